# Optimizing a Trainium2 kernel written in Bass

```python
import jax, jax.numpy as jnp
from jax import lax
import numpy as np

D_MODEL = 1024
BATCH = 2
SEQ = 8192
DEPTH = 2

CHUNK = 64
D_POOL = D_MODEL // 4
POOL_WINDOWS = (2, 4, 8, 16)
N_POOL_GROUPS = len(POOL_WINDOWS)
POOL_GROUP = D_POOL // N_POOL_GROUPS
D_GMLP = 3 * D_MODEL // 8
GMLP_HEADS = 4
GMLP_HEAD_DIM = D_GMLP // GMLP_HEADS
GMLP_BLOCK = 128
D_CONV = D_MODEL - D_POOL - D_GMLP
CONV_WIDTH = 31
D_MIX = D_POOL + D_GMLP + D_CONV
D_IN = D_POOL + 2 * D_GMLP + 2 * D_CONV
D_FF = 2816
N_EXPERTS = 8
TOP_K = 2
D_FF_EXPERT = 3584
N_DENSE = (DEPTH + 1) // 2
N_MOE = DEPTH // 2
EPS = 1e-6

kernel_name = "hybrid_pool_gmlp_conv_moe_trunk"


def rms_norm(x, g):
    xf = x.astype(jnp.float32)
    y = xf * lax.rsqrt(jnp.mean(xf * xf, axis=-1, keepdims=True) + EPS)
    return (y * g.astype(jnp.float32)).astype(x.dtype)


def layer_norm(x, g, b):
    xf = x.astype(jnp.float32)
    mu = jnp.mean(xf, axis=-1, keepdims=True)
    var = jnp.mean(jnp.square(xf - mu), axis=-1, keepdims=True)
    y = (xf - mu) * lax.rsqrt(var + EPS)
    return (y * g.astype(jnp.float32) + b.astype(jnp.float32)).astype(x.dtype)


def pool_mixer(a, w, scale):
    B, S, _ = a.shape
    af = a.astype(jnp.float32)
    csum = jnp.cumsum(af, axis=1)
    t = jnp.arange(1, S + 1, dtype=jnp.float32)
    outs = []
    for gi, win in enumerate(POOL_WINDOWS):
        sl = slice(gi * POOL_GROUP, (gi + 1) * POOL_GROUP)
        c = csum[..., sl]
        c_prev = jnp.pad(c[:, :-win], ((0, 0), (win, 0), (0, 0)))
        mean = (c - c_prev) / jnp.minimum(t, float(win))[None, :, None]
        outs.append(mean - af[..., sl])
    y = jnp.stack(outs, axis=2).astype(a.dtype)
    y = jnp.einsum('bsgc,gcd->bsgd', y, w).reshape(B, S, D_POOL)
    return y * scale


def gmlp_mixer(u, v, g, ws, b):
    B, S, _ = u.shape
    v = rms_norm(v, g)
    n_blk = S // GMLP_BLOCK
    vb = v.reshape(B, n_blk, GMLP_BLOCK, GMLP_HEADS, GMLP_HEAD_DIM)
    ci = jnp.arange(GMLP_BLOCK) // CHUNK
    mask = ci[:, None] >= ci[None, :]
    ws_m = jnp.where(mask[None], ws, jnp.zeros_like(ws))
    z = jnp.einsum('hij,bnjhc->bnihc', ws_m, vb) + b.T[None, None, :, :, None]
    return u * z.reshape(B, S, D_GMLP)


def conv_module(val, gate, w_dw, b_dw, ln_g, ln_b, w_pw, b_pw):
    h = val * jax.nn.sigmoid(gate)
    h = lax.conv_general_dilated(
        h, w_dw[:, None, :], window_strides=(1,),
        padding=((CONV_WIDTH - 1, 0),),
        dimension_numbers=('NWC', 'WIO', 'NWC'),
        feature_group_count=D_CONV) + b_dw
    h = jax.nn.silu(layer_norm(h, ln_g, ln_b))
    return h @ w_pw + b_pw


def swiglu(h, wg, wu, wd):
    return (jax.nn.silu(h @ wg) * (h @ wu)) @ wd


def moe_swiglu(h, router, wg, wu, wd):
    B, S, D = h.shape
    t = h.reshape(B * S, D)
    logits = t.astype(jnp.float32) @ router.astype(jnp.float32)
    top_v, top_i = lax.top_k(logits, TOP_K)
    top_w = jax.nn.softmax(top_v, axis=-1)
    gates = jnp.sum(jax.nn.one_hot(top_i, N_EXPERTS, dtype=jnp.float32)
                    * top_w[..., None], axis=1)
    out = jnp.zeros_like(t)
    for e in range(N_EXPERTS):
        out = out + gates[:, e:e + 1].astype(t.dtype) * swiglu(t, wg[e], wu[e], wd[e])
    return out.reshape(B, S, D)


def setup_inputs(seed: int = 0) -> dict:
    key = jax.random.key(seed)
    k = jax.random.split(key, 26)
    f32 = jnp.float32
    nrm = lambda kk, shape, s: jax.random.normal(kk, shape, f32) * s
    return {
        "x": nrm(k[0], (BATCH, SEQ, D_MODEL), 1.0),
        "norm1_g": 1.0 + nrm(k[1], (DEPTH, D_MODEL), 0.05),
        "w_in": nrm(k[2], (DEPTH, D_MODEL, D_IN), D_MODEL ** -0.5),
        "pool_w": nrm(k[3], (DEPTH, N_POOL_GROUPS, POOL_GROUP, POOL_GROUP), POOL_GROUP ** -0.5),
        "pool_scale": 1.0 + nrm(k[4], (DEPTH, D_POOL), 0.1),
        "gm_norm_g": 1.0 + nrm(k[5], (DEPTH, D_GMLP), 0.05),
        "gm_ws": nrm(k[6], (DEPTH, GMLP_HEADS, GMLP_BLOCK, GMLP_BLOCK), 0.5 * GMLP_BLOCK ** -0.5),
        "gm_b": 1.0 + nrm(k[7], (DEPTH, GMLP_HEADS, GMLP_BLOCK), 0.1),
        "conv_dw_w": nrm(k[8], (DEPTH, CONV_WIDTH, D_CONV), CONV_WIDTH ** -0.5),
        "conv_dw_b": nrm(k[9], (DEPTH, D_CONV), 0.02),
        "conv_ln_g": 1.0 + nrm(k[10], (DEPTH, D_CONV), 0.05),
        "conv_ln_b": nrm(k[11], (DEPTH, D_CONV), 0.02),
        "conv_pw_w": nrm(k[12], (DEPTH, D_CONV, D_CONV), D_CONV ** -0.5),
        "conv_pw_b": nrm(k[13], (DEPTH, D_CONV), 0.02),
        "w_out": nrm(k[14], (DEPTH, D_MIX, D_MODEL), D_MIX ** -0.5),
        "norm2_g": 1.0 + nrm(k[15], (DEPTH, D_MODEL), 0.05),
        "ffn_wg": nrm(k[16], (N_DENSE, D_MODEL, D_FF), D_MODEL ** -0.5),
        "ffn_wu": nrm(k[17], (N_DENSE, D_MODEL, D_FF), D_MODEL ** -0.5),
        "ffn_wd": nrm(k[18], (N_DENSE, D_FF, D_MODEL), D_FF ** -0.5),
        "moe_router": nrm(k[19], (N_MOE, D_MODEL, N_EXPERTS), D_MODEL ** -0.5),
        "moe_wg": nrm(k[20], (N_MOE, N_EXPERTS, D_MODEL, D_FF_EXPERT), D_MODEL ** -0.5),
        "moe_wu": nrm(k[21], (N_MOE, N_EXPERTS, D_MODEL, D_FF_EXPERT), D_MODEL ** -0.5),
        "moe_wd": nrm(k[22], (N_MOE, N_EXPERTS, D_FF_EXPERT, D_MODEL), D_FF_EXPERT ** -0.5),
        "final_g": 1.0 + nrm(k[23], (D_MODEL,), 0.05),
    }


def reference(x, norm1_g, w_in, pool_w, pool_scale, gm_norm_g, gm_ws, gm_b,
              conv_dw_w, conv_dw_b, conv_ln_g, conv_ln_b, conv_pw_w, conv_pw_b,
              w_out, norm2_g, ffn_wg, ffn_wu, ffn_wd,
              moe_router, moe_wg, moe_wu, moe_wd, final_g):
    splits = [D_POOL, D_POOL + D_GMLP, D_POOL + 2 * D_GMLP,
              D_POOL + 2 * D_GMLP + D_CONV]
    for l in range(DEPTH):
        h = rms_norm(x, norm1_g[l])
        p = h @ w_in[l]
        a, u, v, cv, cg = jnp.split(p, splits, axis=-1)
        ya = pool_mixer(a, pool_w[l], pool_scale[l])
        yb = gmlp_mixer(u, v, gm_norm_g[l], gm_ws[l], gm_b[l])
        yc = conv_module(cv, cg, conv_dw_w[l], conv_dw_b[l], conv_ln_g[l],
                         conv_ln_b[l], conv_pw_w[l], conv_pw_b[l])
        x = x + jnp.concatenate([ya, yb, yc], axis=-1) @ w_out[l]
        h = rms_norm(x, norm2_g[l])
        if l % 2 == 0:
            j = l // 2
            x = x + swiglu(h, ffn_wg[j], ffn_wu[j], ffn_wd[j])
        else:
            j = l // 2
            x = x + moe_swiglu(h, moe_router[j], moe_wg[j], moe_wu[j], moe_wd[j])
    return rms_norm(x, final_g)
```

```python
import numpy as np
from contextlib import ExitStack
import concourse.bass as bass
import concourse.mybir as mybir
from concourse.bass_utils import run_bass_kernel_spmd

F32 = mybir.dt.float32
BF16 = mybir.dt.bfloat16
I32 = mybir.dt.int32
AF = mybir.ActivationFunctionType
ALU = mybir.AluOpType
AX = mybir.AxisListType

PE, ACT, DVE, POOL, SP = "pe", "act", "dve", "pool", "sp"
ENGS = (PE, ACT, DVE, POOL, SP)

D = 1024
NT = 17
D_IN = 1792
D_FF = 2816
D_FFE = 3584
NE = 8
EPS = 1e-6
NPP = 112
PP_PSCALE = 0
PP_DWW = 2
PP_DWB = 95
PP_LNG = 98
PP_LNB = 101
PP_PWB = 104
PP_INVW = 107
TGM = 2
GF = 4
GFS = 2
CAPS = (512, 640, 768)
FORCE_CLASS = None


class Prog:
    def __init__(self, nc, stack):
        self.nc = nc
        self.stack = stack
        self.ops = {e: [] for e in ENGS}
        self.sems = {}
        self.cnt = {}
        self.seen = {e: {} for e in ENGS}
        for e in ENGS:
            self._mksem("eng_" + e)

    def _mksem(self, key):
        if key not in self.sems:
            self.sems[key] = self.stack.enter_context(self.nc.semaphore(key))
            self.cnt[key] = 0
        return self.sems[key]

    def _waits(self, eng, deps):
        out = []
        for d in deps:
            if d is None:
                continue
            key, val = d
            if eng == PE and key == "eng_pe":
                continue
            if self.seen[eng].get(key, 0) >= val:
                continue
            self.seen[eng][key] = val
            out.append((self.sems[key], val))
        return out

    def op(self, eng, fn, deps=(), inc=True):
        waits = self._waits(eng, deps)
        key = "eng_" + eng
        tok = None
        if inc:
            self.cnt[key] += 1
            tok = (key, self.cnt[key])
        self.ops[eng].append((waits, fn, (self.sems[key], 1) if inc else None))
        return tok

    def dma(self, eng, out, in_, semname, deps=()):
        self._mksem(semname)
        waits = self._waits(eng, deps)
        self.cnt[semname] += 16
        tok = (semname, self.cnt[semname])
        self.ops[eng].append(
            (waits, lambda e, o=out, i=in_: e.dma_start(out=o, in_=i), (self.sems[semname], 16))
        )
        return tok

    def wait_only(self, eng, deps):
        waits = self._waits(eng, deps)
        if waits:
            self.ops[eng].append((waits, None, None))

    def barrier(self):
        toks = [(k, v) for k, v in self.cnt.items() if v > 0]
        for e in ENGS:
            self.wait_only(e, toks)

    def cond_region(self, cond_ap, cond_deps, then_fn, else_fn):
        for e in ENGS:
            self.ops[e].append(("IF", cond_ap, self._waits(e, cond_deps)))
        snap_cnt = dict(self.cnt)
        snap_seen = {e: dict(d) for e, d in self.seen.items()}
        Buf.reset_all()
        then_fn()
        then_cnt = dict(self.cnt)
        then_end = {e: len(self.ops[e]) for e in ENGS}
        self.cnt = dict(snap_cnt)
        for kk in then_cnt:
            self.cnt.setdefault(kk, 0)
        self.seen = {e: dict(d) for e, d in snap_seen.items()}
        for e in ENGS:
            self.ops[e].append(("ELSE",))
        Buf.reset_all()
        else_fn()
        else_cnt = dict(self.cnt)
        keys = set(then_cnt) | set(else_cnt)
        final = {kk: max(then_cnt.get(kk, 0), else_cnt.get(kk, 0)) for kk in keys}

        def equalizers(branch_cnt):
            per_eng = {e: [] for e in ENGS}
            for kk in sorted(keys):
                diff = final[kk] - branch_cnt.get(kk, 0)
                if diff <= 0:
                    continue
                eng = kk[4:] if kk.startswith("eng_") else SP
                per_eng[eng].append(("EQ", self.sems[kk], branch_cnt.get(kk, 0), diff))
            return per_eng

        eq_then = equalizers(then_cnt)
        eq_else = equalizers(else_cnt)
        for e in ENGS:
            self.ops[e][then_end[e]:then_end[e]] = eq_then[e]
            self.ops[e].extend(eq_else[e])
            self.ops[e].append(("ENDIF",))
        self.cnt = final
        self.seen = snap_seen
        Buf.reset_all()

    def flush(self):
        nc = self.nc
        ops = self.ops

        def run(e, lst):
            cms = []
            for item in lst:
                tag = item[0]
                if tag == "IF":
                    for s_, v in item[2]:
                        e.wait_ge(s_, v)
                    val = e.value_load(item[1])
                    cm = e.If(val == 1)
                    cm.__enter__()
                    cms.append(cm)
                elif tag == "ELSE":
                    cms.pop().__exit__(None, None, None)
                    cm = e.Else()
                    cm.__enter__()
                    cms.append(cm)
                elif tag == "ENDIF":
                    cms.pop().__exit__(None, None, None)
                elif tag == "EQ":
                    _, sem, have, diff = item
                    if have > 0:
                        e.wait_ge(sem, have)
                    e.sem_inc(sem, diff)
                else:
                    waits, fn, inc = item
                    for s_, v in waits:
                        e.wait_ge(s_, v)
                    if fn is not None:
                        ins = fn(e)
                        if inc is not None:
                            ins.then_inc(inc[0], inc[1])

        with nc.Block() as block:
            @block.tensor
            def _(e):
                run(e, ops[PE])

            @block.scalar
            def _(e):
                run(e, ops[ACT])

            @block.vector
            def _(e):
                run(e, ops[DVE])

            @block.gpsimd
            def _(e):
                run(e, ops[POOL])

            @block.sync
            def _(e):
                run(e, ops[SP])
        self.ops = {e: [] for e in ENGS}


class Buf:
    ALL = []

    def __init__(self, name=""):
        self.name = name
        self.w = None
        self.r = {}
        Buf.ALL.append(self)

    @staticmethod
    def reset_all():
        for b in Buf.ALL:
            b.w = None
            b.r = {}

    def add_read(self, tok):
        if tok is None:
            return
        k, v = tok
        if self.r.get(k, 0) < v:
            self.r[k] = v

    def set_write(self, tok):
        self.w = tok
        self.r = {}


class K:
    def __init__(self, nc, stack):
        self.nc = nc
        self.p = Prog(nc, stack)
        self.dma_n = 0

    def deps_of(self, reads, writes):
        deps = []
        for b in reads:
            deps.append(b.w)
        for b in writes:
            deps.append(b.w)
            deps.extend(b.r.items())
        return deps

    def emit(self, eng, fn, reads=(), writes=()):
        tok = self.p.op(eng, fn, self.deps_of(reads, writes), True)
        for b in reads:
            b.add_read(tok)
        for b in writes:
            b.set_write(tok)
        return tok

    def dma(self, eng, out, in_, reads=(), writes=(), sem=None):
        assert sem is not None
        deps = []
        for b in reads:
            deps.append(b.w)
        for b in writes:
            if not (b.w is not None and b.w[0] == sem):
                deps.append(b.w)
            deps.extend(b.r.items())
        tok = self.p.dma(eng, out, in_, sem, deps)
        for b in reads:
            b.add_read(tok)
        for b in writes:
            b.set_write(tok)
        return tok

    def mm_group(self, out, pairs, reads, bank, transpose=False):
        deps = self.deps_of(reads, [bank])
        n = len(pairs)
        tok = None
        for i, (l, r) in enumerate(pairs):
            last = i == n - 1
            if transpose:
                fn = (lambda e, o=out[i], a=l, b=r: e.transpose(o, a, b))
            else:
                fn = (lambda e, o=out, a=l, b=r, s=(i == 0), t=last: e.matmul(o, lhsT=a, rhs=b, start=s, stop=t))
            tok = self.p.op(PE, fn, deps if i == 0 else (), last)
        for b in reads:
            b.add_read(tok)
        bank.set_write(tok)
        return tok

    def mm_multi(self, groups, reads, bank):
        deps = self.deps_of(reads, [bank])
        n = len(groups)
        tok = None
        for i, (o, l, r) in enumerate(groups):
            last = i == n - 1
            fn = (lambda e, o=o, a=l, b=r: e.matmul(o, lhsT=a, rhs=b, start=True, stop=True))
            tok = self.p.op(PE, fn, deps if i == 0 else (), last)
        for b in reads:
            b.add_read(tok)
        bank.set_write(tok)
        return tok

    def act(self, out, in_, func, reads, writes, bias=None, scale=None, accum=None):
        kw = {}
        if bias is not None:
            kw["bias"] = bias
        if scale is not None:
            kw["scale"] = scale
        if accum is not None:
            kw["accum_out"] = accum
        return self.emit(ACT, lambda e: e.activation(out=out, in_=in_, func=func, **kw), reads, writes)

    def tt(self, out, in0, in1, op, reads, writes, eng=DVE):
        return self.emit(eng, lambda e: e.tensor_tensor(out=out, in0=in0, in1=in1, op=op), reads, writes)

    def ts(self, out, in0, s1, s2, op0, op1, reads, writes, eng=DVE):
        return self.emit(eng, lambda e: e.tensor_scalar(out=out, in0=in0, scalar1=s1, scalar2=s2, op0=op0, op1=op1),
                         reads, writes)

    def ts1(self, out, in0, s1, op0, reads, writes, eng=DVE):
        return self.emit(eng, lambda e: e.tensor_scalar(out=out, in0=in0, scalar1=s1, scalar2=None, op0=op0),
                         reads, writes)

    def stt(self, out, in0, scalar, in1, op0, op1, reads, writes, accum=None, eng=DVE):
        kw = {}
        if accum is not None:
            kw["accum_out"] = accum
        return self.emit(eng, lambda e: e.scalar_tensor_tensor(out=out, in0=in0, scalar=scalar, in1=in1,
                                                               op0=op0, op1=op1, **kw), reads, writes)

    def copy(self, out, in_, reads, writes, eng=DVE):
        return self.emit(eng, lambda e: e.tensor_copy(out=out, in_=in_), reads, writes)

    def recip(self, out, in_, reads, writes):
        return self.emit(DVE, lambda e: e.reciprocal(out=out, in_=in_), reads, writes)

    def memset(self, ap, val, writes, eng=DVE):
        return self.emit(eng, lambda e: e.memset(ap, val), (), writes)


def build_program(stop_after=None):
    nc = bass.Bass("TRN2", target_bir_lowering=False)

    def din(name, shape):
        return nc.dram_tensor(name, list(shape), F32, kind="ExternalInput").ap()

    x_d = din("x", [NT * 128, D])
    flag_d = din("flag", [128, 1])
    pinv_d = din("pool_inv", [128, 32])
    ident_d = din("ident", [128, 128])
    ustrict_d = din("ustrict", [128, 128])
    iota_d = din("iota", [128, CAPS[-1]])
    pp_d = din("pp", [128, 2 * NPP])
    n1g_d = din("norm1_g", [2, D])
    n2g_d = din("norm2_g", [2, D])
    fg_d = din("final_g", [1, D])
    gmg_d = din("gm_norm_g", [2, 384])
    gmb_d = din("gm_b", [2, 512])
    w_in_d = din("w_in", [2, D, D_IN])
    pool_w_d = din("pool_w", [2, 4, 64, 64])
    wsT_d = din("gm_wsT", [2, 4, 128, 128])
    pw_d = din("conv_pw_w", [2, 384, 384])
    w_out_d = din("w_out", [2, D, D])
    fwg_d = din("ffn_wg", [D, D_FF])
    fwu_d = din("ffn_wu", [D, D_FF])
    fwd_d = din("ffn_wd", [D_FF, D])
    rT_d = din("routerT", [NE, D])
    mwg_d = din("moe_wg", [NE, D, D_FFE])
    mwu_d = din("moe_wu", [NE, D, D_FFE])
    mwd_d = din("moe_wd", [NE, D_FFE, D])
    y_d = nc.dram_tensor("y", [16 * 128, D], F32, kind="ExternalOutput").ap()

    with ExitStack() as st:
        ARENA_WORDS = 52600
        arena = st.enter_context(nc.sbuf_tensor("arena", [128, ARENA_WORDS], F32))
        atop = [0]
        scopes = {}

        def _release(mark):
            atop[0] = mark

        def sb(stack, name, shape, dt):
            if stack is not st and not getattr(stack, "_arena_marked", False):
                stack._arena_marked = True
                stack.callback(_release, atop[0])
            n = 1
            for d_ in shape[1:]:
                n *= d_
            nbytes = n * (4 if dt in (F32, I32) else 2)
            words = ((nbytes + 3) // 4 + 7) // 8 * 8
            assert atop[0] + words <= ARENA_WORDS, ("SBUF arena overflow", name, atop[0], words)
            v = arena[:, atop[0]:atop[0] + words]
            atop[0] += words
            if dt != F32:
                v = v.bitcast(dt)
            v = v[:, 0:n]
            if len(shape) == 3:
                v = v.rearrange("p (a b) -> p a b", a=shape[1])
            return v

        k = K(nc, st)
        p = k.p

        x_tok = sb(st, "x_tok", [128, NT, D], F32)
        xb = [Buf("x%d" % n) for n in range(NT)]
        ident = sb(st, "ident", [128, 128], BF16)
        ones32 = sb(st, "ones32", [128, 128], F32)
        ustrict = sb(st, "ustrict", [128, 128], F32)
        iota = sb(st, "iota", [128, CAPS[-1]], F32)
        pp = sb(st, "pp", [128, 2 * NPP], F32)
        flag = sb(st, "flag", [128, 1], F32)
        pinv = sb(st, "pinv", [128, 2, 16], F32)
        stt_t = sb(st, "stats", [128, 8, 4], F32)
        gate = sb(st, "gate", [128, NT, NE], F32)
        cb = Buf("consts")
        stb = [Buf("st%d" % i) for i in range(8)]
        gateb = [Buf("gate%d" % n) for n in range(NT)]
        banks = [st.enter_context(nc.psum_tensor("bank%d" % i, [128, 512], F32)) for i in range(8)]
        bankb = [Buf("bank%d" % i) for i in range(8)]
        ring = [0]
        stat_i = [0]

        def next_bank():
            i = ring[0]
            ring[0] = (i + 1) % 8
            return banks[i], bankb[i]

        def next_stat():
            i = stat_i[0]
            stat_i[0] = (i + 1) % 8
            return stt_t[:, i, :], stb[i]

        def ppc(l, col, n=1, lo=0, hi=128):
            return pp[lo:hi, l * NPP + col: l * NPP + col + n]

        k.memset(ones32[:], 1.0, [cb])
        k.dma(POOL, ident[:], ident_d, writes=[cb], sem="consts")
        k.dma(POOL, pp[:], pp_d, writes=[cb], sem="consts")
        k.dma(POOL, ustrict[:], ustrict_d, writes=[cb], sem="consts")
        k.dma(POOL, iota[:], iota_d, writes=[cb], sem="consts")
        k.dma(POOL, flag[:], flag_d, writes=[cb], sem="consts")
        k.dma(POOL, pinv[:].rearrange("p c j -> p (c j)"), pinv_d, writes=[cb], sem="consts")
        for n in range(NT):
            k.dma(SP, x_tok[:, n, :], x_d[n * 128:(n + 1) * 128, :], writes=[xb[n]], sem="x%d" % n)

        def rms_stats(src_ap, srcb, junk, junkb, scale):
            sap, sbf = next_stat()
            k.act(junk, src_ap, AF.Square, [srcb], [junkb, sbf], scale=scale, accum=sap[:, 0:1])
            k.act(sap[:, 1:2], sap[:, 0:1], AF.Sqrt, [sbf], [sbf], bias=EPS)
            k.recip(sap[:, 2:3], sap[:, 1:2], [sbf], [sbf])
            return sap[:, 2:3], sbf

        def norm_and_transpose(n, g_bc, gb, h_tok, h_tokb, junk, junkb, hT_dst, hTb):
            rstd, sbf = rms_stats(x_tok[:, n, :], xb[n], junk, junkb, 1.0 / 32.0)
            k.stt(h_tok, x_tok[:, n, :], rstd, g_bc, ALU.mult, ALU.mult, [xb[n], sbf, gb], [h_tokb])
            bk, bkb = next_bank()
            bkbf = bk.bitcast(BF16)
            outs = [bkbf[:, kc * 128:(kc + 1) * 128] for kc in range(8)]
            pairs = [(h_tok[:, kc * 128:(kc + 1) * 128], ident[:, :]) for kc in range(8)]
            k.mm_group(outs, pairs, [h_tokb, cb], bkb, transpose=True)
            k.act(hT_dst, bkbf[:, :].rearrange("p (k t) -> p k t", k=8), AF.Copy, [bkb], [hTb])
            return rstd, sbf

        def mixer_phase(l):
            with ExitStack() as ms:
                NMAX = TGM * 128
                LMAX = 32 + NMAX
                w_in_sb = sb(ms, "w_in_sb", [128, 8, D_IN], BF16)
                wo_a = sb(ms, "wo_a", [128, 2, D], BF16)
                wo_b = sb(ms, "wo_b", [128, 4, D], BF16)
                wo_c = sb(ms, "wo_c", [128, 3, D], BF16)
                pw_sb = sb(ms, "pw_sb", [128, 3, 384], BF16)
                wblk = sb(ms, "wblk", [128, 2, 128], BF16)
                wsT = sb(ms, "wsT", [128, 4, 128], BF16)
                g1_bc = sb(ms, "g1_bc", [128, D], F32)
                gmg_bc = sb(ms, "gmg_bc", [128, 384], F32)
                gmb_bc = sb(ms, "gmb_bc", [128, 512], F32)
                wb = Buf("mixw")
                h_tok = [sb(ms, "h_tok%d" % i, [128, D], BF16) for i in range(2)]
                h_tokb = [Buf(), Buf()]
                junk = sb(ms, "junk", [128, D], BF16)
                junkb = Buf()
                two = range(2)
                a_ext = [sb(ms, "a_ext%d" % i, [128, 2, LMAX], F32) for i in two]
                hc_ext = [sb(ms, "hc_ext%d" % i, [128, 3, LMAX], BF16) for i in two]
                yb = [sb(ms, "yb%d" % i, [128, 4, NMAX], BF16) for i in two]
                ab, hcb, ybb = ([Buf(), Buf()] for _ in range(3))

                def same2(name, shape, dt):
                    t_ = sb(ms, name, shape, dt)
                    return [t_, t_]

                def same2b():
                    b_ = Buf()
                    return [b_, b_]

                hT = same2("hT", [128, 8, NMAX], BF16)
                sig = same2("sig", [128, 3, NMAX], F32)
                acc = same2("acc", [128, 3, NMAX], F32)
                u_sb = same2("u_sb", [128, 4, NMAX], F32)
                y_p = same2("y_p", [128, 2, NMAX], BF16)
                ya = same2("ya", [128, 2, NMAX], BF16)
                hs = same2("hs", [128, 3, NMAX], BF16)
                yc = same2("yc", [128, 3, NMAX], BF16)
                hTb, sigb, ub, ypb, yab, hsb, ycb = (same2b() for _ in range(7))
                accb1 = [Buf(), Buf(), Buf()]
                accb = [accb1, accb1]
                dg = sb(ms, "dg", [128, 93, 128], BF16)
                dgb = Buf()
                sA = sb(ms, "sA", [128, 2, LMAX], F32)
                sB = sb(ms, "sB", [128, 2, LMAX], F32)
                tmp16 = sb(ms, "tmp16", [128, 16], F32)
                v_n = [sb(ms, "v_n%d" % i, [128, 384], BF16) for i in two]
                ztmp = sb(ms, "ztmp", [128, 512], F32)
                sq = sb(ms, "sq", [128, 3, NMAX], F32)
                mean = sb(ms, "mean", [128, NMAX], F32)
                var = sb(ms, "var", [128, NMAX], F32)
                rstdc = sb(ms, "rstdc", [128, NMAX], F32)
                sAb, sBb, t16b, ztb, sqb, meanb, varb, rsb = (Buf() for _ in range(8))
                vnb = [Buf(), Buf()]

                wblkb = Buf()
                wsTb = Buf()
                k.memset(wblk[:], 0.0, [wblkb])
                for c in range(2):
                    k.dma(POOL, wblk[0:64, c, 0:64], pool_w_d[l, 2 * c], writes=[wblkb], sem="wblk")
                    k.dma(POOL, wblk[64:128, c, 64:128], pool_w_d[l, 2 * c + 1], writes=[wblkb], sem="wblk")
                k.dma(POOL, wsT[:], wsT_d[l].rearrange("h j i -> j h i"), writes=[wsTb], sem="wsT")
                k.memset(wsT[64:128, :, 0:64], 0.0, [wsTb])
                k.dma(SP, g1_bc[:], n1g_d[l:l + 1, :].partition_broadcast(128), writes=[wb], sem="mixw")
                k.dma(SP, gmg_bc[:], gmg_d[l:l + 1, :].partition_broadcast(128), writes=[wb], sem="mixw")
                k.dma(SP, gmb_bc[:], gmb_d[l:l + 1, :].partition_broadcast(128), writes=[wb], sem="mixw")
                k.dma(POOL, w_in_sb[:], w_in_d[l].rearrange("(kc p) n -> p kc n", p=128), writes=[wb], sem="mixw")
                k.dma(POOL, wo_a[:], w_out_d[l, 0:256, :].rearrange("(c p) n -> p c n", p=128), writes=[wb], sem="mixw")
                k.dma(POOL, wo_b[0:96], w_out_d[l, 256:640, :].rearrange("(h p) n -> p h n", p=96), writes=[wb], sem="mixw")
                k.dma(POOL, wo_c[:], w_out_d[l, 640:1024, :].rearrange("(c p) n -> p c n", p=128), writes=[wb], sem="mixw")
                k.dma(POOL, pw_sb[:], pw_d[l].rearrange("(c p) n -> p c n", p=128), writes=[wb], sem="mixw")
                k.memset(a_ext[0][:, :, 0:32], 0.0, [ab[0]])
                k.memset(hc_ext[0][:, :, 0:32], 0.0, [hcb[0]])
                for i in range(93):
                    k.ts1(dg[:, i, :], ident[:, :], ppc(l, PP_DWW + i), ALU.mult, [cb], [dgb])

                groups = [(0, 1)] + [(t0, TGM) for t0 in range(1, NT, TGM)]

                def stage_n(gi):
                    t0, nt = groups[gi]
                    q = gi % 2
                    if gi > 0:
                        pN = groups[gi - 1][1] * 128
                        k.copy(a_ext[q][:, :, 0:32], a_ext[1 - q][:, :, pN:pN + 32], [ab[1 - q]], [ab[q]], eng=POOL)
                        k.copy(hc_ext[q][:, :, 0:32], hc_ext[1 - q][:, :, pN:pN + 32], [hcb[1 - q]], [hcb[q]], eng=POOL)
                    for j in range(nt):
                        n = t0 + j
                        hb = (gi * TGM + j) % 2
                        norm_and_transpose(n, g1_bc[:], wb, h_tok[hb][:], h_tokb[hb], junk[:], junkb,
                                           hT[q][:, :, j * 128:(j + 1) * 128], hTb[q])

                def stage_p(gi):
                    t0, nt = groups[gi]
                    q = gi % 2
                    N = nt * 128
                    L = 32 + N
                    full = not (l == 1 and t0 == 0)

                    def proj(col0, m):
                        bk, bkb = next_bank()
                        pairs = [(w_in_sb[:, kc, col0:col0 + m], hT[q][:, kc, 0:N]) for kc in range(8)]
                        k.mm_group(bk[0:m, 0:N], pairs, [wb, hTb[q]], bkb)
                        return bk, bkb

                    for c in range(2):
                        bk, bkb = proj(c * 128, 128)
                        k.act(a_ext[q][:, c, 32:L], bk[:, 0:N], AF.Copy, [bkb], [ab[q]])
                    cvb = []
                    for c in range(3):
                        cvb.append(proj(1024 + c * 128, 128))
                    for c in range(3):
                        bk, bkb = proj(1408 + c * 128, 128)
                        k.act(sig[q][:, c, 0:N], bk[:, 0:N], AF.Sigmoid, [bkb], [sigb[q]])
                    for c in range(3):
                        bk, bkb = cvb[c]
                        k.tt(hc_ext[q][:, c, 32:L], bk[:, 0:N], sig[q][:, c, 0:N], ALU.mult, [bkb, sigb[q]], [hcb[q]])
                    if full:
                        for h in range(4):
                            bk, bkb = proj(256 + h * 96, 96)
                            k.act(u_sb[q][0:96, h, 0:N], bk[0:96, 0:N], AF.Copy, [bkb], [ub[q]])
                        for j in range(nt):
                            vb = j % 2
                            bk, bkb = next_bank()
                            pairs = [(hT[q][:, kc, j * 128:(j + 1) * 128], w_in_sb[:, kc, 640:1024]) for kc in range(8)]
                            k.mm_group(bk[:, 0:384], pairs, [wb, hTb[q]], bkb)
                            rstd, sbf = rms_stats(bk[:, 0:384], bkb, junk[:, 0:384], junkb, float(384.0 ** -0.5))
                            k.stt(v_n[vb][:], bk[:, 0:384], rstd, gmg_bc[:], ALU.mult, ALU.mult,
                                  [bkb, sbf, wb], [vnb[vb]])
                            zk, zkb = next_bank()
                            grp = [(zk[0:96, h * 128:(h + 1) * 128], v_n[vb][:, h * 96:(h + 1) * 96], wsT[:, h, :])
                                   for h in range(4)]
                            k.mm_multi(grp, [vnb[vb], wsTb], zkb)
                            k.tt(ztmp[0:96, :], zk[0:96, :], gmb_bc[0:96, :], ALU.add, [zkb, wb], [ztb])
                            k.tt(yb[q][0:96, :, j * 128:(j + 1) * 128],
                                 ztmp[0:96, :].rearrange("p (h i) -> p h i", h=4),
                                 u_sb[q][0:96, :, j * 128:(j + 1) * 128], ALU.mult, [ztb, ub[q]], [ybb[q]])

                def stage_b1(gi):
                    t0, nt = groups[gi]
                    q = gi % 2
                    N = nt * 128
                    L = 32 + N
                    full = not (l == 1 and t0 == 0)
                    first_own = (t0 == 1)
                    if not full:
                        return
                    A_ = a_ext[q]
                    H_ = hc_ext[q]

                    def pool_out(sbuf_t, sbuf_b, lo, hi, c):
                        k.stt(y_p[q][lo:hi, c, 0:N], sbuf_t[lo:hi, c, 32:L], ppc(l, PP_INVW + c, 1, lo, hi),
                              A_[lo:hi, c, 32:L], ALU.mult, ALU.subtract, [sbuf_b, ab[q], cb], [ypb[q]])
                        if first_own:
                            k.tt(tmp16[lo:hi, :], sbuf_t[lo:hi, c, 32:48], pinv[lo:hi, c, :], ALU.mult,
                                 [sbuf_b, cb], [t16b])
                            k.tt(y_p[q][lo:hi, c, 0:16], tmp16[lo:hi, :], A_[lo:hi, c, 32:48], ALU.subtract,
                                 [t16b, ab[q]], [ypb[q]])

                    k.tt(sA[:, :, 1:L], A_[:, :, 1:L], A_[:, :, 0:L - 1], ALU.add, [ab[q]], [sAb])
                    pool_out(sA, sAb, 0, 64, 0)
                    k.tt(sB[:, :, 3:L], sA[:, :, 3:L], sA[:, :, 1:L - 2], ALU.add, [sAb], [sBb])
                    pool_out(sB, sBb, 64, 128, 0)
                    k.tt(sA[:, :, 7:L], sB[:, :, 7:L], sB[:, :, 3:L - 4], ALU.add, [sBb], [sAb])
                    pool_out(sA, sAb, 0, 64, 1)
                    k.tt(sB[:, :, 15:L], sA[:, :, 15:L], sA[:, :, 7:L - 8], ALU.add, [sAb], [sBb])
                    pool_out(sB, sBb, 64, 128, 1)
                    for c in range(2):
                        bk, bkb = next_bank()
                        k.mm_group(bk[:, 0:N], [(wblk[:, c, :], y_p[q][:, c, 0:N])], [wblkb, ypb[q]], bkb)
                        k.act(ya[q][:, c, 0:N], bk[:, 0:N], AF.Copy, [bkb, cb], [yab[q]], scale=ppc(l, PP_PSCALE + c))

                    for c in range(3):
                        bk, bkb = next_bank()
                        pairs = [(dg[:, c * 31 + kk, :], H_[:, c, 2 + kk:2 + kk + N]) for kk in range(31)]
                        k.mm_group(bk[:, 0:N], pairs, [dgb, hcb[q]], bkb)
                        k.act(acc[q][:, c, 0:N], bk[:, 0:N], AF.Identity, [bkb, cb], [accb[q][c]],
                              bias=ppc(l, PP_DWB + c))
                    for c in range(3):
                        k.act(sq[:, c, 0:N], acc[q][:, c, 0:N], AF.Square, [accb[q][c]], [sqb])
                    b1, b1b = next_bank()
                    k.mm_group(b1[:, 0:N], [(ones32[:, :], acc[q][:, c, 0:N]) for c in range(3)], accb[q] + [cb], b1b)
                    b2, b2b = next_bank()
                    k.mm_group(b2[:, 0:N], [(ones32[:, :], sq[:, c, 0:N]) for c in range(3)], [sqb, cb], b2b)
                    k.ts(mean[:, 0:N], b1[:, 0:N], 1.0 / 384.0, 0.0, ALU.mult, ALU.add, [b1b], [meanb])
                    k.tt(var[:, 0:N], mean[:, 0:N], mean[:, 0:N], ALU.mult, [meanb], [varb])
                    k.stt(var[:, 0:N], b2[:, 0:N], 1.0 / 384.0, var[:, 0:N], ALU.mult, ALU.subtract,
                          [b2b], [varb])
                    k.act(var[:, 0:N], var[:, 0:N], AF.Sqrt, [], [varb], bias=EPS)
                    k.recip(rstdc[:, 0:N], var[:, 0:N], [varb], [rsb])
                    for c in range(3):
                        k.tt(sq[:, c, 0:N], acc[q][:, c, 0:N], mean[:, 0:N], ALU.subtract, [accb[q][c], meanb], [sqb])
                        k.tt(sq[:, c, 0:N], sq[:, c, 0:N], rstdc[:, 0:N], ALU.mult, [rsb], [sqb])
                        k.act(hs[q][:, c, 0:N], sq[:, c, 0:N], AF.Silu, [sqb, cb], [hsb[q]],
                              bias=ppc(l, PP_LNB + c), scale=ppc(l, PP_LNG + c))
                def stage_b2(gi):
                    t0, nt = groups[gi]
                    q = gi % 2
                    N = nt * 128
                    full = not (l == 1 and t0 == 0)
                    if not full:
                        return
                    for co in range(3):
                        bk, bkb = next_bank()
                        pairs = [(pw_sb[:, ci, co * 128:(co + 1) * 128], hs[q][:, ci, 0:N]) for ci in range(3)]
                        k.mm_group(bk[:, 0:N], pairs, [wb, hsb[q]], bkb)
                        k.act(yc[q][:, co, 0:N], bk[:, 0:N], AF.Identity, [bkb, cb], [ycb[q]], bias=ppc(l, PP_PWB + co))

                    for j in range(nt):
                        n = t0 + j
                        ts_ = slice(j * 128, (j + 1) * 128)
                        for hf in range(2):
                            cs = slice(hf * 512, (hf + 1) * 512)
                            pairs = [(ya[q][:, c, ts_], wo_a[:, c, cs]) for c in range(2)]
                            pairs += [(yb[q][0:96, h, ts_], wo_b[0:96, h, cs]) for h in range(4)]
                            pairs += [(yc[q][:, c, ts_], wo_c[:, c, cs]) for c in range(3)]
                            bk, bkb = next_bank()
                            k.mm_group(bk[:, :], pairs, [wb, yab[q], ybb[q], ycb[q]], bkb)
                            k.tt(x_tok[:, n, cs], x_tok[:, n, cs], bk[:, :], ALU.add, [bkb], [xb[n]])

                ng = len(groups)
                stage_n(0)
                stage_p(0)
                for gi in range(ng):
                    if gi + 1 < ng:
                        stage_n(gi + 1)
                    stage_b1(gi)
                    if gi + 1 < ng:
                        stage_p(gi + 1)
                    stage_b2(gi)
                p.barrier()

        def ffn_stream(scope, moe, tiles, h2T, h2b, gf, experts=None):
            GW = gf * 128
            slots = []
            for s_ in range(2):
                slots.append((sb(scope, "wg%d" % s_, [128, 8, GW], BF16),
                              sb(scope, "wu%d" % s_, [128, 8, GW], BF16),
                              sb(scope, "wd%d" % s_, [128, gf, D], BF16), Buf()))
            actt = [sb(scope, "act%d" % i, [128, gf, 512], BF16) for i in range(2)]
            actb = [Buf(), Buf()]
            sg = [sb(scope, "sg%d" % i, [128, 512], F32) for i in range(2)]
            sgb = [Buf(), Buf()]
            glist = []
            if not moe:
                nch = D_FF // 128
                for c0 in range(0, nch, gf):
                    nf = min(gf, nch - c0)
                    glist.append((fwg_d[:, c0 * 128:(c0 + nf) * 128], fwu_d[:, c0 * 128:(c0 + nf) * 128],
                                  fwd_d[c0 * 128:(c0 + nf) * 128, :], nf, None))
            else:
                nch = D_FFE // 128
                for e in (experts if experts is not None else range(NE)):
                    for c0 in range(0, nch, gf):
                        nf = min(gf, nch - c0)
                        glist.append((mwg_d[e, :, c0 * 128:(c0 + nf) * 128],
                                      mwu_d[e, :, c0 * 128:(c0 + nf) * 128],
                                      mwd_d[e, c0 * 128:(c0 + nf) * 128, :], nf, e))
            tgroups = [tiles[i:i + 4] for i in range(0, len(tiles), 4)]
            cnt = 0
            sgi = 0
            for gi, (wg_ap, wu_ap, wd_ap, nf, e) in enumerate(glist):
                wg_s, wu_s, wd_s, wsb = slots[gi % 2]
                sem = "wslot%d" % (gi % 2)
                k.dma(POOL, wg_s[:, :, 0:nf * 128], wg_ap.rearrange("(kc p) n -> p kc n", p=128),
                      writes=[wsb], sem=sem)
                k.dma(POOL, wu_s[:, :, 0:nf * 128], wu_ap.rearrange("(kc p) n -> p kc n", p=128),
                      writes=[wsb], sem=sem)
                k.dma(POOL, wd_s[:, 0:nf, :], wd_ap.rearrange("(f p) n -> p f n", p=128),
                      writes=[wsb], sem=sem)
                for tg in tgroups:
                    ab_i = cnt % 2
                    cnt += 1
                    t_lo = tg[0] * 128
                    N = len(tg) * 128
                    for f in range(nf):
                        bg, bgb = next_bank()
                        k.mm_group(bg[:, 0:N], [(wg_s[:, kc, f * 128:(f + 1) * 128], h2T[:, kc, t_lo:t_lo + N])
                                                for kc in range(8)], [wsb] + [h2b[n] for n in tg], bgb)
                        bu, bub = next_bank()
                        k.mm_group(bu[:, 0:N], [(wu_s[:, kc, f * 128:(f + 1) * 128], h2T[:, kc, t_lo:t_lo + N])
                                                for kc in range(8)], [wsb] + [h2b[n] for n in tg], bub)
                        si = sgi % 2
                        sgi += 1
                        k.act(sg[si][:, 0:N], bg[:, 0:N], AF.Silu, [bgb], [sgb[si]])
                        k.tt(actt[ab_i][:, f, 0:N], sg[si][:, 0:N], bu[:, 0:N], ALU.mult, [sgb[si], bub],
                             [actb[ab_i]])
                    for j, n in enumerate(tg):
                        for hf in range(2):
                            cs = slice(hf * 512, (hf + 1) * 512)
                            bk, bkb = next_bank()
                            k.mm_group(bk[:, :], [(actt[ab_i][:, f, j * 128:(j + 1) * 128], wd_s[:, f, cs])
                                                  for f in range(nf)], [wsb, actb[ab_i]], bkb)
                            if moe:
                                k.stt(x_tok[:, n, cs], bk[:, :], gate[:, n, e:e + 1], x_tok[:, n, cs],
                                      ALU.mult, ALU.add, [bkb, gateb[n]], [xb[n]])
                            else:
                                k.tt(x_tok[:, n, cs], x_tok[:, n, cs], bk[:, :], ALU.add, [bkb], [xb[n]])

        def ffn_phase0():
            tiles = list(range(0, NT))
            with ExitStack() as fs:
                h2T = sb(fs, "h2T", [128, 8, NT * 128], BF16)
                h2b = [Buf() for _ in range(NT)]
                with ExitStack() as f1:
                    g2_bc = sb(f1, "g2_bc", [128, D], F32)
                    gb = Buf()
                    h_tok = [sb(f1, "h_tokf%d" % i, [128, D], BF16) for i in range(2)]
                    h_tokb = [Buf(), Buf()]
                    junk = sb(f1, "junkf", [128, D], BF16)
                    junkb = Buf()
                    k.dma(SP, g2_bc[:], n2g_d[0:1, :].partition_broadcast(128), writes=[gb], sem="g2")
                    for ti, n in enumerate(tiles):
                        hb = ti % 2
                        norm_and_transpose(n, g2_bc[:], gb, h_tok[hb][:], h_tokb[hb], junk[:], junkb,
                                           h2T[:, :, n * 128:(n + 1) * 128], h2b[n])
                    p.barrier()
                with ExitStack() as f2:
                    ffn_stream(f2, False, tiles, h2T, h2b, GF)
                    p.barrier()

        def moe_phase():
            tiles = list(range(1, NT))
            with ExitStack() as fs:
                h_all = sb(fs, "h_all", [128, 16, D], BF16)
                hab = [Buf() for _ in range(16)]
                sel = sb(fs, "sel", [128, 16, NE], F32)
                pos = sb(fs, "pos", [128, 16, NE], F32)
                tot = sb(fs, "tot", [128, 16, NE], F32)
                offs = sb(fs, "offs", [128, 16, NE], F32)
                misc = sb(fs, "rmisc", [128, 40], F32)
                cond_i = sb(fs, "cond_i", [128, 32], I32)
                selb, posb, totb, offb, miscb, condb = (Buf() for _ in range(6))
                with ExitStack() as f1:
                    g2_bc = sb(f1, "g2_bc", [128, D], F32)
                    gb = Buf()
                    junk = sb(f1, "junkf", [128, D], BF16)
                    junkb = Buf()
                    rg = sb(f1, "rg", [128, NE, D], F32)
                    rgb = Buf()
                    junk32 = sb(f1, "junk32", [128, D], F32)
                    j32b = Buf()
                    lg = sb(f1, "lg", [128, 2, 32], F32)
                    lgb = [Buf(), Buf()]
                    k.dma(SP, g2_bc[:], n2g_d[1:2, :].partition_broadcast(128), writes=[gb], sem="g2")
                    for e in range(NE):
                        k.dma(SP, rg[:, e, :], rT_d[e:e + 1, :].partition_broadcast(128), writes=[rgb], sem="rg")
                    for e in range(NE):
                        k.tt(rg[:, e, :], rg[:, e, :], g2_bc[:], ALU.mult, [gb], [rgb])
                    for ti, n in enumerate(tiles):
                        T = n - 1
                        hb = ti % 2
                        rstd, sbf = rms_stats(x_tok[:, n, :], xb[n], junk[:], junkb, 1.0 / 32.0)
                        k.stt(h_all[:, T, :], x_tok[:, n, :], rstd, g2_bc[:], ALU.mult, ALU.mult,
                              [xb[n], sbf, gb], [hab[T]])
                        L_ = lg[:, hb, :]
                        lb = lgb[hb]
                        for e in range(NE):
                            k.stt(junk32[:], x_tok[:, n, :], rstd, rg[:, e, :], ALU.mult, ALU.mult,
                                  [xb[n], sbf, rgb], [j32b, lb], accum=L_[:, e:e + 1])
                        k.emit(DVE, lambda e_, o=L_[:, 24:25], i=L_[:, 0:8]: e_.reduce_max(out=o, in_=i, axis=AX.X),
                               [], [lb])
                        k.ts1(L_[:, 8:16], L_[:, 0:8], L_[:, 24:25], ALU.is_equal, [], [lb])
                        k.stt(L_[:, 16:24], L_[:, 8:16], -1e30, L_[:, 0:8], ALU.mult, ALU.add, [], [lb])
                        k.emit(DVE, lambda e_, o=L_[:, 25:26], i=L_[:, 16:24]: e_.reduce_max(out=o, in_=i, axis=AX.X),
                               [], [lb])
                        k.ts1(L_[:, 16:24], L_[:, 16:24], L_[:, 25:26], ALU.is_equal, [], [lb])
                        k.tt(sel[:, T, :], L_[:, 8:16], L_[:, 16:24], ALU.add, [lb], [selb])
                        k.tt(L_[:, 26:27], L_[:, 25:26], L_[:, 24:25], ALU.subtract, [], [lb])
                        k.act(L_[:, 26:27], L_[:, 26:27], AF.Exp, [], [lb])
                        k.ts(L_[:, 27:28], L_[:, 26:27], 1.0, 0.0, ALU.add, ALU.add, [], [lb])
                        k.recip(L_[:, 27:28], L_[:, 27:28], [], [lb])
                        k.tt(L_[:, 28:29], L_[:, 26:27], L_[:, 27:28], ALU.mult, [], [lb])
                        k.ts1(L_[:, 8:16], L_[:, 8:16], L_[:, 27:28], ALU.mult, [], [lb])
                        k.stt(gate[:, n, :], L_[:, 16:24], L_[:, 28:29], L_[:, 8:16], ALU.mult, ALU.add,
                              [lb], [gateb[n]])
                    selv = sel[:, :, :].rearrange("p t e -> p (t e)")
                    b1, b1b = next_bank()
                    k.mm_group(b1[:, 0:128], [(ustrict[:, :], selv)], [selb, cb], b1b)
                    k.copy(pos[:, :, :].rearrange("p t e -> p (t e)"), b1[:, 0:128], [b1b], [posb])
                    b2, b2b = next_bank()
                    k.mm_group(b2[:, 0:128], [(ones32[:, :], selv)], [selb, cb], b2b)
                    k.copy(tot[:, :, :].rearrange("p t e -> p (t e)"), b2[:, 0:128], [b2b], [totb])
                    k.memset(offs[:, 0, :], 0.0, [offb])
                    for T in range(1, 16):
                        k.tt(offs[:, T, :], offs[:, T - 1, :], tot[:, T - 1, :], ALU.add, [totb], [offb])
                    k.tt(pos[:, :, :], pos[:, :, :], offs[:, :, :], ALU.add, [offb], [posb])
                    k.tt(misc[:, 0:8], offs[:, 15, :], tot[:, 15, :], ALU.add, [offb, totb], [miscb])
                    for c, cap in enumerate(CAPS):
                        k.ts(misc[:, 8 + 8 * c:16 + 8 * c], misc[:, 0:8], float(cap) + 0.5, 0.0, ALU.is_lt, ALU.add,
                             [], [miscb])
                    k.copy(cond_i[:, 0:8 * len(CAPS)], misc[:, 8:8 + 8 * len(CAPS)], [miscb], [condb])
                    if FORCE_CLASS is not None:
                        for c in range(len(CAPS)):
                            k.memset(cond_i[:, 8 * c:8 * c + 8], 1 if c >= FORCE_CLASS else 0, [condb])
                    p.barrier()

                def expert_sparse(e, cap):
                    NJ = cap // 128
                    chunks = [(0, 512)] + ([(512, cap - 512)] if cap > 512 else [])
                    with ExitStack() as sp_:
                        Pb = [sb(sp_, "P%d" % i, [128, cap], BF16) for i in range(4)]
                        Pbb = [Buf() for _ in range(4)]
                        pi = [0]
                        hTe = sb(sp_, "hTe", [128, 8, cap], BF16)
                        hTeb = Buf()
                        GW = GFS * 128
                        slots = []
                        for s_ in range(2):
                            slots.append((sb(sp_, "swg%d" % s_, [128, 8, GW], BF16),
                                          sb(sp_, "swu%d" % s_, [128, 8, GW], BF16),
                                          sb(sp_, "swd%d" % s_, [128, GFS, D], BF16), Buf()))
                        actt = [sb(sp_, "sact%d" % i, [128, GFS, cap], BF16) for i in range(2)]
                        actb = [Buf(), Buf()]
                        sg = [sb(sp_, "ssg%d" % i, [128, cap], F32) for i in range(2)]
                        sgb = [Buf(), Buf()]
                        oe32 = sb(sp_, "oe32", [128, NJ, D], F32)
                        oe_bf = sb(sp_, "oe_bf", [128, NJ, D], BF16)
                        oeb = [[Buf(), Buf()] for _ in range(NJ)]
                        oebf_b = Buf()
                        PT = [sb(sp_, "PT%d" % i, [128, NJ, 128], BF16) for i in range(2)]
                        PTb = [Buf(), Buf()]

                        def build_P(T, c0, w):
                            i = pi[0] % 4
                            pi[0] += 1
                            k.ts(Pb[i][:, 0:w], iota[:, c0:c0 + w], pos[:, T, e:e + 1], sel[:, T, e:e + 1],
                                 ALU.is_equal, ALU.mult, [cb, posb, selb], [Pbb[i]])
                            return Pb[i], Pbb[i]

                        for (c0, w) in chunks:
                            per_bank = 512 // w
                            nb = 8 // per_bank
                            bks = [next_bank() for _ in range(nb)]
                            allb = [b for _, b in bks]
                            tok = None
                            for T in range(16):
                                Pt, Ptb = build_P(T, c0, w)
                                deps = [Ptb.w, hab[T].w]
                                if T == 0:
                                    deps += k.deps_of([], allb)
                                for kc in range(8):
                                    o = bks[kc // per_bank][0][:, (kc % per_bank) * w:(kc % per_bank + 1) * w]
                                    tok = p.op(PE, (lambda e_, o=o, a=h_all[:, T, kc * 128:(kc + 1) * 128], r=Pt[:, 0:w],
                                                    s_=(T == 0), t_=(T == 15): e_.matmul(o, lhsT=a, rhs=r, start=s_, stop=t_)),
                                               deps if kc == 0 else (), kc == 7)
                                Ptb.add_read(tok)
                                hab[T].add_read(tok)
                            for b in allb:
                                b.set_write(tok)
                            for bi, (bk, bkb) in enumerate(bks):
                                k.act(hTe[:, bi * per_bank:(bi + 1) * per_bank, c0:c0 + w],
                                      bk[:, 0:per_bank * w].rearrange("p (a b) -> p a b", a=per_bank), AF.Copy,
                                      [bkb], [hTeb])
                        nch = D_FFE // 128
                        ngrp = (nch + GFS - 1) // GFS
                        ginfo = {}
                        sgi_box = [0]

                        def ffn_s1(g_):
                            c0f = g_ * GFS
                            nf = min(GFS, nch - c0f)
                            wg_s, wu_s, wd_s, wsb = slots[g_ % 2]
                            sem = "wslot%d" % (g_ % 2)
                            k.dma(POOL, wg_s[:, :, 0:nf * 128],
                                  mwg_d[e, :, c0f * 128:(c0f + nf) * 128].rearrange("(kc p) n -> p kc n", p=128),
                                  writes=[wsb], sem=sem)
                            k.dma(POOL, wu_s[:, :, 0:nf * 128],
                                  mwu_d[e, :, c0f * 128:(c0f + nf) * 128].rearrange("(kc p) n -> p kc n", p=128),
                                  writes=[wsb], sem=sem)
                            k.dma(POOL, wd_s[:, 0:nf, :],
                                  mwd_d[e, c0f * 128:(c0f + nf) * 128, :].rearrange("(f p) n -> p f n", p=128),
                                  writes=[wsb], sem=sem)
                            ab_i = g_ % 2
                            ginfo[g_] = (nf, wd_s, wsb, ab_i)
                            for f in range(nf):
                                fs_ = slice(f * 128, (f + 1) * 128)
                                si = sgi_box[0] % 2
                                sgi_box[0] += 1
                                for (c0, w) in chunks:
                                    if w == 512:
                                        bg, bgb = next_bank()
                                        bu, bub = next_bank()
                                        k.mm_group(bg[:, :], [(wg_s[:, kc, fs_], hTe[:, kc, c0:c0 + w]) for kc in range(8)],
                                                   [wsb, hTeb], bgb)
                                        k.mm_group(bu[:, :], [(wu_s[:, kc, fs_], hTe[:, kc, c0:c0 + w]) for kc in range(8)],
                                                   [wsb, hTeb], bub)
                                        g_ap, u_ap = bg[:, :], bu[:, :]
                                    else:
                                        bs, bsb = next_bank()
                                        deps = k.deps_of([wsb, hTeb], [bsb])
                                        tok = None
                                        for wi, w_s in enumerate((wg_s, wu_s)):
                                            for kc in range(8):
                                                tok = p.op(PE, (lambda e_, o=bs[:, wi * w:(wi + 1) * w], a=w_s[:, kc, fs_],
                                                                r=hTe[:, kc, c0:c0 + w], s_=(kc == 0), t_=(kc == 7):
                                                                e_.matmul(o, lhsT=a, rhs=r, start=s_, stop=t_)),
                                                           deps if (wi == 0 and kc == 0) else (), (wi == 1 and kc == 7))
                                        wsb.add_read(tok)
                                        hTeb.add_read(tok)
                                        bsb.set_write(tok)
                                        bgb = bub = bsb
                                        g_ap, u_ap = bs[:, 0:w], bs[:, w:2 * w]
                                    k.act(sg[si][:, c0:c0 + w], g_ap, AF.Silu, [bgb], [sgb[si]])
                                    k.tt(actt[ab_i][:, f, c0:c0 + w], sg[si][:, c0:c0 + w], u_ap, ALU.mult,
                                         [sgb[si], bub], [actb[ab_i]])

                        def ffn_s2(g_):
                            nf, wd_s, wsb, ab_i = ginfo[g_]
                            for j in range(NJ):
                                for hf in range(2):
                                    cs = slice(hf * 512, (hf + 1) * 512)
                                    bk, bkb = next_bank()
                                    k.mm_group(bk[:, :], [(actt[ab_i][:, f, j * 128:(j + 1) * 128], wd_s[:, f, cs])
                                                          for f in range(nf)], [wsb, actb[ab_i]], bkb)
                                    ob = oeb[j][hf]
                                    if g_ == 0:
                                        k.act(oe32[:, j, cs], bk[:, :], AF.Copy, [bkb], [ob])
                                    elif g_ < ngrp - 1:
                                        k.tt(oe32[:, j, cs], oe32[:, j, cs], bk[:, :], ALU.add, [bkb], [ob])
                                    else:
                                        k.tt(oe_bf[:, j, cs], oe32[:, j, cs], bk[:, :], ALU.add, [bkb, ob], [oebf_b])

                        ffn_s1(0)
                        for g_ in range(ngrp):
                            if g_ + 1 < ngrp:
                                ffn_s1(g_ + 1)
                            ffn_s2(g_)
                        pend = None
                        for T in range(17):
                            cur = None
                            if T < 16:
                                Pt, Ptb = build_P(T, 0, cap)
                                bk, bkb = next_bank()
                                bkbf = bk.bitcast(BF16)
                                outs = [bkbf[:, j * 128:(j + 1) * 128] for j in range(NJ)]
                                pairs = [(Pt[:, j * 128:(j + 1) * 128], ident[:, :]) for j in range(NJ)]
                                k.mm_group(outs, pairs, [Ptb, cb], bkb, transpose=True)
                                pti = T % 2
                                k.act(PT[pti][:, :, :], bkbf[:, 0:cap].rearrange("p (j t) -> p j t", j=NJ), AF.Copy,
                                      [bkb], [PTb[pti]])
                                cur = (T, pti)
                            if pend is not None:
                                Tp, ptp = pend
                                n = Tp + 1
                                for hf in range(2):
                                    cs = slice(hf * 512, (hf + 1) * 512)
                                    bk2, bk2b = next_bank()
                                    k.mm_group(bk2[:, :], [(PT[ptp][:, j, :], oe_bf[:, j, cs]) for j in range(NJ)],
                                               [PTb[ptp], oebf_b], bk2b)
                                    k.stt(x_tok[:, n, cs], bk2[:, :], gate[:, n, e:e + 1], x_tok[:, n, cs],
                                          ALU.mult, ALU.add, [bk2b, gateb[n]], [xb[n]])
                            pend = cur
                        p.barrier()

                def expert_dense(e):
                    with ExitStack() as db:
                        h2T = sb(db, "h2T", [128, 8, NT * 128], BF16)
                        h2b = [Buf() for _ in range(NT)]
                        for n in tiles:
                            T = n - 1
                            bk, bkb = next_bank()
                            bkbf = bk.bitcast(BF16)
                            outs = [bkbf[:, kc * 128:(kc + 1) * 128] for kc in range(8)]
                            pairs = [(h_all[:, T, kc * 128:(kc + 1) * 128], ident[:, :]) for kc in range(8)]
                            k.mm_group(outs, pairs, [hab[T], cb], bkb, transpose=True)
                            k.act(h2T[:, :, n * 128:(n + 1) * 128], bkbf[:, :].rearrange("p (k t) -> p k t", k=8),
                                  AF.Copy, [bkb], [h2b[n]])
                        ffn_stream(db, True, tiles, h2T, h2b, GFS, experts=[e])
                        p.barrier()

                def cflag(c, e):
                    return cond_i[0:1, 8 * c + e:8 * c + e + 1]

                for e in range(NE):
                    p.cond_region(
                        cflag(1, e), [],
                        lambda e=e: p.cond_region(cflag(0, e), [], lambda: expert_sparse(e, CAPS[0]),
                                                  lambda: expert_sparse(e, CAPS[1])),
                        lambda e=e: p.cond_region(cflag(2, e), [], lambda: expert_sparse(e, CAPS[2]),
                                                  lambda: expert_dense(e)))
                    p.barrier()

        def final_phase(do_norm):
            with ExitStack() as os_:
                gf_bc = sb(os_, "gf_bc", [128, D], F32)
                gfb = Buf()
                outt = [sb(os_, "outt%d" % i, [128, D], F32) for i in range(2)]
                outb = [Buf(), Buf()]
                junk = sb(os_, "junko", [128, D], BF16)
                junkb = Buf()
                toks = []
                if do_norm:
                    k.dma(SP, gf_bc[:], fg_d[0:1, :].partition_broadcast(128), writes=[gfb], sem="gf")
                for n in range(1, NT):
                    if do_norm:
                        oi = n % 2
                        rstd, sbf = rms_stats(x_tok[:, n, :], xb[n], junk[:], junkb, 1.0 / 32.0)
                        k.stt(outt[oi][:], x_tok[:, n, :], rstd, gf_bc[:], ALU.mult, ALU.mult,
                              [xb[n], sbf, gfb], [outb[oi]])
                        toks.append(k.dma(SP, y_d[(n - 1) * 128:n * 128, :], outt[oi][:], reads=[outb[oi]], sem="out%d" % oi))
                    else:
                        toks.append(k.dma(SP, y_d[(n - 1) * 128:n * 128, :], x_tok[:, n, :], reads=[xb[n]], sem="out0"))
                p.wait_only(SP, toks)
                p.barrier()

        mixer_phase(0)
        if stop_after == "M0":
            final_phase(False)
            p.flush()
            return nc
        ffn_phase0()
        if stop_after == "F0":
            final_phase(False)
            p.flush()
            return nc
        k.ts1(x_tok[:, 0, :], x_tok[:, 0, :], flag[:, 0:1], ALU.mult, [cb], [xb[0]])
        mixer_phase(1)
        if stop_after == "M1":
            final_phase(False)
            p.flush()
            return nc
        moe_phase()
        final_phase(True)
        p.flush()
    return nc


def _prep_shared(inp):
    f = lambda a: np.ascontiguousarray(np.asarray(a, dtype=np.float32))
    sh = {}
    sh["ident"] = np.eye(128, dtype=np.float32)
    sh["ustrict"] = np.triu(np.ones((128, 128), np.float32), 1)
    sh["iota"] = np.ascontiguousarray(np.broadcast_to(np.arange(CAPS[-1], dtype=np.float32), (128, CAPS[-1])))
    pp = np.zeros((128, 2, NPP), np.float32)
    wins = np.array([[2.0, 4.0], [8.0, 16.0]], np.float32)
    for l in range(2):
        pp[:, l, PP_PSCALE:PP_PSCALE + 2] = f(inp["pool_scale"])[l].reshape(2, 128).T
        dw = f(inp["conv_dw_w"])[l]
        pp[:, l, PP_DWW:PP_DWW + 93] = dw.reshape(31, 3, 128).transpose(2, 1, 0).reshape(128, 93)
        pp[:, l, PP_DWB:PP_DWB + 3] = f(inp["conv_dw_b"])[l].reshape(3, 128).T
        pp[:, l, PP_LNG:PP_LNG + 3] = f(inp["conv_ln_g"])[l].reshape(3, 128).T
        pp[:, l, PP_LNB:PP_LNB + 3] = f(inp["conv_ln_b"])[l].reshape(3, 128).T
        pp[:, l, PP_PWB:PP_PWB + 3] = f(inp["conv_pw_b"])[l].reshape(3, 128).T
        for c in range(2):
            pp[0:64, l, PP_INVW + c] = 1.0 / wins[c, 0]
            pp[64:128, l, PP_INVW + c] = 1.0 / wins[c, 1]
    sh["pp"] = pp.reshape(128, 2 * NPP)
    sh["norm1_g"] = f(inp["norm1_g"])
    sh["norm2_g"] = f(inp["norm2_g"])
    sh["final_g"] = f(inp["final_g"]).reshape(1, D)
    sh["gm_norm_g"] = f(inp["gm_norm_g"])
    sh["gm_b"] = f(inp["gm_b"]).reshape(2, 512)
    sh["w_in"] = f(inp["w_in"])
    sh["pool_w"] = f(inp["pool_w"])
    sh["gm_wsT"] = np.ascontiguousarray(f(inp["gm_ws"]).transpose(0, 1, 3, 2))
    sh["conv_pw_w"] = f(inp["conv_pw_w"])
    sh["w_out"] = f(inp["w_out"])
    sh["ffn_wg"] = f(inp["ffn_wg"])[0]
    sh["ffn_wu"] = f(inp["ffn_wu"])[0]
    sh["ffn_wd"] = f(inp["ffn_wd"])[0]
    sh["routerT"] = np.ascontiguousarray(f(inp["moe_router"])[0].T)
    sh["moe_wg"] = f(inp["moe_wg"])[0]
    sh["moe_wu"] = f(inp["moe_wu"])[0]
    sh["moe_wd"] = f(inp["moe_wd"])[0]
    return sh


def _prep_core(x, c):
    b, q = c // 4, c % 4
    xin = np.zeros((NT * 128, D), np.float32)
    xin[128:] = x[b, q * 2048:(q + 1) * 2048]
    if q > 0:
        xin[:128] = x[b, q * 2048 - 128:q * 2048]
    flag = np.full((128, 1), 1.0 if q > 0 else 0.0, np.float32)
    pinv = np.zeros((128, 2, 16), np.float32)
    wins = [[2, 4], [8, 16]]
    for cc in range(2):
        for half in range(2):
            w = wins[cc][half]
            for j in range(16):
                cntv = min(j + 1, w) if q == 0 else w
                pinv[half * 64:(half + 1) * 64, cc, j] = 1.0 / cntv
    return {"x": xin, "flag": flag, "pool_inv": pinv.reshape(128, 32)}


_NC_CACHE = {}


def run(inputs, stop_after=None, trace=False):
    x = np.asarray(inputs["x"], dtype=np.float32)
    sh = _prep_shared(inputs)
    in_maps = []
    for c in range(8):
        m = dict(sh)
        m.update(_prep_core(x, c))
        in_maps.append(m)
    if stop_after not in _NC_CACHE:
        _NC_CACHE[stop_after] = build_program(stop_after)
    nc = _NC_CACHE[stop_after]
    res = run_bass_kernel_spmd(nc, in_maps, core_ids=list(range(8)), **({"trace": True} if trace else {}))
    out = np.zeros((2, 8192, D), np.float32)
    for c in range(8):
        b, q = c // 4, c % 4
        out[b, q * 2048:(q + 1) * 2048] = res.results[c]["y"]
    return out, res


def kernel(**inputs):
    out, _ = run(inputs)
    return out
```

```python
import numpy as np
from contextlib import ExitStack
import concourse.bass as bass
import concourse.mybir as mybir
from concourse.bass_utils import run_bass_kernel_spmd

F32 = mybir.dt.float32
BF16 = mybir.dt.bfloat16
I32 = mybir.dt.int32
AF = mybir.ActivationFunctionType
ALU = mybir.AluOpType
AX = mybir.AxisListType

PE, ACT, DVE, POOL, SP = "pe", "act", "dve", "pool", "sp"
ENGS = (PE, ACT, DVE, POOL, SP)

D = 1024
NT = 17
D_IN = 1792
D_FF = 2816
D_FFE = 3584
NE = 8
EPS = 1e-6
NPP = 112
PP_PSCALE = 0
PP_DWW = 2
PP_DWB = 95
PP_LNG = 98
PP_LNB = 101
PP_PWB = 104
PP_INVW = 107
TGM = 2
GF = 4
GFS = 2
CAPS = (512, 640, 768)
FORCE_CLASS = None


class Prog:
    def __init__(self, nc, stack):
        self.nc = nc
        self.stack = stack
        self.ops = {e: [] for e in ENGS}
        self.sems = {}
        self.cnt = {}
        self.seen = {e: {} for e in ENGS}
        for e in ENGS:
            self._mksem("eng_" + e)

    def _mksem(self, key):
        if key not in self.sems:
            self.sems[key] = self.stack.enter_context(self.nc.semaphore(key))
            self.cnt[key] = 0
        return self.sems[key]

    def _waits(self, eng, deps):
        out = []
        for d in deps:
            if d is None:
                continue
            key, val = d
            if eng == PE and key == "eng_pe":
                continue
            if self.seen[eng].get(key, 0) >= val:
                continue
            self.seen[eng][key] = val
            out.append((self.sems[key], val))
        return out

    def op(self, eng, fn, deps=(), inc=True):
        waits = self._waits(eng, deps)
        key = "eng_" + eng
        tok = None
        if inc:
            self.cnt[key] += 1
            tok = (key, self.cnt[key])
        self.ops[eng].append((waits, fn, (self.sems[key], 1) if inc else None))
        return tok

    def dma(self, eng, out, in_, semname, deps=()):
        self._mksem(semname)
        waits = self._waits(eng, deps)
        self.cnt[semname] += 16
        tok = (semname, self.cnt[semname])
        self.ops[eng].append(
            (waits, lambda e, o=out, i=in_: e.dma_start(out=o, in_=i), (self.sems[semname], 16))
        )
        return tok

    def wait_only(self, eng, deps):
        waits = self._waits(eng, deps)
        if waits:
            self.ops[eng].append((waits, None, None))

    def barrier(self):
        toks = [(k, v) for k, v in self.cnt.items() if v > 0]
        for e in ENGS:
            self.wait_only(e, toks)

    def cond_region(self, cond_ap, cond_deps, then_fn, else_fn):
        for e in ENGS:
            self.ops[e].append(("IF", cond_ap, self._waits(e, cond_deps)))
        snap_cnt = dict(self.cnt)
        snap_seen = {e: dict(d) for e, d in self.seen.items()}
        Buf.reset_all()
        then_fn()
        then_cnt = dict(self.cnt)
        then_end = {e: len(self.ops[e]) for e in ENGS}
        self.cnt = dict(snap_cnt)
        for kk in then_cnt:
            self.cnt.setdefault(kk, 0)
        self.seen = {e: dict(d) for e, d in snap_seen.items()}
        for e in ENGS:
            self.ops[e].append(("ELSE",))
        Buf.reset_all()
        else_fn()
        else_cnt = dict(self.cnt)
        keys = set(then_cnt) | set(else_cnt)
        final = {kk: max(then_cnt.get(kk, 0), else_cnt.get(kk, 0)) for kk in keys}

        def equalizers(branch_cnt):
            per_eng = {e: [] for e in ENGS}
            for kk in sorted(keys):
                diff = final[kk] - branch_cnt.get(kk, 0)
                if diff <= 0:
                    continue
                eng = kk[4:] if kk.startswith("eng_") else SP
                per_eng[eng].append(("EQ", self.sems[kk], branch_cnt.get(kk, 0), diff))
            return per_eng

        eq_then = equalizers(then_cnt)
        eq_else = equalizers(else_cnt)
        for e in ENGS:
            self.ops[e][then_end[e]:then_end[e]] = eq_then[e]
            self.ops[e].extend(eq_else[e])
            self.ops[e].append(("ENDIF",))
        self.cnt = final
        self.seen = snap_seen
        Buf.reset_all()

    def flush(self):
        nc = self.nc
        ops = self.ops

        def run(e, lst):
            cms = []
            for item in lst:
                tag = item[0]
                if tag == "IF":
                    for s_, v in item[2]:
                        e.wait_ge(s_, v)
                    val = e.value_load(item[1])
                    cm = e.If(val == 1)
                    cm.__enter__()
                    cms.append(cm)
                elif tag == "ELSE":
                    cms.pop().__exit__(None, None, None)
                    cm = e.Else()
                    cm.__enter__()
                    cms.append(cm)
                elif tag == "ENDIF":
                    cms.pop().__exit__(None, None, None)
                elif tag == "EQ":
                    _, sem, have, diff = item
                    if have > 0:
                        e.wait_ge(sem, have)
                    e.sem_inc(sem, diff)
                else:
                    waits, fn, inc = item
                    for s_, v in waits:
                        e.wait_ge(s_, v)
                    if fn is not None:
                        ins = fn(e)
                        if inc is not None:
                            ins.then_inc(inc[0], inc[1])

        with nc.Block() as block:
            @block.tensor
            def _(e):
                run(e, ops[PE])

            @block.scalar
            def _(e):
                run(e, ops[ACT])

            @block.vector
            def _(e):
                run(e, ops[DVE])

            @block.gpsimd
            def _(e):
                run(e, ops[POOL])

            @block.sync
            def _(e):
                run(e, ops[SP])
        self.ops = {e: [] for e in ENGS}


class Buf:
    ALL = []

    def __init__(self, name=""):
        self.name = name
        self.w = None
        self.r = {}
        Buf.ALL.append(self)

    @staticmethod
    def reset_all():
        for b in Buf.ALL:
            b.w = None
            b.r = {}

    def add_read(self, tok):
        if tok is None:
            return
        k, v = tok
        if self.r.get(k, 0) < v:
            self.r[k] = v

    def set_write(self, tok):
        self.w = tok
        self.r = {}


class K:
    def __init__(self, nc, stack):
        self.nc = nc
        self.p = Prog(nc, stack)
        self.dma_n = 0

    def deps_of(self, reads, writes):
        deps = []
        for b in reads:
            deps.append(b.w)
        for b in writes:
            deps.append(b.w)
            deps.extend(b.r.items())
        return deps

    def emit(self, eng, fn, reads=(), writes=()):
        tok = self.p.op(eng, fn, self.deps_of(reads, writes), True)
        for b in reads:
            b.add_read(tok)
        for b in writes:
            b.set_write(tok)
        return tok

    def dma(self, eng, out, in_, reads=(), writes=(), sem=None):
        assert sem is not None
        deps = []
        for b in reads:
            deps.append(b.w)
        for b in writes:
            if not (b.w is not None and b.w[0] == sem):
                deps.append(b.w)
            deps.extend(b.r.items())
        tok = self.p.dma(eng, out, in_, sem, deps)
        for b in reads:
            b.add_read(tok)
        for b in writes:
            b.set_write(tok)
        return tok

    def mm_group(self, out, pairs, reads, bank, transpose=False):
        deps = self.deps_of(reads, [bank])
        n = len(pairs)
        tok = None
        for i, (l, r) in enumerate(pairs):
            last = i == n - 1
            if transpose:
                fn = (lambda e, o=out[i], a=l, b=r: e.transpose(o, a, b))
            else:
                fn = (lambda e, o=out, a=l, b=r, s=(i == 0), t=last: e.matmul(o, lhsT=a, rhs=b, start=s, stop=t))
            tok = self.p.op(PE, fn, deps if i == 0 else (), last)
        for b in reads:
            b.add_read(tok)
        bank.set_write(tok)
        return tok

    def mm_multi(self, groups, reads, bank):
        deps = self.deps_of(reads, [bank])
        n = len(groups)
        tok = None
        for i, (o, l, r) in enumerate(groups):
            last = i == n - 1
            fn = (lambda e, o=o, a=l, b=r: e.matmul(o, lhsT=a, rhs=b, start=True, stop=True))
            tok = self.p.op(PE, fn, deps if i == 0 else (), last)
        for b in reads:
            b.add_read(tok)
        bank.set_write(tok)
        return tok

    def act(self, out, in_, func, reads, writes, bias=None, scale=None, accum=None):
        kw = {}
        if bias is not None:
            kw["bias"] = bias
        if scale is not None:
            kw["scale"] = scale
        if accum is not None:
            kw["accum_out"] = accum
        return self.emit(ACT, lambda e: e.activation(out=out, in_=in_, func=func, **kw), reads, writes)

    def tt(self, out, in0, in1, op, reads, writes, eng=DVE):
        return self.emit(eng, lambda e: e.tensor_tensor(out=out, in0=in0, in1=in1, op=op), reads, writes)

    def ts(self, out, in0, s1, s2, op0, op1, reads, writes, eng=DVE):
        return self.emit(eng, lambda e: e.tensor_scalar(out=out, in0=in0, scalar1=s1, scalar2=s2, op0=op0, op1=op1),
                         reads, writes)

    def ts1(self, out, in0, s1, op0, reads, writes, eng=DVE):
        return self.emit(eng, lambda e: e.tensor_scalar(out=out, in0=in0, scalar1=s1, scalar2=None, op0=op0),
                         reads, writes)

    def stt(self, out, in0, scalar, in1, op0, op1, reads, writes, accum=None, eng=DVE):
        kw = {}
        if accum is not None:
            kw["accum_out"] = accum
        return self.emit(eng, lambda e: e.scalar_tensor_tensor(out=out, in0=in0, scalar=scalar, in1=in1,
                                                               op0=op0, op1=op1, **kw), reads, writes)

    def copy(self, out, in_, reads, writes, eng=DVE):
        return self.emit(eng, lambda e: e.tensor_copy(out=out, in_=in_), reads, writes)

    def recip(self, out, in_, reads, writes):
        return self.emit(DVE, lambda e: e.reciprocal(out=out, in_=in_), reads, writes)

    def memset(self, ap, val, writes, eng=DVE):
        return self.emit(eng, lambda e: e.memset(ap, val), (), writes)


def build_program(stop_after=None):
    nc = bass.Bass("TRN2", target_bir_lowering=False)

    def din(name, shape):
        return nc.dram_tensor(name, list(shape), F32, kind="ExternalInput").ap()

    x_d = din("x", [NT * 128, D])
    flag_d = din("flag", [128, 1])
    pinv_d = din("pool_inv", [128, 32])
    ident_d = din("ident", [128, 128])
    ustrict_d = din("ustrict", [128, 128])
    iota_d = din("iota", [128, CAPS[-1]])
    pp_d = din("pp", [128, 2 * NPP])
    n1g_d = din("norm1_g", [2, D])
    n2g_d = din("norm2_g", [2, D])
    fg_d = din("final_g", [1, D])
    gmg_d = din("gm_norm_g", [2, 384])
    gmb_d = din("gm_b", [2, 512])
    w_in_d = din("w_in", [2, D, D_IN])
    pool_w_d = din("pool_w", [2, 4, 64, 64])
    wsT_d = din("gm_wsT", [2, 4, 128, 128])
    pw_d = din("conv_pw_w", [2, 384, 384])
    w_out_d = din("w_out", [2, D, D])
    fwg_d = din("ffn_wg", [D, D_FF])
    fwu_d = din("ffn_wu", [D, D_FF])
    fwd_d = din("ffn_wd", [D_FF, D])
    rT_d = din("routerT", [NE, D])
    mwg_d = din("moe_wg", [NE, D, D_FFE])
    mwu_d = din("moe_wu", [NE, D, D_FFE])
    mwd_d = din("moe_wd", [NE, D_FFE, D])
    y_d = nc.dram_tensor("y", [16 * 128, D], F32, kind="ExternalOutput").ap()

    with ExitStack() as st:
        ARENA_WORDS = 52600
        arena = st.enter_context(nc.sbuf_tensor("arena", [128, ARENA_WORDS], F32))
        atop = [0]
        scopes = {}

        def _release(mark):
            atop[0] = mark

        def sb(stack, name, shape, dt):
            if stack is not st and not getattr(stack, "_arena_marked", False):
                stack._arena_marked = True
                stack.callback(_release, atop[0])
            n = 1
            for d_ in shape[1:]:
                n *= d_
            nbytes = n * (4 if dt in (F32, I32) else 2)
            words = ((nbytes + 3) // 4 + 7) // 8 * 8
            assert atop[0] + words <= ARENA_WORDS, ("SBUF arena overflow", name, atop[0], words)
            v = arena[:, atop[0]:atop[0] + words]
            atop[0] += words
            if dt != F32:
                v = v.bitcast(dt)
            v = v[:, 0:n]
            if len(shape) == 3:
                v = v.rearrange("p (a b) -> p a b", a=shape[1])
            return v

        k = K(nc, st)
        p = k.p

        x_tok = sb(st, "x_tok", [128, NT, D], F32)
        xb = [Buf("x%d" % n) for n in range(NT)]
        ident = sb(st, "ident", [128, 128], BF16)
        ones32 = sb(st, "ones32", [128, 128], F32)
        ustrict = sb(st, "ustrict", [128, 128], F32)
        iota = sb(st, "iota", [128, CAPS[-1]], F32)
        pp = sb(st, "pp", [128, 2 * NPP], F32)
        flag = sb(st, "flag", [128, 1], F32)
        pinv = sb(st, "pinv", [128, 2, 16], F32)
        stt_t = sb(st, "stats", [128, 8, 4], F32)
        gate = sb(st, "gate", [128, NT, NE], F32)
        cb = Buf("consts")
        stb = [Buf("st%d" % i) for i in range(8)]
        gateb = [Buf("gate%d" % n) for n in range(NT)]
        banks = [st.enter_context(nc.psum_tensor("bank%d" % i, [128, 512], F32)) for i in range(8)]
        bankb = [Buf("bank%d" % i) for i in range(8)]
        ring = [0]
        stat_i = [0]

        def next_bank():
            i = ring[0]
            ring[0] = (i + 1) % 8
            return banks[i], bankb[i]

        def next_stat():
            i = stat_i[0]
            stat_i[0] = (i + 1) % 8
            return stt_t[:, i, :], stb[i]

        def ppc(l, col, n=1, lo=0, hi=128):
            return pp[lo:hi, l * NPP + col: l * NPP + col + n]

        k.memset(ones32[:], 1.0, [cb])
        k.dma(POOL, ident[:], ident_d, writes=[cb], sem="consts")
        k.dma(POOL, pp[:], pp_d, writes=[cb], sem="consts")
        k.dma(POOL, ustrict[:], ustrict_d, writes=[cb], sem="consts")
        k.dma(POOL, iota[:], iota_d, writes=[cb], sem="consts")
        k.dma(POOL, flag[:], flag_d, writes=[cb], sem="consts")
        k.dma(POOL, pinv[:].rearrange("p c j -> p (c j)"), pinv_d, writes=[cb], sem="consts")
        for n in range(NT):
            k.dma(SP, x_tok[:, n, :], x_d[n * 128:(n + 1) * 128, :], writes=[xb[n]], sem="x%d" % n)

        def rms_stats(src_ap, srcb, junk, junkb, scale):
            sap, sbf = next_stat()
            k.act(junk, src_ap, AF.Square, [srcb], [junkb, sbf], scale=scale, accum=sap[:, 0:1])
            k.act(sap[:, 1:2], sap[:, 0:1], AF.Sqrt, [sbf], [sbf], bias=EPS)
            k.recip(sap[:, 2:3], sap[:, 1:2], [sbf], [sbf])
            return sap[:, 2:3], sbf

        def norm_and_transpose(n, g_bc, gb, h_tok, h_tokb, junk, junkb, hT_dst, hTb):
            rstd, sbf = rms_stats(x_tok[:, n, :], xb[n], junk, junkb, 1.0 / 32.0)
            k.stt(h_tok, x_tok[:, n, :], rstd, g_bc, ALU.mult, ALU.mult, [xb[n], sbf, gb], [h_tokb])
            bk, bkb = next_bank()
            bkbf = bk.bitcast(BF16)
            outs = [bkbf[:, kc * 128:(kc + 1) * 128] for kc in range(8)]
            pairs = [(h_tok[:, kc * 128:(kc + 1) * 128], ident[:, :]) for kc in range(8)]
            k.mm_group(outs, pairs, [h_tokb, cb], bkb, transpose=True)
            k.act(hT_dst, bkbf[:, :].rearrange("p (k t) -> p k t", k=8), AF.Copy, [bkb], [hTb])
            return rstd, sbf

        def mixer_phase(l):
            with ExitStack() as ms:
                NMAX = TGM * 128
                LMAX = 32 + NMAX
                w_in_sb = sb(ms, "w_in_sb", [128, 8, D_IN], BF16)
                wo_a = sb(ms, "wo_a", [128, 2, D], BF16)
                wo_b = sb(ms, "wo_b", [128, 4, D], BF16)
                wo_c = sb(ms, "wo_c", [128, 3, D], BF16)
                pw_sb = sb(ms, "pw_sb", [128, 3, 384], BF16)
                wblk = sb(ms, "wblk", [128, 2, 128], BF16)
                wsT = sb(ms, "wsT", [128, 4, 128], BF16)
                g1_bc = sb(ms, "g1_bc", [128, D], F32)
                gmg_bc = sb(ms, "gmg_bc", [128, 384], F32)
                gmb_bc = sb(ms, "gmb_bc", [128, 512], F32)
                wb = Buf("mixw")
                h_tok = [sb(ms, "h_tok%d" % i, [128, D], BF16) for i in range(2)]
                h_tokb = [Buf(), Buf()]
                junk = sb(ms, "junk", [128, D], BF16)
                junkb = Buf()
                two = range(2)
                a_ext = [sb(ms, "a_ext%d" % i, [128, 2, LMAX], F32) for i in two]
                hc_ext = [sb(ms, "hc_ext%d" % i, [128, 3, LMAX], BF16) for i in two]
                yb = [sb(ms, "yb%d" % i, [128, 4, NMAX], BF16) for i in two]
                ab, hcb, ybb = ([Buf(), Buf()] for _ in range(3))

                def same2(name, shape, dt):
                    t_ = sb(ms, name, shape, dt)
                    return [t_, t_]

                def same2b():
                    b_ = Buf()
                    return [b_, b_]

                hT = same2("hT", [128, 8, NMAX], BF16)
                sig = same2("sig", [128, 3, NMAX], F32)
                acc = same2("acc", [128, 3, NMAX], F32)
                u_sb = same2("u_sb", [128, 4, NMAX], F32)
                y_p = same2("y_p", [128, 2, NMAX], BF16)
                ya = same2("ya", [128, 2, NMAX], BF16)
                hs = same2("hs", [128, 3, NMAX], BF16)
                yc = same2("yc", [128, 3, NMAX], BF16)
                hTb, sigb, ub, ypb, yab, hsb, ycb = (same2b() for _ in range(7))
                accb1 = [Buf(), Buf(), Buf()]
                accb = [accb1, accb1]
                dg = sb(ms, "dg", [128, 93, 128], BF16)
                dgb = Buf()
                sA = sb(ms, "sA", [128, 2, LMAX], F32)
                sB = sb(ms, "sB", [128, 2, LMAX], F32)
                tmp16 = sb(ms, "tmp16", [128, 16], F32)
                v_n = [sb(ms, "v_n%d" % i, [128, 384], BF16) for i in two]
                ztmp = sb(ms, "ztmp", [128, 512], F32)
                sq = sb(ms, "sq", [128, 3, NMAX], F32)
                mean = sb(ms, "mean", [128, NMAX], F32)
                var = sb(ms, "var", [128, NMAX], F32)
                rstdc = sb(ms, "rstdc", [128, NMAX], F32)
                sAb, sBb, t16b, ztb, sqb, meanb, varb, rsb = (Buf() for _ in range(8))
                vnb = [Buf(), Buf()]

                wblkb = Buf()
                wsTb = Buf()
                k.memset(wblk[:], 0.0, [wblkb])
                for c in range(2):
                    k.dma(POOL, wblk[0:64, c, 0:64], pool_w_d[l, 2 * c], writes=[wblkb], sem="wblk")
                    k.dma(POOL, wblk[64:128, c, 64:128], pool_w_d[l, 2 * c + 1], writes=[wblkb], sem="wblk")
                k.dma(POOL, wsT[:], wsT_d[l].rearrange("h j i -> j h i"), writes=[wsTb], sem="wsT")
                k.memset(wsT[64:128, :, 0:64], 0.0, [wsTb])
                k.dma(SP, g1_bc[:], n1g_d[l:l + 1, :].partition_broadcast(128), writes=[wb], sem="mixw")
                k.dma(SP, gmg_bc[:], gmg_d[l:l + 1, :].partition_broadcast(128), writes=[wb], sem="mixw")
                k.dma(SP, gmb_bc[:], gmb_d[l:l + 1, :].partition_broadcast(128), writes=[wb], sem="mixw")
                k.dma(POOL, w_in_sb[:], w_in_d[l].rearrange("(kc p) n -> p kc n", p=128), writes=[wb], sem="mixw")
                k.dma(POOL, wo_a[:], w_out_d[l, 0:256, :].rearrange("(c p) n -> p c n", p=128), writes=[wb], sem="mixw")
                k.dma(POOL, wo_b[0:96], w_out_d[l, 256:640, :].rearrange("(h p) n -> p h n", p=96), writes=[wb], sem="mixw")
                k.dma(POOL, wo_c[:], w_out_d[l, 640:1024, :].rearrange("(c p) n -> p c n", p=128), writes=[wb], sem="mixw")
                k.dma(POOL, pw_sb[:], pw_d[l].rearrange("(c p) n -> p c n", p=128), writes=[wb], sem="mixw")
                k.memset(a_ext[0][:, :, 0:32], 0.0, [ab[0]])
                k.memset(hc_ext[0][:, :, 0:32], 0.0, [hcb[0]])
                for i in range(93):
                    k.ts1(dg[:, i, :], ident[:, :], ppc(l, PP_DWW + i), ALU.mult, [cb], [dgb])

                groups = [(0, 1)] + [(t0, TGM) for t0 in range(1, NT, TGM)]

                def stage_n(gi):
                    t0, nt = groups[gi]
                    q = gi % 2
                    if gi > 0:
                        pN = groups[gi - 1][1] * 128
                        k.copy(a_ext[q][:, :, 0:32], a_ext[1 - q][:, :, pN:pN + 32], [ab[1 - q]], [ab[q]], eng=POOL)
                        k.copy(hc_ext[q][:, :, 0:32], hc_ext[1 - q][:, :, pN:pN + 32], [hcb[1 - q]], [hcb[q]], eng=POOL)
                    for j in range(nt):
                        n = t0 + j
                        hb = (gi * TGM + j) % 2
                        norm_and_transpose(n, g1_bc[:], wb, h_tok[hb][:], h_tokb[hb], junk[:], junkb,
                                           hT[q][:, :, j * 128:(j + 1) * 128], hTb[q])

                def stage_p(gi):
                    t0, nt = groups[gi]
                    q = gi % 2
                    N = nt * 128
                    L = 32 + N
                    full = not (l == 1 and t0 == 0)

                    def proj(col0, m):
                        bk, bkb = next_bank()
                        pairs = [(w_in_sb[:, kc, col0:col0 + m], hT[q][:, kc, 0:N]) for kc in range(8)]
                        k.mm_group(bk[0:m, 0:N], pairs, [wb, hTb[q]], bkb)
                        return bk, bkb

                    for c in range(2):
                        bk, bkb = proj(c * 128, 128)
                        k.act(a_ext[q][:, c, 32:L], bk[:, 0:N], AF.Copy, [bkb], [ab[q]])
                    cvb = []
                    for c in range(3):
                        cvb.append(proj(1024 + c * 128, 128))
                    for c in range(3):
                        bk, bkb = proj(1408 + c * 128, 128)
                        k.act(sig[q][:, c, 0:N], bk[:, 0:N], AF.Sigmoid, [bkb], [sigb[q]])
                    for c in range(3):
                        bk, bkb = cvb[c]
                        k.tt(hc_ext[q][:, c, 32:L], bk[:, 0:N], sig[q][:, c, 0:N], ALU.mult, [bkb, sigb[q]], [hcb[q]])
                    if full:
                        for h in range(4):
                            bk, bkb = proj(256 + h * 96, 96)
                            k.act(u_sb[q][0:96, h, 0:N], bk[0:96, 0:N], AF.Copy, [bkb], [ub[q]])
                        for j in range(nt):
                            vb = j % 2
                            bk, bkb = next_bank()
                            pairs = [(hT[q][:, kc, j * 128:(j + 1) * 128], w_in_sb[:, kc, 640:1024]) for kc in range(8)]
                            k.mm_group(bk[:, 0:384], pairs, [wb, hTb[q]], bkb)
                            rstd, sbf = rms_stats(bk[:, 0:384], bkb, junk[:, 0:384], junkb, float(384.0 ** -0.5))
                            k.stt(v_n[vb][:], bk[:, 0:384], rstd, gmg_bc[:], ALU.mult, ALU.mult,
                                  [bkb, sbf, wb], [vnb[vb]])
                            zk, zkb = next_bank()
                            grp = [(zk[0:96, h * 128:(h + 1) * 128], v_n[vb][:, h * 96:(h + 1) * 96], wsT[:, h, :])
                                   for h in range(4)]
                            k.mm_multi(grp, [vnb[vb], wsTb], zkb)
                            k.tt(ztmp[0:96, :], zk[0:96, :], gmb_bc[0:96, :], ALU.add, [zkb, wb], [ztb])
                            k.tt(yb[q][0:96, :, j * 128:(j + 1) * 128],
                                 ztmp[0:96, :].rearrange("p (h i) -> p h i", h=4),
                                 u_sb[q][0:96, :, j * 128:(j + 1) * 128], ALU.mult, [ztb, ub[q]], [ybb[q]])

                def stage_b1(gi):
                    t0, nt = groups[gi]
                    q = gi % 2
                    N = nt * 128
                    L = 32 + N
                    full = not (l == 1 and t0 == 0)
                    first_own = (t0 == 1)
                    if not full:
                        return
                    A_ = a_ext[q]
                    H_ = hc_ext[q]

                    def pool_out(sbuf_t, sbuf_b, lo, hi, c):
                        k.stt(y_p[q][lo:hi, c, 0:N], sbuf_t[lo:hi, c, 32:L], ppc(l, PP_INVW + c, 1, lo, hi),
                              A_[lo:hi, c, 32:L], ALU.mult, ALU.subtract, [sbuf_b, ab[q], cb], [ypb[q]])
                        if first_own:
                            k.tt(tmp16[lo:hi, :], sbuf_t[lo:hi, c, 32:48], pinv[lo:hi, c, :], ALU.mult,
                                 [sbuf_b, cb], [t16b])
                            k.tt(y_p[q][lo:hi, c, 0:16], tmp16[lo:hi, :], A_[lo:hi, c, 32:48], ALU.subtract,
                                 [t16b, ab[q]], [ypb[q]])

                    k.tt(sA[:, :, 1:L], A_[:, :, 1:L], A_[:, :, 0:L - 1], ALU.add, [ab[q]], [sAb])
                    pool_out(sA, sAb, 0, 64, 0)
                    k.tt(sB[:, :, 3:L], sA[:, :, 3:L], sA[:, :, 1:L - 2], ALU.add, [sAb], [sBb])
                    pool_out(sB, sBb, 64, 128, 0)
                    k.tt(sA[:, :, 7:L], sB[:, :, 7:L], sB[:, :, 3:L - 4], ALU.add, [sBb], [sAb])
                    pool_out(sA, sAb, 0, 64, 1)
                    k.tt(sB[:, :, 15:L], sA[:, :, 15:L], sA[:, :, 7:L - 8], ALU.add, [sAb], [sBb])
                    pool_out(sB, sBb, 64, 128, 1)
                    for c in range(2):
                        bk, bkb = next_bank()
                        k.mm_group(bk[:, 0:N], [(wblk[:, c, :], y_p[q][:, c, 0:N])], [wblkb, ypb[q]], bkb)
                        k.act(ya[q][:, c, 0:N], bk[:, 0:N], AF.Copy, [bkb, cb], [yab[q]], scale=ppc(l, PP_PSCALE + c))

                    for c in range(3):
                        bk, bkb = next_bank()
                        pairs = [(dg[:, c * 31 + kk, :], H_[:, c, 2 + kk:2 + kk + N]) for kk in range(31)]
                        k.mm_group(bk[:, 0:N], pairs, [dgb, hcb[q]], bkb)
                        k.act(acc[q][:, c, 0:N], bk[:, 0:N], AF.Identity, [bkb, cb], [accb[q][c]],
                              bias=ppc(l, PP_DWB + c))
                    for c in range(3):
                        k.act(sq[:, c, 0:N], acc[q][:, c, 0:N], AF.Square, [accb[q][c]], [sqb])
                    b1, b1b = next_bank()
                    k.mm_group(b1[:, 0:N], [(ones32[:, :], acc[q][:, c, 0:N]) for c in range(3)], accb[q] + [cb], b1b)
                    b2, b2b = next_bank()
                    k.mm_group(b2[:, 0:N], [(ones32[:, :], sq[:, c, 0:N]) for c in range(3)], [sqb, cb], b2b)
                    k.ts(mean[:, 0:N], b1[:, 0:N], 1.0 / 384.0, 0.0, ALU.mult, ALU.add, [b1b], [meanb])
                    k.tt(var[:, 0:N], mean[:, 0:N], mean[:, 0:N], ALU.mult, [meanb], [varb])
                    k.stt(var[:, 0:N], b2[:, 0:N], 1.0 / 384.0, var[:, 0:N], ALU.mult, ALU.subtract,
                          [b2b], [varb])
                    k.act(var[:, 0:N], var[:, 0:N], AF.Sqrt, [], [varb], bias=EPS)
                    k.recip(rstdc[:, 0:N], var[:, 0:N], [varb], [rsb])
                    for c in range(3):
                        k.tt(sq[:, c, 0:N], acc[q][:, c, 0:N], mean[:, 0:N], ALU.subtract, [accb[q][c], meanb], [sqb])
                        k.tt(sq[:, c, 0:N], sq[:, c, 0:N], rstdc[:, 0:N], ALU.mult, [rsb], [sqb])
                        k.act(hs[q][:, c, 0:N], sq[:, c, 0:N], AF.Silu, [sqb, cb], [hsb[q]],
                              bias=ppc(l, PP_LNB + c), scale=ppc(l, PP_LNG + c))
                def stage_b2(gi):
                    t0, nt = groups[gi]
                    q = gi % 2
                    N = nt * 128
                    full = not (l == 1 and t0 == 0)
                    if not full:
                        return
                    for co in range(3):
                        bk, bkb = next_bank()
                        pairs = [(pw_sb[:, ci, co * 128:(co + 1) * 128], hs[q][:, ci, 0:N]) for ci in range(3)]
                        k.mm_group(bk[:, 0:N], pairs, [wb, hsb[q]], bkb)
                        k.act(yc[q][:, co, 0:N], bk[:, 0:N], AF.Identity, [bkb, cb], [ycb[q]], bias=ppc(l, PP_PWB + co))

                    for j in range(nt):
                        n = t0 + j
                        ts_ = slice(j * 128, (j + 1) * 128)
                        for hf in range(2):
                            cs = slice(hf * 512, (hf + 1) * 512)
                            pairs = [(ya[q][:, c, ts_], wo_a[:, c, cs]) for c in range(2)]
                            pairs += [(yb[q][0:96, h, ts_], wo_b[0:96, h, cs]) for h in range(4)]
                            pairs += [(yc[q][:, c, ts_], wo_c[:, c, cs]) for c in range(3)]
                            bk, bkb = next_bank()
                            k.mm_group(bk[:, :], pairs, [wb, yab[q], ybb[q], ycb[q]], bkb)
                            k.tt(x_tok[:, n, cs], x_tok[:, n, cs], bk[:, :], ALU.add, [bkb], [xb[n]])

                ng = len(groups)
                stage_n(0)
                stage_p(0)
                for gi in range(ng):
                    if gi + 1 < ng:
                        stage_n(gi + 1)
                    stage_b1(gi)
                    if gi + 1 < ng:
                        stage_p(gi + 1)
                    stage_b2(gi)
                p.barrier()

        def ffn_stream(scope, moe, tiles, h2T, h2b, gf, experts=None):
            GW = gf * 128
            slots = []
            for s_ in range(2):
                slots.append((sb(scope, "wg%d" % s_, [128, 8, GW], BF16),
                              sb(scope, "wu%d" % s_, [128, 8, GW], BF16),
                              sb(scope, "wd%d" % s_, [128, gf, D], BF16), Buf()))
            actt = [sb(scope, "act%d" % i, [128, gf, 512], BF16) for i in range(2)]
            actb = [Buf(), Buf()]
            sg = [sb(scope, "sg%d" % i, [128, 512], F32) for i in range(2)]
            sgb = [Buf(), Buf()]
            glist = []
            if not moe:
                nch = D_FF // 128
                for c0 in range(0, nch, gf):
                    nf = min(gf, nch - c0)
                    glist.append((fwg_d[:, c0 * 128:(c0 + nf) * 128], fwu_d[:, c0 * 128:(c0 + nf) * 128],
                                  fwd_d[c0 * 128:(c0 + nf) * 128, :], nf, None))
            else:
                nch = D_FFE // 128
                for e in (experts if experts is not None else range(NE)):
                    for c0 in range(0, nch, gf):
                        nf = min(gf, nch - c0)
                        glist.append((mwg_d[e, :, c0 * 128:(c0 + nf) * 128],
                                      mwu_d[e, :, c0 * 128:(c0 + nf) * 128],
                                      mwd_d[e, c0 * 128:(c0 + nf) * 128, :], nf, e))
            tgroups = [tiles[i:i + 4] for i in range(0, len(tiles), 4)]
            cnt = 0
            sgi = 0
            for gi, (wg_ap, wu_ap, wd_ap, nf, e) in enumerate(glist):
                wg_s, wu_s, wd_s, wsb = slots[gi % 2]
                sem = "wslot%d" % (gi % 2)
                k.dma(POOL, wg_s[:, :, 0:nf * 128], wg_ap.rearrange("(kc p) n -> p kc n", p=128),
                      writes=[wsb], sem=sem)
                k.dma(POOL, wu_s[:, :, 0:nf * 128], wu_ap.rearrange("(kc p) n -> p kc n", p=128),
                      writes=[wsb], sem=sem)
                k.dma(POOL, wd_s[:, 0:nf, :], wd_ap.rearrange("(f p) n -> p f n", p=128),
                      writes=[wsb], sem=sem)
                for tg in tgroups:
                    ab_i = cnt % 2
                    cnt += 1
                    t_lo = tg[0] * 128
                    N = len(tg) * 128
                    for f in range(nf):
                        bg, bgb = next_bank()
                        k.mm_group(bg[:, 0:N], [(wg_s[:, kc, f * 128:(f + 1) * 128], h2T[:, kc, t_lo:t_lo + N])
                                                for kc in range(8)], [wsb] + [h2b[n] for n in tg], bgb)
                        bu, bub = next_bank()
                        k.mm_group(bu[:, 0:N], [(wu_s[:, kc, f * 128:(f + 1) * 128], h2T[:, kc, t_lo:t_lo + N])
                                                for kc in range(8)], [wsb] + [h2b[n] for n in tg], bub)
                        si = sgi % 2
                        sgi += 1
                        k.act(sg[si][:, 0:N], bg[:, 0:N], AF.Silu, [bgb], [sgb[si]])
                        k.tt(actt[ab_i][:, f, 0:N], sg[si][:, 0:N], bu[:, 0:N], ALU.mult, [sgb[si], bub],
                             [actb[ab_i]])
                    for j, n in enumerate(tg):
                        for hf in range(2):
                            cs = slice(hf * 512, (hf + 1) * 512)
                            bk, bkb = next_bank()
                            k.mm_group(bk[:, :], [(actt[ab_i][:, f, j * 128:(j + 1) * 128], wd_s[:, f, cs])
                                                  for f in range(nf)], [wsb, actb[ab_i]], bkb)
                            if moe:
                                k.stt(x_tok[:, n, cs], bk[:, :], gate[:, n, e:e + 1], x_tok[:, n, cs],
                                      ALU.mult, ALU.add, [bkb, gateb[n]], [xb[n]])
                            else:
                                k.tt(x_tok[:, n, cs], x_tok[:, n, cs], bk[:, :], ALU.add, [bkb], [xb[n]])

        def ffn_phase0():
            tiles = list(range(0, NT))
            with ExitStack() as fs:
                h2T = sb(fs, "h2T", [128, 8, NT * 128], BF16)
                h2b = [Buf() for _ in range(NT)]
                with ExitStack() as f1:
                    g2_bc = sb(f1, "g2_bc", [128, D], F32)
                    gb = Buf()
                    h_tok = [sb(f1, "h_tokf%d" % i, [128, D], BF16) for i in range(2)]
                    h_tokb = [Buf(), Buf()]
                    junk = sb(f1, "junkf", [128, D], BF16)
                    junkb = Buf()
                    k.dma(SP, g2_bc[:], n2g_d[0:1, :].partition_broadcast(128), writes=[gb], sem="g2")
                    for ti, n in enumerate(tiles):
                        hb = ti % 2
                        norm_and_transpose(n, g2_bc[:], gb, h_tok[hb][:], h_tokb[hb], junk[:], junkb,
                                           h2T[:, :, n * 128:(n + 1) * 128], h2b[n])
                    p.barrier()
                with ExitStack() as f2:
                    ffn_stream(f2, False, tiles, h2T, h2b, GF)
                    p.barrier()

        def moe_phase():
            tiles = list(range(1, NT))
            with ExitStack() as fs:
                h_all = sb(fs, "h_all", [128, 16, D], BF16)
                hab = [Buf() for _ in range(16)]
                sel = sb(fs, "sel", [128, 16, NE], F32)
                pos = sb(fs, "pos", [128, 16, NE], F32)
                tot = sb(fs, "tot", [128, 16, NE], F32)
                offs = sb(fs, "offs", [128, 16, NE], F32)
                misc = sb(fs, "rmisc", [128, 40], F32)
                cond_i = sb(fs, "cond_i", [128, 32], I32)
                selb, posb, totb, offb, miscb, condb = (Buf() for _ in range(6))
                with ExitStack() as f1:
                    g2_bc = sb(f1, "g2_bc", [128, D], F32)
                    gb = Buf()
                    junk = sb(f1, "junkf", [128, D], BF16)
                    junkb = Buf()
                    rg = sb(f1, "rg", [128, NE, D], F32)
                    rgb = Buf()
                    junk32 = sb(f1, "junk32", [128, D], F32)
                    j32b = Buf()
                    lg = sb(f1, "lg", [128, 2, 32], F32)
                    lgb = [Buf(), Buf()]
                    k.dma(SP, g2_bc[:], n2g_d[1:2, :].partition_broadcast(128), writes=[gb], sem="g2")
                    for e in range(NE):
                        k.dma(SP, rg[:, e, :], rT_d[e:e + 1, :].partition_broadcast(128), writes=[rgb], sem="rg")
                    for e in range(NE):
                        k.tt(rg[:, e, :], rg[:, e, :], g2_bc[:], ALU.mult, [gb], [rgb])
                    for ti, n in enumerate(tiles):
                        T = n - 1
                        hb = ti % 2
                        rstd, sbf = rms_stats(x_tok[:, n, :], xb[n], junk[:], junkb, 1.0 / 32.0)
                        k.stt(h_all[:, T, :], x_tok[:, n, :], rstd, g2_bc[:], ALU.mult, ALU.mult,
                              [xb[n], sbf, gb], [hab[T]])
                        L_ = lg[:, hb, :]
                        lb = lgb[hb]
                        for e in range(NE):
                            k.stt(junk32[:], x_tok[:, n, :], rstd, rg[:, e, :], ALU.mult, ALU.mult,
                                  [xb[n], sbf, rgb], [j32b, lb], accum=L_[:, e:e + 1])
                        k.emit(DVE, lambda e_, o=L_[:, 24:25], i=L_[:, 0:8]: e_.reduce_max(out=o, in_=i, axis=AX.X),
                               [], [lb])
                        k.ts1(L_[:, 8:16], L_[:, 0:8], L_[:, 24:25], ALU.is_equal, [], [lb])
                        k.stt(L_[:, 16:24], L_[:, 8:16], -1e30, L_[:, 0:8], ALU.mult, ALU.add, [], [lb])
                        k.emit(DVE, lambda e_, o=L_[:, 25:26], i=L_[:, 16:24]: e_.reduce_max(out=o, in_=i, axis=AX.X),
                               [], [lb])
                        k.ts1(L_[:, 16:24], L_[:, 16:24], L_[:, 25:26], ALU.is_equal, [], [lb])
                        k.tt(sel[:, T, :], L_[:, 8:16], L_[:, 16:24], ALU.add, [lb], [selb])
                        k.tt(L_[:, 26:27], L_[:, 25:26], L_[:, 24:25], ALU.subtract, [], [lb])
                        k.act(L_[:, 26:27], L_[:, 26:27], AF.Exp, [], [lb])
                        k.ts(L_[:, 27:28], L_[:, 26:27], 1.0, 0.0, ALU.add, ALU.add, [], [lb])
                        k.recip(L_[:, 27:28], L_[:, 27:28], [], [lb])
                        k.tt(L_[:, 28:29], L_[:, 26:27], L_[:, 27:28], ALU.mult, [], [lb])
                        k.ts1(L_[:, 8:16], L_[:, 8:16], L_[:, 27:28], ALU.mult, [], [lb])
                        k.stt(gate[:, n, :], L_[:, 16:24], L_[:, 28:29], L_[:, 8:16], ALU.mult, ALU.add,
                              [lb], [gateb[n]])
                    selv = sel[:, :, :].rearrange("p t e -> p (t e)")
                    b1, b1b = next_bank()
                    k.mm_group(b1[:, 0:128], [(ustrict[:, :], selv)], [selb, cb], b1b)
                    k.copy(pos[:, :, :].rearrange("p t e -> p (t e)"), b1[:, 0:128], [b1b], [posb])
                    b2, b2b = next_bank()
                    k.mm_group(b2[:, 0:128], [(ones32[:, :], selv)], [selb, cb], b2b)
                    k.copy(tot[:, :, :].rearrange("p t e -> p (t e)"), b2[:, 0:128], [b2b], [totb])
                    k.memset(offs[:, 0, :], 0.0, [offb])
                    for T in range(1, 16):
                        k.tt(offs[:, T, :], offs[:, T - 1, :], tot[:, T - 1, :], ALU.add, [totb], [offb])
                    k.tt(pos[:, :, :], pos[:, :, :], offs[:, :, :], ALU.add, [offb], [posb])
                    k.tt(misc[:, 0:8], offs[:, 15, :], tot[:, 15, :], ALU.add, [offb, totb], [miscb])
                    for c, cap in enumerate(CAPS):
                        k.ts(misc[:, 8 + 8 * c:16 + 8 * c], misc[:, 0:8], float(cap) + 0.5, 0.0, ALU.is_lt, ALU.add,
                             [], [miscb])
                    k.copy(cond_i[:, 0:8 * len(CAPS)], misc[:, 8:8 + 8 * len(CAPS)], [miscb], [condb])
                    if FORCE_CLASS is not None:
                        for c in range(len(CAPS)):
                            k.memset(cond_i[:, 8 * c:8 * c + 8], 1 if c >= FORCE_CLASS else 0, [condb])
                    p.barrier()

                def expert_sparse(e, cap):
                    NJ = cap // 128
                    chunks = [(0, 512)] + ([(512, cap - 512)] if cap > 512 else [])
                    with ExitStack() as sp_:
                        Pb = [sb(sp_, "P%d" % i, [128, cap], BF16) for i in range(4)]
                        Pbb = [Buf() for _ in range(4)]
                        pi = [0]
                        hTe = sb(sp_, "hTe", [128, 8, cap], BF16)
                        hTeb = Buf()
                        GW = GFS * 128
                        slots = []
                        for s_ in range(2):
                            slots.append((sb(sp_, "swg%d" % s_, [128, 8, GW], BF16),
                                          sb(sp_, "swu%d" % s_, [128, 8, GW], BF16),
                                          sb(sp_, "swd%d" % s_, [128, GFS, D], BF16), Buf(), Buf()))
                        actt = [sb(sp_, "sact%d" % i, [128, GFS, cap], BF16) for i in range(2)]
                        actb = [Buf(), Buf()]
                        sg = [sb(sp_, "ssg%d" % i, [128, cap], F32) for i in range(2)]
                        sgb = [Buf(), Buf()]
                        oe32 = sb(sp_, "oe32", [128, NJ, D], F32)
                        oe_bf = sb(sp_, "oe_bf", [128, NJ, D], BF16)
                        oeb = [[Buf(), Buf()] for _ in range(NJ)]
                        oebf_b = Buf()
                        PT = [sb(sp_, "PT%d" % i, [128, NJ, 128], BF16) for i in range(2)]
                        PTb = [Buf(), Buf()]

                        def build_P(T, c0, w):
                            i = pi[0] % 4
                            pi[0] += 1
                            k.ts(Pb[i][:, 0:w], iota[:, c0:c0 + w], pos[:, T, e:e + 1], sel[:, T, e:e + 1],
                                 ALU.is_equal, ALU.mult, [cb, posb, selb], [Pbb[i]])
                            return Pb[i], Pbb[i]

                        for (c0, w) in chunks:
                            per_bank = 512 // w
                            nb = 8 // per_bank
                            bks = [next_bank() for _ in range(nb)]
                            allb = [b for _, b in bks]
                            tok = None
                            for T in range(16):
                                Pt, Ptb = build_P(T, c0, w)
                                deps = [Ptb.w, hab[T].w]
                                if T == 0:
                                    deps += k.deps_of([], allb)
                                for kc in range(8):
                                    o = bks[kc // per_bank][0][:, (kc % per_bank) * w:(kc % per_bank + 1) * w]
                                    tok = p.op(PE, (lambda e_, o=o, a=h_all[:, T, kc * 128:(kc + 1) * 128], r=Pt[:, 0:w],
                                                    s_=(T == 0), t_=(T == 15): e_.matmul(o, lhsT=a, rhs=r, start=s_, stop=t_)),
                                               deps if kc == 0 else (), kc == 7)
                                Ptb.add_read(tok)
                                hab[T].add_read(tok)
                            for b in allb:
                                b.set_write(tok)
                            for bi, (bk, bkb) in enumerate(bks):
                                k.act(hTe[:, bi * per_bank:(bi + 1) * per_bank, c0:c0 + w],
                                      bk[:, 0:per_bank * w].rearrange("p (a b) -> p a b", a=per_bank), AF.Copy,
                                      [bkb], [hTeb])
                        nch = D_FFE // 128
                        ngrp = (nch + GFS - 1) // GFS
                        ginfo = {}
                        sgi_box = [0]

                        def ffn_s1(g_):
                            c0f = g_ * GFS
                            nf = min(GFS, nch - c0f)
                            wg_s, wu_s, wd_s, wsb, wdb = slots[g_ % 2]
                            k.dma(POOL, wg_s[:, :, 0:nf * 128],
                                  mwg_d[e, :, c0f * 128:(c0f + nf) * 128].rearrange("(kc p) n -> p kc n", p=128),
                                  writes=[wsb], sem="wsA%d" % (g_ % 2))
                            k.dma(POOL, wu_s[:, :, 0:nf * 128],
                                  mwu_d[e, :, c0f * 128:(c0f + nf) * 128].rearrange("(kc p) n -> p kc n", p=128),
                                  writes=[wsb], sem="wsA%d" % (g_ % 2))
                            k.dma(POOL, wd_s[:, 0:nf, :],
                                  mwd_d[e, c0f * 128:(c0f + nf) * 128, :].rearrange("(f p) n -> p f n", p=128),
                                  writes=[wdb], sem="wsB%d" % (g_ % 2))
                            ab_i = g_ % 2
                            ginfo[g_] = (nf, wd_s, wdb, ab_i)
                            for f in range(nf):
                                fs_ = slice(f * 128, (f + 1) * 128)
                                si = sgi_box[0] % 2
                                sgi_box[0] += 1
                                for (c0, w) in chunks:
                                    if w == 512:
                                        bg, bgb = next_bank()
                                        bu, bub = next_bank()
                                        k.mm_group(bg[:, :], [(wg_s[:, kc, fs_], hTe[:, kc, c0:c0 + w]) for kc in range(8)],
                                                   [wsb, hTeb], bgb)
                                        k.mm_group(bu[:, :], [(wu_s[:, kc, fs_], hTe[:, kc, c0:c0 + w]) for kc in range(8)],
                                                   [wsb, hTeb], bub)
                                        g_ap, u_ap = bg[:, :], bu[:, :]
                                    else:
                                        bs, bsb = next_bank()
                                        deps = k.deps_of([wsb, hTeb], [bsb])
                                        tok = None
                                        for wi, w_s in enumerate((wg_s, wu_s)):
                                            for kc in range(8):
                                                tok = p.op(PE, (lambda e_, o=bs[:, wi * w:(wi + 1) * w], a=w_s[:, kc, fs_],
                                                                r=hTe[:, kc, c0:c0 + w], s_=(kc == 0), t_=(kc == 7):
                                                                e_.matmul(o, lhsT=a, rhs=r, start=s_, stop=t_)),
                                                           deps if (wi == 0 and kc == 0) else (), (wi == 1 and kc == 7))
                                        wsb.add_read(tok)
                                        hTeb.add_read(tok)
                                        bsb.set_write(tok)
                                        bgb = bub = bsb
                                        g_ap, u_ap = bs[:, 0:w], bs[:, w:2 * w]
                                    k.act(sg[si][:, c0:c0 + w], g_ap, AF.Silu, [bgb], [sgb[si]])
                                    k.tt(actt[ab_i][:, f, c0:c0 + w], sg[si][:, c0:c0 + w], u_ap, ALU.mult,
                                         [sgb[si], bub], [actb[ab_i]])

                        def ffn_s2(g_):
                            nf, wd_s, wsb, ab_i = ginfo[g_]
                            for j in range(NJ):
                                for hf in range(2):
                                    cs = slice(hf * 512, (hf + 1) * 512)
                                    bk, bkb = next_bank()
                                    k.mm_group(bk[:, :], [(actt[ab_i][:, f, j * 128:(j + 1) * 128], wd_s[:, f, cs])
                                                          for f in range(nf)], [wsb, actb[ab_i]], bkb)
                                    ob = oeb[j][hf]
                                    if g_ == 0:
                                        k.act(oe32[:, j, cs], bk[:, :], AF.Copy, [bkb], [ob])
                                    elif g_ < ngrp - 1:
                                        k.tt(oe32[:, j, cs], oe32[:, j, cs], bk[:, :], ALU.add, [bkb], [ob])
                                    else:
                                        k.tt(oe_bf[:, j, cs], oe32[:, j, cs], bk[:, :], ALU.add, [bkb, ob], [oebf_b])

                        ffn_s1(0)
                        for g_ in range(ngrp):
                            if g_ + 1 < ngrp:
                                ffn_s1(g_ + 1)
                            ffn_s2(g_)
                        pend = None
                        for T in range(17):
                            cur = None
                            if T < 16:
                                Pt, Ptb = build_P(T, 0, cap)
                                bk, bkb = next_bank()
                                bkbf = bk.bitcast(BF16)
                                outs = [bkbf[:, j * 128:(j + 1) * 128] for j in range(NJ)]
                                pairs = [(Pt[:, j * 128:(j + 1) * 128], ident[:, :]) for j in range(NJ)]
                                k.mm_group(outs, pairs, [Ptb, cb], bkb, transpose=True)
                                pti = T % 2
                                k.act(PT[pti][:, :, :], bkbf[:, 0:cap].rearrange("p (j t) -> p j t", j=NJ), AF.Copy,
                                      [bkb], [PTb[pti]])
                                cur = (T, pti)
                            if pend is not None:
                                Tp, ptp = pend
                                n = Tp + 1
                                for hf in range(2):
                                    cs = slice(hf * 512, (hf + 1) * 512)
                                    bk2, bk2b = next_bank()
                                    k.mm_group(bk2[:, :], [(PT[ptp][:, j, :], oe_bf[:, j, cs]) for j in range(NJ)],
                                               [PTb[ptp], oebf_b], bk2b)
                                    k.stt(x_tok[:, n, cs], bk2[:, :], gate[:, n, e:e + 1], x_tok[:, n, cs],
                                          ALU.mult, ALU.add, [bk2b, gateb[n]], [xb[n]])
                            pend = cur
                        p.barrier()

                def expert_dense(e):
                    with ExitStack() as db:
                        h2T = sb(db, "h2T", [128, 8, NT * 128], BF16)
                        h2b = [Buf() for _ in range(NT)]
                        for n in tiles:
                            T = n - 1
                            bk, bkb = next_bank()
                            bkbf = bk.bitcast(BF16)
                            outs = [bkbf[:, kc * 128:(kc + 1) * 128] for kc in range(8)]
                            pairs = [(h_all[:, T, kc * 128:(kc + 1) * 128], ident[:, :]) for kc in range(8)]
                            k.mm_group(outs, pairs, [hab[T], cb], bkb, transpose=True)
                            k.act(h2T[:, :, n * 128:(n + 1) * 128], bkbf[:, :].rearrange("p (k t) -> p k t", k=8),
                                  AF.Copy, [bkb], [h2b[n]])
                        ffn_stream(db, True, tiles, h2T, h2b, GFS, experts=[e])
                        p.barrier()

                def cflag(c, e):
                    return cond_i[0:1, 8 * c + e:8 * c + e + 1]

                for e in range(NE):
                    p.cond_region(
                        cflag(1, e), [],
                        lambda e=e: p.cond_region(cflag(0, e), [], lambda: expert_sparse(e, CAPS[0]),
                                                  lambda: expert_sparse(e, CAPS[1])),
                        lambda e=e: p.cond_region(cflag(2, e), [], lambda: expert_sparse(e, CAPS[2]),
                                                  lambda: expert_dense(e)))
                    p.barrier()

        def final_phase(do_norm):
            with ExitStack() as os_:
                gf_bc = sb(os_, "gf_bc", [128, D], F32)
                gfb = Buf()
                outt = [sb(os_, "outt%d" % i, [128, D], F32) for i in range(2)]
                outb = [Buf(), Buf()]
                junk = sb(os_, "junko", [128, D], BF16)
                junkb = Buf()
                toks = []
                if do_norm:
                    k.dma(SP, gf_bc[:], fg_d[0:1, :].partition_broadcast(128), writes=[gfb], sem="gf")
                for n in range(1, NT):
                    if do_norm:
                        oi = n % 2
                        rstd, sbf = rms_stats(x_tok[:, n, :], xb[n], junk[:], junkb, 1.0 / 32.0)
                        k.stt(outt[oi][:], x_tok[:, n, :], rstd, gf_bc[:], ALU.mult, ALU.mult,
                              [xb[n], sbf, gfb], [outb[oi]])
                        toks.append(k.dma(SP, y_d[(n - 1) * 128:n * 128, :], outt[oi][:], reads=[outb[oi]], sem="out%d" % oi))
                    else:
                        toks.append(k.dma(SP, y_d[(n - 1) * 128:n * 128, :], x_tok[:, n, :], reads=[xb[n]], sem="out0"))
                p.wait_only(SP, toks)
                p.barrier()

        mixer_phase(0)
        if stop_after == "M0":
            final_phase(False)
            p.flush()
            return nc
        ffn_phase0()
        if stop_after == "F0":
            final_phase(False)
            p.flush()
            return nc
        k.ts1(x_tok[:, 0, :], x_tok[:, 0, :], flag[:, 0:1], ALU.mult, [cb], [xb[0]])
        mixer_phase(1)
        if stop_after == "M1":
            final_phase(False)
            p.flush()
            return nc
        moe_phase()
        final_phase(True)
        p.flush()
    return nc


def _prep_shared(inp):
    f = lambda a: np.ascontiguousarray(np.asarray(a, dtype=np.float32))
    sh = {}
    sh["ident"] = np.eye(128, dtype=np.float32)
    sh["ustrict"] = np.triu(np.ones((128, 128), np.float32), 1)
    sh["iota"] = np.ascontiguousarray(np.broadcast_to(np.arange(CAPS[-1], dtype=np.float32), (128, CAPS[-1])))
    pp = np.zeros((128, 2, NPP), np.float32)
    wins = np.array([[2.0, 4.0], [8.0, 16.0]], np.float32)
    for l in range(2):
        pp[:, l, PP_PSCALE:PP_PSCALE + 2] = f(inp["pool_scale"])[l].reshape(2, 128).T
        dw = f(inp["conv_dw_w"])[l]
        pp[:, l, PP_DWW:PP_DWW + 93] = dw.reshape(31, 3, 128).transpose(2, 1, 0).reshape(128, 93)
        pp[:, l, PP_DWB:PP_DWB + 3] = f(inp["conv_dw_b"])[l].reshape(3, 128).T
        pp[:, l, PP_LNG:PP_LNG + 3] = f(inp["conv_ln_g"])[l].reshape(3, 128).T
        pp[:, l, PP_LNB:PP_LNB + 3] = f(inp["conv_ln_b"])[l].reshape(3, 128).T
        pp[:, l, PP_PWB:PP_PWB + 3] = f(inp["conv_pw_b"])[l].reshape(3, 128).T
        for c in range(2):
            pp[0:64, l, PP_INVW + c] = 1.0 / wins[c, 0]
            pp[64:128, l, PP_INVW + c] = 1.0 / wins[c, 1]
    sh["pp"] = pp.reshape(128, 2 * NPP)
    sh["norm1_g"] = f(inp["norm1_g"])
    sh["norm2_g"] = f(inp["norm2_g"])
    sh["final_g"] = f(inp["final_g"]).reshape(1, D)
    sh["gm_norm_g"] = f(inp["gm_norm_g"])
    sh["gm_b"] = f(inp["gm_b"]).reshape(2, 512)
    sh["w_in"] = f(inp["w_in"])
    sh["pool_w"] = f(inp["pool_w"])
    sh["gm_wsT"] = np.ascontiguousarray(f(inp["gm_ws"]).transpose(0, 1, 3, 2))
    sh["conv_pw_w"] = f(inp["conv_pw_w"])
    sh["w_out"] = f(inp["w_out"])
    sh["ffn_wg"] = f(inp["ffn_wg"])[0]
    sh["ffn_wu"] = f(inp["ffn_wu"])[0]
    sh["ffn_wd"] = f(inp["ffn_wd"])[0]
    sh["routerT"] = np.ascontiguousarray(f(inp["moe_router"])[0].T)
    sh["moe_wg"] = f(inp["moe_wg"])[0]
    sh["moe_wu"] = f(inp["moe_wu"])[0]
    sh["moe_wd"] = f(inp["moe_wd"])[0]
    return sh


def _prep_core(x, c):
    b, q = c // 4, c % 4
    xin = np.zeros((NT * 128, D), np.float32)
    xin[128:] = x[b, q * 2048:(q + 1) * 2048]
    if q > 0:
        xin[:128] = x[b, q * 2048 - 128:q * 2048]
    flag = np.full((128, 1), 1.0 if q > 0 else 0.0, np.float32)
    pinv = np.zeros((128, 2, 16), np.float32)
    wins = [[2, 4], [8, 16]]
    for cc in range(2):
        for half in range(2):
            w = wins[cc][half]
            for j in range(16):
                cntv = min(j + 1, w) if q == 0 else w
                pinv[half * 64:(half + 1) * 64, cc, j] = 1.0 / cntv
    return {"x": xin, "flag": flag, "pool_inv": pinv.reshape(128, 32)}


_NC_CACHE = {}


def run(inputs, stop_after=None, trace=False):
    x = np.asarray(inputs["x"], dtype=np.float32)
    sh = _prep_shared(inputs)
    in_maps = []
    for c in range(8):
        m = dict(sh)
        m.update(_prep_core(x, c))
        in_maps.append(m)
    if stop_after not in _NC_CACHE:
        _NC_CACHE[stop_after] = build_program(stop_after)
    nc = _NC_CACHE[stop_after]
    res = run_bass_kernel_spmd(nc, in_maps, core_ids=list(range(8)), **({"trace": True} if trace else {}))
    out = np.zeros((2, 8192, D), np.float32)
    for c in range(8):
        b, q = c // 4, c % 4
        out[b, q * 2048:(q + 1) * 2048] = res.results[c]["y"]
    return out, res


def kernel(**inputs):
    out, _ = run(inputs)
    return out
```

```python
import numpy as np
from contextlib import ExitStack
import concourse.bass as bass
import concourse.mybir as mybir
from concourse.bass_utils import run_bass_kernel_spmd

F32 = mybir.dt.float32
BF16 = mybir.dt.bfloat16
I32 = mybir.dt.int32
AF = mybir.ActivationFunctionType
ALU = mybir.AluOpType
AX = mybir.AxisListType

PE, ACT, DVE, POOL, SP = "pe", "act", "dve", "pool", "sp"
ENGS = (PE, ACT, DVE, POOL, SP)

D = 1024
NT = 17
D_IN = 1792
D_FF = 2816
D_FFE = 3584
NE = 8
EPS = 1e-6
NPP = 112
PP_PSCALE = 0
PP_DWW = 2
PP_DWB = 95
PP_LNG = 98
PP_LNB = 101
PP_PWB = 104
PP_INVW = 107
TGM = 2
GF = 4
GFS = 2
CAPS = (512, 640, 768)
FORCE_CLASS = None


class Prog:
    def __init__(self, nc, stack):
        self.nc = nc
        self.stack = stack
        self.ops = {e: [] for e in ENGS}
        self.sems = {}
        self.cnt = {}
        self.seen = {e: {} for e in ENGS}
        for e in ENGS:
            self._mksem("eng_" + e)

    def _mksem(self, key):
        if key not in self.sems:
            self.sems[key] = self.stack.enter_context(self.nc.semaphore(key))
            self.cnt[key] = 0
        return self.sems[key]

    def _waits(self, eng, deps):
        out = []
        for d in deps:
            if d is None:
                continue
            key, val = d
            if eng == PE and key == "eng_pe":
                continue
            if self.seen[eng].get(key, 0) >= val:
                continue
            self.seen[eng][key] = val
            out.append((self.sems[key], val))
        return out

    def op(self, eng, fn, deps=(), inc=True):
        waits = self._waits(eng, deps)
        key = "eng_" + eng
        tok = None
        if inc:
            self.cnt[key] += 1
            tok = (key, self.cnt[key])
        self.ops[eng].append((waits, fn, (self.sems[key], 1) if inc else None))
        return tok

    def dma(self, eng, out, in_, semname, deps=()):
        self._mksem(semname)
        waits = self._waits(eng, deps)
        self.cnt[semname] += 16
        tok = (semname, self.cnt[semname])
        self.ops[eng].append(
            (waits, lambda e, o=out, i=in_: e.dma_start(out=o, in_=i), (self.sems[semname], 16))
        )
        return tok

    def wait_only(self, eng, deps):
        waits = self._waits(eng, deps)
        if waits:
            self.ops[eng].append((waits, None, None))

    def barrier(self):
        toks = [(k, v) for k, v in self.cnt.items() if v > 0]
        for e in ENGS:
            self.wait_only(e, toks)

    def cond_region(self, cond_ap, cond_deps, then_fn, else_fn):
        for e in ENGS:
            self.ops[e].append(("IF", cond_ap, self._waits(e, cond_deps)))
        snap_cnt = dict(self.cnt)
        snap_seen = {e: dict(d) for e, d in self.seen.items()}
        Buf.reset_all()
        then_fn()
        then_cnt = dict(self.cnt)
        then_end = {e: len(self.ops[e]) for e in ENGS}
        self.cnt = dict(snap_cnt)
        for kk in then_cnt:
            self.cnt.setdefault(kk, 0)
        self.seen = {e: dict(d) for e, d in snap_seen.items()}
        for e in ENGS:
            self.ops[e].append(("ELSE",))
        Buf.reset_all()
        else_fn()
        else_cnt = dict(self.cnt)
        keys = set(then_cnt) | set(else_cnt)
        final = {kk: max(then_cnt.get(kk, 0), else_cnt.get(kk, 0)) for kk in keys}

        def equalizers(branch_cnt):
            per_eng = {e: [] for e in ENGS}
            for kk in sorted(keys):
                diff = final[kk] - branch_cnt.get(kk, 0)
                if diff <= 0:
                    continue
                eng = kk[4:] if kk.startswith("eng_") else SP
                per_eng[eng].append(("EQ", self.sems[kk], branch_cnt.get(kk, 0), diff))
            return per_eng

        eq_then = equalizers(then_cnt)
        eq_else = equalizers(else_cnt)
        for e in ENGS:
            self.ops[e][then_end[e]:then_end[e]] = eq_then[e]
            self.ops[e].extend(eq_else[e])
            self.ops[e].append(("ENDIF",))
        self.cnt = final
        self.seen = snap_seen
        Buf.reset_all()

    def flush(self):
        nc = self.nc
        ops = self.ops

        def run(e, lst):
            cms = []
            for item in lst:
                tag = item[0]
                if tag == "IF":
                    for s_, v in item[2]:
                        e.wait_ge(s_, v)
                    val = e.value_load(item[1])
                    cm = e.If(val == 1)
                    cm.__enter__()
                    cms.append(cm)
                elif tag == "ELSE":
                    cms.pop().__exit__(None, None, None)
                    cm = e.Else()
                    cm.__enter__()
                    cms.append(cm)
                elif tag == "ENDIF":
                    cms.pop().__exit__(None, None, None)
                elif tag == "EQ":
                    _, sem, have, diff = item
                    if have > 0:
                        e.wait_ge(sem, have)
                    e.sem_inc(sem, diff)
                else:
                    waits, fn, inc = item
                    for s_, v in waits:
                        e.wait_ge(s_, v)
                    if fn is not None:
                        ins = fn(e)
                        if inc is not None:
                            ins.then_inc(inc[0], inc[1])

        with nc.Block() as block:
            @block.tensor
            def _(e):
                run(e, ops[PE])

            @block.scalar
            def _(e):
                run(e, ops[ACT])

            @block.vector
            def _(e):
                run(e, ops[DVE])

            @block.gpsimd
            def _(e):
                run(e, ops[POOL])

            @block.sync
            def _(e):
                run(e, ops[SP])
        self.ops = {e: [] for e in ENGS}


class Buf:
    ALL = []

    def __init__(self, name=""):
        self.name = name
        self.w = None
        self.r = {}
        Buf.ALL.append(self)

    @staticmethod
    def reset_all():
        for b in Buf.ALL:
            b.w = None
            b.r = {}

    def add_read(self, tok):
        if tok is None:
            return
        k, v = tok
        if self.r.get(k, 0) < v:
            self.r[k] = v

    def set_write(self, tok):
        self.w = tok
        self.r = {}


class K:
    def __init__(self, nc, stack):
        self.nc = nc
        self.p = Prog(nc, stack)
        self.dma_n = 0

    def deps_of(self, reads, writes):
        deps = []
        for b in reads:
            deps.append(b.w)
        for b in writes:
            deps.append(b.w)
            deps.extend(b.r.items())
        return deps

    def emit(self, eng, fn, reads=(), writes=()):
        tok = self.p.op(eng, fn, self.deps_of(reads, writes), True)
        for b in reads:
            b.add_read(tok)
        for b in writes:
            b.set_write(tok)
        return tok

    def dma(self, eng, out, in_, reads=(), writes=(), sem=None):
        assert sem is not None
        deps = []
        for b in reads:
            deps.append(b.w)
        for b in writes:
            if not (b.w is not None and b.w[0] == sem):
                deps.append(b.w)
            deps.extend(b.r.items())
        tok = self.p.dma(eng, out, in_, sem, deps)
        for b in reads:
            b.add_read(tok)
        for b in writes:
            b.set_write(tok)
        return tok

    def mm_group(self, out, pairs, reads, bank, transpose=False):
        deps = self.deps_of(reads, [bank])
        n = len(pairs)
        tok = None
        for i, (l, r) in enumerate(pairs):
            last = i == n - 1
            if transpose:
                fn = (lambda e, o=out[i], a=l, b=r: e.transpose(o, a, b))
            else:
                fn = (lambda e, o=out, a=l, b=r, s=(i == 0), t=last: e.matmul(o, lhsT=a, rhs=b, start=s, stop=t))
            tok = self.p.op(PE, fn, deps if i == 0 else (), last)
        for b in reads:
            b.add_read(tok)
        bank.set_write(tok)
        return tok

    def mm_multi(self, groups, reads, bank):
        deps = self.deps_of(reads, [bank])
        n = len(groups)
        tok = None
        for i, (o, l, r) in enumerate(groups):
            last = i == n - 1
            fn = (lambda e, o=o, a=l, b=r: e.matmul(o, lhsT=a, rhs=b, start=True, stop=True))
            tok = self.p.op(PE, fn, deps if i == 0 else (), last)
        for b in reads:
            b.add_read(tok)
        bank.set_write(tok)
        return tok

    def act(self, out, in_, func, reads, writes, bias=None, scale=None, accum=None):
        kw = {}
        if bias is not None:
            kw["bias"] = bias
        if scale is not None:
            kw["scale"] = scale
        if accum is not None:
            kw["accum_out"] = accum
        return self.emit(ACT, lambda e: e.activation(out=out, in_=in_, func=func, **kw), reads, writes)

    def tt(self, out, in0, in1, op, reads, writes, eng=DVE):
        return self.emit(eng, lambda e: e.tensor_tensor(out=out, in0=in0, in1=in1, op=op), reads, writes)

    def ts(self, out, in0, s1, s2, op0, op1, reads, writes, eng=DVE):
        return self.emit(eng, lambda e: e.tensor_scalar(out=out, in0=in0, scalar1=s1, scalar2=s2, op0=op0, op1=op1),
                         reads, writes)

    def ts1(self, out, in0, s1, op0, reads, writes, eng=DVE):
        return self.emit(eng, lambda e: e.tensor_scalar(out=out, in0=in0, scalar1=s1, scalar2=None, op0=op0),
                         reads, writes)

    def stt(self, out, in0, scalar, in1, op0, op1, reads, writes, accum=None, eng=DVE):
        kw = {}
        if accum is not None:
            kw["accum_out"] = accum
        return self.emit(eng, lambda e: e.scalar_tensor_tensor(out=out, in0=in0, scalar=scalar, in1=in1,
                                                               op0=op0, op1=op1, **kw), reads, writes)

    def copy(self, out, in_, reads, writes, eng=DVE):
        return self.emit(eng, lambda e: e.tensor_copy(out=out, in_=in_), reads, writes)

    def recip(self, out, in_, reads, writes):
        return self.emit(DVE, lambda e: e.reciprocal(out=out, in_=in_), reads, writes)

    def memset(self, ap, val, writes, eng=DVE):
        return self.emit(eng, lambda e: e.memset(ap, val), (), writes)


def build_program(stop_after=None):
    nc = bass.Bass("TRN2", target_bir_lowering=False)

    def din(name, shape):
        return nc.dram_tensor(name, list(shape), F32, kind="ExternalInput").ap()

    x_d = din("x", [NT * 128, D])
    flag_d = din("flag", [128, 1])
    pinv_d = din("pool_inv", [128, 32])
    ident_d = din("ident", [128, 128])
    ustrict_d = din("ustrict", [128, 128])
    iota_d = din("iota", [128, CAPS[-1]])
    pp_d = din("pp", [128, 2 * NPP])
    n1g_d = din("norm1_g", [2, D])
    n2g_d = din("norm2_g", [2, D])
    fg_d = din("final_g", [1, D])
    gmg_d = din("gm_norm_g", [2, 384])
    gmb_d = din("gm_b", [2, 512])
    w_in_d = din("w_in", [2, D, D_IN])
    pool_w_d = din("pool_w", [2, 4, 64, 64])
    wsT_d = din("gm_wsT", [2, 4, 128, 128])
    pw_d = din("conv_pw_w", [2, 384, 384])
    w_out_d = din("w_out", [2, D, D])
    fwg_d = din("ffn_wg", [D, D_FF])
    fwu_d = din("ffn_wu", [D, D_FF])
    fwd_d = din("ffn_wd", [D_FF, D])
    rT_d = din("routerT", [NE, D])
    mwg_d = din("moe_wg", [NE, D, D_FFE])
    mwu_d = din("moe_wu", [NE, D, D_FFE])
    mwd_d = din("moe_wd", [NE, D_FFE, D])
    y_d = nc.dram_tensor("y", [16 * 128, D], F32, kind="ExternalOutput").ap()

    with ExitStack() as st:
        ARENA_WORDS = 52600
        arena = st.enter_context(nc.sbuf_tensor("arena", [128, ARENA_WORDS], F32))
        atop = [0]
        scopes = {}

        def _release(mark):
            atop[0] = mark

        def sb(stack, name, shape, dt):
            if stack is not st and not getattr(stack, "_arena_marked", False):
                stack._arena_marked = True
                stack.callback(_release, atop[0])
            n = 1
            for d_ in shape[1:]:
                n *= d_
            nbytes = n * (4 if dt in (F32, I32) else 2)
            words = ((nbytes + 3) // 4 + 7) // 8 * 8
            assert atop[0] + words <= ARENA_WORDS, ("SBUF arena overflow", name, atop[0], words)
            v = arena[:, atop[0]:atop[0] + words]
            atop[0] += words
            if dt != F32:
                v = v.bitcast(dt)
            v = v[:, 0:n]
            if len(shape) == 3:
                v = v.rearrange("p (a b) -> p a b", a=shape[1])
            return v

        k = K(nc, st)
        p = k.p

        x_tok = sb(st, "x_tok", [128, NT, D], F32)
        xb = [Buf("x%d" % n) for n in range(NT)]
        ident = sb(st, "ident", [128, 128], BF16)
        ones32 = sb(st, "ones32", [128, 128], F32)
        ustrict = sb(st, "ustrict", [128, 128], F32)
        iota = sb(st, "iota", [128, CAPS[-1]], F32)
        pp = sb(st, "pp", [128, 2 * NPP], F32)
        flag = sb(st, "flag", [128, 1], F32)
        pinv = sb(st, "pinv", [128, 2, 16], F32)
        stt_t = sb(st, "stats", [128, 8, 4], F32)
        gate = sb(st, "gate", [128, NT, NE], F32)
        cb = Buf("consts")
        stb = [Buf("st%d" % i) for i in range(8)]
        gateb = [Buf("gate%d" % n) for n in range(NT)]
        banks = [st.enter_context(nc.psum_tensor("bank%d" % i, [128, 512], F32)) for i in range(8)]
        bankb = [Buf("bank%d" % i) for i in range(8)]
        ring = [0]
        stat_i = [0]

        def next_bank():
            i = ring[0]
            ring[0] = (i + 1) % 8
            return banks[i], bankb[i]

        def next_stat():
            i = stat_i[0]
            stat_i[0] = (i + 1) % 8
            return stt_t[:, i, :], stb[i]

        def ppc(l, col, n=1, lo=0, hi=128):
            return pp[lo:hi, l * NPP + col: l * NPP + col + n]

        k.memset(ones32[:], 1.0, [cb])
        k.dma(POOL, ident[:], ident_d, writes=[cb], sem="consts")
        k.dma(POOL, pp[:], pp_d, writes=[cb], sem="consts")
        k.dma(POOL, ustrict[:], ustrict_d, writes=[cb], sem="consts")
        k.dma(POOL, iota[:], iota_d, writes=[cb], sem="consts")
        k.dma(POOL, flag[:], flag_d, writes=[cb], sem="consts")
        k.dma(POOL, pinv[:].rearrange("p c j -> p (c j)"), pinv_d, writes=[cb], sem="consts")
        for n in range(NT):
            k.dma(SP, x_tok[:, n, :], x_d[n * 128:(n + 1) * 128, :], writes=[xb[n]], sem="x%d" % n)

        def rms_stats(src_ap, srcb, junk, junkb, scale):
            sap, sbf = next_stat()
            k.act(junk, src_ap, AF.Square, [srcb], [junkb, sbf], scale=scale, accum=sap[:, 0:1])
            k.act(sap[:, 1:2], sap[:, 0:1], AF.Sqrt, [sbf], [sbf], bias=EPS)
            k.recip(sap[:, 2:3], sap[:, 1:2], [sbf], [sbf])
            return sap[:, 2:3], sbf

        def norm_and_transpose(n, g_bc, gb, h_tok, h_tokb, junk, junkb, hT_dst, hTb):
            rstd, sbf = rms_stats(x_tok[:, n, :], xb[n], junk, junkb, 1.0 / 32.0)
            k.stt(h_tok, x_tok[:, n, :], rstd, g_bc, ALU.mult, ALU.mult, [xb[n], sbf, gb], [h_tokb])
            bk, bkb = next_bank()
            bkbf = bk.bitcast(BF16)
            outs = [bkbf[:, kc * 128:(kc + 1) * 128] for kc in range(8)]
            pairs = [(h_tok[:, kc * 128:(kc + 1) * 128], ident[:, :]) for kc in range(8)]
            k.mm_group(outs, pairs, [h_tokb, cb], bkb, transpose=True)
            k.act(hT_dst, bkbf[:, :].rearrange("p (k t) -> p k t", k=8), AF.Copy, [bkb], [hTb])
            return rstd, sbf

        def mixer_phase(l):
            with ExitStack() as ms:
                NMAX = TGM * 128
                LMAX = 32 + NMAX
                w_in_sb = sb(ms, "w_in_sb", [128, 8, D_IN], BF16)
                wo_a = sb(ms, "wo_a", [128, 2, D], BF16)
                wo_b = sb(ms, "wo_b", [128, 4, D], BF16)
                wo_c = sb(ms, "wo_c", [128, 3, D], BF16)
                pw_sb = sb(ms, "pw_sb", [128, 3, 384], BF16)
                wblk = sb(ms, "wblk", [128, 2, 128], BF16)
                wsT = sb(ms, "wsT", [128, 4, 128], BF16)
                g1_bc = sb(ms, "g1_bc", [128, D], F32)
                gmg_bc = sb(ms, "gmg_bc", [128, 384], F32)
                gmb_bc = sb(ms, "gmb_bc", [128, 512], F32)
                wb = Buf("mixw")
                h_tok = [sb(ms, "h_tok%d" % i, [128, D], BF16) for i in range(2)]
                h_tokb = [Buf(), Buf()]
                junk = sb(ms, "junk", [128, D], BF16)
                junkb = Buf()
                two = range(2)
                a_ext = [sb(ms, "a_ext%d" % i, [128, 2, LMAX], F32) for i in two]
                hc_ext = [sb(ms, "hc_ext%d" % i, [128, 3, LMAX], BF16) for i in two]
                yb = [sb(ms, "yb%d" % i, [128, 4, NMAX], BF16) for i in two]
                ab, hcb, ybb = ([Buf(), Buf()] for _ in range(3))

                def same2(name, shape, dt):
                    t_ = sb(ms, name, shape, dt)
                    return [t_, t_]

                def same2b():
                    b_ = Buf()
                    return [b_, b_]

                hT = same2("hT", [128, 8, NMAX], BF16)
                sig = same2("sig", [128, 3, NMAX], F32)
                acc = same2("acc", [128, 3, NMAX], F32)
                u_sb = same2("u_sb", [128, 4, NMAX], F32)
                y_p = same2("y_p", [128, 2, NMAX], BF16)
                ya = same2("ya", [128, 2, NMAX], BF16)
                hs = same2("hs", [128, 3, NMAX], BF16)
                yc = same2("yc", [128, 3, NMAX], BF16)
                hTb, sigb, ub, ypb, yab, hsb, ycb = (same2b() for _ in range(7))
                accb1 = [Buf(), Buf(), Buf()]
                accb = [accb1, accb1]
                dg = sb(ms, "dg", [128, 93, 128], BF16)
                dgb = Buf()
                sA = sb(ms, "sA", [128, 2, LMAX], F32)
                sB = sb(ms, "sB", [128, 2, LMAX], F32)
                tmp16 = sb(ms, "tmp16", [128, 16], F32)
                v_n = [sb(ms, "v_n%d" % i, [128, 384], BF16) for i in two]
                ztmp = sb(ms, "ztmp", [128, 512], F32)
                sq = sb(ms, "sq", [128, 3, NMAX], F32)
                mean = sb(ms, "mean", [128, NMAX], F32)
                var = sb(ms, "var", [128, NMAX], F32)
                rstdc = sb(ms, "rstdc", [128, NMAX], F32)
                sAb, sBb, t16b, ztb, sqb, meanb, varb, rsb = (Buf() for _ in range(8))
                vnb = [Buf(), Buf()]

                wblkb = Buf()
                wsTb = Buf()
                k.memset(wblk[:], 0.0, [wblkb])
                for c in range(2):
                    k.dma(POOL, wblk[0:64, c, 0:64], pool_w_d[l, 2 * c], writes=[wblkb], sem="wblk")
                    k.dma(POOL, wblk[64:128, c, 64:128], pool_w_d[l, 2 * c + 1], writes=[wblkb], sem="wblk")
                k.dma(POOL, wsT[:], wsT_d[l].rearrange("h j i -> j h i"), writes=[wsTb], sem="wsT")
                k.memset(wsT[64:128, :, 0:64], 0.0, [wsTb])
                k.dma(SP, g1_bc[:], n1g_d[l:l + 1, :].partition_broadcast(128), writes=[wb], sem="mixw")
                k.dma(SP, gmg_bc[:], gmg_d[l:l + 1, :].partition_broadcast(128), writes=[wb], sem="mixw")
                k.dma(SP, gmb_bc[:], gmb_d[l:l + 1, :].partition_broadcast(128), writes=[wb], sem="mixw")
                k.dma(POOL, w_in_sb[:], w_in_d[l].rearrange("(kc p) n -> p kc n", p=128), writes=[wb], sem="mixw")
                k.dma(POOL, wo_a[:], w_out_d[l, 0:256, :].rearrange("(c p) n -> p c n", p=128), writes=[wb], sem="mixw")
                k.dma(POOL, wo_b[0:96], w_out_d[l, 256:640, :].rearrange("(h p) n -> p h n", p=96), writes=[wb], sem="mixw")
                k.dma(POOL, wo_c[:], w_out_d[l, 640:1024, :].rearrange("(c p) n -> p c n", p=128), writes=[wb], sem="mixw")
                k.dma(POOL, pw_sb[:], pw_d[l].rearrange("(c p) n -> p c n", p=128), writes=[wb], sem="mixw")
                k.memset(a_ext[0][:, :, 0:32], 0.0, [ab[0]])
                k.memset(hc_ext[0][:, :, 0:32], 0.0, [hcb[0]])
                for i in range(93):
                    k.ts1(dg[:, i, :], ident[:, :], ppc(l, PP_DWW + i), ALU.mult, [cb], [dgb])

                groups = [(0, 1)] + [(t0, TGM) for t0 in range(1, NT, TGM)]

                def stage_ne(gi):
                    t0, nt = groups[gi]
                    for j in range(nt):
                        n = t0 + j
                        hb = (gi * TGM + j) % 2
                        rstd, sbf = rms_stats(x_tok[:, n, :], xb[n], junk[:], junkb, 1.0 / 32.0)
                        k.stt(h_tok[hb][:], x_tok[:, n, :], rstd, g1_bc[:], ALU.mult, ALU.mult,
                              [xb[n], sbf, wb], [h_tokb[hb]])

                def stage_nt(gi):
                    t0, nt = groups[gi]
                    q = gi % 2
                    for j in range(nt):
                        hb = (gi * TGM + j) % 2
                        bk, bkb = next_bank()
                        bkbf = bk.bitcast(BF16)
                        outs = [bkbf[:, kc * 128:(kc + 1) * 128] for kc in range(8)]
                        pairs = [(h_tok[hb][:, kc * 128:(kc + 1) * 128], ident[:, :]) for kc in range(8)]
                        k.mm_group(outs, pairs, [h_tokb[hb], cb], bkb, transpose=True)
                        k.act(hT[q][:, :, j * 128:(j + 1) * 128], bkbf[:, :].rearrange("p (k t) -> p k t", k=8),
                              AF.Copy, [bkb], [hTb[q]])

                def stage_p(gi):
                    t0, nt = groups[gi]
                    q = gi % 2
                    N = nt * 128
                    L = 32 + N
                    full = not (l == 1 and t0 == 0)
                    first_own = (t0 == 1)
                    if gi > 0:
                        pN = groups[gi - 1][1] * 128
                        k.copy(a_ext[q][:, :, 0:32], a_ext[1 - q][:, :, pN:pN + 32], [ab[1 - q]], [ab[q]], eng=POOL)
                        k.copy(hc_ext[q][:, :, 0:32], hc_ext[1 - q][:, :, pN:pN + 32], [hcb[1 - q]], [hcb[q]], eng=POOL)

                    def proj(col0, m):
                        bk, bkb = next_bank()
                        pairs = [(w_in_sb[:, kc, col0:col0 + m], hT[q][:, kc, 0:N]) for kc in range(8)]
                        k.mm_group(bk[0:m, 0:N], pairs, [wb, hTb[q]], bkb)
                        return bk, bkb

                    for c in range(2):
                        bk, bkb = proj(c * 128, 128)
                        k.act(a_ext[q][:, c, 32:L], bk[:, 0:N], AF.Copy, [bkb], [ab[q]])
                    vinfo = []
                    if full:
                        for j in range(nt):
                            vb = j % 2
                            bk, bkb = next_bank()
                            pairs = [(hT[q][:, kc, j * 128:(j + 1) * 128], w_in_sb[:, kc, 640:1024]) for kc in range(8)]
                            k.mm_group(bk[:, 0:384], pairs, [wb, hTb[q]], bkb)
                            rstd, sbf = rms_stats(bk[:, 0:384], bkb, junk[:, 0:384], junkb, float(384.0 ** -0.5))
                            k.stt(v_n[vb][:], bk[:, 0:384], rstd, gmg_bc[:], ALU.mult, ALU.mult,
                                  [bkb, sbf, wb], [vnb[vb]])
                    for c in range(3):
                        bk, bkb = proj(1408 + c * 128, 128)
                        k.act(sig[q][:, c, 0:N], bk[:, 0:N], AF.Sigmoid, [bkb], [sigb[q]])
                    for c in range(3):
                        bk, bkb = proj(1024 + c * 128, 128)
                        k.tt(hc_ext[q][:, c, 32:L], bk[:, 0:N], sig[q][:, c, 0:N], ALU.mult, [bkb, sigb[q]], [hcb[q]])
                    if full:
                        for h in range(4):
                            bk, bkb = proj(256 + h * 96, 96)
                            k.act(u_sb[q][0:96, h, 0:N], bk[0:96, 0:N], AF.Copy, [bkb], [ub[q]])
                        for j in range(nt):
                            vb = j % 2
                            zk, zkb = next_bank()
                            grp = [(zk[0:96, h * 128:(h + 1) * 128], v_n[vb][:, h * 96:(h + 1) * 96], wsT[:, h, :])
                                   for h in range(4)]
                            k.mm_multi(grp, [vnb[vb], wsTb], zkb)
                            k.tt(ztmp[0:96, :], zk[0:96, :], gmb_bc[0:96, :], ALU.add, [zkb, wb], [ztb])
                            k.tt(yb[q][0:96, :, j * 128:(j + 1) * 128],
                                 ztmp[0:96, :].rearrange("p (h i) -> p h i", h=4),
                                 u_sb[q][0:96, :, j * 128:(j + 1) * 128], ALU.mult, [ztb, ub[q]], [ybb[q]])
                        A_ = a_ext[q]

                        def pool_out(sbuf_t, sbuf_b, lo, hi, c):
                            k.stt(y_p[q][lo:hi, c, 0:N], sbuf_t[lo:hi, c, 32:L], ppc(l, PP_INVW + c, 1, lo, hi),
                                  A_[lo:hi, c, 32:L], ALU.mult, ALU.subtract, [sbuf_b, ab[q], cb], [ypb[q]])
                            if first_own:
                                k.tt(tmp16[lo:hi, :], sbuf_t[lo:hi, c, 32:48], pinv[lo:hi, c, :], ALU.mult,
                                     [sbuf_b, cb], [t16b])
                                k.tt(y_p[q][lo:hi, c, 0:16], tmp16[lo:hi, :], A_[lo:hi, c, 32:48], ALU.subtract,
                                     [t16b, ab[q]], [ypb[q]])

                        k.tt(sA[:, :, 1:L], A_[:, :, 1:L], A_[:, :, 0:L - 1], ALU.add, [ab[q]], [sAb])
                        pool_out(sA, sAb, 0, 64, 0)
                        k.tt(sB[:, :, 3:L], sA[:, :, 3:L], sA[:, :, 1:L - 2], ALU.add, [sAb], [sBb])
                        pool_out(sB, sBb, 64, 128, 0)
                        k.tt(sA[:, :, 7:L], sB[:, :, 7:L], sB[:, :, 3:L - 4], ALU.add, [sBb], [sAb])
                        pool_out(sA, sAb, 0, 64, 1)
                        k.tt(sB[:, :, 15:L], sA[:, :, 15:L], sA[:, :, 7:L - 8], ALU.add, [sAb], [sBb])
                        pool_out(sB, sBb, 64, 128, 1)

                def stage_b1(gi):
                    t0, nt = groups[gi]
                    q = gi % 2
                    N = nt * 128
                    L = 32 + N
                    full = not (l == 1 and t0 == 0)
                    first_own = (t0 == 1)
                    if not full:
                        return
                    A_ = a_ext[q]
                    H_ = hc_ext[q]

                    for c in range(2):
                        bk, bkb = next_bank()
                        k.mm_group(bk[:, 0:N], [(wblk[:, c, :], y_p[q][:, c, 0:N])], [wblkb, ypb[q]], bkb)
                        k.act(ya[q][:, c, 0:N], bk[:, 0:N], AF.Copy, [bkb, cb], [yab[q]], scale=ppc(l, PP_PSCALE + c))

                    for c in range(3):
                        bk, bkb = next_bank()
                        pairs = [(dg[:, c * 31 + kk, :], H_[:, c, 2 + kk:2 + kk + N]) for kk in range(31)]
                        k.mm_group(bk[:, 0:N], pairs, [dgb, hcb[q]], bkb)
                        k.act(acc[q][:, c, 0:N], bk[:, 0:N], AF.Identity, [bkb, cb], [accb[q][c]],
                              bias=ppc(l, PP_DWB + c))
                    for c in range(3):
                        k.act(sq[:, c, 0:N], acc[q][:, c, 0:N], AF.Square, [accb[q][c]], [sqb])
                    b1, b1b = next_bank()
                    k.mm_group(b1[:, 0:N], [(ones32[:, :], acc[q][:, c, 0:N]) for c in range(3)], accb[q] + [cb], b1b)
                    b2, b2b = next_bank()
                    k.mm_group(b2[:, 0:N], [(ones32[:, :], sq[:, c, 0:N]) for c in range(3)], [sqb, cb], b2b)
                    k.ts(mean[:, 0:N], b1[:, 0:N], 1.0 / 384.0, 0.0, ALU.mult, ALU.add, [b1b], [meanb])
                    k.tt(var[:, 0:N], mean[:, 0:N], mean[:, 0:N], ALU.mult, [meanb], [varb])
                    k.stt(var[:, 0:N], b2[:, 0:N], 1.0 / 384.0, var[:, 0:N], ALU.mult, ALU.subtract,
                          [b2b], [varb])
                    k.act(var[:, 0:N], var[:, 0:N], AF.Sqrt, [], [varb], bias=EPS)
                    k.recip(rstdc[:, 0:N], var[:, 0:N], [varb], [rsb])
                    for c in range(3):
                        k.tt(sq[:, c, 0:N], acc[q][:, c, 0:N], mean[:, 0:N], ALU.subtract, [accb[q][c], meanb], [sqb])
                        k.tt(sq[:, c, 0:N], sq[:, c, 0:N], rstdc[:, 0:N], ALU.mult, [rsb], [sqb])
                        k.act(hs[q][:, c, 0:N], sq[:, c, 0:N], AF.Silu, [sqb, cb], [hsb[q]],
                              bias=ppc(l, PP_LNB + c), scale=ppc(l, PP_LNG + c))
                def stage_b2(gi):
                    t0, nt = groups[gi]
                    q = gi % 2
                    N = nt * 128
                    full = not (l == 1 and t0 == 0)
                    if not full:
                        return
                    for co in range(3):
                        bk, bkb = next_bank()
                        pairs = [(pw_sb[:, ci, co * 128:(co + 1) * 128], hs[q][:, ci, 0:N]) for ci in range(3)]
                        k.mm_group(bk[:, 0:N], pairs, [wb, hsb[q]], bkb)
                        k.act(yc[q][:, co, 0:N], bk[:, 0:N], AF.Identity, [bkb, cb], [ycb[q]], bias=ppc(l, PP_PWB + co))

                    for j in range(nt):
                        n = t0 + j
                        ts_ = slice(j * 128, (j + 1) * 128)
                        for hf in range(2):
                            cs = slice(hf * 512, (hf + 1) * 512)
                            pairs = [(ya[q][:, c, ts_], wo_a[:, c, cs]) for c in range(2)]
                            pairs += [(yb[q][0:96, h, ts_], wo_b[0:96, h, cs]) for h in range(4)]
                            pairs += [(yc[q][:, c, ts_], wo_c[:, c, cs]) for c in range(3)]
                            bk, bkb = next_bank()
                            k.mm_group(bk[:, :], pairs, [wb, yab[q], ybb[q], ycb[q]], bkb)
                            k.tt(x_tok[:, n, cs], x_tok[:, n, cs], bk[:, :], ALU.add, [bkb], [xb[n]])

                ng = len(groups)
                stage_ne(0)
                stage_nt(0)
                if ng > 1:
                    stage_ne(1)
                stage_p(0)
                for gi in range(ng):
                    if gi + 1 < ng:
                        stage_nt(gi + 1)
                    stage_b1(gi)
                    if gi + 2 < ng:
                        stage_ne(gi + 2)
                    if gi + 1 < ng:
                        stage_p(gi + 1)
                    stage_b2(gi)
                p.barrier()

        def ffn_stream(scope, moe, tiles, h2T, h2b, gf, experts=None):
            GW = gf * 128
            slots = []
            for s_ in range(2):
                slots.append((sb(scope, "wg%d" % s_, [128, 8, GW], BF16),
                              sb(scope, "wu%d" % s_, [128, 8, GW], BF16),
                              sb(scope, "wd%d" % s_, [128, gf, D], BF16), Buf()))
            actt = [sb(scope, "act%d" % i, [128, gf, 512], BF16) for i in range(2)]
            actb = [Buf(), Buf()]
            sg = [sb(scope, "sg%d" % i, [128, 512], F32) for i in range(2)]
            sgb = [Buf(), Buf()]
            glist = []
            if not moe:
                nch = D_FF // 128
                for c0 in range(0, nch, gf):
                    nf = min(gf, nch - c0)
                    glist.append((fwg_d[:, c0 * 128:(c0 + nf) * 128], fwu_d[:, c0 * 128:(c0 + nf) * 128],
                                  fwd_d[c0 * 128:(c0 + nf) * 128, :], nf, None))
            else:
                nch = D_FFE // 128
                for e in (experts if experts is not None else range(NE)):
                    for c0 in range(0, nch, gf):
                        nf = min(gf, nch - c0)
                        glist.append((mwg_d[e, :, c0 * 128:(c0 + nf) * 128],
                                      mwu_d[e, :, c0 * 128:(c0 + nf) * 128],
                                      mwd_d[e, c0 * 128:(c0 + nf) * 128, :], nf, e))
            tgroups = [tiles[i:i + 4] for i in range(0, len(tiles), 4)]
            cnt = 0
            sgi = 0
            for gi, (wg_ap, wu_ap, wd_ap, nf, e) in enumerate(glist):
                wg_s, wu_s, wd_s, wsb = slots[gi % 2]
                sem = "wslot%d" % (gi % 2)
                k.dma(POOL, wg_s[:, :, 0:nf * 128], wg_ap.rearrange("(kc p) n -> p kc n", p=128),
                      writes=[wsb], sem=sem)
                k.dma(POOL, wu_s[:, :, 0:nf * 128], wu_ap.rearrange("(kc p) n -> p kc n", p=128),
                      writes=[wsb], sem=sem)
                k.dma(POOL, wd_s[:, 0:nf, :], wd_ap.rearrange("(f p) n -> p f n", p=128),
                      writes=[wsb], sem=sem)
                for tg in tgroups:
                    ab_i = cnt % 2
                    cnt += 1
                    t_lo = tg[0] * 128
                    N = len(tg) * 128
                    for f in range(nf):
                        bg, bgb = next_bank()
                        k.mm_group(bg[:, 0:N], [(wg_s[:, kc, f * 128:(f + 1) * 128], h2T[:, kc, t_lo:t_lo + N])
                                                for kc in range(8)], [wsb] + [h2b[n] for n in tg], bgb)
                        bu, bub = next_bank()
                        k.mm_group(bu[:, 0:N], [(wu_s[:, kc, f * 128:(f + 1) * 128], h2T[:, kc, t_lo:t_lo + N])
                                                for kc in range(8)], [wsb] + [h2b[n] for n in tg], bub)
                        si = sgi % 2
                        sgi += 1
                        k.act(sg[si][:, 0:N], bg[:, 0:N], AF.Silu, [bgb], [sgb[si]])
                        k.tt(actt[ab_i][:, f, 0:N], sg[si][:, 0:N], bu[:, 0:N], ALU.mult, [sgb[si], bub],
                             [actb[ab_i]])
                    for j, n in enumerate(tg):
                        for hf in range(2):
                            cs = slice(hf * 512, (hf + 1) * 512)
                            bk, bkb = next_bank()
                            k.mm_group(bk[:, :], [(actt[ab_i][:, f, j * 128:(j + 1) * 128], wd_s[:, f, cs])
                                                  for f in range(nf)], [wsb, actb[ab_i]], bkb)
                            if moe:
                                k.stt(x_tok[:, n, cs], bk[:, :], gate[:, n, e:e + 1], x_tok[:, n, cs],
                                      ALU.mult, ALU.add, [bkb, gateb[n]], [xb[n]])
                            else:
                                k.tt(x_tok[:, n, cs], x_tok[:, n, cs], bk[:, :], ALU.add, [bkb], [xb[n]])

        def ffn_phase0():
            tiles = list(range(0, NT))
            with ExitStack() as fs:
                h2T = sb(fs, "h2T", [128, 8, NT * 128], BF16)
                h2b = [Buf() for _ in range(NT)]
                with ExitStack() as f1:
                    g2_bc = sb(f1, "g2_bc", [128, D], F32)
                    gb = Buf()
                    h_tok = [sb(f1, "h_tokf%d" % i, [128, D], BF16) for i in range(2)]
                    h_tokb = [Buf(), Buf()]
                    junk = sb(f1, "junkf", [128, D], BF16)
                    junkb = Buf()
                    k.dma(SP, g2_bc[:], n2g_d[0:1, :].partition_broadcast(128), writes=[gb], sem="g2")
                    for ti, n in enumerate(tiles):
                        hb = ti % 2
                        norm_and_transpose(n, g2_bc[:], gb, h_tok[hb][:], h_tokb[hb], junk[:], junkb,
                                           h2T[:, :, n * 128:(n + 1) * 128], h2b[n])
                    p.barrier()
                with ExitStack() as f2:
                    ffn_stream(f2, False, tiles, h2T, h2b, GF)
                    p.barrier()

        def moe_phase():
            tiles = list(range(1, NT))
            with ExitStack() as fs:
                h_all = sb(fs, "h_all", [128, 16, D], BF16)
                hab = [Buf() for _ in range(16)]
                sel = sb(fs, "sel", [128, 16, NE], F32)
                pos = sb(fs, "pos", [128, 16, NE], F32)
                tot = sb(fs, "tot", [128, 16, NE], F32)
                offs = sb(fs, "offs", [128, 16, NE], F32)
                misc = sb(fs, "rmisc", [128, 40], F32)
                cond_i = sb(fs, "cond_i", [128, 32], I32)
                selb, posb, totb, offb, miscb, condb = (Buf() for _ in range(6))
                with ExitStack() as f1:
                    g2_bc = sb(f1, "g2_bc", [128, D], F32)
                    gb = Buf()
                    junk = sb(f1, "junkf", [128, D], BF16)
                    junkb = Buf()
                    rg = sb(f1, "rg", [128, NE, D], F32)
                    rgb = Buf()
                    junk32 = sb(f1, "junk32", [128, D], F32)
                    j32b = Buf()
                    lg = sb(f1, "lg", [128, 2, 32], F32)
                    lgb = [Buf(), Buf()]
                    k.dma(SP, g2_bc[:], n2g_d[1:2, :].partition_broadcast(128), writes=[gb], sem="g2")
                    for e in range(NE):
                        k.dma(SP, rg[:, e, :], rT_d[e:e + 1, :].partition_broadcast(128), writes=[rgb], sem="rg")
                    for e in range(NE):
                        k.tt(rg[:, e, :], rg[:, e, :], g2_bc[:], ALU.mult, [gb], [rgb])
                    for ti, n in enumerate(tiles):
                        T = n - 1
                        hb = ti % 2
                        rstd, sbf = rms_stats(x_tok[:, n, :], xb[n], junk[:], junkb, 1.0 / 32.0)
                        k.stt(h_all[:, T, :], x_tok[:, n, :], rstd, g2_bc[:], ALU.mult, ALU.mult,
                              [xb[n], sbf, gb], [hab[T]])
                        L_ = lg[:, hb, :]
                        lb = lgb[hb]
                        for e in range(NE):
                            k.stt(junk32[:], x_tok[:, n, :], rstd, rg[:, e, :], ALU.mult, ALU.mult,
                                  [xb[n], sbf, rgb], [j32b, lb], accum=L_[:, e:e + 1])
                        k.emit(DVE, lambda e_, o=L_[:, 24:25], i=L_[:, 0:8]: e_.reduce_max(out=o, in_=i, axis=AX.X),
                               [], [lb])
                        k.ts1(L_[:, 8:16], L_[:, 0:8], L_[:, 24:25], ALU.is_equal, [], [lb])
                        k.stt(L_[:, 16:24], L_[:, 8:16], -1e30, L_[:, 0:8], ALU.mult, ALU.add, [], [lb])
                        k.emit(DVE, lambda e_, o=L_[:, 25:26], i=L_[:, 16:24]: e_.reduce_max(out=o, in_=i, axis=AX.X),
                               [], [lb])
                        k.ts1(L_[:, 16:24], L_[:, 16:24], L_[:, 25:26], ALU.is_equal, [], [lb])
                        k.tt(sel[:, T, :], L_[:, 8:16], L_[:, 16:24], ALU.add, [lb], [selb])
                        k.tt(L_[:, 26:27], L_[:, 25:26], L_[:, 24:25], ALU.subtract, [], [lb])
                        k.act(L_[:, 26:27], L_[:, 26:27], AF.Exp, [], [lb])
                        k.ts(L_[:, 27:28], L_[:, 26:27], 1.0, 0.0, ALU.add, ALU.add, [], [lb])
                        k.recip(L_[:, 27:28], L_[:, 27:28], [], [lb])
                        k.tt(L_[:, 28:29], L_[:, 26:27], L_[:, 27:28], ALU.mult, [], [lb])
                        k.ts1(L_[:, 8:16], L_[:, 8:16], L_[:, 27:28], ALU.mult, [], [lb])
                        k.stt(gate[:, n, :], L_[:, 16:24], L_[:, 28:29], L_[:, 8:16], ALU.mult, ALU.add,
                              [lb], [gateb[n]])
                    selv = sel[:, :, :].rearrange("p t e -> p (t e)")
                    b1, b1b = next_bank()
                    k.mm_group(b1[:, 0:128], [(ustrict[:, :], selv)], [selb, cb], b1b)
                    k.copy(pos[:, :, :].rearrange("p t e -> p (t e)"), b1[:, 0:128], [b1b], [posb])
                    b2, b2b = next_bank()
                    k.mm_group(b2[:, 0:128], [(ones32[:, :], selv)], [selb, cb], b2b)
                    k.copy(tot[:, :, :].rearrange("p t e -> p (t e)"), b2[:, 0:128], [b2b], [totb])
                    k.memset(offs[:, 0, :], 0.0, [offb])
                    for T in range(1, 16):
                        k.tt(offs[:, T, :], offs[:, T - 1, :], tot[:, T - 1, :], ALU.add, [totb], [offb])
                    k.tt(pos[:, :, :], pos[:, :, :], offs[:, :, :], ALU.add, [offb], [posb])
                    k.tt(misc[:, 0:8], offs[:, 15, :], tot[:, 15, :], ALU.add, [offb, totb], [miscb])
                    for c, cap in enumerate(CAPS):
                        k.ts(misc[:, 8 + 8 * c:16 + 8 * c], misc[:, 0:8], float(cap) + 0.5, 0.0, ALU.is_lt, ALU.add,
                             [], [miscb])
                    k.copy(cond_i[:, 0:8 * len(CAPS)], misc[:, 8:8 + 8 * len(CAPS)], [miscb], [condb])
                    if FORCE_CLASS is not None:
                        for c in range(len(CAPS)):
                            k.memset(cond_i[:, 8 * c:8 * c + 8], 1 if c >= FORCE_CLASS else 0, [condb])
                    p.barrier()

                def expert_sparse(e, cap):
                    NJ = cap // 128
                    chunks = [(0, 512)] + ([(512, cap - 512)] if cap > 512 else [])
                    with ExitStack() as sp_:
                        Pb = [sb(sp_, "P%d" % i, [128, cap], BF16) for i in range(4)]
                        Pbb = [Buf() for _ in range(4)]
                        pi = [0]
                        hTe = sb(sp_, "hTe", [128, 8, cap], BF16)
                        hTeb = Buf()
                        GW = GFS * 128
                        slots = []
                        for s_ in range(2):
                            slots.append((sb(sp_, "swg%d" % s_, [128, 8, GW], BF16),
                                          sb(sp_, "swu%d" % s_, [128, 8, GW], BF16),
                                          sb(sp_, "swd%d" % s_, [128, GFS, D], BF16), Buf(), Buf()))
                        actt = [sb(sp_, "sact%d" % i, [128, GFS, cap], BF16) for i in range(2)]
                        actb = [Buf(), Buf()]
                        sg = [sb(sp_, "ssg%d" % i, [128, cap], F32) for i in range(2)]
                        sgb = [Buf(), Buf()]
                        oe32 = sb(sp_, "oe32", [128, NJ, D], F32)
                        oe_bf = sb(sp_, "oe_bf", [128, NJ, D], BF16)
                        oeb = [[Buf(), Buf()] for _ in range(NJ)]
                        oebf_b = Buf()
                        PT = [sb(sp_, "PT%d" % i, [128, NJ, 128], BF16) for i in range(2)]
                        PTb = [Buf(), Buf()]

                        def build_P(T, c0, w):
                            i = pi[0] % 4
                            pi[0] += 1
                            k.ts(Pb[i][:, 0:w], iota[:, c0:c0 + w], pos[:, T, e:e + 1], sel[:, T, e:e + 1],
                                 ALU.is_equal, ALU.mult, [cb, posb, selb], [Pbb[i]])
                            return Pb[i], Pbb[i]

                        for (c0, w) in chunks:
                            per_bank = 512 // w
                            nb = 8 // per_bank
                            bks = [next_bank() for _ in range(nb)]
                            allb = [b for _, b in bks]
                            tok = None
                            for T in range(16):
                                Pt, Ptb = build_P(T, c0, w)
                                deps = [Ptb.w, hab[T].w]
                                if T == 0:
                                    deps += k.deps_of([], allb)
                                for kc in range(8):
                                    o = bks[kc // per_bank][0][:, (kc % per_bank) * w:(kc % per_bank + 1) * w]
                                    tok = p.op(PE, (lambda e_, o=o, a=h_all[:, T, kc * 128:(kc + 1) * 128], r=Pt[:, 0:w],
                                                    s_=(T == 0), t_=(T == 15): e_.matmul(o, lhsT=a, rhs=r, start=s_, stop=t_)),
                                               deps if kc == 0 else (), kc == 7)
                                Ptb.add_read(tok)
                                hab[T].add_read(tok)
                            for b in allb:
                                b.set_write(tok)
                            for bi, (bk, bkb) in enumerate(bks):
                                k.act(hTe[:, bi * per_bank:(bi + 1) * per_bank, c0:c0 + w],
                                      bk[:, 0:per_bank * w].rearrange("p (a b) -> p a b", a=per_bank), AF.Copy,
                                      [bkb], [hTeb])
                        nch = D_FFE // 128
                        ngrp = (nch + GFS - 1) // GFS
                        ginfo = {}
                        sgi_box = [0]

                        def ffn_s1(g_):
                            c0f = g_ * GFS
                            nf = min(GFS, nch - c0f)
                            wg_s, wu_s, wd_s, wsb, wdb = slots[g_ % 2]
                            k.dma(POOL, wg_s[:, :, 0:nf * 128],
                                  mwg_d[e, :, c0f * 128:(c0f + nf) * 128].rearrange("(kc p) n -> p kc n", p=128),
                                  writes=[wsb], sem="wsA%d" % (g_ % 2))
                            k.dma(POOL, wu_s[:, :, 0:nf * 128],
                                  mwu_d[e, :, c0f * 128:(c0f + nf) * 128].rearrange("(kc p) n -> p kc n", p=128),
                                  writes=[wsb], sem="wsA%d" % (g_ % 2))
                            k.dma(POOL, wd_s[:, 0:nf, :],
                                  mwd_d[e, c0f * 128:(c0f + nf) * 128, :].rearrange("(f p) n -> p f n", p=128),
                                  writes=[wdb], sem="wsB%d" % (g_ % 2))
                            ab_i = g_ % 2
                            ginfo[g_] = (nf, wd_s, wdb, ab_i)
                            for f in range(nf):
                                fs_ = slice(f * 128, (f + 1) * 128)
                                si = sgi_box[0] % 2
                                sgi_box[0] += 1
                                for (c0, w) in chunks:
                                    if w == 512:
                                        bg, bgb = next_bank()
                                        bu, bub = next_bank()
                                        k.mm_group(bg[:, :], [(wg_s[:, kc, fs_], hTe[:, kc, c0:c0 + w]) for kc in range(8)],
                                                   [wsb, hTeb], bgb)
                                        k.mm_group(bu[:, :], [(wu_s[:, kc, fs_], hTe[:, kc, c0:c0 + w]) for kc in range(8)],
                                                   [wsb, hTeb], bub)
                                        g_ap, u_ap = bg[:, :], bu[:, :]
                                    else:
                                        bs, bsb = next_bank()
                                        deps = k.deps_of([wsb, hTeb], [bsb])
                                        tok = None
                                        for wi, w_s in enumerate((wg_s, wu_s)):
                                            for kc in range(8):
                                                tok = p.op(PE, (lambda e_, o=bs[:, wi * w:(wi + 1) * w], a=w_s[:, kc, fs_],
                                                                r=hTe[:, kc, c0:c0 + w], s_=(kc == 0), t_=(kc == 7):
                                                                e_.matmul(o, lhsT=a, rhs=r, start=s_, stop=t_)),
                                                           deps if (wi == 0 and kc == 0) else (), (wi == 1 and kc == 7))
                                        wsb.add_read(tok)
                                        hTeb.add_read(tok)
                                        bsb.set_write(tok)
                                        bgb = bub = bsb
                                        g_ap, u_ap = bs[:, 0:w], bs[:, w:2 * w]
                                    k.act(sg[si][:, c0:c0 + w], g_ap, AF.Silu, [bgb], [sgb[si]])
                                    k.tt(actt[ab_i][:, f, c0:c0 + w], sg[si][:, c0:c0 + w], u_ap, ALU.mult,
                                         [sgb[si], bub], [actb[ab_i]])

                        def ffn_s2(g_):
                            nf, wd_s, wsb, ab_i = ginfo[g_]
                            for j in range(NJ):
                                for hf in range(2):
                                    cs = slice(hf * 512, (hf + 1) * 512)
                                    bk, bkb = next_bank()
                                    k.mm_group(bk[:, :], [(actt[ab_i][:, f, j * 128:(j + 1) * 128], wd_s[:, f, cs])
                                                          for f in range(nf)], [wsb, actb[ab_i]], bkb)
                                    ob = oeb[j][hf]
                                    if g_ == 0:
                                        k.act(oe32[:, j, cs], bk[:, :], AF.Copy, [bkb], [ob])
                                    elif g_ < ngrp - 1:
                                        k.tt(oe32[:, j, cs], oe32[:, j, cs], bk[:, :], ALU.add, [bkb], [ob])
                                    else:
                                        k.tt(oe_bf[:, j, cs], oe32[:, j, cs], bk[:, :], ALU.add, [bkb, ob], [oebf_b])

                        ffn_s1(0)
                        for g_ in range(ngrp):
                            if g_ + 1 < ngrp:
                                ffn_s1(g_ + 1)
                            ffn_s2(g_)
                        pend = None
                        for T in range(17):
                            cur = None
                            if T < 16:
                                Pt, Ptb = build_P(T, 0, cap)
                                bk, bkb = next_bank()
                                bkbf = bk.bitcast(BF16)
                                outs = [bkbf[:, j * 128:(j + 1) * 128] for j in range(NJ)]
                                pairs = [(Pt[:, j * 128:(j + 1) * 128], ident[:, :]) for j in range(NJ)]
                                k.mm_group(outs, pairs, [Ptb, cb], bkb, transpose=True)
                                pti = T % 2
                                k.act(PT[pti][:, :, :], bkbf[:, 0:cap].rearrange("p (j t) -> p j t", j=NJ), AF.Copy,
                                      [bkb], [PTb[pti]])
                                cur = (T, pti)
                            if pend is not None:
                                Tp, ptp = pend
                                n = Tp + 1
                                for hf in range(2):
                                    cs = slice(hf * 512, (hf + 1) * 512)
                                    bk2, bk2b = next_bank()
                                    k.mm_group(bk2[:, :], [(PT[ptp][:, j, :], oe_bf[:, j, cs]) for j in range(NJ)],
                                               [PTb[ptp], oebf_b], bk2b)
                                    k.stt(x_tok[:, n, cs], bk2[:, :], gate[:, n, e:e + 1], x_tok[:, n, cs],
                                          ALU.mult, ALU.add, [bk2b, gateb[n]], [xb[n]])
                            pend = cur
                        p.barrier()

                def expert_dense(e):
                    with ExitStack() as db:
                        h2T = sb(db, "h2T", [128, 8, NT * 128], BF16)
                        h2b = [Buf() for _ in range(NT)]
                        for n in tiles:
                            T = n - 1
                            bk, bkb = next_bank()
                            bkbf = bk.bitcast(BF16)
                            outs = [bkbf[:, kc * 128:(kc + 1) * 128] for kc in range(8)]
                            pairs = [(h_all[:, T, kc * 128:(kc + 1) * 128], ident[:, :]) for kc in range(8)]
                            k.mm_group(outs, pairs, [hab[T], cb], bkb, transpose=True)
                            k.act(h2T[:, :, n * 128:(n + 1) * 128], bkbf[:, :].rearrange("p (k t) -> p k t", k=8),
                                  AF.Copy, [bkb], [h2b[n]])
                        ffn_stream(db, True, tiles, h2T, h2b, GFS, experts=[e])
                        p.barrier()

                def cflag(c, e):
                    return cond_i[0:1, 8 * c + e:8 * c + e + 1]

                for e in range(NE):
                    p.cond_region(
                        cflag(1, e), [],
                        lambda e=e: p.cond_region(cflag(0, e), [], lambda: expert_sparse(e, CAPS[0]),
                                                  lambda: expert_sparse(e, CAPS[1])),
                        lambda e=e: p.cond_region(cflag(2, e), [], lambda: expert_sparse(e, CAPS[2]),
                                                  lambda: expert_dense(e)))
                    p.barrier()

        def final_phase(do_norm):
            with ExitStack() as os_:
                gf_bc = sb(os_, "gf_bc", [128, D], F32)
                gfb = Buf()
                outt = [sb(os_, "outt%d" % i, [128, D], F32) for i in range(2)]
                outb = [Buf(), Buf()]
                junk = sb(os_, "junko", [128, D], BF16)
                junkb = Buf()
                toks = []
                if do_norm:
                    k.dma(SP, gf_bc[:], fg_d[0:1, :].partition_broadcast(128), writes=[gfb], sem="gf")
                for n in range(1, NT):
                    if do_norm:
                        oi = n % 2
                        rstd, sbf = rms_stats(x_tok[:, n, :], xb[n], junk[:], junkb, 1.0 / 32.0)
                        k.stt(outt[oi][:], x_tok[:, n, :], rstd, gf_bc[:], ALU.mult, ALU.mult,
                              [xb[n], sbf, gfb], [outb[oi]])
                        toks.append(k.dma(SP, y_d[(n - 1) * 128:n * 128, :], outt[oi][:], reads=[outb[oi]], sem="out%d" % oi))
                    else:
                        toks.append(k.dma(SP, y_d[(n - 1) * 128:n * 128, :], x_tok[:, n, :], reads=[xb[n]], sem="out0"))
                p.wait_only(SP, toks)
                p.barrier()

        mixer_phase(0)
        if stop_after == "M0":
            final_phase(False)
            p.flush()
            return nc
        ffn_phase0()
        if stop_after == "F0":
            final_phase(False)
            p.flush()
            return nc
        k.ts1(x_tok[:, 0, :], x_tok[:, 0, :], flag[:, 0:1], ALU.mult, [cb], [xb[0]])
        mixer_phase(1)
        if stop_after == "M1":
            final_phase(False)
            p.flush()
            return nc
        moe_phase()
        final_phase(True)
        p.flush()
    return nc


def _prep_shared(inp):
    f = lambda a: np.ascontiguousarray(np.asarray(a, dtype=np.float32))
    sh = {}
    sh["ident"] = np.eye(128, dtype=np.float32)
    sh["ustrict"] = np.triu(np.ones((128, 128), np.float32), 1)
    sh["iota"] = np.ascontiguousarray(np.broadcast_to(np.arange(CAPS[-1], dtype=np.float32), (128, CAPS[-1])))
    pp = np.zeros((128, 2, NPP), np.float32)
    wins = np.array([[2.0, 4.0], [8.0, 16.0]], np.float32)
    for l in range(2):
        pp[:, l, PP_PSCALE:PP_PSCALE + 2] = f(inp["pool_scale"])[l].reshape(2, 128).T
        dw = f(inp["conv_dw_w"])[l]
        pp[:, l, PP_DWW:PP_DWW + 93] = dw.reshape(31, 3, 128).transpose(2, 1, 0).reshape(128, 93)
        pp[:, l, PP_DWB:PP_DWB + 3] = f(inp["conv_dw_b"])[l].reshape(3, 128).T
        pp[:, l, PP_LNG:PP_LNG + 3] = f(inp["conv_ln_g"])[l].reshape(3, 128).T
        pp[:, l, PP_LNB:PP_LNB + 3] = f(inp["conv_ln_b"])[l].reshape(3, 128).T
        pp[:, l, PP_PWB:PP_PWB + 3] = f(inp["conv_pw_b"])[l].reshape(3, 128).T
        for c in range(2):
            pp[0:64, l, PP_INVW + c] = 1.0 / wins[c, 0]
            pp[64:128, l, PP_INVW + c] = 1.0 / wins[c, 1]
    sh["pp"] = pp.reshape(128, 2 * NPP)
    sh["norm1_g"] = f(inp["norm1_g"])
    sh["norm2_g"] = f(inp["norm2_g"])
    sh["final_g"] = f(inp["final_g"]).reshape(1, D)
    sh["gm_norm_g"] = f(inp["gm_norm_g"])
    sh["gm_b"] = f(inp["gm_b"]).reshape(2, 512)
    sh["w_in"] = f(inp["w_in"])
    sh["pool_w"] = f(inp["pool_w"])
    sh["gm_wsT"] = np.ascontiguousarray(f(inp["gm_ws"]).transpose(0, 1, 3, 2))
    sh["conv_pw_w"] = f(inp["conv_pw_w"])
    sh["w_out"] = f(inp["w_out"])
    sh["ffn_wg"] = f(inp["ffn_wg"])[0]
    sh["ffn_wu"] = f(inp["ffn_wu"])[0]
    sh["ffn_wd"] = f(inp["ffn_wd"])[0]
    sh["routerT"] = np.ascontiguousarray(f(inp["moe_router"])[0].T)
    sh["moe_wg"] = f(inp["moe_wg"])[0]
    sh["moe_wu"] = f(inp["moe_wu"])[0]
    sh["moe_wd"] = f(inp["moe_wd"])[0]
    return sh


def _prep_core(x, c):
    b, q = c // 4, c % 4
    xin = np.zeros((NT * 128, D), np.float32)
    xin[128:] = x[b, q * 2048:(q + 1) * 2048]
    if q > 0:
        xin[:128] = x[b, q * 2048 - 128:q * 2048]
    flag = np.full((128, 1), 1.0 if q > 0 else 0.0, np.float32)
    pinv = np.zeros((128, 2, 16), np.float32)
    wins = [[2, 4], [8, 16]]
    for cc in range(2):
        for half in range(2):
            w = wins[cc][half]
            for j in range(16):
                cntv = min(j + 1, w) if q == 0 else w
                pinv[half * 64:(half + 1) * 64, cc, j] = 1.0 / cntv
    return {"x": xin, "flag": flag, "pool_inv": pinv.reshape(128, 32)}


_NC_CACHE = {}


def run(inputs, stop_after=None, trace=False):
    x = np.asarray(inputs["x"], dtype=np.float32)
    sh = _prep_shared(inputs)
    in_maps = []
    for c in range(8):
        m = dict(sh)
        m.update(_prep_core(x, c))
        in_maps.append(m)
    if stop_after not in _NC_CACHE:
        _NC_CACHE[stop_after] = build_program(stop_after)
    nc = _NC_CACHE[stop_after]
    res = run_bass_kernel_spmd(nc, in_maps, core_ids=list(range(8)), **({"trace": True} if trace else {}))
    out = np.zeros((2, 8192, D), np.float32)
    for c in range(8):
        b, q = c // 4, c % 4
        out[b, q * 2048:(q + 1) * 2048] = res.results[c]["y"]
    return out, res


def kernel(**inputs):
    out, _ = run(inputs)
    return out
```

```python
import numpy as np
from contextlib import ExitStack
import concourse.bass as bass
import concourse.mybir as mybir
from concourse.bass_utils import run_bass_kernel_spmd

F32 = mybir.dt.float32
BF16 = mybir.dt.bfloat16
I32 = mybir.dt.int32
AF = mybir.ActivationFunctionType
ALU = mybir.AluOpType
AX = mybir.AxisListType

PE, ACT, DVE, POOL, SP = "pe", "act", "dve", "pool", "sp"
ENGS = (PE, ACT, DVE, POOL, SP)

D = 1024
NT = 17
D_IN = 1792
D_FF = 2816
D_FFE = 3584
NE = 8
EPS = 1e-6
NPP = 112
PP_PSCALE = 0
PP_DWW = 2
PP_DWB = 95
PP_LNG = 98
PP_LNB = 101
PP_PWB = 104
PP_INVW = 107
TGM = 2
GF = 4
GFS = 2
CAPS = (512, 640, 768)
FORCE_CLASS = None


class Prog:
    def __init__(self, nc, stack):
        self.nc = nc
        self.stack = stack
        self.ops = {e: [] for e in ENGS}
        self.sems = {}
        self.cnt = {}
        self.seen = {e: {} for e in ENGS}
        for e in ENGS:
            self._mksem("eng_" + e)

    def _mksem(self, key):
        if key not in self.sems:
            self.sems[key] = self.stack.enter_context(self.nc.semaphore(key))
            self.cnt[key] = 0
        return self.sems[key]

    def _waits(self, eng, deps):
        out = []
        for d in deps:
            if d is None:
                continue
            key, val = d
            if eng == PE and key == "eng_pe":
                continue
            if self.seen[eng].get(key, 0) >= val:
                continue
            self.seen[eng][key] = val
            out.append((self.sems[key], val))
        return out

    def op(self, eng, fn, deps=(), inc=True):
        waits = self._waits(eng, deps)
        key = "eng_" + eng
        tok = None
        if inc:
            self.cnt[key] += 1
            tok = (key, self.cnt[key])
        self.ops[eng].append((waits, fn, (self.sems[key], 1) if inc else None))
        return tok

    def dma(self, eng, out, in_, semname, deps=()):
        self._mksem(semname)
        waits = self._waits(eng, deps)
        self.cnt[semname] += 16
        tok = (semname, self.cnt[semname])
        self.ops[eng].append(
            (waits, lambda e, o=out, i=in_: e.dma_start(out=o, in_=i), (self.sems[semname], 16))
        )
        return tok

    def wait_only(self, eng, deps):
        waits = self._waits(eng, deps)
        if waits:
            self.ops[eng].append((waits, None, None))

    def barrier(self):
        toks = [(k, v) for k, v in self.cnt.items() if v > 0]
        for e in ENGS:
            self.wait_only(e, toks)

    def cond_region(self, cond_ap, cond_deps, then_fn, else_fn):
        for e in ENGS:
            self.ops[e].append(("IF", cond_ap, self._waits(e, cond_deps)))
        snap_cnt = dict(self.cnt)
        snap_seen = {e: dict(d) for e, d in self.seen.items()}
        Buf.reset_all()
        then_fn()
        then_cnt = dict(self.cnt)
        then_end = {e: len(self.ops[e]) for e in ENGS}
        self.cnt = dict(snap_cnt)
        for kk in then_cnt:
            self.cnt.setdefault(kk, 0)
        self.seen = {e: dict(d) for e, d in snap_seen.items()}
        for e in ENGS:
            self.ops[e].append(("ELSE",))
        Buf.reset_all()
        else_fn()
        else_cnt = dict(self.cnt)
        keys = set(then_cnt) | set(else_cnt)
        final = {kk: max(then_cnt.get(kk, 0), else_cnt.get(kk, 0)) for kk in keys}

        def equalizers(branch_cnt):
            per_eng = {e: [] for e in ENGS}
            for kk in sorted(keys):
                diff = final[kk] - branch_cnt.get(kk, 0)
                if diff <= 0:
                    continue
                eng = kk[4:] if kk.startswith("eng_") else SP
                per_eng[eng].append(("EQ", self.sems[kk], branch_cnt.get(kk, 0), diff))
            return per_eng

        eq_then = equalizers(then_cnt)
        eq_else = equalizers(else_cnt)
        for e in ENGS:
            self.ops[e][then_end[e]:then_end[e]] = eq_then[e]
            self.ops[e].extend(eq_else[e])
            self.ops[e].append(("ENDIF",))
        self.cnt = final
        self.seen = snap_seen
        Buf.reset_all()

    def flush(self):
        nc = self.nc
        ops = self.ops

        def run(e, lst):
            cms = []
            for item in lst:
                tag = item[0]
                if tag == "IF":
                    for s_, v in item[2]:
                        e.wait_ge(s_, v)
                    val = e.value_load(item[1])
                    cm = e.If(val == 1)
                    cm.__enter__()
                    cms.append(cm)
                elif tag == "ELSE":
                    cms.pop().__exit__(None, None, None)
                    cm = e.Else()
                    cm.__enter__()
                    cms.append(cm)
                elif tag == "ENDIF":
                    cms.pop().__exit__(None, None, None)
                elif tag == "EQ":
                    _, sem, have, diff = item
                    if have > 0:
                        e.wait_ge(sem, have)
                    e.sem_inc(sem, diff)
                else:
                    waits, fn, inc = item
                    for s_, v in waits:
                        e.wait_ge(s_, v)
                    if fn is not None:
                        ins = fn(e)
                        if inc is not None:
                            ins.then_inc(inc[0], inc[1])

        with nc.Block() as block:
            @block.tensor
            def _(e):
                run(e, ops[PE])

            @block.scalar
            def _(e):
                run(e, ops[ACT])

            @block.vector
            def _(e):
                run(e, ops[DVE])

            @block.gpsimd
            def _(e):
                run(e, ops[POOL])

            @block.sync
            def _(e):
                run(e, ops[SP])
        self.ops = {e: [] for e in ENGS}


class Buf:
    ALL = []

    def __init__(self, name=""):
        self.name = name
        self.w = None
        self.r = {}
        Buf.ALL.append(self)

    @staticmethod
    def reset_all():
        for b in Buf.ALL:
            b.w = None
            b.r = {}

    def add_read(self, tok):
        if tok is None:
            return
        k, v = tok
        if self.r.get(k, 0) < v:
            self.r[k] = v

    def set_write(self, tok):
        self.w = tok
        self.r = {}


class K:
    def __init__(self, nc, stack):
        self.nc = nc
        self.p = Prog(nc, stack)
        self.dma_n = 0

    def deps_of(self, reads, writes):
        deps = []
        for b in reads:
            deps.append(b.w)
        for b in writes:
            deps.append(b.w)
            deps.extend(b.r.items())
        return deps

    def emit(self, eng, fn, reads=(), writes=()):
        tok = self.p.op(eng, fn, self.deps_of(reads, writes), True)
        for b in reads:
            b.add_read(tok)
        for b in writes:
            b.set_write(tok)
        return tok

    def dma(self, eng, out, in_, reads=(), writes=(), sem=None):
        assert sem is not None
        deps = []
        for b in reads:
            deps.append(b.w)
        for b in writes:
            if not (b.w is not None and b.w[0] == sem):
                deps.append(b.w)
            deps.extend(b.r.items())
        tok = self.p.dma(eng, out, in_, sem, deps)
        for b in reads:
            b.add_read(tok)
        for b in writes:
            b.set_write(tok)
        return tok

    def mm_group(self, out, pairs, reads, bank, transpose=False):
        deps = self.deps_of(reads, [bank])
        n = len(pairs)
        tok = None
        for i, (l, r) in enumerate(pairs):
            last = i == n - 1
            if transpose:
                fn = (lambda e, o=out[i], a=l, b=r: e.transpose(o, a, b))
            else:
                fn = (lambda e, o=out, a=l, b=r, s=(i == 0), t=last: e.matmul(o, lhsT=a, rhs=b, start=s, stop=t))
            tok = self.p.op(PE, fn, deps if i == 0 else (), last)
        for b in reads:
            b.add_read(tok)
        bank.set_write(tok)
        return tok

    def mm_multi(self, groups, reads, bank):
        deps = self.deps_of(reads, [bank])
        n = len(groups)
        tok = None
        for i, (o, l, r) in enumerate(groups):
            last = i == n - 1
            fn = (lambda e, o=o, a=l, b=r: e.matmul(o, lhsT=a, rhs=b, start=True, stop=True))
            tok = self.p.op(PE, fn, deps if i == 0 else (), last)
        for b in reads:
            b.add_read(tok)
        bank.set_write(tok)
        return tok

    def act(self, out, in_, func, reads, writes, bias=None, scale=None, accum=None):
        kw = {}
        if bias is not None:
            kw["bias"] = bias
        if scale is not None:
            kw["scale"] = scale
        if accum is not None:
            kw["accum_out"] = accum
        return self.emit(ACT, lambda e: e.activation(out=out, in_=in_, func=func, **kw), reads, writes)

    def tt(self, out, in0, in1, op, reads, writes, eng=DVE):
        return self.emit(eng, lambda e: e.tensor_tensor(out=out, in0=in0, in1=in1, op=op), reads, writes)

    def ts(self, out, in0, s1, s2, op0, op1, reads, writes, eng=DVE):
        return self.emit(eng, lambda e: e.tensor_scalar(out=out, in0=in0, scalar1=s1, scalar2=s2, op0=op0, op1=op1),
                         reads, writes)

    def ts1(self, out, in0, s1, op0, reads, writes, eng=DVE):
        return self.emit(eng, lambda e: e.tensor_scalar(out=out, in0=in0, scalar1=s1, scalar2=None, op0=op0),
                         reads, writes)

    def stt(self, out, in0, scalar, in1, op0, op1, reads, writes, accum=None, eng=DVE):
        kw = {}
        if accum is not None:
            kw["accum_out"] = accum
        return self.emit(eng, lambda e: e.scalar_tensor_tensor(out=out, in0=in0, scalar=scalar, in1=in1,
                                                               op0=op0, op1=op1, **kw), reads, writes)

    def copy(self, out, in_, reads, writes, eng=DVE):
        return self.emit(eng, lambda e: e.tensor_copy(out=out, in_=in_), reads, writes)

    def recip(self, out, in_, reads, writes):
        return self.emit(DVE, lambda e: e.reciprocal(out=out, in_=in_), reads, writes)

    def memset(self, ap, val, writes, eng=DVE):
        return self.emit(eng, lambda e: e.memset(ap, val), (), writes)


def build_program(stop_after=None):
    nc = bass.Bass("TRN2", target_bir_lowering=False)

    def din(name, shape):
        return nc.dram_tensor(name, list(shape), F32, kind="ExternalInput").ap()

    x_d = din("x", [NT * 128, D])
    flag_d = din("flag", [128, 1])
    pinv_d = din("pool_inv", [128, 32])
    ident_d = din("ident", [128, 128])
    ustrict_d = din("ustrict", [128, 128])
    iota_d = din("iota", [128, CAPS[-1]])
    pp_d = din("pp", [128, 2 * NPP])
    n1g_d = din("norm1_g", [2, D])
    n2g_d = din("norm2_g", [2, D])
    fg_d = din("final_g", [1, D])
    gmg_d = din("gm_norm_g", [2, 384])
    gmb_d = din("gm_b", [2, 512])
    w_in_d = din("w_in", [2, D, D_IN])
    pool_w_d = din("pool_w", [2, 4, 64, 64])
    wsT_d = din("gm_wsT", [2, 4, 128, 128])
    pw_d = din("conv_pw_w", [2, 384, 384])
    w_out_d = din("w_out", [2, D, D])
    fwg_d = din("ffn_wg", [D, D_FF])
    fwu_d = din("ffn_wu", [D, D_FF])
    fwd_d = din("ffn_wd", [D_FF, D])
    rT_d = din("routerT", [NE, D])
    mwg_d = din("moe_wg", [NE, D, D_FFE])
    mwu_d = din("moe_wu", [NE, D, D_FFE])
    mwd_d = din("moe_wd", [NE, D_FFE, D])
    y_d = nc.dram_tensor("y", [16 * 128, D], F32, kind="ExternalOutput").ap()

    with ExitStack() as st:
        ARENA_WORDS = 52600
        arena = st.enter_context(nc.sbuf_tensor("arena", [128, ARENA_WORDS], F32))
        atop = [0]
        scopes = {}

        def _release(mark):
            atop[0] = mark

        def sb(stack, name, shape, dt):
            if stack is not st and not getattr(stack, "_arena_marked", False):
                stack._arena_marked = True
                stack.callback(_release, atop[0])
            n = 1
            for d_ in shape[1:]:
                n *= d_
            nbytes = n * (4 if dt in (F32, I32) else 2)
            words = ((nbytes + 3) // 4 + 7) // 8 * 8
            assert atop[0] + words <= ARENA_WORDS, ("SBUF arena overflow", name, atop[0], words)
            v = arena[:, atop[0]:atop[0] + words]
            atop[0] += words
            if dt != F32:
                v = v.bitcast(dt)
            v = v[:, 0:n]
            if len(shape) == 3:
                v = v.rearrange("p (a b) -> p a b", a=shape[1])
            return v

        k = K(nc, st)
        p = k.p

        x_tok = sb(st, "x_tok", [128, NT, D], F32)
        xb = [Buf("x%d" % n) for n in range(NT)]
        ident = sb(st, "ident", [128, 128], BF16)
        ones32 = sb(st, "ones32", [128, 128], F32)
        ustrict = sb(st, "ustrict", [128, 128], F32)
        iota = sb(st, "iota", [128, CAPS[-1]], F32)
        pp = sb(st, "pp", [128, 2 * NPP], F32)
        flag = sb(st, "flag", [128, 1], F32)
        pinv = sb(st, "pinv", [128, 2, 16], F32)
        stt_t = sb(st, "stats", [128, 8, 4], F32)
        gate = sb(st, "gate", [128, NT, NE], F32)
        cb = Buf("consts")
        stb = [Buf("st%d" % i) for i in range(8)]
        gateb = [Buf("gate%d" % n) for n in range(NT)]
        banks = [st.enter_context(nc.psum_tensor("bank%d" % i, [128, 512], F32)) for i in range(8)]
        bankb = [Buf("bank%d" % i) for i in range(8)]
        ring = [0]
        stat_i = [0]

        def next_bank():
            i = ring[0]
            ring[0] = (i + 1) % 8
            return banks[i], bankb[i]

        def next_stat():
            i = stat_i[0]
            stat_i[0] = (i + 1) % 8
            return stt_t[:, i, :], stb[i]

        def ppc(l, col, n=1, lo=0, hi=128):
            return pp[lo:hi, l * NPP + col: l * NPP + col + n]

        k.memset(ones32[:], 1.0, [cb])
        k.dma(POOL, ident[:], ident_d, writes=[cb], sem="consts")
        k.dma(POOL, pp[:], pp_d, writes=[cb], sem="consts")
        k.dma(POOL, ustrict[:], ustrict_d, writes=[cb], sem="consts")
        k.dma(POOL, iota[:], iota_d, writes=[cb], sem="consts")
        k.dma(POOL, flag[:], flag_d, writes=[cb], sem="consts")
        k.dma(POOL, pinv[:].rearrange("p c j -> p (c j)"), pinv_d, writes=[cb], sem="consts")
        for n in range(NT):
            k.dma(SP, x_tok[:, n, :], x_d[n * 128:(n + 1) * 128, :], writes=[xb[n]], sem="x%d" % n)

        def rms_stats(src_ap, srcb, junk, junkb, scale):
            sap, sbf = next_stat()
            k.act(junk, src_ap, AF.Square, [srcb], [junkb, sbf], scale=scale, accum=sap[:, 0:1])
            k.act(sap[:, 1:2], sap[:, 0:1], AF.Sqrt, [sbf], [sbf], bias=EPS)
            k.recip(sap[:, 2:3], sap[:, 1:2], [sbf], [sbf])
            return sap[:, 2:3], sbf

        def norm_and_transpose(n, g_bc, gb, h_tok, h_tokb, junk, junkb, hT_dst, hTb):
            rstd, sbf = rms_stats(x_tok[:, n, :], xb[n], junk, junkb, 1.0 / 32.0)
            k.stt(h_tok, x_tok[:, n, :], rstd, g_bc, ALU.mult, ALU.mult, [xb[n], sbf, gb], [h_tokb])
            bk, bkb = next_bank()
            bkbf = bk.bitcast(BF16)
            outs = [bkbf[:, kc * 128:(kc + 1) * 128] for kc in range(8)]
            pairs = [(h_tok[:, kc * 128:(kc + 1) * 128], ident[:, :]) for kc in range(8)]
            k.mm_group(outs, pairs, [h_tokb, cb], bkb, transpose=True)
            k.act(hT_dst, bkbf[:, :].rearrange("p (k t) -> p k t", k=8), AF.Copy, [bkb], [hTb])
            return rstd, sbf

        def mixer_phase(l):
            with ExitStack() as ms:
                NMAX = TGM * 128
                LMAX = 32 + NMAX
                w_in_sb = sb(ms, "w_in_sb", [128, 8, D_IN], BF16)
                wo_a = sb(ms, "wo_a", [128, 2, D], BF16)
                wo_b = sb(ms, "wo_b", [128, 4, D], BF16)
                wo_c = sb(ms, "wo_c", [128, 3, D], BF16)
                pw_sb = sb(ms, "pw_sb", [128, 3, 384], BF16)
                wblk = sb(ms, "wblk", [128, 2, 128], BF16)
                wsT = sb(ms, "wsT", [128, 4, 128], BF16)
                g1_bc = sb(ms, "g1_bc", [128, D], F32)
                gmg_bc = sb(ms, "gmg_bc", [128, 384], F32)
                gmb_bc = sb(ms, "gmb_bc", [128, 512], F32)
                wb = Buf("mixw")
                h_tok = [sb(ms, "h_tok%d" % i, [128, D], BF16) for i in range(2)]
                h_tokb = [Buf(), Buf()]
                junk = sb(ms, "junk", [128, D], BF16)
                junkb = Buf()
                two = range(2)
                a_ext = [sb(ms, "a_ext%d" % i, [128, 2, LMAX], F32) for i in two]
                hc_ext = [sb(ms, "hc_ext%d" % i, [128, 3, LMAX], BF16) for i in two]
                yb = [sb(ms, "yb%d" % i, [128, 4, NMAX], BF16) for i in two]
                ab, hcb, ybb = ([Buf(), Buf()] for _ in range(3))

                def same2(name, shape, dt):
                    t_ = sb(ms, name, shape, dt)
                    return [t_, t_]

                def same2b():
                    b_ = Buf()
                    return [b_, b_]

                hT = same2("hT", [128, 8, NMAX], BF16)
                sig = same2("sig", [128, 3, NMAX], F32)
                acc = same2("acc", [128, 3, NMAX], F32)
                u_sb = same2("u_sb", [128, 4, NMAX], F32)
                y_p = same2("y_p", [128, 2, NMAX], BF16)
                ya = same2("ya", [128, 2, NMAX], BF16)
                hs = same2("hs", [128, 3, NMAX], BF16)
                yc = same2("yc", [128, 3, NMAX], BF16)
                hTb, sigb, ub, ypb, yab, hsb, ycb = (same2b() for _ in range(7))
                accb1 = [Buf(), Buf(), Buf()]
                accb = [accb1, accb1]
                dg = sb(ms, "dg", [128, 93, 128], BF16)
                dgb = Buf()
                sA = sb(ms, "sA", [128, 2, LMAX], F32)
                sB = sb(ms, "sB", [128, 2, LMAX], F32)
                tmp16 = sb(ms, "tmp16", [128, 16], F32)
                v_n = [sb(ms, "v_n%d" % i, [128, 384], BF16) for i in two]
                ztmp = sb(ms, "ztmp", [128, 512], F32)
                sq = sb(ms, "sq", [128, 3, NMAX], F32)
                mean = sb(ms, "mean", [128, NMAX], F32)
                var = sb(ms, "var", [128, NMAX], F32)
                rstdc = sb(ms, "rstdc", [128, NMAX], F32)
                sAb, sBb, t16b, ztb, sqb, meanb, varb, rsb = (Buf() for _ in range(8))
                vnb = [Buf(), Buf()]

                wblkb = Buf()
                wsTb = Buf()
                k.memset(wblk[:], 0.0, [wblkb])
                for c in range(2):
                    k.dma(POOL, wblk[0:64, c, 0:64], pool_w_d[l, 2 * c], writes=[wblkb], sem="wblk")
                    k.dma(POOL, wblk[64:128, c, 64:128], pool_w_d[l, 2 * c + 1], writes=[wblkb], sem="wblk")
                k.dma(POOL, wsT[:], wsT_d[l].rearrange("h j i -> j h i"), writes=[wsTb], sem="wsT")
                k.memset(wsT[64:128, :, 0:64], 0.0, [wsTb])
                k.dma(SP, g1_bc[:], n1g_d[l:l + 1, :].partition_broadcast(128), writes=[wb], sem="mixw")
                k.dma(SP, gmg_bc[:], gmg_d[l:l + 1, :].partition_broadcast(128), writes=[wb], sem="mixw")
                k.dma(SP, gmb_bc[:], gmb_d[l:l + 1, :].partition_broadcast(128), writes=[wb], sem="mixw")
                k.dma(POOL, w_in_sb[:], w_in_d[l].rearrange("(kc p) n -> p kc n", p=128), writes=[wb], sem="mixw")
                k.dma(POOL, wo_a[:], w_out_d[l, 0:256, :].rearrange("(c p) n -> p c n", p=128), writes=[wb], sem="mixw")
                k.dma(POOL, wo_b[0:96], w_out_d[l, 256:640, :].rearrange("(h p) n -> p h n", p=96), writes=[wb], sem="mixw")
                k.dma(POOL, wo_c[:], w_out_d[l, 640:1024, :].rearrange("(c p) n -> p c n", p=128), writes=[wb], sem="mixw")
                k.dma(POOL, pw_sb[:], pw_d[l].rearrange("(c p) n -> p c n", p=128), writes=[wb], sem="mixw")
                k.memset(a_ext[0][:, :, 0:32], 0.0, [ab[0]])
                k.memset(hc_ext[0][:, :, 0:32], 0.0, [hcb[0]])
                for i in range(93):
                    k.ts1(dg[:, i, :], ident[:, :], ppc(l, PP_DWW + i), ALU.mult, [cb], [dgb])

                groups = [(0, 1)] + [(t0, TGM) for t0 in range(1, NT, TGM)]

                def stage_ne(gi):
                    t0, nt = groups[gi]
                    for j in range(nt):
                        n = t0 + j
                        hb = (gi * TGM + j) % 2
                        rstd, sbf = rms_stats(x_tok[:, n, :], xb[n], junk[:], junkb, 1.0 / 32.0)
                        k.stt(h_tok[hb][:], x_tok[:, n, :], rstd, g1_bc[:], ALU.mult, ALU.mult,
                              [xb[n], sbf, wb], [h_tokb[hb]])

                def stage_nt(gi):
                    t0, nt = groups[gi]
                    q = gi % 2
                    for j in range(nt):
                        hb = (gi * TGM + j) % 2
                        bk, bkb = next_bank()
                        bkbf = bk.bitcast(BF16)
                        outs = [bkbf[:, kc * 128:(kc + 1) * 128] for kc in range(8)]
                        pairs = [(h_tok[hb][:, kc * 128:(kc + 1) * 128], ident[:, :]) for kc in range(8)]
                        k.mm_group(outs, pairs, [h_tokb[hb], cb], bkb, transpose=True)
                        k.act(hT[q][:, :, j * 128:(j + 1) * 128], bkbf[:, :].rearrange("p (k t) -> p k t", k=8),
                              AF.Copy, [bkb], [hTb[q]])

                def stage_p(gi):
                    t0, nt = groups[gi]
                    q = gi % 2
                    N = nt * 128
                    L = 32 + N
                    full = not (l == 1 and t0 == 0)
                    first_own = (t0 == 1)
                    if gi > 0:
                        pN = groups[gi - 1][1] * 128
                        k.copy(a_ext[q][:, :, 0:32], a_ext[1 - q][:, :, pN:pN + 32], [ab[1 - q]], [ab[q]], eng=POOL)
                        k.copy(hc_ext[q][:, :, 0:32], hc_ext[1 - q][:, :, pN:pN + 32], [hcb[1 - q]], [hcb[q]], eng=POOL)

                    def proj(col0, m):
                        bk, bkb = next_bank()
                        pairs = [(w_in_sb[:, kc, col0:col0 + m], hT[q][:, kc, 0:N]) for kc in range(8)]
                        k.mm_group(bk[0:m, 0:N], pairs, [wb, hTb[q]], bkb)
                        return bk, bkb

                    for c in range(2):
                        bk, bkb = proj(c * 128, 128)
                        k.act(a_ext[q][:, c, 32:L], bk[:, 0:N], AF.Copy, [bkb], [ab[q]])
                    vinfo = []
                    if full:
                        for j in range(nt):
                            vb = j % 2
                            bk, bkb = next_bank()
                            pairs = [(hT[q][:, kc, j * 128:(j + 1) * 128], w_in_sb[:, kc, 640:1024]) for kc in range(8)]
                            k.mm_group(bk[:, 0:384], pairs, [wb, hTb[q]], bkb)
                            rstd, sbf = rms_stats(bk[:, 0:384], bkb, junk[:, 0:384], junkb, float(384.0 ** -0.5))
                            k.stt(v_n[vb][:], bk[:, 0:384], rstd, gmg_bc[:], ALU.mult, ALU.mult,
                                  [bkb, sbf, wb], [vnb[vb]])
                    for c in range(3):
                        bk, bkb = proj(1408 + c * 128, 128)
                        k.act(sig[q][:, c, 0:N], bk[:, 0:N], AF.Sigmoid, [bkb], [sigb[q]])
                    for c in range(3):
                        bk, bkb = proj(1024 + c * 128, 128)
                        k.tt(hc_ext[q][:, c, 32:L], bk[:, 0:N], sig[q][:, c, 0:N], ALU.mult, [bkb, sigb[q]], [hcb[q]])
                    if full:
                        for h in range(4):
                            bk, bkb = proj(256 + h * 96, 96)
                            k.act(u_sb[q][0:96, h, 0:N], bk[0:96, 0:N], AF.Copy, [bkb], [ub[q]])
                        for j in range(nt):
                            vb = j % 2
                            zk, zkb = next_bank()
                            grp = [(zk[0:96, h * 128:(h + 1) * 128], v_n[vb][:, h * 96:(h + 1) * 96], wsT[:, h, :])
                                   for h in range(4)]
                            k.mm_multi(grp, [vnb[vb], wsTb], zkb)
                            k.tt(ztmp[0:96, :], zk[0:96, :], gmb_bc[0:96, :], ALU.add, [zkb, wb], [ztb])
                            k.tt(yb[q][0:96, :, j * 128:(j + 1) * 128],
                                 ztmp[0:96, :].rearrange("p (h i) -> p h i", h=4),
                                 u_sb[q][0:96, :, j * 128:(j + 1) * 128], ALU.mult, [ztb, ub[q]], [ybb[q]])
                        A_ = a_ext[q]

                        def pool_out(sbuf_t, sbuf_b, lo, hi, c):
                            k.stt(y_p[q][lo:hi, c, 0:N], sbuf_t[lo:hi, c, 32:L], ppc(l, PP_INVW + c, 1, lo, hi),
                                  A_[lo:hi, c, 32:L], ALU.mult, ALU.subtract, [sbuf_b, ab[q], cb], [ypb[q]])
                            if first_own:
                                k.tt(tmp16[lo:hi, :], sbuf_t[lo:hi, c, 32:48], pinv[lo:hi, c, :], ALU.mult,
                                     [sbuf_b, cb], [t16b])
                                k.tt(y_p[q][lo:hi, c, 0:16], tmp16[lo:hi, :], A_[lo:hi, c, 32:48], ALU.subtract,
                                     [t16b, ab[q]], [ypb[q]])

                        k.tt(sA[:, :, 1:L], A_[:, :, 1:L], A_[:, :, 0:L - 1], ALU.add, [ab[q]], [sAb])
                        pool_out(sA, sAb, 0, 64, 0)
                        k.tt(sB[:, :, 3:L], sA[:, :, 3:L], sA[:, :, 1:L - 2], ALU.add, [sAb], [sBb])
                        pool_out(sB, sBb, 64, 128, 0)
                        k.tt(sA[:, :, 7:L], sB[:, :, 7:L], sB[:, :, 3:L - 4], ALU.add, [sBb], [sAb])
                        pool_out(sA, sAb, 0, 64, 1)
                        k.tt(sB[:, :, 15:L], sA[:, :, 15:L], sA[:, :, 7:L - 8], ALU.add, [sAb], [sBb])
                        pool_out(sB, sBb, 64, 128, 1)

                def stage_b1(gi):
                    t0, nt = groups[gi]
                    q = gi % 2
                    N = nt * 128
                    L = 32 + N
                    full = not (l == 1 and t0 == 0)
                    first_own = (t0 == 1)
                    if not full:
                        return
                    A_ = a_ext[q]
                    H_ = hc_ext[q]

                    for c in range(2):
                        bk, bkb = next_bank()
                        k.mm_group(bk[:, 0:N], [(wblk[:, c, :], y_p[q][:, c, 0:N])], [wblkb, ypb[q]], bkb)
                        k.act(ya[q][:, c, 0:N], bk[:, 0:N], AF.Copy, [bkb, cb], [yab[q]], scale=ppc(l, PP_PSCALE + c))

                    for c in range(3):
                        bk, bkb = next_bank()
                        pairs = [(dg[:, c * 31 + kk, :], H_[:, c, 2 + kk:2 + kk + N]) for kk in range(31)]
                        k.mm_group(bk[:, 0:N], pairs, [dgb, hcb[q]], bkb)
                        k.act(acc[q][:, c, 0:N], bk[:, 0:N], AF.Identity, [bkb, cb], [accb[q][c]],
                              bias=ppc(l, PP_DWB + c))
                    for c in range(3):
                        k.act(sq[:, c, 0:N], acc[q][:, c, 0:N], AF.Square, [accb[q][c]], [sqb])
                    b1, b1b = next_bank()
                    k.mm_group(b1[:, 0:N], [(ones32[:, :], acc[q][:, c, 0:N]) for c in range(3)], accb[q] + [cb], b1b)
                    b2, b2b = next_bank()
                    k.mm_group(b2[:, 0:N], [(ones32[:, :], sq[:, c, 0:N]) for c in range(3)], [sqb, cb], b2b)
                    k.ts(mean[:, 0:N], b1[:, 0:N], 1.0 / 384.0, 0.0, ALU.mult, ALU.add, [b1b], [meanb])
                    k.tt(var[:, 0:N], mean[:, 0:N], mean[:, 0:N], ALU.mult, [meanb], [varb])
                    k.stt(var[:, 0:N], b2[:, 0:N], 1.0 / 384.0, var[:, 0:N], ALU.mult, ALU.subtract,
                          [b2b], [varb])
                    k.act(var[:, 0:N], var[:, 0:N], AF.Sqrt, [], [varb], bias=EPS)
                    k.recip(rstdc[:, 0:N], var[:, 0:N], [varb], [rsb])
                    for c in range(3):
                        k.tt(sq[:, c, 0:N], acc[q][:, c, 0:N], mean[:, 0:N], ALU.subtract, [accb[q][c], meanb], [sqb])
                        k.tt(sq[:, c, 0:N], sq[:, c, 0:N], rstdc[:, 0:N], ALU.mult, [rsb], [sqb])
                        k.act(hs[q][:, c, 0:N], sq[:, c, 0:N], AF.Silu, [sqb, cb], [hsb[q]],
                              bias=ppc(l, PP_LNB + c), scale=ppc(l, PP_LNG + c))
                def stage_b2(gi):
                    t0, nt = groups[gi]
                    q = gi % 2
                    N = nt * 128
                    full = not (l == 1 and t0 == 0)
                    if not full:
                        return
                    for co in range(3):
                        bk, bkb = next_bank()
                        pairs = [(pw_sb[:, ci, co * 128:(co + 1) * 128], hs[q][:, ci, 0:N]) for ci in range(3)]
                        k.mm_group(bk[:, 0:N], pairs, [wb, hsb[q]], bkb)
                        k.act(yc[q][:, co, 0:N], bk[:, 0:N], AF.Identity, [bkb, cb], [ycb[q]], bias=ppc(l, PP_PWB + co))

                    for j in range(nt):
                        n = t0 + j
                        ts_ = slice(j * 128, (j + 1) * 128)
                        for hf in range(2):
                            cs = slice(hf * 512, (hf + 1) * 512)
                            pairs = [(ya[q][:, c, ts_], wo_a[:, c, cs]) for c in range(2)]
                            pairs += [(yb[q][0:96, h, ts_], wo_b[0:96, h, cs]) for h in range(4)]
                            pairs += [(yc[q][:, c, ts_], wo_c[:, c, cs]) for c in range(3)]
                            bk, bkb = next_bank()
                            k.mm_group(bk[:, :], pairs, [wb, yab[q], ybb[q], ycb[q]], bkb)
                            k.tt(x_tok[:, n, cs], x_tok[:, n, cs], bk[:, :], ALU.add, [bkb], [xb[n]])

                ng = len(groups)
                stage_ne(0)
                stage_nt(0)
                if ng > 1:
                    stage_ne(1)
                stage_p(0)
                for gi in range(ng):
                    if gi + 1 < ng:
                        stage_nt(gi + 1)
                    stage_b1(gi)
                    if gi + 2 < ng:
                        stage_ne(gi + 2)
                    if gi + 1 < ng:
                        stage_p(gi + 1)
                    stage_b2(gi)
                p.barrier()

        def ffn_stream(scope, moe, tiles, h2T, h2b, gf, experts=None):
            GW = gf * 128
            slots = []
            for s_ in range(2):
                slots.append((sb(scope, "wg%d" % s_, [128, 8, GW], BF16),
                              sb(scope, "wu%d" % s_, [128, 8, GW], BF16),
                              sb(scope, "wd%d" % s_, [128, gf, D], BF16), Buf()))
            actt = [sb(scope, "act%d" % i, [128, gf, 512], BF16) for i in range(2)]
            actb = [Buf(), Buf()]
            sg = [sb(scope, "sg%d" % i, [128, 512], F32) for i in range(2)]
            sgb = [Buf(), Buf()]
            glist = []
            if not moe:
                nch = D_FF // 128
                for c0 in range(0, nch, gf):
                    nf = min(gf, nch - c0)
                    glist.append((fwg_d[:, c0 * 128:(c0 + nf) * 128], fwu_d[:, c0 * 128:(c0 + nf) * 128],
                                  fwd_d[c0 * 128:(c0 + nf) * 128, :], nf, None))
            else:
                nch = D_FFE // 128
                for e in (experts if experts is not None else range(NE)):
                    for c0 in range(0, nch, gf):
                        nf = min(gf, nch - c0)
                        glist.append((mwg_d[e, :, c0 * 128:(c0 + nf) * 128],
                                      mwu_d[e, :, c0 * 128:(c0 + nf) * 128],
                                      mwd_d[e, c0 * 128:(c0 + nf) * 128, :], nf, e))
            tgroups = [tiles[i:i + 4] for i in range(0, len(tiles), 4)]
            cnt = 0
            sgi = 0
            for gi, (wg_ap, wu_ap, wd_ap, nf, e) in enumerate(glist):
                wg_s, wu_s, wd_s, wsb = slots[gi % 2]
                sem = "wslot%d" % (gi % 2)
                k.dma(POOL, wg_s[:, :, 0:nf * 128], wg_ap.rearrange("(kc p) n -> p kc n", p=128),
                      writes=[wsb], sem=sem)
                k.dma(POOL, wu_s[:, :, 0:nf * 128], wu_ap.rearrange("(kc p) n -> p kc n", p=128),
                      writes=[wsb], sem=sem)
                k.dma(POOL, wd_s[:, 0:nf, :], wd_ap.rearrange("(f p) n -> p f n", p=128),
                      writes=[wsb], sem=sem)
                for tg in tgroups:
                    ab_i = cnt % 2
                    cnt += 1
                    t_lo = tg[0] * 128
                    N = len(tg) * 128
                    for f in range(nf):
                        bg, bgb = next_bank()
                        k.mm_group(bg[:, 0:N], [(wg_s[:, kc, f * 128:(f + 1) * 128], h2T[:, kc, t_lo:t_lo + N])
                                                for kc in range(8)], [wsb] + [h2b[n] for n in tg], bgb)
                        bu, bub = next_bank()
                        k.mm_group(bu[:, 0:N], [(wu_s[:, kc, f * 128:(f + 1) * 128], h2T[:, kc, t_lo:t_lo + N])
                                                for kc in range(8)], [wsb] + [h2b[n] for n in tg], bub)
                        si = sgi % 2
                        sgi += 1
                        k.act(sg[si][:, 0:N], bg[:, 0:N], AF.Silu, [bgb], [sgb[si]])
                        k.tt(actt[ab_i][:, f, 0:N], sg[si][:, 0:N], bu[:, 0:N], ALU.mult, [sgb[si], bub],
                             [actb[ab_i]])
                    for j, n in enumerate(tg):
                        for hf in range(2):
                            cs = slice(hf * 512, (hf + 1) * 512)
                            bk, bkb = next_bank()
                            k.mm_group(bk[:, :], [(actt[ab_i][:, f, j * 128:(j + 1) * 128], wd_s[:, f, cs])
                                                  for f in range(nf)], [wsb, actb[ab_i]], bkb)
                            if moe:
                                k.stt(x_tok[:, n, cs], bk[:, :], gate[:, n, e:e + 1], x_tok[:, n, cs],
                                      ALU.mult, ALU.add, [bkb, gateb[n]], [xb[n]])
                            else:
                                k.tt(x_tok[:, n, cs], x_tok[:, n, cs], bk[:, :], ALU.add, [bkb], [xb[n]])

        def ffn_phase0():
            tiles = list(range(0, NT))
            with ExitStack() as fs:
                h2T = sb(fs, "h2T", [128, 8, NT * 128], BF16)
                h2b = [Buf() for _ in range(NT)]
                with ExitStack() as f1:
                    g2_bc = sb(f1, "g2_bc", [128, D], F32)
                    gb = Buf()
                    h_tok = [sb(f1, "h_tokf%d" % i, [128, D], BF16) for i in range(2)]
                    h_tokb = [Buf(), Buf()]
                    junk = sb(f1, "junkf", [128, D], BF16)
                    junkb = Buf()
                    k.dma(SP, g2_bc[:], n2g_d[0:1, :].partition_broadcast(128), writes=[gb], sem="g2")
                    for ti, n in enumerate(tiles):
                        hb = ti % 2
                        norm_and_transpose(n, g2_bc[:], gb, h_tok[hb][:], h_tokb[hb], junk[:], junkb,
                                           h2T[:, :, n * 128:(n + 1) * 128], h2b[n])
                    p.barrier()
                with ExitStack() as f2:
                    ffn_stream(f2, False, tiles, h2T, h2b, GF)
                    p.barrier()

        def moe_phase():
            tiles = list(range(1, NT))
            with ExitStack() as fs:
                h_all = sb(fs, "h_all", [128, 16, D], BF16)
                hab = [Buf() for _ in range(16)]
                sel = sb(fs, "sel", [128, 16, NE], F32)
                pos = sb(fs, "pos", [128, 16, NE], F32)
                tot = sb(fs, "tot", [128, 16, NE], F32)
                offs = sb(fs, "offs", [128, 16, NE], F32)
                misc = sb(fs, "rmisc", [128, 40], F32)
                cond_i = sb(fs, "cond_i", [128, 32], I32)
                selb, posb, totb, offb, miscb, condb = (Buf() for _ in range(6))
                with ExitStack() as f1:
                    g2_bc = sb(f1, "g2_bc", [128, D], F32)
                    gb = Buf()
                    junk = sb(f1, "junkf", [128, D], BF16)
                    junkb = Buf()
                    rg = sb(f1, "rg", [128, NE, D], F32)
                    rgb = Buf()
                    junk32 = sb(f1, "junk32", [128, D], F32)
                    j32b = Buf()
                    lg = sb(f1, "lg", [128, 2, 32], F32)
                    lgb = [Buf(), Buf()]
                    k.dma(SP, g2_bc[:], n2g_d[1:2, :].partition_broadcast(128), writes=[gb], sem="g2")
                    for e in range(NE):
                        k.dma(SP, rg[:, e, :], rT_d[e:e + 1, :].partition_broadcast(128), writes=[rgb], sem="rg")
                    for e in range(NE):
                        k.tt(rg[:, e, :], rg[:, e, :], g2_bc[:], ALU.mult, [gb], [rgb])
                    for ti, n in enumerate(tiles):
                        T = n - 1
                        hb = ti % 2
                        rstd, sbf = rms_stats(x_tok[:, n, :], xb[n], junk[:], junkb, 1.0 / 32.0)
                        k.stt(h_all[:, T, :], x_tok[:, n, :], rstd, g2_bc[:], ALU.mult, ALU.mult,
                              [xb[n], sbf, gb], [hab[T]])
                        L_ = lg[:, hb, :]
                        lb = lgb[hb]
                        for e in range(NE):
                            k.stt(junk32[:], x_tok[:, n, :], rstd, rg[:, e, :], ALU.mult, ALU.mult,
                                  [xb[n], sbf, rgb], [j32b, lb], accum=L_[:, e:e + 1])
                        k.emit(DVE, lambda e_, o=L_[:, 24:25], i=L_[:, 0:8]: e_.reduce_max(out=o, in_=i, axis=AX.X),
                               [], [lb])
                        k.ts1(L_[:, 8:16], L_[:, 0:8], L_[:, 24:25], ALU.is_equal, [], [lb])
                        k.stt(L_[:, 16:24], L_[:, 8:16], -1e30, L_[:, 0:8], ALU.mult, ALU.add, [], [lb])
                        k.emit(DVE, lambda e_, o=L_[:, 25:26], i=L_[:, 16:24]: e_.reduce_max(out=o, in_=i, axis=AX.X),
                               [], [lb])
                        k.ts1(L_[:, 16:24], L_[:, 16:24], L_[:, 25:26], ALU.is_equal, [], [lb])
                        k.tt(sel[:, T, :], L_[:, 8:16], L_[:, 16:24], ALU.add, [lb], [selb])
                        k.tt(L_[:, 26:27], L_[:, 25:26], L_[:, 24:25], ALU.subtract, [], [lb])
                        k.act(L_[:, 26:27], L_[:, 26:27], AF.Exp, [], [lb])
                        k.ts(L_[:, 27:28], L_[:, 26:27], 1.0, 0.0, ALU.add, ALU.add, [], [lb])
                        k.recip(L_[:, 27:28], L_[:, 27:28], [], [lb])
                        k.tt(L_[:, 28:29], L_[:, 26:27], L_[:, 27:28], ALU.mult, [], [lb])
                        k.ts1(L_[:, 8:16], L_[:, 8:16], L_[:, 27:28], ALU.mult, [], [lb])
                        k.stt(gate[:, n, :], L_[:, 16:24], L_[:, 28:29], L_[:, 8:16], ALU.mult, ALU.add,
                              [lb], [gateb[n]])
                    selv = sel[:, :, :].rearrange("p t e -> p (t e)")
                    b1, b1b = next_bank()
                    k.mm_group(b1[:, 0:128], [(ustrict[:, :], selv)], [selb, cb], b1b)
                    k.copy(pos[:, :, :].rearrange("p t e -> p (t e)"), b1[:, 0:128], [b1b], [posb])
                    b2, b2b = next_bank()
                    k.mm_group(b2[:, 0:128], [(ones32[:, :], selv)], [selb, cb], b2b)
                    k.copy(tot[:, :, :].rearrange("p t e -> p (t e)"), b2[:, 0:128], [b2b], [totb])
                    k.memset(offs[:, 0, :], 0.0, [offb])
                    for T in range(1, 16):
                        k.tt(offs[:, T, :], offs[:, T - 1, :], tot[:, T - 1, :], ALU.add, [totb], [offb])
                    k.tt(pos[:, :, :], pos[:, :, :], offs[:, :, :], ALU.add, [offb], [posb])
                    k.tt(misc[:, 0:8], offs[:, 15, :], tot[:, 15, :], ALU.add, [offb, totb], [miscb])
                    for c, cap in enumerate(CAPS):
                        k.ts(misc[:, 8 + 8 * c:16 + 8 * c], misc[:, 0:8], float(cap) + 0.5, 0.0, ALU.is_lt, ALU.add,
                             [], [miscb])
                    k.copy(cond_i[:, 0:8 * len(CAPS)], misc[:, 8:8 + 8 * len(CAPS)], [miscb], [condb])
                    if FORCE_CLASS is not None:
                        for c in range(len(CAPS)):
                            k.memset(cond_i[:, 8 * c:8 * c + 8], 1 if c >= FORCE_CLASS else 0, [condb])
                    p.barrier()

                def expert_sparse(e, cap):
                    NJ = cap // 128
                    chunks = [(0, 512)] + ([(512, cap - 512)] if cap > 512 else [])
                    with ExitStack() as sp_:
                        Pb = [sb(sp_, "P%d" % i, [128, cap], BF16) for i in range(4)]
                        Pbb = [Buf() for _ in range(4)]
                        pi = [0]
                        hTe = sb(sp_, "hTe", [128, 8, cap], BF16)
                        hTeb = Buf()
                        GW = GFS * 128
                        slots = []
                        for s_ in range(2):
                            slots.append((sb(sp_, "swg%d" % s_, [128, 8, GW], BF16),
                                          sb(sp_, "swu%d" % s_, [128, 8, GW], BF16),
                                          sb(sp_, "swd%d" % s_, [128, GFS, D], BF16), Buf(), Buf()))
                        actt = [sb(sp_, "sact%d" % i, [128, GFS, cap], BF16) for i in range(2)]
                        actb = [Buf(), Buf()]
                        sg = [sb(sp_, "ssg%d" % i, [128, cap], F32) for i in range(2)]
                        sgb = [Buf(), Buf()]
                        oe32 = sb(sp_, "oe32", [128, NJ, D], F32)
                        oe_bf = sb(sp_, "oe_bf", [128, NJ, D], BF16)
                        oeb = [[Buf(), Buf()] for _ in range(NJ)]
                        oebf_b = Buf()
                        PT = [sb(sp_, "PT%d" % i, [128, NJ, 128], BF16) for i in range(2)]
                        PTb = [Buf(), Buf()]

                        def build_P(T, c0, w):
                            i = pi[0] % 4
                            pi[0] += 1
                            k.ts(Pb[i][:, 0:w], iota[:, c0:c0 + w], pos[:, T, e:e + 1], sel[:, T, e:e + 1],
                                 ALU.is_equal, ALU.mult, [cb, posb, selb], [Pbb[i]])
                            return Pb[i], Pbb[i]

                        for (c0, w) in chunks:
                            per_bank = 512 // w
                            nb = 8 // per_bank
                            bks = [next_bank() for _ in range(nb)]
                            allb = [b for _, b in bks]
                            tok = None
                            for T in range(16):
                                Pt, Ptb = build_P(T, c0, w)
                                deps = [Ptb.w, hab[T].w]
                                if T == 0:
                                    deps += k.deps_of([], allb)
                                for kc in range(8):
                                    o = bks[kc // per_bank][0][:, (kc % per_bank) * w:(kc % per_bank + 1) * w]
                                    tok = p.op(PE, (lambda e_, o=o, a=h_all[:, T, kc * 128:(kc + 1) * 128], r=Pt[:, 0:w],
                                                    s_=(T == 0), t_=(T == 15): e_.matmul(o, lhsT=a, rhs=r, start=s_, stop=t_)),
                                               deps if kc == 0 else (), kc == 7)
                                Ptb.add_read(tok)
                                hab[T].add_read(tok)
                            for b in allb:
                                b.set_write(tok)
                            for bi, (bk, bkb) in enumerate(bks):
                                k.act(hTe[:, bi * per_bank:(bi + 1) * per_bank, c0:c0 + w],
                                      bk[:, 0:per_bank * w].rearrange("p (a b) -> p a b", a=per_bank), AF.Copy,
                                      [bkb], [hTeb])
                        nch = D_FFE // 128
                        ngrp = (nch + GFS - 1) // GFS
                        ginfo = {}
                        sgi_box = [0]

                        def ffn_s1(g_):
                            c0f = g_ * GFS
                            nf = min(GFS, nch - c0f)
                            wg_s, wu_s, wd_s, wsb, wdb = slots[g_ % 2]
                            k.dma(POOL, wg_s[:, :, 0:nf * 128],
                                  mwg_d[e, :, c0f * 128:(c0f + nf) * 128].rearrange("(kc p) n -> p kc n", p=128),
                                  writes=[wsb], sem="wsA%d" % (g_ % 2))
                            k.dma(POOL, wu_s[:, :, 0:nf * 128],
                                  mwu_d[e, :, c0f * 128:(c0f + nf) * 128].rearrange("(kc p) n -> p kc n", p=128),
                                  writes=[wsb], sem="wsA%d" % (g_ % 2))
                            k.dma(POOL, wd_s[:, 0:nf, :],
                                  mwd_d[e, c0f * 128:(c0f + nf) * 128, :].rearrange("(f p) n -> p f n", p=128),
                                  writes=[wdb], sem="wsB%d" % (g_ % 2))
                            ab_i = g_ % 2
                            ginfo[g_] = (nf, wd_s, wdb, ab_i)
                            for f in range(nf):
                                fs_ = slice(f * 128, (f + 1) * 128)
                                si = sgi_box[0] % 2
                                sgi_box[0] += 1
                                for (c0, w) in chunks:
                                    if w == 512:
                                        bg, bgb = next_bank()
                                        bu, bub = next_bank()
                                        k.mm_group(bg[:, :], [(wg_s[:, kc, fs_], hTe[:, kc, c0:c0 + w]) for kc in range(8)],
                                                   [wsb, hTeb], bgb)
                                        k.mm_group(bu[:, :], [(wu_s[:, kc, fs_], hTe[:, kc, c0:c0 + w]) for kc in range(8)],
                                                   [wsb, hTeb], bub)
                                        g_ap, u_ap = bg[:, :], bu[:, :]
                                    else:
                                        bs, bsb = next_bank()
                                        deps = k.deps_of([wsb, hTeb], [bsb])
                                        tok = None
                                        for wi, w_s in enumerate((wg_s, wu_s)):
                                            for kc in range(8):
                                                tok = p.op(PE, (lambda e_, o=bs[:, wi * w:(wi + 1) * w], a=w_s[:, kc, fs_],
                                                                r=hTe[:, kc, c0:c0 + w], s_=(kc == 0), t_=(kc == 7):
                                                                e_.matmul(o, lhsT=a, rhs=r, start=s_, stop=t_)),
                                                           deps if (wi == 0 and kc == 0) else (), (wi == 1 and kc == 7))
                                        wsb.add_read(tok)
                                        hTeb.add_read(tok)
                                        bsb.set_write(tok)
                                        bgb = bub = bsb
                                        g_ap, u_ap = bs[:, 0:w], bs[:, w:2 * w]
                                    k.act(sg[si][:, c0:c0 + w], g_ap, AF.Silu, [bgb], [sgb[si]])
                                    k.tt(actt[ab_i][:, f, c0:c0 + w], sg[si][:, c0:c0 + w], u_ap, ALU.mult,
                                         [sgb[si], bub], [actb[ab_i]])

                        def ffn_s2(g_):
                            nf, wd_s, wsb, ab_i = ginfo[g_]
                            for j in range(NJ):
                                for hf in range(2):
                                    cs = slice(hf * 512, (hf + 1) * 512)
                                    bk, bkb = next_bank()
                                    k.mm_group(bk[:, :], [(actt[ab_i][:, f, j * 128:(j + 1) * 128], wd_s[:, f, cs])
                                                          for f in range(nf)], [wsb, actb[ab_i]], bkb)
                                    ob = oeb[j][hf]
                                    if g_ == 0:
                                        k.act(oe32[:, j, cs], bk[:, :], AF.Copy, [bkb], [ob])
                                    elif g_ < ngrp - 1:
                                        k.tt(oe32[:, j, cs], oe32[:, j, cs], bk[:, :], ALU.add, [bkb], [ob])
                                    else:
                                        k.tt(oe_bf[:, j, cs], oe32[:, j, cs], bk[:, :], ALU.add, [bkb, ob], [oebf_b])

                        ffn_s1(0)
                        for g_ in range(ngrp):
                            if g_ + 1 < ngrp:
                                ffn_s1(g_ + 1)
                            ffn_s2(g_)
                        Pq = {}

                        def st_x(T):
                            Pq[T] = build_P(T, 0, cap)

                        def st_y(T):
                            Pt, Ptb = Pq.pop(T)
                            bk, bkb = next_bank()
                            bkbf = bk.bitcast(BF16)
                            outs = [bkbf[:, j * 128:(j + 1) * 128] for j in range(NJ)]
                            pairs = [(Pt[:, j * 128:(j + 1) * 128], ident[:, :]) for j in range(NJ)]
                            k.mm_group(outs, pairs, [Ptb, cb], bkb, transpose=True)
                            pti = T % 2
                            k.act(PT[pti][:, :, :], bkbf[:, 0:cap].rearrange("p (j t) -> p j t", j=NJ), AF.Copy,
                                  [bkb], [PTb[pti]])

                        def st_z(T):
                            ptp = T % 2
                            n = T + 1
                            for hf in range(2):
                                cs = slice(hf * 512, (hf + 1) * 512)
                                bk2, bk2b = next_bank()
                                k.mm_group(bk2[:, :], [(PT[ptp][:, j, :], oe_bf[:, j, cs]) for j in range(NJ)],
                                           [PTb[ptp], oebf_b], bk2b)
                                k.stt(x_tok[:, n, cs], bk2[:, :], gate[:, n, e:e + 1], x_tok[:, n, cs],
                                      ALU.mult, ALU.add, [bk2b, gateb[n]], [xb[n]])

                        st_x(0)
                        st_x(1)
                        st_y(0)
                        for T in range(16):
                            if T + 2 < 16:
                                st_x(T + 2)
                            if T + 1 < 16:
                                st_y(T + 1)
                            st_z(T)
                        p.barrier()

                def expert_dense(e):
                    with ExitStack() as db:
                        h2T = sb(db, "h2T", [128, 8, NT * 128], BF16)
                        h2b = [Buf() for _ in range(NT)]
                        for n in tiles:
                            T = n - 1
                            bk, bkb = next_bank()
                            bkbf = bk.bitcast(BF16)
                            outs = [bkbf[:, kc * 128:(kc + 1) * 128] for kc in range(8)]
                            pairs = [(h_all[:, T, kc * 128:(kc + 1) * 128], ident[:, :]) for kc in range(8)]
                            k.mm_group(outs, pairs, [hab[T], cb], bkb, transpose=True)
                            k.act(h2T[:, :, n * 128:(n + 1) * 128], bkbf[:, :].rearrange("p (k t) -> p k t", k=8),
                                  AF.Copy, [bkb], [h2b[n]])
                        ffn_stream(db, True, tiles, h2T, h2b, GFS, experts=[e])
                        p.barrier()

                def cflag(c, e):
                    return cond_i[0:1, 8 * c + e:8 * c + e + 1]

                for e in range(NE):
                    p.cond_region(
                        cflag(1, e), [],
                        lambda e=e: p.cond_region(cflag(0, e), [], lambda: expert_sparse(e, CAPS[0]),
                                                  lambda: expert_sparse(e, CAPS[1])),
                        lambda e=e: p.cond_region(cflag(2, e), [], lambda: expert_sparse(e, CAPS[2]),
                                                  lambda: expert_dense(e)))
                    p.barrier()

        def final_phase(do_norm):
            with ExitStack() as os_:
                gf_bc = sb(os_, "gf_bc", [128, D], F32)
                gfb = Buf()
                outt = [sb(os_, "outt%d" % i, [128, D], F32) for i in range(2)]
                outb = [Buf(), Buf()]
                junk = sb(os_, "junko", [128, D], BF16)
                junkb = Buf()
                toks = []
                if do_norm:
                    k.dma(SP, gf_bc[:], fg_d[0:1, :].partition_broadcast(128), writes=[gfb], sem="gf")
                for n in range(1, NT):
                    if do_norm:
                        oi = n % 2
                        rstd, sbf = rms_stats(x_tok[:, n, :], xb[n], junk[:], junkb, 1.0 / 32.0)
                        k.stt(outt[oi][:], x_tok[:, n, :], rstd, gf_bc[:], ALU.mult, ALU.mult,
                              [xb[n], sbf, gfb], [outb[oi]])
                        toks.append(k.dma(SP, y_d[(n - 1) * 128:n * 128, :], outt[oi][:], reads=[outb[oi]], sem="out%d" % oi))
                    else:
                        toks.append(k.dma(SP, y_d[(n - 1) * 128:n * 128, :], x_tok[:, n, :], reads=[xb[n]], sem="out0"))
                p.wait_only(SP, toks)
                p.barrier()

        mixer_phase(0)
        if stop_after == "M0":
            final_phase(False)
            p.flush()
            return nc
        ffn_phase0()
        if stop_after == "F0":
            final_phase(False)
            p.flush()
            return nc
        k.ts1(x_tok[:, 0, :], x_tok[:, 0, :], flag[:, 0:1], ALU.mult, [cb], [xb[0]])
        mixer_phase(1)
        if stop_after == "M1":
            final_phase(False)
            p.flush()
            return nc
        moe_phase()
        final_phase(True)
        p.flush()
    return nc


def _prep_shared(inp):
    f = lambda a: np.ascontiguousarray(np.asarray(a, dtype=np.float32))
    sh = {}
    sh["ident"] = np.eye(128, dtype=np.float32)
    sh["ustrict"] = np.triu(np.ones((128, 128), np.float32), 1)
    sh["iota"] = np.ascontiguousarray(np.broadcast_to(np.arange(CAPS[-1], dtype=np.float32), (128, CAPS[-1])))
    pp = np.zeros((128, 2, NPP), np.float32)
    wins = np.array([[2.0, 4.0], [8.0, 16.0]], np.float32)
    for l in range(2):
        pp[:, l, PP_PSCALE:PP_PSCALE + 2] = f(inp["pool_scale"])[l].reshape(2, 128).T
        dw = f(inp["conv_dw_w"])[l]
        pp[:, l, PP_DWW:PP_DWW + 93] = dw.reshape(31, 3, 128).transpose(2, 1, 0).reshape(128, 93)
        pp[:, l, PP_DWB:PP_DWB + 3] = f(inp["conv_dw_b"])[l].reshape(3, 128).T
        pp[:, l, PP_LNG:PP_LNG + 3] = f(inp["conv_ln_g"])[l].reshape(3, 128).T
        pp[:, l, PP_LNB:PP_LNB + 3] = f(inp["conv_ln_b"])[l].reshape(3, 128).T
        pp[:, l, PP_PWB:PP_PWB + 3] = f(inp["conv_pw_b"])[l].reshape(3, 128).T
        for c in range(2):
            pp[0:64, l, PP_INVW + c] = 1.0 / wins[c, 0]
            pp[64:128, l, PP_INVW + c] = 1.0 / wins[c, 1]
    sh["pp"] = pp.reshape(128, 2 * NPP)
    sh["norm1_g"] = f(inp["norm1_g"])
    sh["norm2_g"] = f(inp["norm2_g"])
    sh["final_g"] = f(inp["final_g"]).reshape(1, D)
    sh["gm_norm_g"] = f(inp["gm_norm_g"])
    sh["gm_b"] = f(inp["gm_b"]).reshape(2, 512)
    sh["w_in"] = f(inp["w_in"])
    sh["pool_w"] = f(inp["pool_w"])
    sh["gm_wsT"] = np.ascontiguousarray(f(inp["gm_ws"]).transpose(0, 1, 3, 2))
    sh["conv_pw_w"] = f(inp["conv_pw_w"])
    sh["w_out"] = f(inp["w_out"])
    sh["ffn_wg"] = f(inp["ffn_wg"])[0]
    sh["ffn_wu"] = f(inp["ffn_wu"])[0]
    sh["ffn_wd"] = f(inp["ffn_wd"])[0]
    sh["routerT"] = np.ascontiguousarray(f(inp["moe_router"])[0].T)
    sh["moe_wg"] = f(inp["moe_wg"])[0]
    sh["moe_wu"] = f(inp["moe_wu"])[0]
    sh["moe_wd"] = f(inp["moe_wd"])[0]
    return sh


def _prep_core(x, c):
    b, q = c // 4, c % 4
    xin = np.zeros((NT * 128, D), np.float32)
    xin[128:] = x[b, q * 2048:(q + 1) * 2048]
    if q > 0:
        xin[:128] = x[b, q * 2048 - 128:q * 2048]
    flag = np.full((128, 1), 1.0 if q > 0 else 0.0, np.float32)
    pinv = np.zeros((128, 2, 16), np.float32)
    wins = [[2, 4], [8, 16]]
    for cc in range(2):
        for half in range(2):
            w = wins[cc][half]
            for j in range(16):
                cntv = min(j + 1, w) if q == 0 else w
                pinv[half * 64:(half + 1) * 64, cc, j] = 1.0 / cntv
    return {"x": xin, "flag": flag, "pool_inv": pinv.reshape(128, 32)}


_NC_CACHE = {}


def run(inputs, stop_after=None, trace=False):
    x = np.asarray(inputs["x"], dtype=np.float32)
    sh = _prep_shared(inputs)
    in_maps = []
    for c in range(8):
        m = dict(sh)
        m.update(_prep_core(x, c))
        in_maps.append(m)
    if stop_after not in _NC_CACHE:
        _NC_CACHE[stop_after] = build_program(stop_after)
    nc = _NC_CACHE[stop_after]
    res = run_bass_kernel_spmd(nc, in_maps, core_ids=list(range(8)), **({"trace": True} if trace else {}))
    out = np.zeros((2, 8192, D), np.float32)
    for c in range(8):
        b, q = c // 4, c % 4
        out[b, q * 2048:(q + 1) * 2048] = res.results[c]["y"]
    return out, res


def kernel(**inputs):
    out, _ = run(inputs)
    return out
```

```python
import numpy as np
from contextlib import ExitStack
import concourse.bass as bass
import concourse.mybir as mybir
from concourse.bass_utils import run_bass_kernel_spmd

F32 = mybir.dt.float32
BF16 = mybir.dt.bfloat16
I32 = mybir.dt.int32
AF = mybir.ActivationFunctionType
ALU = mybir.AluOpType
AX = mybir.AxisListType

PE, ACT, DVE, POOL, SP = "pe", "act", "dve", "pool", "sp"
ENGS = (PE, ACT, DVE, POOL, SP)

D = 1024
NT = 17
D_IN = 1792
D_FF = 2816
D_FFE = 3584
NE = 8
EPS = 1e-6
NPP = 120
PP_PSCALE = 0
PP_DWW = 2
PP_DWB = 95
PP_LNG = 98
PP_LNB = 101
PP_PWB = 104
PP_INVW = 107
PP_G2 = 109
TGM = 2
GF = 4
GFS = 2
CAPS = (512, 640, 768)
FORCE_CLASS = None


class Prog:
    def __init__(self, nc, stack):
        self.nc = nc
        self.stack = stack
        self.ops = {e: [] for e in ENGS}
        self.sems = {}
        self.cnt = {}
        self.seen = {e: {} for e in ENGS}
        for e in ENGS:
            self._mksem("eng_" + e)

    def _mksem(self, key):
        if key not in self.sems:
            self.sems[key] = self.stack.enter_context(self.nc.semaphore(key))
            self.cnt[key] = 0
        return self.sems[key]

    def _waits(self, eng, deps):
        out = []
        for d in deps:
            if d is None:
                continue
            key, val = d
            if eng == PE and key == "eng_pe":
                continue
            if self.seen[eng].get(key, 0) >= val:
                continue
            self.seen[eng][key] = val
            out.append((self.sems[key], val))
        return out

    def op(self, eng, fn, deps=(), inc=True):
        waits = self._waits(eng, deps)
        key = "eng_" + eng
        tok = None
        if inc:
            self.cnt[key] += 1
            tok = (key, self.cnt[key])
        self.ops[eng].append((waits, fn, (self.sems[key], 1) if inc else None))
        return tok

    def dma(self, eng, out, in_, semname, deps=()):
        self._mksem(semname)
        waits = self._waits(eng, deps)
        self.cnt[semname] += 16
        tok = (semname, self.cnt[semname])
        self.ops[eng].append(
            (waits, lambda e, o=out, i=in_: e.dma_start(out=o, in_=i), (self.sems[semname], 16))
        )
        return tok

    def wait_only(self, eng, deps):
        waits = self._waits(eng, deps)
        if waits:
            self.ops[eng].append((waits, None, None))

    def barrier(self):
        toks = [(k, v) for k, v in self.cnt.items() if v > 0]
        for e in ENGS:
            self.wait_only(e, toks)

    def cond_region(self, cond_ap, cond_deps, then_fn, else_fn):
        for e in ENGS:
            self.ops[e].append(("IF", cond_ap, self._waits(e, cond_deps)))
        snap_cnt = dict(self.cnt)
        snap_seen = {e: dict(d) for e, d in self.seen.items()}
        Buf.reset_all()
        then_fn()
        then_cnt = dict(self.cnt)
        then_end = {e: len(self.ops[e]) for e in ENGS}
        self.cnt = dict(snap_cnt)
        for kk in then_cnt:
            self.cnt.setdefault(kk, 0)
        self.seen = {e: dict(d) for e, d in snap_seen.items()}
        for e in ENGS:
            self.ops[e].append(("ELSE",))
        Buf.reset_all()
        else_fn()
        else_cnt = dict(self.cnt)
        keys = set(then_cnt) | set(else_cnt)
        final = {kk: max(then_cnt.get(kk, 0), else_cnt.get(kk, 0)) for kk in keys}

        def equalizers(branch_cnt):
            per_eng = {e: [] for e in ENGS}
            for kk in sorted(keys):
                diff = final[kk] - branch_cnt.get(kk, 0)
                if diff <= 0:
                    continue
                eng = kk[4:] if kk.startswith("eng_") else SP
                per_eng[eng].append(("EQ", self.sems[kk], branch_cnt.get(kk, 0), diff))
            return per_eng

        eq_then = equalizers(then_cnt)
        eq_else = equalizers(else_cnt)
        for e in ENGS:
            self.ops[e][then_end[e]:then_end[e]] = eq_then[e]
            self.ops[e].extend(eq_else[e])
            self.ops[e].append(("ENDIF",))
        self.cnt = final
        self.seen = snap_seen
        Buf.reset_all()

    def flush(self):
        nc = self.nc
        ops = self.ops

        def run(e, lst):
            cms = []
            for item in lst:
                tag = item[0]
                if tag == "IF":
                    for s_, v in item[2]:
                        e.wait_ge(s_, v)
                    val = e.value_load(item[1])
                    cm = e.If(val == 1)
                    cm.__enter__()
                    cms.append(cm)
                elif tag == "ELSE":
                    cms.pop().__exit__(None, None, None)
                    cm = e.Else()
                    cm.__enter__()
                    cms.append(cm)
                elif tag == "ENDIF":
                    cms.pop().__exit__(None, None, None)
                elif tag == "EQ":
                    _, sem, have, diff = item
                    if have > 0:
                        e.wait_ge(sem, have)
                    e.sem_inc(sem, diff)
                else:
                    waits, fn, inc = item
                    for s_, v in waits:
                        e.wait_ge(s_, v)
                    if fn is not None:
                        ins = fn(e)
                        if inc is not None:
                            ins.then_inc(inc[0], inc[1])

        with nc.Block() as block:
            @block.tensor
            def _(e):
                run(e, ops[PE])

            @block.scalar
            def _(e):
                run(e, ops[ACT])

            @block.vector
            def _(e):
                run(e, ops[DVE])

            @block.gpsimd
            def _(e):
                run(e, ops[POOL])

            @block.sync
            def _(e):
                run(e, ops[SP])
        self.ops = {e: [] for e in ENGS}


class Buf:
    ALL = []

    def __init__(self, name=""):
        self.name = name
        self.w = None
        self.r = {}
        Buf.ALL.append(self)

    @staticmethod
    def reset_all():
        for b in Buf.ALL:
            b.w = None
            b.r = {}

    def add_read(self, tok):
        if tok is None:
            return
        k, v = tok
        if self.r.get(k, 0) < v:
            self.r[k] = v

    def set_write(self, tok):
        self.w = tok
        self.r = {}


class K:
    def __init__(self, nc, stack):
        self.nc = nc
        self.p = Prog(nc, stack)
        self.dma_n = 0

    def deps_of(self, reads, writes):
        deps = []
        for b in reads:
            deps.append(b.w)
        for b in writes:
            deps.append(b.w)
            deps.extend(b.r.items())
        return deps

    def emit(self, eng, fn, reads=(), writes=()):
        tok = self.p.op(eng, fn, self.deps_of(reads, writes), True)
        for b in reads:
            b.add_read(tok)
        for b in writes:
            b.set_write(tok)
        return tok

    def dma(self, eng, out, in_, reads=(), writes=(), sem=None):
        assert sem is not None
        deps = []
        for b in reads:
            deps.append(b.w)
        for b in writes:
            if not (b.w is not None and b.w[0] == sem):
                deps.append(b.w)
            deps.extend(b.r.items())
        tok = self.p.dma(eng, out, in_, sem, deps)
        for b in reads:
            b.add_read(tok)
        for b in writes:
            b.set_write(tok)
        return tok

    def mm_group(self, out, pairs, reads, bank, transpose=False):
        deps = self.deps_of(reads, [bank])
        n = len(pairs)
        tok = None
        for i, (l, r) in enumerate(pairs):
            last = i == n - 1
            if transpose:
                fn = (lambda e, o=out[i], a=l, b=r: e.transpose(o, a, b))
            else:
                fn = (lambda e, o=out, a=l, b=r, s=(i == 0), t=last: e.matmul(o, lhsT=a, rhs=b, start=s, stop=t))
            tok = self.p.op(PE, fn, deps if i == 0 else (), last)
        for b in reads:
            b.add_read(tok)
        bank.set_write(tok)
        return tok

    def mm_multi(self, groups, reads, bank):
        deps = self.deps_of(reads, [bank])
        n = len(groups)
        tok = None
        for i, (o, l, r) in enumerate(groups):
            last = i == n - 1
            fn = (lambda e, o=o, a=l, b=r: e.matmul(o, lhsT=a, rhs=b, start=True, stop=True))
            tok = self.p.op(PE, fn, deps if i == 0 else (), last)
        for b in reads:
            b.add_read(tok)
        bank.set_write(tok)
        return tok

    def act(self, out, in_, func, reads, writes, bias=None, scale=None, accum=None):
        kw = {}
        if bias is not None:
            kw["bias"] = bias
        if scale is not None:
            kw["scale"] = scale
        if accum is not None:
            kw["accum_out"] = accum
        return self.emit(ACT, lambda e: e.activation(out=out, in_=in_, func=func, **kw), reads, writes)

    def tt(self, out, in0, in1, op, reads, writes, eng=DVE):
        return self.emit(eng, lambda e: e.tensor_tensor(out=out, in0=in0, in1=in1, op=op), reads, writes)

    def ts(self, out, in0, s1, s2, op0, op1, reads, writes, eng=DVE):
        return self.emit(eng, lambda e: e.tensor_scalar(out=out, in0=in0, scalar1=s1, scalar2=s2, op0=op0, op1=op1),
                         reads, writes)

    def ts1(self, out, in0, s1, op0, reads, writes, eng=DVE):
        return self.emit(eng, lambda e: e.tensor_scalar(out=out, in0=in0, scalar1=s1, scalar2=None, op0=op0),
                         reads, writes)

    def stt(self, out, in0, scalar, in1, op0, op1, reads, writes, accum=None, eng=DVE):
        kw = {}
        if accum is not None:
            kw["accum_out"] = accum
        return self.emit(eng, lambda e: e.scalar_tensor_tensor(out=out, in0=in0, scalar=scalar, in1=in1,
                                                               op0=op0, op1=op1, **kw), reads, writes)

    def copy(self, out, in_, reads, writes, eng=DVE):
        return self.emit(eng, lambda e: e.tensor_copy(out=out, in_=in_), reads, writes)

    def recip(self, out, in_, reads, writes):
        return self.emit(DVE, lambda e: e.reciprocal(out=out, in_=in_), reads, writes)

    def memset(self, ap, val, writes, eng=DVE):
        return self.emit(eng, lambda e: e.memset(ap, val), (), writes)


def build_program(stop_after=None):
    nc = bass.Bass("TRN2", target_bir_lowering=False)

    def din(name, shape):
        return nc.dram_tensor(name, list(shape), F32, kind="ExternalInput").ap()

    x_d = din("x", [NT * 128, D])
    flag_d = din("flag", [128, 1])
    pinv_d = din("pool_inv", [128, 32])
    ident_d = din("ident", [128, 128])
    ustrict_d = din("ustrict", [128, 128])
    iota_d = din("iota", [128, CAPS[-1]])
    pp_d = din("pp", [128, 2 * NPP])
    n1g_d = din("norm1_g", [2, D])
    n2g_d = din("norm2_g", [2, D])
    fg_d = din("final_g", [1, D])
    gmg_d = din("gm_norm_g", [2, 384])
    gmb_d = din("gm_b", [2, 512])
    w_in_d = din("w_in", [2, D, D_IN])
    pool_w_d = din("pool_w", [2, 4, 64, 64])
    wsT_d = din("gm_wsT", [2, 4, 128, 128])
    pw_d = din("conv_pw_w", [2, 384, 384])
    w_out_d = din("w_out", [2, D, D])
    fwg_d = din("ffn_wg", [D, D_FF])
    fwu_d = din("ffn_wu", [D, D_FF])
    fwd_d = din("ffn_wd", [D_FF, D])
    router_d = din("router", [D, NE])
    mwg_d = din("moe_wg", [NE, D, D_FFE])
    mwu_d = din("moe_wu", [NE, D, D_FFE])
    mwd_d = din("moe_wd", [NE, D_FFE, D])
    y_d = nc.dram_tensor("y", [16 * 128, D], F32, kind="ExternalOutput").ap()

    with ExitStack() as st:
        ARENA_WORDS = 53100
        arena = st.enter_context(nc.sbuf_tensor("arena", [128, ARENA_WORDS], F32))
        atop = [0]
        scopes = {}

        def _release(mark):
            atop[0] = mark

        def sb(stack, name, shape, dt):
            if stack is not st and not getattr(stack, "_arena_marked", False):
                stack._arena_marked = True
                stack.callback(_release, atop[0])
            n = 1
            for d_ in shape[1:]:
                n *= d_
            nbytes = n * (4 if dt in (F32, I32) else 2)
            words = ((nbytes + 3) // 4 + 7) // 8 * 8
            assert atop[0] + words <= ARENA_WORDS, ("SBUF arena overflow", name, atop[0], words)
            v = arena[:, atop[0]:atop[0] + words]
            atop[0] += words
            if dt != F32:
                v = v.bitcast(dt)
            v = v[:, 0:n]
            if len(shape) == 3:
                v = v.rearrange("p (a b) -> p a b", a=shape[1])
            return v

        k = K(nc, st)
        p = k.p

        x_tok = sb(st, "x_tok", [128, NT, D], F32)
        xb = [Buf("x%d" % n) for n in range(NT)]
        ident = sb(st, "ident", [128, 128], BF16)
        ones32 = sb(st, "ones32", [128, 128], F32)
        ustrict = sb(st, "ustrict", [128, 128], F32)
        ident32 = sb(st, "ident32", [128, 128], F32)
        iota = sb(st, "iota", [128, CAPS[-1]], F32)
        pp = sb(st, "pp", [128, 2 * NPP], F32)
        flag = sb(st, "flag", [128, 1], F32)
        pinv = sb(st, "pinv", [128, 2, 16], F32)
        stt_t = sb(st, "stats", [128, 8, 4], F32)
        gate = sb(st, "gate", [128, NT, NE], F32)
        cb = Buf("consts")
        stb = [Buf("st%d" % i) for i in range(8)]
        gateb = [Buf("gate%d" % n) for n in range(NT)]
        banks = [st.enter_context(nc.psum_tensor("bank%d" % i, [128, 512], F32)) for i in range(8)]
        bankb = [Buf("bank%d" % i) for i in range(8)]
        ring = [0]
        stat_i = [0]

        def next_bank():
            i = ring[0]
            ring[0] = (i + 1) % 8
            return banks[i], bankb[i]

        def next_stat():
            i = stat_i[0]
            stat_i[0] = (i + 1) % 8
            return stt_t[:, i, :], stb[i]

        def ppc(l, col, n=1, lo=0, hi=128):
            return pp[lo:hi, l * NPP + col: l * NPP + col + n]

        k.memset(ones32[:], 1.0, [cb])
        k.dma(POOL, ident[:], ident_d, writes=[cb], sem="consts")
        k.dma(POOL, pp[:], pp_d, writes=[cb], sem="consts")
        k.dma(POOL, ustrict[:], ustrict_d, writes=[cb], sem="consts")
        k.dma(POOL, ident32[:], ident_d, writes=[cb], sem="consts")
        k.dma(POOL, iota[:], iota_d, writes=[cb], sem="consts")
        k.dma(POOL, flag[:], flag_d, writes=[cb], sem="consts")
        k.dma(POOL, pinv[:].rearrange("p c j -> p (c j)"), pinv_d, writes=[cb], sem="consts")
        for n in range(NT):
            k.dma(SP, x_tok[:, n, :], x_d[n * 128:(n + 1) * 128, :], writes=[xb[n]], sem="x%d" % n)

        def rms_stats(src_ap, srcb, junk, junkb, scale):
            sap, sbf = next_stat()
            k.act(junk, src_ap, AF.Square, [srcb], [junkb, sbf], scale=scale, accum=sap[:, 0:1])
            k.act(sap[:, 1:2], sap[:, 0:1], AF.Sqrt, [sbf], [sbf], bias=EPS)
            k.recip(sap[:, 2:3], sap[:, 1:2], [sbf], [sbf])
            return sap[:, 2:3], sbf

        def norm_and_transpose(n, g_bc, gb, h_tok, h_tokb, junk, junkb, hT_dst, hTb):
            rstd, sbf = rms_stats(x_tok[:, n, :], xb[n], junk, junkb, 1.0 / 32.0)
            k.stt(h_tok, x_tok[:, n, :], rstd, g_bc, ALU.mult, ALU.mult, [xb[n], sbf, gb], [h_tokb])
            bk, bkb = next_bank()
            bkbf = bk.bitcast(BF16)
            outs = [bkbf[:, kc * 128:(kc + 1) * 128] for kc in range(8)]
            pairs = [(h_tok[:, kc * 128:(kc + 1) * 128], ident[:, :]) for kc in range(8)]
            k.mm_group(outs, pairs, [h_tokb, cb], bkb, transpose=True)
            k.act(hT_dst, bkbf[:, :].rearrange("p (k t) -> p k t", k=8), AF.Copy, [bkb], [hTb])
            return rstd, sbf

        def mixer_phase(l):
            with ExitStack() as ms:
                NMAX = TGM * 128
                LMAX = 32 + NMAX
                w_in_sb = sb(ms, "w_in_sb", [128, 8, D_IN], BF16)
                wo_a = sb(ms, "wo_a", [128, 2, D], BF16)
                wo_b = sb(ms, "wo_b", [128, 4, D], BF16)
                wo_c = sb(ms, "wo_c", [128, 3, D], BF16)
                pw_sb = sb(ms, "pw_sb", [128, 3, 384], BF16)
                wblk = sb(ms, "wblk", [128, 2, 128], BF16)
                wsT = sb(ms, "wsT", [128, 4, 128], BF16)
                g1_bc = sb(ms, "g1_bc", [128, D], F32)
                gmg_bc = sb(ms, "gmg_bc", [128, 384], F32)
                gmb_bc = sb(ms, "gmb_bc", [128, 512], F32)
                wb = Buf("mixw")
                h_tok = [sb(ms, "h_tok%d" % i, [128, D], BF16) for i in range(2)]
                h_tokb = [Buf(), Buf()]
                junk = sb(ms, "junk", [128, D], BF16)
                junkb = Buf()
                two = range(2)
                a_ext = [sb(ms, "a_ext%d" % i, [128, 2, LMAX], F32) for i in two]
                hc_ext = [sb(ms, "hc_ext%d" % i, [128, 3, LMAX], BF16) for i in two]
                yb = [sb(ms, "yb%d" % i, [128, 4, NMAX], BF16) for i in two]
                ab, hcb, ybb = ([Buf(), Buf()] for _ in range(3))

                def same2(name, shape, dt):
                    t_ = sb(ms, name, shape, dt)
                    return [t_, t_]

                def same2b():
                    b_ = Buf()
                    return [b_, b_]

                hT = same2("hT", [128, 8, NMAX], BF16)
                sig = same2("sig", [128, 3, NMAX], F32)
                acc = same2("acc", [128, 3, NMAX], F32)
                u_sb = same2("u_sb", [128, 4, NMAX], F32)
                y_p = same2("y_p", [128, 2, NMAX], BF16)
                ya = same2("ya", [128, 2, NMAX], BF16)
                hs = same2("hs", [128, 3, NMAX], BF16)
                yc = same2("yc", [128, 3, NMAX], BF16)
                hTb, sigb, ub, ypb, yab, hsb, ycb = (same2b() for _ in range(7))
                accb1 = [Buf(), Buf(), Buf()]
                accb = [accb1, accb1]
                dg = sb(ms, "dg", [128, 93, 128], BF16)
                dgb = Buf()
                sA = sb(ms, "sA", [128, 2, LMAX], F32)
                sB = sb(ms, "sB", [128, 2, LMAX], F32)
                tmp16 = sb(ms, "tmp16", [128, 16], F32)
                v_n = [sb(ms, "v_n%d" % i, [128, 384], BF16) for i in two]
                ztmp = sb(ms, "ztmp", [128, 512], F32)
                sq = sb(ms, "sq", [128, 3, NMAX], F32)
                mean = sb(ms, "mean", [128, NMAX], F32)
                var = sb(ms, "var", [128, NMAX], F32)
                rstdc = sb(ms, "rstdc", [128, NMAX], F32)
                sAb, sBb, t16b, ztb, sqb, meanb, varb, rsb = (Buf() for _ in range(8))
                vnb = [Buf(), Buf()]

                wblkb = Buf()
                wsTb = Buf()
                k.memset(wblk[:], 0.0, [wblkb])
                for c in range(2):
                    k.dma(POOL, wblk[0:64, c, 0:64], pool_w_d[l, 2 * c], writes=[wblkb], sem="wblk")
                    k.dma(POOL, wblk[64:128, c, 64:128], pool_w_d[l, 2 * c + 1], writes=[wblkb], sem="wblk")
                k.dma(POOL, wsT[:], wsT_d[l].rearrange("h j i -> j h i"), writes=[wsTb], sem="wsT")
                k.memset(wsT[64:128, :, 0:64], 0.0, [wsTb])
                k.dma(SP, g1_bc[:], n1g_d[l:l + 1, :].partition_broadcast(128), writes=[wb], sem="mixw")
                k.dma(SP, gmg_bc[:], gmg_d[l:l + 1, :].partition_broadcast(128), writes=[wb], sem="mixw")
                k.dma(SP, gmb_bc[:], gmb_d[l:l + 1, :].partition_broadcast(128), writes=[wb], sem="mixw")
                k.dma(POOL, w_in_sb[:], w_in_d[l].rearrange("(kc p) n -> p kc n", p=128), writes=[wb], sem="mixw")
                k.dma(POOL, wo_a[:], w_out_d[l, 0:256, :].rearrange("(c p) n -> p c n", p=128), writes=[wb], sem="mixw")
                k.dma(POOL, wo_b[0:96], w_out_d[l, 256:640, :].rearrange("(h p) n -> p h n", p=96), writes=[wb], sem="mixw")
                k.dma(POOL, wo_c[:], w_out_d[l, 640:1024, :].rearrange("(c p) n -> p c n", p=128), writes=[wb], sem="mixw")
                k.dma(POOL, pw_sb[:], pw_d[l].rearrange("(c p) n -> p c n", p=128), writes=[wb], sem="mixw")
                k.memset(a_ext[0][:, :, 0:32], 0.0, [ab[0]])
                k.memset(hc_ext[0][:, :, 0:32], 0.0, [hcb[0]])
                for i in range(93):
                    k.ts1(dg[:, i, :], ident[:, :], ppc(l, PP_DWW + i), ALU.mult, [cb], [dgb])

                groups = [(0, 1)] + [(t0, TGM) for t0 in range(1, NT, TGM)]

                def stage_ne(gi):
                    t0, nt = groups[gi]
                    for j in range(nt):
                        n = t0 + j
                        hb = (gi * TGM + j) % 2
                        rstd, sbf = rms_stats(x_tok[:, n, :], xb[n], junk[:], junkb, 1.0 / 32.0)
                        k.stt(h_tok[hb][:], x_tok[:, n, :], rstd, g1_bc[:], ALU.mult, ALU.mult,
                              [xb[n], sbf, wb], [h_tokb[hb]])

                def stage_nt(gi):
                    t0, nt = groups[gi]
                    q = gi % 2
                    for j in range(nt):
                        hb = (gi * TGM + j) % 2
                        bk, bkb = next_bank()
                        bkbf = bk.bitcast(BF16)
                        outs = [bkbf[:, kc * 128:(kc + 1) * 128] for kc in range(8)]
                        pairs = [(h_tok[hb][:, kc * 128:(kc + 1) * 128], ident[:, :]) for kc in range(8)]
                        k.mm_group(outs, pairs, [h_tokb[hb], cb], bkb, transpose=True)
                        k.act(hT[q][:, :, j * 128:(j + 1) * 128], bkbf[:, :].rearrange("p (k t) -> p k t", k=8),
                              AF.Copy, [bkb], [hTb[q]])

                def stage_p(gi):
                    t0, nt = groups[gi]
                    q = gi % 2
                    N = nt * 128
                    L = 32 + N
                    full = not (l == 1 and t0 == 0)
                    first_own = (t0 == 1)
                    if gi > 0:
                        pN = groups[gi - 1][1] * 128
                        k.copy(a_ext[q][:, :, 0:32], a_ext[1 - q][:, :, pN:pN + 32], [ab[1 - q]], [ab[q]], eng=POOL)
                        k.copy(hc_ext[q][:, :, 0:32], hc_ext[1 - q][:, :, pN:pN + 32], [hcb[1 - q]], [hcb[q]], eng=POOL)

                    def proj(col0, m):
                        bk, bkb = next_bank()
                        pairs = [(w_in_sb[:, kc, col0:col0 + m], hT[q][:, kc, 0:N]) for kc in range(8)]
                        k.mm_group(bk[0:m, 0:N], pairs, [wb, hTb[q]], bkb)
                        return bk, bkb

                    for c in range(2):
                        bk, bkb = proj(c * 128, 128)
                        k.act(a_ext[q][:, c, 32:L], bk[:, 0:N], AF.Copy, [bkb], [ab[q]])
                    vinfo = []
                    if full:
                        for j in range(nt):
                            vb = j % 2
                            bk, bkb = next_bank()
                            pairs = [(hT[q][:, kc, j * 128:(j + 1) * 128], w_in_sb[:, kc, 640:1024]) for kc in range(8)]
                            k.mm_group(bk[:, 0:384], pairs, [wb, hTb[q]], bkb)
                            rstd, sbf = rms_stats(bk[:, 0:384], bkb, junk[:, 0:384], junkb, float(384.0 ** -0.5))
                            k.stt(v_n[vb][:], bk[:, 0:384], rstd, gmg_bc[:], ALU.mult, ALU.mult,
                                  [bkb, sbf, wb], [vnb[vb]])
                    for c in range(3):
                        bk, bkb = proj(1408 + c * 128, 128)
                        k.act(sig[q][:, c, 0:N], bk[:, 0:N], AF.Sigmoid, [bkb], [sigb[q]])
                    for c in range(3):
                        bk, bkb = proj(1024 + c * 128, 128)
                        k.tt(hc_ext[q][:, c, 32:L], bk[:, 0:N], sig[q][:, c, 0:N], ALU.mult, [bkb, sigb[q]], [hcb[q]])
                    if full:
                        for h in range(4):
                            bk, bkb = proj(256 + h * 96, 96)
                            k.act(u_sb[q][0:96, h, 0:N], bk[0:96, 0:N], AF.Copy, [bkb], [ub[q]])
                        for j in range(nt):
                            vb = j % 2
                            zk, zkb = next_bank()
                            grp = [(zk[0:96, h * 128:(h + 1) * 128], v_n[vb][:, h * 96:(h + 1) * 96], wsT[:, h, :])
                                   for h in range(4)]
                            k.mm_multi(grp, [vnb[vb], wsTb], zkb)
                            k.tt(ztmp[0:96, :], zk[0:96, :], gmb_bc[0:96, :], ALU.add, [zkb, wb], [ztb])
                            k.tt(yb[q][0:96, :, j * 128:(j + 1) * 128],
                                 ztmp[0:96, :].rearrange("p (h i) -> p h i", h=4),
                                 u_sb[q][0:96, :, j * 128:(j + 1) * 128], ALU.mult, [ztb, ub[q]], [ybb[q]])
                        A_ = a_ext[q]

                        def pool_out(sbuf_t, sbuf_b, lo, hi, c):
                            k.stt(y_p[q][lo:hi, c, 0:N], sbuf_t[lo:hi, c, 32:L], ppc(l, PP_INVW + c, 1, lo, hi),
                                  A_[lo:hi, c, 32:L], ALU.mult, ALU.subtract, [sbuf_b, ab[q], cb], [ypb[q]])
                            if first_own:
                                k.tt(tmp16[lo:hi, :], sbuf_t[lo:hi, c, 32:48], pinv[lo:hi, c, :], ALU.mult,
                                     [sbuf_b, cb], [t16b])
                                k.tt(y_p[q][lo:hi, c, 0:16], tmp16[lo:hi, :], A_[lo:hi, c, 32:48], ALU.subtract,
                                     [t16b, ab[q]], [ypb[q]])

                        k.tt(sA[:, :, 1:L], A_[:, :, 1:L], A_[:, :, 0:L - 1], ALU.add, [ab[q]], [sAb])
                        pool_out(sA, sAb, 0, 64, 0)
                        k.tt(sB[:, :, 3:L], sA[:, :, 3:L], sA[:, :, 1:L - 2], ALU.add, [sAb], [sBb])
                        pool_out(sB, sBb, 64, 128, 0)
                        k.tt(sA[:, :, 7:L], sB[:, :, 7:L], sB[:, :, 3:L - 4], ALU.add, [sBb], [sAb])
                        pool_out(sA, sAb, 0, 64, 1)
                        k.tt(sB[:, :, 15:L], sA[:, :, 15:L], sA[:, :, 7:L - 8], ALU.add, [sAb], [sBb])
                        pool_out(sB, sBb, 64, 128, 1)

                def stage_b1(gi):
                    t0, nt = groups[gi]
                    q = gi % 2
                    N = nt * 128
                    L = 32 + N
                    full = not (l == 1 and t0 == 0)
                    first_own = (t0 == 1)
                    if not full:
                        return
                    A_ = a_ext[q]
                    H_ = hc_ext[q]

                    for c in range(2):
                        bk, bkb = next_bank()
                        k.mm_group(bk[:, 0:N], [(wblk[:, c, :], y_p[q][:, c, 0:N])], [wblkb, ypb[q]], bkb)
                        k.act(ya[q][:, c, 0:N], bk[:, 0:N], AF.Copy, [bkb, cb], [yab[q]], scale=ppc(l, PP_PSCALE + c))

                    for c in range(3):
                        bk, bkb = next_bank()
                        pairs = [(dg[:, c * 31 + kk, :], H_[:, c, 2 + kk:2 + kk + N]) for kk in range(31)]
                        k.mm_group(bk[:, 0:N], pairs, [dgb, hcb[q]], bkb)
                        k.act(acc[q][:, c, 0:N], bk[:, 0:N], AF.Identity, [bkb, cb], [accb[q][c]],
                              bias=ppc(l, PP_DWB + c))
                    for c in range(3):
                        k.act(sq[:, c, 0:N], acc[q][:, c, 0:N], AF.Square, [accb[q][c]], [sqb])
                    b1, b1b = next_bank()
                    k.mm_group(b1[:, 0:N], [(ones32[:, :], acc[q][:, c, 0:N]) for c in range(3)], accb[q] + [cb], b1b)
                    b2, b2b = next_bank()
                    k.mm_group(b2[:, 0:N], [(ones32[:, :], sq[:, c, 0:N]) for c in range(3)], [sqb, cb], b2b)
                    k.ts(mean[:, 0:N], b1[:, 0:N], 1.0 / 384.0, 0.0, ALU.mult, ALU.add, [b1b], [meanb])
                    k.tt(var[:, 0:N], mean[:, 0:N], mean[:, 0:N], ALU.mult, [meanb], [varb])
                    k.stt(var[:, 0:N], b2[:, 0:N], 1.0 / 384.0, var[:, 0:N], ALU.mult, ALU.subtract,
                          [b2b], [varb])
                    k.act(var[:, 0:N], var[:, 0:N], AF.Sqrt, [], [varb], bias=EPS)
                    k.recip(rstdc[:, 0:N], var[:, 0:N], [varb], [rsb])
                    for c in range(3):
                        k.tt(sq[:, c, 0:N], acc[q][:, c, 0:N], mean[:, 0:N], ALU.subtract, [accb[q][c], meanb], [sqb])
                        k.tt(sq[:, c, 0:N], sq[:, c, 0:N], rstdc[:, 0:N], ALU.mult, [rsb], [sqb])
                        k.act(hs[q][:, c, 0:N], sq[:, c, 0:N], AF.Silu, [sqb, cb], [hsb[q]],
                              bias=ppc(l, PP_LNB + c), scale=ppc(l, PP_LNG + c))
                def stage_b2(gi):
                    t0, nt = groups[gi]
                    q = gi % 2
                    N = nt * 128
                    full = not (l == 1 and t0 == 0)
                    if not full:
                        return
                    for co in range(3):
                        bk, bkb = next_bank()
                        pairs = [(pw_sb[:, ci, co * 128:(co + 1) * 128], hs[q][:, ci, 0:N]) for ci in range(3)]
                        k.mm_group(bk[:, 0:N], pairs, [wb, hsb[q]], bkb)
                        k.act(yc[q][:, co, 0:N], bk[:, 0:N], AF.Identity, [bkb, cb], [ycb[q]], bias=ppc(l, PP_PWB + co))

                    for j in range(nt):
                        n = t0 + j
                        ts_ = slice(j * 128, (j + 1) * 128)
                        for hf in range(2):
                            cs = slice(hf * 512, (hf + 1) * 512)
                            pairs = [(ya[q][:, c, ts_], wo_a[:, c, cs]) for c in range(2)]
                            pairs += [(yb[q][0:96, h, ts_], wo_b[0:96, h, cs]) for h in range(4)]
                            pairs += [(yc[q][:, c, ts_], wo_c[:, c, cs]) for c in range(3)]
                            bk, bkb = next_bank()
                            k.mm_group(bk[:, :], pairs, [wb, yab[q], ybb[q], ycb[q]], bkb)
                            k.tt(x_tok[:, n, cs], x_tok[:, n, cs], bk[:, :], ALU.add, [bkb], [xb[n]])

                ng = len(groups)
                stage_ne(0)
                stage_nt(0)
                if ng > 1:
                    stage_ne(1)
                stage_p(0)
                for gi in range(ng):
                    if gi + 1 < ng:
                        stage_nt(gi + 1)
                    stage_b1(gi)
                    if gi + 2 < ng:
                        stage_ne(gi + 2)
                    if gi + 1 < ng:
                        stage_p(gi + 1)
                    stage_b2(gi)
                p.barrier()

        def ffn_stream(scope, moe, tiles, h2T, h2b, gf, experts=None):
            GW = gf * 128
            slots = []
            for s_ in range(2):
                slots.append((sb(scope, "wg%d" % s_, [128, 8, GW], BF16),
                              sb(scope, "wu%d" % s_, [128, 8, GW], BF16),
                              sb(scope, "wd%d" % s_, [128, gf, D], BF16), Buf()))
            actt = [sb(scope, "act%d" % i, [128, gf, 512], BF16) for i in range(2)]
            actb = [Buf(), Buf()]
            sg = [sb(scope, "sg%d" % i, [128, 512], F32) for i in range(2)]
            sgb = [Buf(), Buf()]
            glist = []
            if not moe:
                nch = D_FF // 128
                for c0 in range(0, nch, gf):
                    nf = min(gf, nch - c0)
                    glist.append((fwg_d[:, c0 * 128:(c0 + nf) * 128], fwu_d[:, c0 * 128:(c0 + nf) * 128],
                                  fwd_d[c0 * 128:(c0 + nf) * 128, :], nf, None))
            else:
                nch = D_FFE // 128
                for e in (experts if experts is not None else range(NE)):
                    for c0 in range(0, nch, gf):
                        nf = min(gf, nch - c0)
                        glist.append((mwg_d[e, :, c0 * 128:(c0 + nf) * 128],
                                      mwu_d[e, :, c0 * 128:(c0 + nf) * 128],
                                      mwd_d[e, c0 * 128:(c0 + nf) * 128, :], nf, e))
            tgroups = [tiles[i:i + 4] for i in range(0, len(tiles), 4)]
            cnt = 0
            sgi = 0
            for gi, (wg_ap, wu_ap, wd_ap, nf, e) in enumerate(glist):
                wg_s, wu_s, wd_s, wsb = slots[gi % 2]
                sem = "wslot%d" % (gi % 2)
                k.dma(POOL, wg_s[:, :, 0:nf * 128], wg_ap.rearrange("(kc p) n -> p kc n", p=128),
                      writes=[wsb], sem=sem)
                k.dma(POOL, wu_s[:, :, 0:nf * 128], wu_ap.rearrange("(kc p) n -> p kc n", p=128),
                      writes=[wsb], sem=sem)
                k.dma(POOL, wd_s[:, 0:nf, :], wd_ap.rearrange("(f p) n -> p f n", p=128),
                      writes=[wsb], sem=sem)
                for tg in tgroups:
                    ab_i = cnt % 2
                    cnt += 1
                    t_lo = tg[0] * 128
                    N = len(tg) * 128
                    for f in range(nf):
                        bg, bgb = next_bank()
                        k.mm_group(bg[:, 0:N], [(wg_s[:, kc, f * 128:(f + 1) * 128], h2T[:, kc, t_lo:t_lo + N])
                                                for kc in range(8)], [wsb] + [h2b[n] for n in tg], bgb)
                        bu, bub = next_bank()
                        k.mm_group(bu[:, 0:N], [(wu_s[:, kc, f * 128:(f + 1) * 128], h2T[:, kc, t_lo:t_lo + N])
                                                for kc in range(8)], [wsb] + [h2b[n] for n in tg], bub)
                        si = sgi % 2
                        sgi += 1
                        k.act(sg[si][:, 0:N], bg[:, 0:N], AF.Silu, [bgb], [sgb[si]])
                        k.tt(actt[ab_i][:, f, 0:N], sg[si][:, 0:N], bu[:, 0:N], ALU.mult, [sgb[si], bub],
                             [actb[ab_i]])
                    for j, n in enumerate(tg):
                        for hf in range(2):
                            cs = slice(hf * 512, (hf + 1) * 512)
                            bk, bkb = next_bank()
                            k.mm_group(bk[:, :], [(actt[ab_i][:, f, j * 128:(j + 1) * 128], wd_s[:, f, cs])
                                                  for f in range(nf)], [wsb, actb[ab_i]], bkb)
                            if moe:
                                k.stt(x_tok[:, n, cs], bk[:, :], gate[:, n, e:e + 1], x_tok[:, n, cs],
                                      ALU.mult, ALU.add, [bkb, gateb[n]], [xb[n]])
                            else:
                                k.tt(x_tok[:, n, cs], x_tok[:, n, cs], bk[:, :], ALU.add, [bkb], [xb[n]])

        def ffn_phase0():
            tiles = list(range(0, NT))
            with ExitStack() as fs:
                h2T = sb(fs, "h2T", [128, 8, NT * 128], BF16)
                h2b = [Buf() for _ in range(NT)]
                with ExitStack() as f1:
                    g2_bc = sb(f1, "g2_bc", [128, D], F32)
                    gb = Buf()
                    h_tok = [sb(f1, "h_tokf%d" % i, [128, D], BF16) for i in range(2)]
                    h_tokb = [Buf(), Buf()]
                    junk = sb(f1, "junkf", [128, D], BF16)
                    junkb = Buf()
                    k.dma(SP, g2_bc[:], n2g_d[0:1, :].partition_broadcast(128), writes=[gb], sem="g2")
                    for ti, n in enumerate(tiles):
                        hb = ti % 2
                        norm_and_transpose(n, g2_bc[:], gb, h_tok[hb][:], h_tokb[hb], junk[:], junkb,
                                           h2T[:, :, n * 128:(n + 1) * 128], h2b[n])
                    p.barrier()
                with ExitStack() as f2:
                    ffn_stream(f2, False, tiles, h2T, h2b, GF)
                    p.barrier()

        def moe_phase():
            tiles = list(range(1, NT))
            with ExitStack() as fs:
                h_all = sb(fs, "h_all", [128, 16, D], BF16)
                hab = [Buf() for _ in range(16)]
                sel = sb(fs, "sel", [128, 16, NE], F32)
                pos = sb(fs, "pos", [128, 16, NE], F32)
                tot = sb(fs, "tot", [128, 16, NE], F32)
                offs = sb(fs, "offs", [128, 16, NE], F32)
                misc = sb(fs, "rmisc", [128, 40], F32)
                cond_i = sb(fs, "cond_i", [128, 32], I32)
                selb, posb, totb, offb, miscb, condb = (Buf() for _ in range(6))
                with ExitStack() as f1:
                    g2_bc = sb(f1, "g2_bc", [128, D], F32)
                    gb = Buf()
                    junk = sb(f1, "junkf", [128, D], BF16)
                    junkb = Buf()
                    rk = sb(f1, "rk", [128, 8, NE], F32)
                    rkb = Buf()
                    xT32 = [sb(f1, "xT32_%d" % i, [128, 8, 128], F32) for i in range(2)]
                    xTb = [Buf(), Buf()]
                    lg = sb(f1, "lg", [128, 2, 32], F32)
                    lgb = [Buf(), Buf()]
                    k.dma(SP, g2_bc[:], n2g_d[1:2, :].partition_broadcast(128), writes=[gb], sem="g2")
                    k.dma(SP, rk[:], router_d.rearrange("(kc p) e -> p kc e", p=128), writes=[rkb], sem="rg")
                    for kc in range(8):
                        k.ts1(rk[:, kc, :], rk[:, kc, :], ppc(1, PP_G2 + kc), ALU.mult, [cb], [rkb])
                    for ti, n in enumerate(tiles):
                        T = n - 1
                        hb = ti % 2
                        rstd, sbf = rms_stats(x_tok[:, n, :], xb[n], junk[:], junkb, 1.0 / 32.0)
                        k.stt(h_all[:, T, :], x_tok[:, n, :], rstd, g2_bc[:], ALU.mult, ALU.mult,
                              [xb[n], sbf, gb], [hab[T]])
                        L_ = lg[:, hb, :]
                        lb = lgb[hb]
                        for half in range(2):
                            bk, bkb = next_bank()
                            outs = [bk[:, i * 128:(i + 1) * 128] for i in range(4)]
                            pairs = [(x_tok[:, n, (4 * half + i) * 128:(4 * half + i + 1) * 128], ident32[:, :])
                                     for i in range(4)]
                            k.mm_group(outs, pairs, [xb[n], cb], bkb, transpose=True)
                            k.act(xT32[hb][:, 4 * half:4 * half + 4, :], bk[:, :].rearrange("p (a b) -> p a b", a=4),
                                  AF.Copy, [bkb], [xTb[hb]])
                        bl, blb = next_bank()
                        k.mm_group(bl[:, 0:NE], [(xT32[hb][:, kc, :], rk[:, kc, :]) for kc in range(8)],
                                   [xTb[hb], rkb], blb)
                        k.ts1(L_[:, 0:8], bl[:, 0:NE], rstd, ALU.mult, [blb, sbf], [lb])
                        k.emit(DVE, lambda e_, o=L_[:, 24:25], i=L_[:, 0:8]: e_.reduce_max(out=o, in_=i, axis=AX.X),
                               [], [lb])
                        k.ts1(L_[:, 8:16], L_[:, 0:8], L_[:, 24:25], ALU.is_equal, [], [lb])
                        k.stt(L_[:, 16:24], L_[:, 8:16], -1e30, L_[:, 0:8], ALU.mult, ALU.add, [], [lb])
                        k.emit(DVE, lambda e_, o=L_[:, 25:26], i=L_[:, 16:24]: e_.reduce_max(out=o, in_=i, axis=AX.X),
                               [], [lb])
                        k.ts1(L_[:, 16:24], L_[:, 16:24], L_[:, 25:26], ALU.is_equal, [], [lb])
                        k.tt(sel[:, T, :], L_[:, 8:16], L_[:, 16:24], ALU.add, [lb], [selb])
                        k.tt(L_[:, 26:27], L_[:, 25:26], L_[:, 24:25], ALU.subtract, [], [lb])
                        k.act(L_[:, 26:27], L_[:, 26:27], AF.Exp, [], [lb])
                        k.ts(L_[:, 27:28], L_[:, 26:27], 1.0, 0.0, ALU.add, ALU.add, [], [lb])
                        k.recip(L_[:, 27:28], L_[:, 27:28], [], [lb])
                        k.tt(L_[:, 28:29], L_[:, 26:27], L_[:, 27:28], ALU.mult, [], [lb])
                        k.ts1(L_[:, 8:16], L_[:, 8:16], L_[:, 27:28], ALU.mult, [], [lb])
                        k.stt(gate[:, n, :], L_[:, 16:24], L_[:, 28:29], L_[:, 8:16], ALU.mult, ALU.add,
                              [lb], [gateb[n]])
                    selv = sel[:, :, :].rearrange("p t e -> p (t e)")
                    b1, b1b = next_bank()
                    k.mm_group(b1[:, 0:128], [(ustrict[:, :], selv)], [selb, cb], b1b)
                    k.copy(pos[:, :, :].rearrange("p t e -> p (t e)"), b1[:, 0:128], [b1b], [posb])
                    b2, b2b = next_bank()
                    k.mm_group(b2[:, 0:128], [(ones32[:, :], selv)], [selb, cb], b2b)
                    k.copy(tot[:, :, :].rearrange("p t e -> p (t e)"), b2[:, 0:128], [b2b], [totb])
                    k.memset(offs[:, 0, :], 0.0, [offb])
                    for T in range(1, 16):
                        k.tt(offs[:, T, :], offs[:, T - 1, :], tot[:, T - 1, :], ALU.add, [totb], [offb])
                    k.tt(pos[:, :, :], pos[:, :, :], offs[:, :, :], ALU.add, [offb], [posb])
                    k.tt(misc[:, 0:8], offs[:, 15, :], tot[:, 15, :], ALU.add, [offb, totb], [miscb])
                    for c, cap in enumerate(CAPS):
                        k.ts(misc[:, 8 + 8 * c:16 + 8 * c], misc[:, 0:8], float(cap) + 0.5, 0.0, ALU.is_lt, ALU.add,
                             [], [miscb])
                    k.copy(cond_i[:, 0:8 * len(CAPS)], misc[:, 8:8 + 8 * len(CAPS)], [miscb], [condb])
                    if FORCE_CLASS is not None:
                        for c in range(len(CAPS)):
                            k.memset(cond_i[:, 8 * c:8 * c + 8], 1 if c >= FORCE_CLASS else 0, [condb])
                    p.barrier()

                def expert_sparse(e, cap):
                    NJ = cap // 128
                    chunks = [(0, 512)] + ([(512, cap - 512)] if cap > 512 else [])
                    with ExitStack() as sp_:
                        Pb = [sb(sp_, "P%d" % i, [128, cap], BF16) for i in range(4)]
                        Pbb = [Buf() for _ in range(4)]
                        pi = [0]
                        hTe = sb(sp_, "hTe", [128, 8, cap], BF16)
                        hTeb = Buf()
                        GW = GFS * 128
                        slots = []
                        for s_ in range(2):
                            slots.append((sb(sp_, "swg%d" % s_, [128, 8, GW], BF16),
                                          sb(sp_, "swu%d" % s_, [128, 8, GW], BF16),
                                          sb(sp_, "swd%d" % s_, [128, GFS, D], BF16), Buf(), Buf()))
                        actt = [sb(sp_, "sact%d" % i, [128, GFS, cap], BF16) for i in range(2)]
                        actb = [Buf(), Buf()]
                        sg = [sb(sp_, "ssg%d" % i, [128, cap], F32) for i in range(2)]
                        sgb = [Buf(), Buf()]
                        oe32 = sb(sp_, "oe32", [128, NJ, D], F32)
                        oe_bf = sb(sp_, "oe_bf", [128, NJ, D], BF16)
                        oeb = [[Buf(), Buf()] for _ in range(NJ)]
                        oebf_b = Buf()
                        PT = [sb(sp_, "PT%d" % i, [128, NJ, 128], BF16) for i in range(2)]
                        PTb = [Buf(), Buf()]

                        def build_P(T, c0, w):
                            i = pi[0] % 4
                            pi[0] += 1
                            k.ts(Pb[i][:, 0:w], iota[:, c0:c0 + w], pos[:, T, e:e + 1], sel[:, T, e:e + 1],
                                 ALU.is_equal, ALU.mult, [cb, posb, selb], [Pbb[i]])
                            return Pb[i], Pbb[i]

                        for (c0, w) in chunks:
                            per_bank = 512 // w
                            nb = 8 // per_bank
                            bks = [next_bank() for _ in range(nb)]
                            allb = [b for _, b in bks]
                            tok = None
                            for T in range(16):
                                Pt, Ptb = build_P(T, c0, w)
                                deps = [Ptb.w, hab[T].w]
                                if T == 0:
                                    deps += k.deps_of([], allb)
                                for kc in range(8):
                                    o = bks[kc // per_bank][0][:, (kc % per_bank) * w:(kc % per_bank + 1) * w]
                                    tok = p.op(PE, (lambda e_, o=o, a=h_all[:, T, kc * 128:(kc + 1) * 128], r=Pt[:, 0:w],
                                                    s_=(T == 0), t_=(T == 15): e_.matmul(o, lhsT=a, rhs=r, start=s_, stop=t_)),
                                               deps if kc == 0 else (), kc == 7)
                                Ptb.add_read(tok)
                                hab[T].add_read(tok)
                            for b in allb:
                                b.set_write(tok)
                            for bi, (bk, bkb) in enumerate(bks):
                                k.act(hTe[:, bi * per_bank:(bi + 1) * per_bank, c0:c0 + w],
                                      bk[:, 0:per_bank * w].rearrange("p (a b) -> p a b", a=per_bank), AF.Copy,
                                      [bkb], [hTeb])
                        nch = D_FFE // 128
                        ngrp = (nch + GFS - 1) // GFS
                        ginfo = {}
                        sgi_box = [0]

                        def ffn_s1(g_):
                            c0f = g_ * GFS
                            nf = min(GFS, nch - c0f)
                            wg_s, wu_s, wd_s, wsb, wdb = slots[g_ % 2]
                            k.dma(POOL, wg_s[:, :, 0:nf * 128],
                                  mwg_d[e, :, c0f * 128:(c0f + nf) * 128].rearrange("(kc p) n -> p kc n", p=128),
                                  writes=[wsb], sem="wsA%d" % (g_ % 2))
                            k.dma(POOL, wu_s[:, :, 0:nf * 128],
                                  mwu_d[e, :, c0f * 128:(c0f + nf) * 128].rearrange("(kc p) n -> p kc n", p=128),
                                  writes=[wsb], sem="wsA%d" % (g_ % 2))
                            k.dma(POOL, wd_s[:, 0:nf, :],
                                  mwd_d[e, c0f * 128:(c0f + nf) * 128, :].rearrange("(f p) n -> p f n", p=128),
                                  writes=[wdb], sem="wsB%d" % (g_ % 2))
                            ab_i = g_ % 2
                            ginfo[g_] = (nf, wd_s, wdb, ab_i)
                            for f in range(nf):
                                fs_ = slice(f * 128, (f + 1) * 128)
                                si = sgi_box[0] % 2
                                sgi_box[0] += 1
                                for (c0, w) in chunks:
                                    if w == 512:
                                        bg, bgb = next_bank()
                                        bu, bub = next_bank()
                                        k.mm_group(bg[:, :], [(wg_s[:, kc, fs_], hTe[:, kc, c0:c0 + w]) for kc in range(8)],
                                                   [wsb, hTeb], bgb)
                                        k.mm_group(bu[:, :], [(wu_s[:, kc, fs_], hTe[:, kc, c0:c0 + w]) for kc in range(8)],
                                                   [wsb, hTeb], bub)
                                        g_ap, u_ap = bg[:, :], bu[:, :]
                                    else:
                                        bs, bsb = next_bank()
                                        deps = k.deps_of([wsb, hTeb], [bsb])
                                        tok = None
                                        for wi, w_s in enumerate((wg_s, wu_s)):
                                            for kc in range(8):
                                                tok = p.op(PE, (lambda e_, o=bs[:, wi * w:(wi + 1) * w], a=w_s[:, kc, fs_],
                                                                r=hTe[:, kc, c0:c0 + w], s_=(kc == 0), t_=(kc == 7):
                                                                e_.matmul(o, lhsT=a, rhs=r, start=s_, stop=t_)),
                                                           deps if (wi == 0 and kc == 0) else (), (wi == 1 and kc == 7))
                                        wsb.add_read(tok)
                                        hTeb.add_read(tok)
                                        bsb.set_write(tok)
                                        bgb = bub = bsb
                                        g_ap, u_ap = bs[:, 0:w], bs[:, w:2 * w]
                                    k.act(sg[si][:, c0:c0 + w], g_ap, AF.Silu, [bgb], [sgb[si]])
                                    k.tt(actt[ab_i][:, f, c0:c0 + w], sg[si][:, c0:c0 + w], u_ap, ALU.mult,
                                         [sgb[si], bub], [actb[ab_i]])

                        def ffn_s2(g_):
                            nf, wd_s, wsb, ab_i = ginfo[g_]
                            for j in range(NJ):
                                for hf in range(2):
                                    cs = slice(hf * 512, (hf + 1) * 512)
                                    bk, bkb = next_bank()
                                    k.mm_group(bk[:, :], [(actt[ab_i][:, f, j * 128:(j + 1) * 128], wd_s[:, f, cs])
                                                          for f in range(nf)], [wsb, actb[ab_i]], bkb)
                                    ob = oeb[j][hf]
                                    if g_ == 0:
                                        k.act(oe32[:, j, cs], bk[:, :], AF.Copy, [bkb], [ob])
                                    elif g_ < ngrp - 1:
                                        k.tt(oe32[:, j, cs], oe32[:, j, cs], bk[:, :], ALU.add, [bkb], [ob])
                                    else:
                                        k.tt(oe_bf[:, j, cs], oe32[:, j, cs], bk[:, :], ALU.add, [bkb, ob], [oebf_b])

                        ffn_s1(0)
                        for g_ in range(ngrp):
                            if g_ + 1 < ngrp:
                                ffn_s1(g_ + 1)
                            ffn_s2(g_)
                        Pq = {}

                        def st_x(T):
                            Pq[T] = build_P(T, 0, cap)

                        def st_y(T):
                            Pt, Ptb = Pq.pop(T)
                            bk, bkb = next_bank()
                            bkbf = bk.bitcast(BF16)
                            outs = [bkbf[:, j * 128:(j + 1) * 128] for j in range(NJ)]
                            pairs = [(Pt[:, j * 128:(j + 1) * 128], ident[:, :]) for j in range(NJ)]
                            k.mm_group(outs, pairs, [Ptb, cb], bkb, transpose=True)
                            pti = T % 2
                            k.act(PT[pti][:, :, :], bkbf[:, 0:cap].rearrange("p (j t) -> p j t", j=NJ), AF.Copy,
                                  [bkb], [PTb[pti]])

                        def st_z(T):
                            ptp = T % 2
                            n = T + 1
                            for hf in range(2):
                                cs = slice(hf * 512, (hf + 1) * 512)
                                bk2, bk2b = next_bank()
                                k.mm_group(bk2[:, :], [(PT[ptp][:, j, :], oe_bf[:, j, cs]) for j in range(NJ)],
                                           [PTb[ptp], oebf_b], bk2b)
                                k.stt(x_tok[:, n, cs], bk2[:, :], gate[:, n, e:e + 1], x_tok[:, n, cs],
                                      ALU.mult, ALU.add, [bk2b, gateb[n]], [xb[n]])

                        st_x(0)
                        st_x(1)
                        st_y(0)
                        for T in range(16):
                            if T + 2 < 16:
                                st_x(T + 2)
                            if T + 1 < 16:
                                st_y(T + 1)
                            st_z(T)
                        p.barrier()

                def expert_dense(e):
                    with ExitStack() as db:
                        h2T = sb(db, "h2T", [128, 8, NT * 128], BF16)
                        h2b = [Buf() for _ in range(NT)]
                        for n in tiles:
                            T = n - 1
                            bk, bkb = next_bank()
                            bkbf = bk.bitcast(BF16)
                            outs = [bkbf[:, kc * 128:(kc + 1) * 128] for kc in range(8)]
                            pairs = [(h_all[:, T, kc * 128:(kc + 1) * 128], ident[:, :]) for kc in range(8)]
                            k.mm_group(outs, pairs, [hab[T], cb], bkb, transpose=True)
                            k.act(h2T[:, :, n * 128:(n + 1) * 128], bkbf[:, :].rearrange("p (k t) -> p k t", k=8),
                                  AF.Copy, [bkb], [h2b[n]])
                        ffn_stream(db, True, tiles, h2T, h2b, GFS, experts=[e])
                        p.barrier()

                def cflag(c, e):
                    return cond_i[0:1, 8 * c + e:8 * c + e + 1]

                for e in range(NE):
                    p.cond_region(
                        cflag(1, e), [],
                        lambda e=e: p.cond_region(cflag(0, e), [], lambda: expert_sparse(e, CAPS[0]),
                                                  lambda: expert_sparse(e, CAPS[1])),
                        lambda e=e: p.cond_region(cflag(2, e), [], lambda: expert_sparse(e, CAPS[2]),
                                                  lambda: expert_dense(e)))
                    p.barrier()

        def final_phase(do_norm):
            with ExitStack() as os_:
                gf_bc = sb(os_, "gf_bc", [128, D], F32)
                gfb = Buf()
                outt = [sb(os_, "outt%d" % i, [128, D], F32) for i in range(2)]
                outb = [Buf(), Buf()]
                junk = sb(os_, "junko", [128, D], BF16)
                junkb = Buf()
                toks = []
                if do_norm:
                    k.dma(SP, gf_bc[:], fg_d[0:1, :].partition_broadcast(128), writes=[gfb], sem="gf")
                for n in range(1, NT):
                    if do_norm:
                        oi = n % 2
                        rstd, sbf = rms_stats(x_tok[:, n, :], xb[n], junk[:], junkb, 1.0 / 32.0)
                        k.stt(outt[oi][:], x_tok[:, n, :], rstd, gf_bc[:], ALU.mult, ALU.mult,
                              [xb[n], sbf, gfb], [outb[oi]])
                        toks.append(k.dma(SP, y_d[(n - 1) * 128:n * 128, :], outt[oi][:], reads=[outb[oi]], sem="out%d" % oi))
                    else:
                        toks.append(k.dma(SP, y_d[(n - 1) * 128:n * 128, :], x_tok[:, n, :], reads=[xb[n]], sem="out0"))
                p.wait_only(SP, toks)
                p.barrier()

        mixer_phase(0)
        if stop_after == "M0":
            final_phase(False)
            p.flush()
            return nc
        ffn_phase0()
        if stop_after == "F0":
            final_phase(False)
            p.flush()
            return nc
        k.ts1(x_tok[:, 0, :], x_tok[:, 0, :], flag[:, 0:1], ALU.mult, [cb], [xb[0]])
        mixer_phase(1)
        if stop_after == "M1":
            final_phase(False)
            p.flush()
            return nc
        moe_phase()
        final_phase(True)
        p.flush()
    return nc


def _prep_shared(inp):
    f = lambda a: np.ascontiguousarray(np.asarray(a, dtype=np.float32))
    sh = {}
    sh["ident"] = np.eye(128, dtype=np.float32)
    sh["ustrict"] = np.triu(np.ones((128, 128), np.float32), 1)
    sh["iota"] = np.ascontiguousarray(np.broadcast_to(np.arange(CAPS[-1], dtype=np.float32), (128, CAPS[-1])))
    pp = np.zeros((128, 2, NPP), np.float32)
    wins = np.array([[2.0, 4.0], [8.0, 16.0]], np.float32)
    for l in range(2):
        pp[:, l, PP_PSCALE:PP_PSCALE + 2] = f(inp["pool_scale"])[l].reshape(2, 128).T
        dw = f(inp["conv_dw_w"])[l]
        pp[:, l, PP_DWW:PP_DWW + 93] = dw.reshape(31, 3, 128).transpose(2, 1, 0).reshape(128, 93)
        pp[:, l, PP_DWB:PP_DWB + 3] = f(inp["conv_dw_b"])[l].reshape(3, 128).T
        pp[:, l, PP_LNG:PP_LNG + 3] = f(inp["conv_ln_g"])[l].reshape(3, 128).T
        pp[:, l, PP_LNB:PP_LNB + 3] = f(inp["conv_ln_b"])[l].reshape(3, 128).T
        pp[:, l, PP_PWB:PP_PWB + 3] = f(inp["conv_pw_b"])[l].reshape(3, 128).T
        for c in range(2):
            pp[0:64, l, PP_INVW + c] = 1.0 / wins[c, 0]
            pp[64:128, l, PP_INVW + c] = 1.0 / wins[c, 1]
        pp[:, l, PP_G2:PP_G2 + 8] = f(inp["norm2_g"])[l].reshape(8, 128).T
    sh["pp"] = pp.reshape(128, 2 * NPP)
    sh["norm1_g"] = f(inp["norm1_g"])
    sh["norm2_g"] = f(inp["norm2_g"])
    sh["final_g"] = f(inp["final_g"]).reshape(1, D)
    sh["gm_norm_g"] = f(inp["gm_norm_g"])
    sh["gm_b"] = f(inp["gm_b"]).reshape(2, 512)
    sh["w_in"] = f(inp["w_in"])
    sh["pool_w"] = f(inp["pool_w"])
    sh["gm_wsT"] = np.ascontiguousarray(f(inp["gm_ws"]).transpose(0, 1, 3, 2))
    sh["conv_pw_w"] = f(inp["conv_pw_w"])
    sh["w_out"] = f(inp["w_out"])
    sh["ffn_wg"] = f(inp["ffn_wg"])[0]
    sh["ffn_wu"] = f(inp["ffn_wu"])[0]
    sh["ffn_wd"] = f(inp["ffn_wd"])[0]
    sh["router"] = f(inp["moe_router"])[0]
    sh["moe_wg"] = f(inp["moe_wg"])[0]
    sh["moe_wu"] = f(inp["moe_wu"])[0]
    sh["moe_wd"] = f(inp["moe_wd"])[0]
    return sh


def _prep_core(x, c):
    b, q = c // 4, c % 4
    xin = np.zeros((NT * 128, D), np.float32)
    xin[128:] = x[b, q * 2048:(q + 1) * 2048]
    if q > 0:
        xin[:128] = x[b, q * 2048 - 128:q * 2048]
    flag = np.full((128, 1), 1.0 if q > 0 else 0.0, np.float32)
    pinv = np.zeros((128, 2, 16), np.float32)
    wins = [[2, 4], [8, 16]]
    for cc in range(2):
        for half in range(2):
            w = wins[cc][half]
            for j in range(16):
                cntv = min(j + 1, w) if q == 0 else w
                pinv[half * 64:(half + 1) * 64, cc, j] = 1.0 / cntv
    return {"x": xin, "flag": flag, "pool_inv": pinv.reshape(128, 32)}


_NC_CACHE = {}


def run(inputs, stop_after=None, trace=False):
    x = np.asarray(inputs["x"], dtype=np.float32)
    sh = _prep_shared(inputs)
    in_maps = []
    for c in range(8):
        m = dict(sh)
        m.update(_prep_core(x, c))
        in_maps.append(m)
    if stop_after not in _NC_CACHE:
        _NC_CACHE[stop_after] = build_program(stop_after)
    nc = _NC_CACHE[stop_after]
    res = run_bass_kernel_spmd(nc, in_maps, core_ids=list(range(8)), **({"trace": True} if trace else {}))
    out = np.zeros((2, 8192, D), np.float32)
    for c in range(8):
        b, q = c // 4, c % 4
        out[b, q * 2048:(q + 1) * 2048] = res.results[c]["y"]
    return out, res


def kernel(**inputs):
    out, _ = run(inputs)
    return out
```

```python
import numpy as np
from contextlib import ExitStack
import concourse.bass as bass
import concourse.mybir as mybir
from concourse.bass_utils import run_bass_kernel_spmd

F32 = mybir.dt.float32
BF16 = mybir.dt.bfloat16
I32 = mybir.dt.int32
AF = mybir.ActivationFunctionType
ALU = mybir.AluOpType
AX = mybir.AxisListType

PE, ACT, DVE, POOL, SP = "pe", "act", "dve", "pool", "sp"
ENGS = (PE, ACT, DVE, POOL, SP)

D = 1024
NT = 17
D_IN = 1792
D_FF = 2816
D_FFE = 3584
NE = 8
EPS = 1e-6
NPP = 120
PP_PSCALE = 0
PP_DWW = 2
PP_DWB = 95
PP_LNG = 98
PP_LNB = 101
PP_PWB = 104
PP_INVW = 107
PP_G2 = 109
TGM = 2
GF = 4
GFS = 2
CAPS = (512, 640, 768)
FORCE_CLASS = None


class Prog:
    def __init__(self, nc, stack):
        self.nc = nc
        self.stack = stack
        self.ops = {e: [] for e in ENGS}
        self.sems = {}
        self.cnt = {}
        self.seen = {e: {} for e in ENGS}
        for e in ENGS:
            self._mksem("eng_" + e)

    def _mksem(self, key):
        if key not in self.sems:
            self.sems[key] = self.stack.enter_context(self.nc.semaphore(key))
            self.cnt[key] = 0
        return self.sems[key]

    def _waits(self, eng, deps):
        out = []
        for d in deps:
            if d is None:
                continue
            key, val = d
            if eng == PE and key == "eng_pe":
                continue
            if self.seen[eng].get(key, 0) >= val:
                continue
            self.seen[eng][key] = val
            out.append((self.sems[key], val))
        return out

    def op(self, eng, fn, deps=(), inc=True):
        waits = self._waits(eng, deps)
        key = "eng_" + eng
        tok = None
        if inc:
            self.cnt[key] += 1
            tok = (key, self.cnt[key])
        self.ops[eng].append((waits, fn, (self.sems[key], 1) if inc else None))
        return tok

    def dma(self, eng, out, in_, semname, deps=()):
        self._mksem(semname)
        waits = self._waits(eng, deps)
        self.cnt[semname] += 16
        tok = (semname, self.cnt[semname])
        self.ops[eng].append(
            (waits, lambda e, o=out, i=in_: e.dma_start(out=o, in_=i), (self.sems[semname], 16))
        )
        return tok

    def wait_only(self, eng, deps):
        waits = self._waits(eng, deps)
        if waits:
            self.ops[eng].append((waits, None, None))

    def barrier(self):
        toks = [(k, v) for k, v in self.cnt.items() if v > 0]
        for e in ENGS:
            self.wait_only(e, toks)

    def cond_region(self, cond_ap, cond_deps, then_fn, else_fn):
        for e in ENGS:
            self.ops[e].append(("IF", cond_ap, self._waits(e, cond_deps)))
        snap_cnt = dict(self.cnt)
        snap_seen = {e: dict(d) for e, d in self.seen.items()}
        Buf.reset_all()
        then_fn()
        then_cnt = dict(self.cnt)
        then_end = {e: len(self.ops[e]) for e in ENGS}
        self.cnt = dict(snap_cnt)
        for kk in then_cnt:
            self.cnt.setdefault(kk, 0)
        self.seen = {e: dict(d) for e, d in snap_seen.items()}
        for e in ENGS:
            self.ops[e].append(("ELSE",))
        Buf.reset_all()
        else_fn()
        else_cnt = dict(self.cnt)
        keys = set(then_cnt) | set(else_cnt)
        final = {kk: max(then_cnt.get(kk, 0), else_cnt.get(kk, 0)) for kk in keys}

        def equalizers(branch_cnt):
            per_eng = {e: [] for e in ENGS}
            for kk in sorted(keys):
                diff = final[kk] - branch_cnt.get(kk, 0)
                if diff <= 0:
                    continue
                eng = kk[4:] if kk.startswith("eng_") else SP
                per_eng[eng].append(("EQ", self.sems[kk], branch_cnt.get(kk, 0), diff))
            return per_eng

        eq_then = equalizers(then_cnt)
        eq_else = equalizers(else_cnt)
        for e in ENGS:
            self.ops[e][then_end[e]:then_end[e]] = eq_then[e]
            self.ops[e].extend(eq_else[e])
            self.ops[e].append(("ENDIF",))
        self.cnt = final
        self.seen = snap_seen
        Buf.reset_all()

    def flush(self):
        nc = self.nc
        ops = self.ops

        def run(e, lst):
            cms = []
            for item in lst:
                tag = item[0]
                if tag == "IF":
                    for s_, v in item[2]:
                        e.wait_ge(s_, v)
                    val = e.value_load(item[1])
                    cm = e.If(val == 1)
                    cm.__enter__()
                    cms.append(cm)
                elif tag == "ELSE":
                    cms.pop().__exit__(None, None, None)
                    cm = e.Else()
                    cm.__enter__()
                    cms.append(cm)
                elif tag == "ENDIF":
                    cms.pop().__exit__(None, None, None)
                elif tag == "EQ":
                    _, sem, have, diff = item
                    if have > 0:
                        e.wait_ge(sem, have)
                    e.sem_inc(sem, diff)
                else:
                    waits, fn, inc = item
                    for s_, v in waits:
                        e.wait_ge(s_, v)
                    if fn is not None:
                        ins = fn(e)
                        if inc is not None:
                            ins.then_inc(inc[0], inc[1])

        with nc.Block() as block:
            @block.tensor
            def _(e):
                run(e, ops[PE])

            @block.scalar
            def _(e):
                run(e, ops[ACT])

            @block.vector
            def _(e):
                run(e, ops[DVE])

            @block.gpsimd
            def _(e):
                run(e, ops[POOL])

            @block.sync
            def _(e):
                run(e, ops[SP])
        self.ops = {e: [] for e in ENGS}


class Buf:
    ALL = []

    def __init__(self, name=""):
        self.name = name
        self.w = None
        self.r = {}
        Buf.ALL.append(self)

    @staticmethod
    def reset_all():
        for b in Buf.ALL:
            b.w = None
            b.r = {}

    def add_read(self, tok):
        if tok is None:
            return
        k, v = tok
        if self.r.get(k, 0) < v:
            self.r[k] = v

    def set_write(self, tok):
        self.w = tok
        self.r = {}


class K:
    def __init__(self, nc, stack):
        self.nc = nc
        self.p = Prog(nc, stack)
        self.dma_n = 0

    def deps_of(self, reads, writes):
        deps = []
        for b in reads:
            deps.append(b.w)
        for b in writes:
            deps.append(b.w)
            deps.extend(b.r.items())
        return deps

    def emit(self, eng, fn, reads=(), writes=()):
        tok = self.p.op(eng, fn, self.deps_of(reads, writes), True)
        for b in reads:
            b.add_read(tok)
        for b in writes:
            b.set_write(tok)
        return tok

    def dma(self, eng, out, in_, reads=(), writes=(), sem=None):
        assert sem is not None
        deps = []
        for b in reads:
            deps.append(b.w)
        for b in writes:
            if not (b.w is not None and b.w[0] == sem):
                deps.append(b.w)
            deps.extend(b.r.items())
        tok = self.p.dma(eng, out, in_, sem, deps)
        for b in reads:
            b.add_read(tok)
        for b in writes:
            b.set_write(tok)
        return tok

    def mm_group(self, out, pairs, reads, bank, transpose=False):
        deps = self.deps_of(reads, [bank])
        n = len(pairs)
        tok = None
        for i, (l, r) in enumerate(pairs):
            last = i == n - 1
            if transpose:
                fn = (lambda e, o=out[i], a=l, b=r: e.transpose(o, a, b))
            else:
                fn = (lambda e, o=out, a=l, b=r, s=(i == 0), t=last: e.matmul(o, lhsT=a, rhs=b, start=s, stop=t))
            tok = self.p.op(PE, fn, deps if i == 0 else (), last)
        for b in reads:
            b.add_read(tok)
        bank.set_write(tok)
        return tok

    def mm_multi(self, groups, reads, bank):
        deps = self.deps_of(reads, [bank])
        n = len(groups)
        tok = None
        for i, (o, l, r) in enumerate(groups):
            last = i == n - 1
            fn = (lambda e, o=o, a=l, b=r: e.matmul(o, lhsT=a, rhs=b, start=True, stop=True))
            tok = self.p.op(PE, fn, deps if i == 0 else (), last)
        for b in reads:
            b.add_read(tok)
        bank.set_write(tok)
        return tok

    def act(self, out, in_, func, reads, writes, bias=None, scale=None, accum=None):
        kw = {}
        if bias is not None:
            kw["bias"] = bias
        if scale is not None:
            kw["scale"] = scale
        if accum is not None:
            kw["accum_out"] = accum
        return self.emit(ACT, lambda e: e.activation(out=out, in_=in_, func=func, **kw), reads, writes)

    def tt(self, out, in0, in1, op, reads, writes, eng=DVE):
        return self.emit(eng, lambda e: e.tensor_tensor(out=out, in0=in0, in1=in1, op=op), reads, writes)

    def ts(self, out, in0, s1, s2, op0, op1, reads, writes, eng=DVE):
        return self.emit(eng, lambda e: e.tensor_scalar(out=out, in0=in0, scalar1=s1, scalar2=s2, op0=op0, op1=op1),
                         reads, writes)

    def ts1(self, out, in0, s1, op0, reads, writes, eng=DVE):
        return self.emit(eng, lambda e: e.tensor_scalar(out=out, in0=in0, scalar1=s1, scalar2=None, op0=op0),
                         reads, writes)

    def stt(self, out, in0, scalar, in1, op0, op1, reads, writes, accum=None, eng=DVE):
        kw = {}
        if accum is not None:
            kw["accum_out"] = accum
        return self.emit(eng, lambda e: e.scalar_tensor_tensor(out=out, in0=in0, scalar=scalar, in1=in1,
                                                               op0=op0, op1=op1, **kw), reads, writes)

    def copy(self, out, in_, reads, writes, eng=DVE):
        return self.emit(eng, lambda e: e.tensor_copy(out=out, in_=in_), reads, writes)

    def recip(self, out, in_, reads, writes):
        return self.emit(DVE, lambda e: e.reciprocal(out=out, in_=in_), reads, writes)

    def memset(self, ap, val, writes, eng=DVE):
        return self.emit(eng, lambda e: e.memset(ap, val), (), writes)


def build_program(stop_after=None):
    nc = bass.Bass("TRN2", target_bir_lowering=False)

    def din(name, shape):
        return nc.dram_tensor(name, list(shape), F32, kind="ExternalInput").ap()

    x_d = din("x", [NT * 128, D])
    flag_d = din("flag", [128, 1])
    pinv_d = din("pool_inv", [128, 32])
    ident_d = din("ident", [128, 128])
    ustrict_d = din("ustrict", [128, 128])
    iota_d = din("iota", [128, CAPS[-1]])
    pp_d = din("pp", [128, 2 * NPP])
    n1g_d = din("norm1_g", [2, D])
    n2g_d = din("norm2_g", [2, D])
    fg_d = din("final_g", [1, D])
    gmg_d = din("gm_norm_g", [2, 384])
    gmb_d = din("gm_b", [2, 512])
    w_in_d = din("w_in", [2, D, D_IN])
    pool_w_d = din("pool_w", [2, 4, 64, 64])
    wsT_d = din("gm_wsT", [2, 4, 128, 128])
    pw_d = din("conv_pw_w", [2, 384, 384])
    w_out_d = din("w_out", [2, D, D])
    fwg_d = din("ffn_wg", [D, D_FF])
    fwu_d = din("ffn_wu", [D, D_FF])
    fwd_d = din("ffn_wd", [D_FF, D])
    router_d = din("router", [D, NE])
    mwg_d = din("moe_wg", [NE, D, D_FFE])
    mwu_d = din("moe_wu", [NE, D, D_FFE])
    mwd_d = din("moe_wd", [NE, D_FFE, D])
    y_d = nc.dram_tensor("y", [16 * 128, D], F32, kind="ExternalOutput").ap()

    with ExitStack() as st:
        ARENA_WORDS = 53100
        arena = st.enter_context(nc.sbuf_tensor("arena", [128, ARENA_WORDS], F32))
        atop = [0]
        scopes = {}

        def _release(mark):
            atop[0] = mark

        def sb(stack, name, shape, dt):
            if stack is not st and not getattr(stack, "_arena_marked", False):
                stack._arena_marked = True
                stack.callback(_release, atop[0])
            n = 1
            for d_ in shape[1:]:
                n *= d_
            nbytes = n * (4 if dt in (F32, I32) else 2)
            words = ((nbytes + 3) // 4 + 7) // 8 * 8
            assert atop[0] + words <= ARENA_WORDS, ("SBUF arena overflow", name, atop[0], words)
            v = arena[:, atop[0]:atop[0] + words]
            atop[0] += words
            if dt != F32:
                v = v.bitcast(dt)
            v = v[:, 0:n]
            if len(shape) == 3:
                v = v.rearrange("p (a b) -> p a b", a=shape[1])
            return v

        k = K(nc, st)
        p = k.p

        x_tok = sb(st, "x_tok", [128, NT, D], F32)
        xb = [Buf("x%d" % n) for n in range(NT)]
        ident = sb(st, "ident", [128, 128], BF16)
        ones32 = sb(st, "ones32", [128, 128], F32)
        ustrict = sb(st, "ustrict", [128, 128], F32)
        ident32 = sb(st, "ident32", [128, 128], F32)
        iota = sb(st, "iota", [128, CAPS[-1]], F32)
        pp = sb(st, "pp", [128, 2 * NPP], F32)
        flag = sb(st, "flag", [128, 1], F32)
        pinv = sb(st, "pinv", [128, 2, 16], F32)
        stt_t = sb(st, "stats", [128, 8, 4], F32)
        gate = sb(st, "gate", [128, NT, NE], F32)
        cb = Buf("consts")
        stb = [Buf("st%d" % i) for i in range(8)]
        gateb = [Buf("gate%d" % n) for n in range(NT)]
        banks = [st.enter_context(nc.psum_tensor("bank%d" % i, [128, 512], F32)) for i in range(8)]
        bankb = [Buf("bank%d" % i) for i in range(8)]
        ring = [0]
        stat_i = [0]

        def next_bank():
            i = ring[0]
            ring[0] = (i + 1) % 8
            return banks[i], bankb[i]

        def next_stat():
            i = stat_i[0]
            stat_i[0] = (i + 1) % 8
            return stt_t[:, i, :], stb[i]

        def ppc(l, col, n=1, lo=0, hi=128):
            return pp[lo:hi, l * NPP + col: l * NPP + col + n]

        k.memset(ones32[:], 1.0, [cb])
        k.dma(POOL, ident[:], ident_d, writes=[cb], sem="consts")
        k.dma(POOL, pp[:], pp_d, writes=[cb], sem="consts")
        k.dma(POOL, ustrict[:], ustrict_d, writes=[cb], sem="consts")
        k.dma(POOL, ident32[:], ident_d, writes=[cb], sem="consts")
        k.dma(POOL, iota[:], iota_d, writes=[cb], sem="consts")
        k.dma(POOL, flag[:], flag_d, writes=[cb], sem="consts")
        k.dma(POOL, pinv[:].rearrange("p c j -> p (c j)"), pinv_d, writes=[cb], sem="consts")
        for n in range(NT):
            k.dma(SP, x_tok[:, n, :], x_d[n * 128:(n + 1) * 128, :], writes=[xb[n]], sem="x%d" % n)

        def rms_stats(src_ap, srcb, junk, junkb, scale):
            sap, sbf = next_stat()
            k.act(junk, src_ap, AF.Square, [srcb], [junkb, sbf], scale=scale, accum=sap[:, 0:1])
            k.act(sap[:, 1:2], sap[:, 0:1], AF.Sqrt, [sbf], [sbf], bias=EPS)
            k.recip(sap[:, 2:3], sap[:, 1:2], [sbf], [sbf])
            return sap[:, 2:3], sbf

        def norm_and_transpose(n, g_bc, gb, h_tok, h_tokb, junk, junkb, hT_dst, hTb):
            rstd, sbf = rms_stats(x_tok[:, n, :], xb[n], junk, junkb, 1.0 / 32.0)
            k.stt(h_tok, x_tok[:, n, :], rstd, g_bc, ALU.mult, ALU.mult, [xb[n], sbf, gb], [h_tokb])
            bk, bkb = next_bank()
            bkbf = bk.bitcast(BF16)
            outs = [bkbf[:, kc * 128:(kc + 1) * 128] for kc in range(8)]
            pairs = [(h_tok[:, kc * 128:(kc + 1) * 128], ident[:, :]) for kc in range(8)]
            k.mm_group(outs, pairs, [h_tokb, cb], bkb, transpose=True)
            k.act(hT_dst, bkbf[:, :].rearrange("p (k t) -> p k t", k=8), AF.Copy, [bkb], [hTb])
            return rstd, sbf

        def mixer_phase(l):
            with ExitStack() as ms:
                NMAX = TGM * 128
                LMAX = 32 + NMAX
                w_in_sb = sb(ms, "w_in_sb", [128, 8, D_IN], BF16)
                wo_a = sb(ms, "wo_a", [128, 2, D], BF16)
                wo_b = sb(ms, "wo_b", [128, 4, D], BF16)
                wo_c = sb(ms, "wo_c", [128, 3, D], BF16)
                pw_sb = sb(ms, "pw_sb", [128, 3, 384], BF16)
                wblk = sb(ms, "wblk", [128, 2, 128], BF16)
                wsT = sb(ms, "wsT", [128, 4, 128], BF16)
                g1_bc = sb(ms, "g1_bc", [128, D], F32)
                gmg_bc = sb(ms, "gmg_bc", [128, 384], F32)
                gmb_bc = sb(ms, "gmb_bc", [128, 512], F32)
                wb = Buf("mixw")
                h_tok = [sb(ms, "h_tok%d" % i, [128, D], BF16) for i in range(2)]
                h_tokb = [Buf(), Buf()]
                junk = sb(ms, "junk", [128, D], BF16)
                junkb = Buf()
                two = range(2)
                a_ext = [sb(ms, "a_ext%d" % i, [128, 2, LMAX], F32) for i in two]
                hc_ext = [sb(ms, "hc_ext%d" % i, [128, 3, LMAX], BF16) for i in two]
                yb = [sb(ms, "yb%d" % i, [128, 4, NMAX], BF16) for i in two]
                ab, hcb, ybb = ([Buf(), Buf()] for _ in range(3))

                def same2(name, shape, dt):
                    t_ = sb(ms, name, shape, dt)
                    return [t_, t_]

                def same2b():
                    b_ = Buf()
                    return [b_, b_]

                hT = same2("hT", [128, 8, NMAX], BF16)
                sig = same2("sig", [128, 3, NMAX], F32)
                acc = same2("acc", [128, 3, NMAX], F32)
                u_sb = same2("u_sb", [128, 4, NMAX], F32)
                y_p = same2("y_p", [128, 2, NMAX], BF16)
                ya = same2("ya", [128, 2, NMAX], BF16)
                hs = same2("hs", [128, 3, NMAX], BF16)
                yc = same2("yc", [128, 3, NMAX], BF16)
                hTb, sigb, ub, ypb, yab, hsb, ycb = (same2b() for _ in range(7))
                accb1 = [Buf(), Buf(), Buf()]
                accb = [accb1, accb1]
                dg = sb(ms, "dg", [128, 93, 128], BF16)
                dgb = Buf()
                sA = sb(ms, "sA", [128, 2, LMAX], F32)
                sB = sb(ms, "sB", [128, 2, LMAX], F32)
                tmp16 = sb(ms, "tmp16", [128, 16], F32)
                v_n = [sb(ms, "v_n%d" % i, [128, 384], BF16) for i in two]
                ztmp = sb(ms, "ztmp", [128, 512], F32)
                sq = sb(ms, "sq", [128, 3, NMAX], F32)
                mean = sb(ms, "mean", [128, NMAX], F32)
                var = sb(ms, "var", [128, NMAX], F32)
                rstdc = sb(ms, "rstdc", [128, NMAX], F32)
                sAb, sBb, t16b, ztb, sqb, meanb, varb, rsb = (Buf() for _ in range(8))
                vnb = [Buf(), Buf()]

                wblkb = Buf()
                wsTb = Buf()
                k.memset(wblk[:], 0.0, [wblkb])
                for c in range(2):
                    k.dma(POOL, wblk[0:64, c, 0:64], pool_w_d[l, 2 * c], writes=[wblkb], sem="wblk")
                    k.dma(POOL, wblk[64:128, c, 64:128], pool_w_d[l, 2 * c + 1], writes=[wblkb], sem="wblk")
                k.dma(POOL, wsT[:], wsT_d[l].rearrange("h j i -> j h i"), writes=[wsTb], sem="wsT")
                k.memset(wsT[64:128, :, 0:64], 0.0, [wsTb])
                k.dma(SP, g1_bc[:], n1g_d[l:l + 1, :].partition_broadcast(128), writes=[wb], sem="mixw")
                k.dma(SP, gmg_bc[:], gmg_d[l:l + 1, :].partition_broadcast(128), writes=[wb], sem="mixw")
                k.dma(SP, gmb_bc[:], gmb_d[l:l + 1, :].partition_broadcast(128), writes=[wb], sem="mixw")
                k.dma(POOL, w_in_sb[:], w_in_d[l].rearrange("(kc p) n -> p kc n", p=128), writes=[wb], sem="mixw")
                k.dma(POOL, wo_a[:], w_out_d[l, 0:256, :].rearrange("(c p) n -> p c n", p=128), writes=[wb], sem="mixw")
                k.dma(POOL, wo_b[0:96], w_out_d[l, 256:640, :].rearrange("(h p) n -> p h n", p=96), writes=[wb], sem="mixw")
                k.dma(POOL, wo_c[:], w_out_d[l, 640:1024, :].rearrange("(c p) n -> p c n", p=128), writes=[wb], sem="mixw")
                k.dma(POOL, pw_sb[:], pw_d[l].rearrange("(c p) n -> p c n", p=128), writes=[wb], sem="mixw")
                k.memset(a_ext[0][:, :, 0:32], 0.0, [ab[0]])
                k.memset(hc_ext[0][:, :, 0:32], 0.0, [hcb[0]])
                for i in range(93):
                    k.ts1(dg[:, i, :], ident[:, :], ppc(l, PP_DWW + i), ALU.mult, [cb], [dgb])

                groups = [(0, 1)] + [(t0, TGM) for t0 in range(1, NT, TGM)]

                def stage_ne(gi):
                    t0, nt = groups[gi]
                    for j in range(nt):
                        n = t0 + j
                        hb = (gi * TGM + j) % 2
                        rstd, sbf = rms_stats(x_tok[:, n, :], xb[n], junk[:], junkb, 1.0 / 32.0)
                        k.stt(h_tok[hb][:], x_tok[:, n, :], rstd, g1_bc[:], ALU.mult, ALU.mult,
                              [xb[n], sbf, wb], [h_tokb[hb]])

                def stage_nt(gi):
                    t0, nt = groups[gi]
                    q = gi % 2
                    for j in range(nt):
                        hb = (gi * TGM + j) % 2
                        bk, bkb = next_bank()
                        bkbf = bk.bitcast(BF16)
                        outs = [bkbf[:, kc * 128:(kc + 1) * 128] for kc in range(8)]
                        pairs = [(h_tok[hb][:, kc * 128:(kc + 1) * 128], ident[:, :]) for kc in range(8)]
                        k.mm_group(outs, pairs, [h_tokb[hb], cb], bkb, transpose=True)
                        k.act(hT[q][:, :, j * 128:(j + 1) * 128], bkbf[:, :].rearrange("p (k t) -> p k t", k=8),
                              AF.Copy, [bkb], [hTb[q]])

                def stage_p(gi):
                    t0, nt = groups[gi]
                    q = gi % 2
                    N = nt * 128
                    L = 32 + N
                    full = not (l == 1 and t0 == 0)
                    first_own = (t0 == 1)
                    if gi > 0:
                        pN = groups[gi - 1][1] * 128
                        k.copy(a_ext[q][:, :, 0:32], a_ext[1 - q][:, :, pN:pN + 32], [ab[1 - q]], [ab[q]], eng=POOL)
                        k.copy(hc_ext[q][:, :, 0:32], hc_ext[1 - q][:, :, pN:pN + 32], [hcb[1 - q]], [hcb[q]], eng=POOL)

                    def proj(col0, m):
                        bk, bkb = next_bank()
                        pairs = [(w_in_sb[:, kc, col0:col0 + m], hT[q][:, kc, 0:N]) for kc in range(8)]
                        k.mm_group(bk[0:m, 0:N], pairs, [wb, hTb[q]], bkb)
                        return bk, bkb

                    for c in range(2):
                        bk, bkb = proj(c * 128, 128)
                        k.act(a_ext[q][:, c, 32:L], bk[:, 0:N], AF.Copy, [bkb], [ab[q]])
                    vinfo = []
                    if full:
                        for j in range(nt):
                            vb = j % 2
                            bk, bkb = next_bank()
                            pairs = [(hT[q][:, kc, j * 128:(j + 1) * 128], w_in_sb[:, kc, 640:1024]) for kc in range(8)]
                            k.mm_group(bk[:, 0:384], pairs, [wb, hTb[q]], bkb)
                            rstd, sbf = rms_stats(bk[:, 0:384], bkb, junk[:, 0:384], junkb, float(384.0 ** -0.5))
                            k.stt(v_n[vb][:], bk[:, 0:384], rstd, gmg_bc[:], ALU.mult, ALU.mult,
                                  [bkb, sbf, wb], [vnb[vb]])
                    for c in range(3):
                        bk, bkb = proj(1408 + c * 128, 128)
                        k.act(sig[q][:, c, 0:N], bk[:, 0:N], AF.Sigmoid, [bkb], [sigb[q]])
                    for c in range(3):
                        bk, bkb = proj(1024 + c * 128, 128)
                        k.tt(hc_ext[q][:, c, 32:L], bk[:, 0:N], sig[q][:, c, 0:N], ALU.mult, [bkb, sigb[q]], [hcb[q]])
                    if full:
                        for h in range(4):
                            bk, bkb = proj(256 + h * 96, 96)
                            k.act(u_sb[q][0:96, h, 0:N], bk[0:96, 0:N], AF.Copy, [bkb], [ub[q]])
                        for j in range(nt):
                            vb = j % 2
                            zk, zkb = next_bank()
                            grp = [(zk[0:96, h * 128:(h + 1) * 128], v_n[vb][:, h * 96:(h + 1) * 96], wsT[:, h, :])
                                   for h in range(4)]
                            k.mm_multi(grp, [vnb[vb], wsTb], zkb)
                            k.tt(ztmp[0:96, :], zk[0:96, :], gmb_bc[0:96, :], ALU.add, [zkb, wb], [ztb])
                            k.tt(yb[q][0:96, :, j * 128:(j + 1) * 128],
                                 ztmp[0:96, :].rearrange("p (h i) -> p h i", h=4),
                                 u_sb[q][0:96, :, j * 128:(j + 1) * 128], ALU.mult, [ztb, ub[q]], [ybb[q]])
                        A_ = a_ext[q]

                        def pool_out(sbuf_t, sbuf_b, lo, hi, c):
                            k.stt(y_p[q][lo:hi, c, 0:N], sbuf_t[lo:hi, c, 32:L], ppc(l, PP_INVW + c, 1, lo, hi),
                                  A_[lo:hi, c, 32:L], ALU.mult, ALU.subtract, [sbuf_b, ab[q], cb], [ypb[q]])
                            if first_own:
                                k.tt(tmp16[lo:hi, :], sbuf_t[lo:hi, c, 32:48], pinv[lo:hi, c, :], ALU.mult,
                                     [sbuf_b, cb], [t16b])
                                k.tt(y_p[q][lo:hi, c, 0:16], tmp16[lo:hi, :], A_[lo:hi, c, 32:48], ALU.subtract,
                                     [t16b, ab[q]], [ypb[q]])

                        k.tt(sA[:, :, 1:L], A_[:, :, 1:L], A_[:, :, 0:L - 1], ALU.add, [ab[q]], [sAb])
                        pool_out(sA, sAb, 0, 64, 0)
                        k.tt(sB[:, :, 3:L], sA[:, :, 3:L], sA[:, :, 1:L - 2], ALU.add, [sAb], [sBb])
                        pool_out(sB, sBb, 64, 128, 0)
                        k.tt(sA[:, :, 7:L], sB[:, :, 7:L], sB[:, :, 3:L - 4], ALU.add, [sBb], [sAb])
                        pool_out(sA, sAb, 0, 64, 1)
                        k.tt(sB[:, :, 15:L], sA[:, :, 15:L], sA[:, :, 7:L - 8], ALU.add, [sAb], [sBb])
                        pool_out(sB, sBb, 64, 128, 1)

                def stage_b1(gi):
                    t0, nt = groups[gi]
                    q = gi % 2
                    N = nt * 128
                    L = 32 + N
                    full = not (l == 1 and t0 == 0)
                    first_own = (t0 == 1)
                    if not full:
                        return
                    A_ = a_ext[q]
                    H_ = hc_ext[q]

                    for c in range(2):
                        bk, bkb = next_bank()
                        k.mm_group(bk[:, 0:N], [(wblk[:, c, :], y_p[q][:, c, 0:N])], [wblkb, ypb[q]], bkb)
                        k.act(ya[q][:, c, 0:N], bk[:, 0:N], AF.Copy, [bkb, cb], [yab[q]], scale=ppc(l, PP_PSCALE + c))

                    for c in range(3):
                        bk, bkb = next_bank()
                        pairs = [(dg[:, c * 31 + kk, :], H_[:, c, 2 + kk:2 + kk + N]) for kk in range(31)]
                        k.mm_group(bk[:, 0:N], pairs, [dgb, hcb[q]], bkb)
                        k.act(acc[q][:, c, 0:N], bk[:, 0:N], AF.Identity, [bkb, cb], [accb[q][c]],
                              bias=ppc(l, PP_DWB + c))
                    for c in range(3):
                        k.act(sq[:, c, 0:N], acc[q][:, c, 0:N], AF.Square, [accb[q][c]], [sqb])
                    b1, b1b = next_bank()
                    k.mm_group(b1[:, 0:N], [(ones32[:, :], acc[q][:, c, 0:N]) for c in range(3)], accb[q] + [cb], b1b)
                    b2, b2b = next_bank()
                    k.mm_group(b2[:, 0:N], [(ones32[:, :], sq[:, c, 0:N]) for c in range(3)], [sqb, cb], b2b)
                    k.ts(mean[:, 0:N], b1[:, 0:N], 1.0 / 384.0, 0.0, ALU.mult, ALU.add, [b1b], [meanb])
                    k.tt(var[:, 0:N], mean[:, 0:N], mean[:, 0:N], ALU.mult, [meanb], [varb])
                    k.stt(var[:, 0:N], b2[:, 0:N], 1.0 / 384.0, var[:, 0:N], ALU.mult, ALU.subtract,
                          [b2b], [varb])
                    k.act(var[:, 0:N], var[:, 0:N], AF.Sqrt, [], [varb], bias=EPS)
                    k.recip(rstdc[:, 0:N], var[:, 0:N], [varb], [rsb])
                    for c in range(3):
                        k.tt(sq[:, c, 0:N], acc[q][:, c, 0:N], mean[:, 0:N], ALU.subtract, [accb[q][c], meanb], [sqb])
                        k.tt(sq[:, c, 0:N], sq[:, c, 0:N], rstdc[:, 0:N], ALU.mult, [rsb], [sqb])
                        k.act(hs[q][:, c, 0:N], sq[:, c, 0:N], AF.Silu, [sqb, cb], [hsb[q]],
                              bias=ppc(l, PP_LNB + c), scale=ppc(l, PP_LNG + c))
                def stage_b2(gi):
                    t0, nt = groups[gi]
                    q = gi % 2
                    N = nt * 128
                    full = not (l == 1 and t0 == 0)
                    if not full:
                        return
                    for co in range(3):
                        bk, bkb = next_bank()
                        pairs = [(pw_sb[:, ci, co * 128:(co + 1) * 128], hs[q][:, ci, 0:N]) for ci in range(3)]
                        k.mm_group(bk[:, 0:N], pairs, [wb, hsb[q]], bkb)
                        k.act(yc[q][:, co, 0:N], bk[:, 0:N], AF.Identity, [bkb, cb], [ycb[q]], bias=ppc(l, PP_PWB + co))

                    for j in range(nt):
                        n = t0 + j
                        ts_ = slice(j * 128, (j + 1) * 128)
                        for hf in range(2):
                            cs = slice(hf * 512, (hf + 1) * 512)
                            pairs = [(ya[q][:, c, ts_], wo_a[:, c, cs]) for c in range(2)]
                            pairs += [(yb[q][0:96, h, ts_], wo_b[0:96, h, cs]) for h in range(4)]
                            pairs += [(yc[q][:, c, ts_], wo_c[:, c, cs]) for c in range(3)]
                            bk, bkb = next_bank()
                            k.mm_group(bk[:, :], pairs, [wb, yab[q], ybb[q], ycb[q]], bkb)
                            k.tt(x_tok[:, n, cs], x_tok[:, n, cs], bk[:, :], ALU.add, [bkb], [xb[n]])

                ng = len(groups)
                stage_ne(0)
                stage_nt(0)
                if ng > 1:
                    stage_ne(1)
                stage_p(0)
                for gi in range(ng):
                    if gi + 1 < ng:
                        stage_nt(gi + 1)
                    stage_b1(gi)
                    if gi + 2 < ng:
                        stage_ne(gi + 2)
                    if gi + 1 < ng:
                        stage_p(gi + 1)
                    stage_b2(gi)
                p.barrier()

        def ffn_stream(scope, moe, tiles, h2T, h2b, gf, experts=None):
            GW = gf * 128
            slots = []
            for s_ in range(2):
                slots.append((sb(scope, "wg%d" % s_, [128, 8, GW], BF16),
                              sb(scope, "wu%d" % s_, [128, 8, GW], BF16),
                              sb(scope, "wd%d" % s_, [128, gf, D], BF16), Buf()))
            actt = [sb(scope, "act%d" % i, [128, gf, 512], BF16) for i in range(2)]
            actb = [Buf(), Buf()]
            sg = [sb(scope, "sg%d" % i, [128, 512], F32) for i in range(2)]
            sgb = [Buf(), Buf()]
            glist = []
            if not moe:
                nch = D_FF // 128
                for c0 in range(0, nch, gf):
                    nf = min(gf, nch - c0)
                    glist.append((fwg_d[:, c0 * 128:(c0 + nf) * 128], fwu_d[:, c0 * 128:(c0 + nf) * 128],
                                  fwd_d[c0 * 128:(c0 + nf) * 128, :], nf, None))
            else:
                nch = D_FFE // 128
                for e in (experts if experts is not None else range(NE)):
                    for c0 in range(0, nch, gf):
                        nf = min(gf, nch - c0)
                        glist.append((mwg_d[e, :, c0 * 128:(c0 + nf) * 128],
                                      mwu_d[e, :, c0 * 128:(c0 + nf) * 128],
                                      mwd_d[e, c0 * 128:(c0 + nf) * 128, :], nf, e))
            tgroups = [tiles[i:i + 4] for i in range(0, len(tiles), 4)]
            cnt = 0
            sgi = 0
            for gi, (wg_ap, wu_ap, wd_ap, nf, e) in enumerate(glist):
                wg_s, wu_s, wd_s, wsb = slots[gi % 2]
                sem = "wslot%d" % (gi % 2)
                k.dma(POOL, wg_s[:, :, 0:nf * 128], wg_ap.rearrange("(kc p) n -> p kc n", p=128),
                      writes=[wsb], sem=sem)
                k.dma(POOL, wu_s[:, :, 0:nf * 128], wu_ap.rearrange("(kc p) n -> p kc n", p=128),
                      writes=[wsb], sem=sem)
                k.dma(POOL, wd_s[:, 0:nf, :], wd_ap.rearrange("(f p) n -> p f n", p=128),
                      writes=[wsb], sem=sem)
                for tg in tgroups:
                    ab_i = cnt % 2
                    cnt += 1
                    t_lo = tg[0] * 128
                    N = len(tg) * 128
                    for f in range(nf):
                        bg, bgb = next_bank()
                        k.mm_group(bg[:, 0:N], [(wg_s[:, kc, f * 128:(f + 1) * 128], h2T[:, kc, t_lo:t_lo + N])
                                                for kc in range(8)], [wsb] + [h2b[n] for n in tg], bgb)
                        bu, bub = next_bank()
                        k.mm_group(bu[:, 0:N], [(wu_s[:, kc, f * 128:(f + 1) * 128], h2T[:, kc, t_lo:t_lo + N])
                                                for kc in range(8)], [wsb] + [h2b[n] for n in tg], bub)
                        si = sgi % 2
                        sgi += 1
                        k.act(sg[si][:, 0:N], bg[:, 0:N], AF.Silu, [bgb], [sgb[si]])
                        k.tt(actt[ab_i][:, f, 0:N], sg[si][:, 0:N], bu[:, 0:N], ALU.mult, [sgb[si], bub],
                             [actb[ab_i]])
                    for j, n in enumerate(tg):
                        for hf in range(2):
                            cs = slice(hf * 512, (hf + 1) * 512)
                            bk, bkb = next_bank()
                            k.mm_group(bk[:, :], [(actt[ab_i][:, f, j * 128:(j + 1) * 128], wd_s[:, f, cs])
                                                  for f in range(nf)], [wsb, actb[ab_i]], bkb)
                            if moe:
                                k.stt(x_tok[:, n, cs], bk[:, :], gate[:, n, e:e + 1], x_tok[:, n, cs],
                                      ALU.mult, ALU.add, [bkb, gateb[n]], [xb[n]])
                            else:
                                k.tt(x_tok[:, n, cs], x_tok[:, n, cs], bk[:, :], ALU.add, [bkb], [xb[n]])

        def ffn_phase0():
            tiles = list(range(0, NT))
            with ExitStack() as fs:
                h2T = sb(fs, "h2T", [128, 8, NT * 128], BF16)
                h2b = [Buf() for _ in range(NT)]
                g2_bc = sb(fs, "g2_bc", [128, D], F32)
                gb = Buf()
                h_tok = [sb(fs, "h_tokf%d" % i, [128, D], BF16) for i in range(2)]
                h_tokb = [Buf(), Buf()]
                junk = sb(fs, "junkf", [128, D], BF16)
                junkb = Buf()
                k.dma(SP, g2_bc[:], n2g_d[0:1, :].partition_broadcast(128), writes=[gb], sem="g2")
                for ti, n in enumerate(tiles):
                    hb = ti % 2
                    norm_and_transpose(n, g2_bc[:], gb, h_tok[hb][:], h_tokb[hb], junk[:], junkb,
                                       h2T[:, :, n * 128:(n + 1) * 128], h2b[n])
                ffn_stream(fs, False, tiles, h2T, h2b, GF)
                p.barrier()

        def moe_phase():
            tiles = list(range(1, NT))
            with ExitStack() as fs:
                h_all = sb(fs, "h_all", [128, 16, D], BF16)
                hab = [Buf() for _ in range(16)]
                sel = sb(fs, "sel", [128, 16, NE], F32)
                pos = sb(fs, "pos", [128, 16, NE], F32)
                tot = sb(fs, "tot", [128, 16, NE], F32)
                offs = sb(fs, "offs", [128, 16, NE], F32)
                misc = sb(fs, "rmisc", [128, 40], F32)
                cond_i = sb(fs, "cond_i", [128, 32], I32)
                selb, posb, totb, offb, miscb, condb = (Buf() for _ in range(6))
                with ExitStack() as f1:
                    g2_bc = sb(f1, "g2_bc", [128, D], F32)
                    gb = Buf()
                    junk = sb(f1, "junkf", [128, D], BF16)
                    junkb = Buf()
                    rk = sb(f1, "rk", [128, 8, NE], F32)
                    rkb = Buf()
                    xT32 = [sb(f1, "xT32_%d" % i, [128, 8, 128], F32) for i in range(2)]
                    xTb = [Buf(), Buf()]
                    lg = sb(f1, "lg", [128, 2, 32], F32)
                    lgb = [Buf(), Buf()]
                    k.dma(SP, g2_bc[:], n2g_d[1:2, :].partition_broadcast(128), writes=[gb], sem="g2")
                    k.dma(SP, rk[:], router_d.rearrange("(kc p) e -> p kc e", p=128), writes=[rkb], sem="rg")
                    for kc in range(8):
                        k.ts1(rk[:, kc, :], rk[:, kc, :], ppc(1, PP_G2 + kc), ALU.mult, [cb], [rkb])
                    for ti, n in enumerate(tiles):
                        T = n - 1
                        hb = ti % 2
                        rstd, sbf = rms_stats(x_tok[:, n, :], xb[n], junk[:], junkb, 1.0 / 32.0)
                        k.stt(h_all[:, T, :], x_tok[:, n, :], rstd, g2_bc[:], ALU.mult, ALU.mult,
                              [xb[n], sbf, gb], [hab[T]])
                        L_ = lg[:, hb, :]
                        lb = lgb[hb]
                        for half in range(2):
                            bk, bkb = next_bank()
                            outs = [bk[:, i * 128:(i + 1) * 128] for i in range(4)]
                            pairs = [(x_tok[:, n, (4 * half + i) * 128:(4 * half + i + 1) * 128], ident32[:, :])
                                     for i in range(4)]
                            k.mm_group(outs, pairs, [xb[n], cb], bkb, transpose=True)
                            k.act(xT32[hb][:, 4 * half:4 * half + 4, :], bk[:, :].rearrange("p (a b) -> p a b", a=4),
                                  AF.Copy, [bkb], [xTb[hb]])
                        bl, blb = next_bank()
                        k.mm_group(bl[:, 0:NE], [(xT32[hb][:, kc, :], rk[:, kc, :]) for kc in range(8)],
                                   [xTb[hb], rkb], blb)
                        k.ts1(L_[:, 0:8], bl[:, 0:NE], rstd, ALU.mult, [blb, sbf], [lb])
                        k.emit(DVE, lambda e_, o=L_[:, 24:25], i=L_[:, 0:8]: e_.reduce_max(out=o, in_=i, axis=AX.X),
                               [], [lb])
                        k.ts1(L_[:, 8:16], L_[:, 0:8], L_[:, 24:25], ALU.is_equal, [], [lb])
                        k.stt(L_[:, 16:24], L_[:, 8:16], -1e30, L_[:, 0:8], ALU.mult, ALU.add, [], [lb])
                        k.emit(DVE, lambda e_, o=L_[:, 25:26], i=L_[:, 16:24]: e_.reduce_max(out=o, in_=i, axis=AX.X),
                               [], [lb])
                        k.ts1(L_[:, 16:24], L_[:, 16:24], L_[:, 25:26], ALU.is_equal, [], [lb])
                        k.tt(sel[:, T, :], L_[:, 8:16], L_[:, 16:24], ALU.add, [lb], [selb])
                        k.tt(L_[:, 26:27], L_[:, 25:26], L_[:, 24:25], ALU.subtract, [], [lb])
                        k.act(L_[:, 26:27], L_[:, 26:27], AF.Exp, [], [lb])
                        k.ts(L_[:, 27:28], L_[:, 26:27], 1.0, 0.0, ALU.add, ALU.add, [], [lb])
                        k.recip(L_[:, 27:28], L_[:, 27:28], [], [lb])
                        k.tt(L_[:, 28:29], L_[:, 26:27], L_[:, 27:28], ALU.mult, [], [lb])
                        k.ts1(L_[:, 8:16], L_[:, 8:16], L_[:, 27:28], ALU.mult, [], [lb])
                        k.stt(gate[:, n, :], L_[:, 16:24], L_[:, 28:29], L_[:, 8:16], ALU.mult, ALU.add,
                              [lb], [gateb[n]])
                    selv = sel[:, :, :].rearrange("p t e -> p (t e)")
                    b1, b1b = next_bank()
                    k.mm_group(b1[:, 0:128], [(ustrict[:, :], selv)], [selb, cb], b1b)
                    k.copy(pos[:, :, :].rearrange("p t e -> p (t e)"), b1[:, 0:128], [b1b], [posb])
                    b2, b2b = next_bank()
                    k.mm_group(b2[:, 0:128], [(ones32[:, :], selv)], [selb, cb], b2b)
                    k.copy(tot[:, :, :].rearrange("p t e -> p (t e)"), b2[:, 0:128], [b2b], [totb])
                    k.memset(offs[:, 0, :], 0.0, [offb])
                    for T in range(1, 16):
                        k.tt(offs[:, T, :], offs[:, T - 1, :], tot[:, T - 1, :], ALU.add, [totb], [offb])
                    k.tt(pos[:, :, :], pos[:, :, :], offs[:, :, :], ALU.add, [offb], [posb])
                    k.tt(misc[:, 0:8], offs[:, 15, :], tot[:, 15, :], ALU.add, [offb, totb], [miscb])
                    for c, cap in enumerate(CAPS):
                        k.ts(misc[:, 8 + 8 * c:16 + 8 * c], misc[:, 0:8], float(cap) + 0.5, 0.0, ALU.is_lt, ALU.add,
                             [], [miscb])
                    k.copy(cond_i[:, 0:8 * len(CAPS)], misc[:, 8:8 + 8 * len(CAPS)], [miscb], [condb])
                    if FORCE_CLASS is not None:
                        for c in range(len(CAPS)):
                            k.memset(cond_i[:, 8 * c:8 * c + 8], 1 if c >= FORCE_CLASS else 0, [condb])
                    p.barrier()

                def expert_sparse(e, cap):
                    NJ = cap // 128
                    chunks = [(0, 512)] + ([(512, cap - 512)] if cap > 512 else [])
                    with ExitStack() as sp_:
                        Pb = [sb(sp_, "P%d" % i, [128, cap], BF16) for i in range(4)]
                        Pbb = [Buf() for _ in range(4)]
                        pi = [0]
                        hTe = sb(sp_, "hTe", [128, 8, cap], BF16)
                        hTeb = Buf()
                        GW = GFS * 128
                        slots = []
                        for s_ in range(2):
                            slots.append((sb(sp_, "swg%d" % s_, [128, 8, GW], BF16),
                                          sb(sp_, "swu%d" % s_, [128, 8, GW], BF16),
                                          sb(sp_, "swd%d" % s_, [128, GFS, D], BF16), Buf(), Buf()))
                        actt = [sb(sp_, "sact%d" % i, [128, GFS, cap], BF16) for i in range(2)]
                        actb = [Buf(), Buf()]
                        sg = [sb(sp_, "ssg%d" % i, [128, cap], F32) for i in range(2)]
                        sgb = [Buf(), Buf()]
                        oe32 = sb(sp_, "oe32", [128, NJ, D], F32)
                        oe_bf = sb(sp_, "oe_bf", [128, NJ, D], BF16)
                        oeb = [[Buf(), Buf()] for _ in range(NJ)]
                        oebf_b = Buf()
                        PT = [sb(sp_, "PT%d" % i, [128, NJ, 128], BF16) for i in range(2)]
                        PTb = [Buf(), Buf()]

                        def build_P(T, c0, w):
                            i = pi[0] % 4
                            pi[0] += 1
                            k.ts(Pb[i][:, 0:w], iota[:, c0:c0 + w], pos[:, T, e:e + 1], sel[:, T, e:e + 1],
                                 ALU.is_equal, ALU.mult, [cb, posb, selb], [Pbb[i]])
                            return Pb[i], Pbb[i]

                        for (c0, w) in chunks:
                            per_bank = 512 // w
                            nb = 8 // per_bank
                            bks = [next_bank() for _ in range(nb)]
                            allb = [b for _, b in bks]
                            tok = None
                            for T in range(16):
                                Pt, Ptb = build_P(T, c0, w)
                                deps = [Ptb.w, hab[T].w]
                                if T == 0:
                                    deps += k.deps_of([], allb)
                                for kc in range(8):
                                    o = bks[kc // per_bank][0][:, (kc % per_bank) * w:(kc % per_bank + 1) * w]
                                    tok = p.op(PE, (lambda e_, o=o, a=h_all[:, T, kc * 128:(kc + 1) * 128], r=Pt[:, 0:w],
                                                    s_=(T == 0), t_=(T == 15): e_.matmul(o, lhsT=a, rhs=r, start=s_, stop=t_)),
                                               deps if kc == 0 else (), kc == 7)
                                Ptb.add_read(tok)
                                hab[T].add_read(tok)
                            for b in allb:
                                b.set_write(tok)
                            for bi, (bk, bkb) in enumerate(bks):
                                k.act(hTe[:, bi * per_bank:(bi + 1) * per_bank, c0:c0 + w],
                                      bk[:, 0:per_bank * w].rearrange("p (a b) -> p a b", a=per_bank), AF.Copy,
                                      [bkb], [hTeb])
                        nch = D_FFE // 128
                        ngrp = (nch + GFS - 1) // GFS
                        ginfo = {}
                        sgi_box = [0]

                        def ffn_s1(g_):
                            c0f = g_ * GFS
                            nf = min(GFS, nch - c0f)
                            wg_s, wu_s, wd_s, wsb, wdb = slots[g_ % 2]
                            k.dma(POOL, wg_s[:, :, 0:nf * 128],
                                  mwg_d[e, :, c0f * 128:(c0f + nf) * 128].rearrange("(kc p) n -> p kc n", p=128),
                                  writes=[wsb], sem="wsA%d" % (g_ % 2))
                            k.dma(POOL, wu_s[:, :, 0:nf * 128],
                                  mwu_d[e, :, c0f * 128:(c0f + nf) * 128].rearrange("(kc p) n -> p kc n", p=128),
                                  writes=[wsb], sem="wsA%d" % (g_ % 2))
                            k.dma(POOL, wd_s[:, 0:nf, :],
                                  mwd_d[e, c0f * 128:(c0f + nf) * 128, :].rearrange("(f p) n -> p f n", p=128),
                                  writes=[wdb], sem="wsB%d" % (g_ % 2))
                            ab_i = g_ % 2
                            ginfo[g_] = (nf, wd_s, wdb, ab_i)
                            for f in range(nf):
                                fs_ = slice(f * 128, (f + 1) * 128)
                                si = sgi_box[0] % 2
                                sgi_box[0] += 1
                                for (c0, w) in chunks:
                                    if w == 512:
                                        bg, bgb = next_bank()
                                        bu, bub = next_bank()
                                        k.mm_group(bg[:, :], [(wg_s[:, kc, fs_], hTe[:, kc, c0:c0 + w]) for kc in range(8)],
                                                   [wsb, hTeb], bgb)
                                        k.mm_group(bu[:, :], [(wu_s[:, kc, fs_], hTe[:, kc, c0:c0 + w]) for kc in range(8)],
                                                   [wsb, hTeb], bub)
                                        g_ap, u_ap = bg[:, :], bu[:, :]
                                    else:
                                        bs, bsb = next_bank()
                                        deps = k.deps_of([wsb, hTeb], [bsb])
                                        tok = None
                                        for wi, w_s in enumerate((wg_s, wu_s)):
                                            for kc in range(8):
                                                tok = p.op(PE, (lambda e_, o=bs[:, wi * w:(wi + 1) * w], a=w_s[:, kc, fs_],
                                                                r=hTe[:, kc, c0:c0 + w], s_=(kc == 0), t_=(kc == 7):
                                                                e_.matmul(o, lhsT=a, rhs=r, start=s_, stop=t_)),
                                                           deps if (wi == 0 and kc == 0) else (), (wi == 1 and kc == 7))
                                        wsb.add_read(tok)
                                        hTeb.add_read(tok)
                                        bsb.set_write(tok)
                                        bgb = bub = bsb
                                        g_ap, u_ap = bs[:, 0:w], bs[:, w:2 * w]
                                    k.act(sg[si][:, c0:c0 + w], g_ap, AF.Silu, [bgb], [sgb[si]])
                                    k.tt(actt[ab_i][:, f, c0:c0 + w], sg[si][:, c0:c0 + w], u_ap, ALU.mult,
                                         [sgb[si], bub], [actb[ab_i]])

                        def ffn_s2(g_):
                            nf, wd_s, wsb, ab_i = ginfo[g_]
                            for j in range(NJ):
                                for hf in range(2):
                                    cs = slice(hf * 512, (hf + 1) * 512)
                                    bk, bkb = next_bank()
                                    k.mm_group(bk[:, :], [(actt[ab_i][:, f, j * 128:(j + 1) * 128], wd_s[:, f, cs])
                                                          for f in range(nf)], [wsb, actb[ab_i]], bkb)
                                    ob = oeb[j][hf]
                                    if g_ == 0:
                                        k.act(oe32[:, j, cs], bk[:, :], AF.Copy, [bkb], [ob])
                                    elif g_ < ngrp - 1:
                                        k.tt(oe32[:, j, cs], oe32[:, j, cs], bk[:, :], ALU.add, [bkb], [ob])
                                    else:
                                        k.tt(oe_bf[:, j, cs], oe32[:, j, cs], bk[:, :], ALU.add, [bkb, ob], [oebf_b])

                        ffn_s1(0)
                        for g_ in range(ngrp):
                            if g_ + 1 < ngrp:
                                ffn_s1(g_ + 1)
                            ffn_s2(g_)
                        Pq = {}

                        def st_x(T):
                            Pq[T] = build_P(T, 0, cap)

                        def st_y(T):
                            Pt, Ptb = Pq.pop(T)
                            bk, bkb = next_bank()
                            bkbf = bk.bitcast(BF16)
                            outs = [bkbf[:, j * 128:(j + 1) * 128] for j in range(NJ)]
                            pairs = [(Pt[:, j * 128:(j + 1) * 128], ident[:, :]) for j in range(NJ)]
                            k.mm_group(outs, pairs, [Ptb, cb], bkb, transpose=True)
                            pti = T % 2
                            k.act(PT[pti][:, :, :], bkbf[:, 0:cap].rearrange("p (j t) -> p j t", j=NJ), AF.Copy,
                                  [bkb], [PTb[pti]])

                        def st_z(T):
                            ptp = T % 2
                            n = T + 1
                            for hf in range(2):
                                cs = slice(hf * 512, (hf + 1) * 512)
                                bk2, bk2b = next_bank()
                                k.mm_group(bk2[:, :], [(PT[ptp][:, j, :], oe_bf[:, j, cs]) for j in range(NJ)],
                                           [PTb[ptp], oebf_b], bk2b)
                                k.stt(x_tok[:, n, cs], bk2[:, :], gate[:, n, e:e + 1], x_tok[:, n, cs],
                                      ALU.mult, ALU.add, [bk2b, gateb[n]], [xb[n]])

                        st_x(0)
                        st_x(1)
                        st_y(0)
                        for T in range(16):
                            if T + 2 < 16:
                                st_x(T + 2)
                            if T + 1 < 16:
                                st_y(T + 1)
                            st_z(T)
                        p.barrier()

                def expert_dense(e):
                    with ExitStack() as db:
                        h2T = sb(db, "h2T", [128, 8, NT * 128], BF16)
                        h2b = [Buf() for _ in range(NT)]
                        for n in tiles:
                            T = n - 1
                            bk, bkb = next_bank()
                            bkbf = bk.bitcast(BF16)
                            outs = [bkbf[:, kc * 128:(kc + 1) * 128] for kc in range(8)]
                            pairs = [(h_all[:, T, kc * 128:(kc + 1) * 128], ident[:, :]) for kc in range(8)]
                            k.mm_group(outs, pairs, [hab[T], cb], bkb, transpose=True)
                            k.act(h2T[:, :, n * 128:(n + 1) * 128], bkbf[:, :].rearrange("p (k t) -> p k t", k=8),
                                  AF.Copy, [bkb], [h2b[n]])
                        ffn_stream(db, True, tiles, h2T, h2b, GFS, experts=[e])
                        p.barrier()

                def cflag(c, e):
                    return cond_i[0:1, 8 * c + e:8 * c + e + 1]

                for e in range(NE):
                    p.cond_region(
                        cflag(1, e), [],
                        lambda e=e: p.cond_region(cflag(0, e), [], lambda: expert_sparse(e, CAPS[0]),
                                                  lambda: expert_sparse(e, CAPS[1])),
                        lambda e=e: p.cond_region(cflag(2, e), [], lambda: expert_sparse(e, CAPS[2]),
                                                  lambda: expert_dense(e)))
                    p.barrier()

        def final_phase(do_norm):
            with ExitStack() as os_:
                gf_bc = sb(os_, "gf_bc", [128, D], F32)
                gfb = Buf()
                outt = [sb(os_, "outt%d" % i, [128, D], F32) for i in range(2)]
                outb = [Buf(), Buf()]
                junk = sb(os_, "junko", [128, D], BF16)
                junkb = Buf()
                toks = []
                if do_norm:
                    k.dma(SP, gf_bc[:], fg_d[0:1, :].partition_broadcast(128), writes=[gfb], sem="gf")
                for n in range(1, NT):
                    if do_norm:
                        oi = n % 2
                        rstd, sbf = rms_stats(x_tok[:, n, :], xb[n], junk[:], junkb, 1.0 / 32.0)
                        k.stt(outt[oi][:], x_tok[:, n, :], rstd, gf_bc[:], ALU.mult, ALU.mult,
                              [xb[n], sbf, gfb], [outb[oi]])
                        toks.append(k.dma(SP, y_d[(n - 1) * 128:n * 128, :], outt[oi][:], reads=[outb[oi]], sem="out%d" % oi))
                    else:
                        toks.append(k.dma(SP, y_d[(n - 1) * 128:n * 128, :], x_tok[:, n, :], reads=[xb[n]], sem="out0"))
                p.wait_only(SP, toks)
                p.barrier()

        mixer_phase(0)
        if stop_after == "M0":
            final_phase(False)
            p.flush()
            return nc
        ffn_phase0()
        if stop_after == "F0":
            final_phase(False)
            p.flush()
            return nc
        k.ts1(x_tok[:, 0, :], x_tok[:, 0, :], flag[:, 0:1], ALU.mult, [cb], [xb[0]])
        mixer_phase(1)
        if stop_after == "M1":
            final_phase(False)
            p.flush()
            return nc
        moe_phase()
        final_phase(True)
        p.flush()
    return nc


def _prep_shared(inp):
    f = lambda a: np.ascontiguousarray(np.asarray(a, dtype=np.float32))
    sh = {}
    sh["ident"] = np.eye(128, dtype=np.float32)
    sh["ustrict"] = np.triu(np.ones((128, 128), np.float32), 1)
    sh["iota"] = np.ascontiguousarray(np.broadcast_to(np.arange(CAPS[-1], dtype=np.float32), (128, CAPS[-1])))
    pp = np.zeros((128, 2, NPP), np.float32)
    wins = np.array([[2.0, 4.0], [8.0, 16.0]], np.float32)
    for l in range(2):
        pp[:, l, PP_PSCALE:PP_PSCALE + 2] = f(inp["pool_scale"])[l].reshape(2, 128).T
        dw = f(inp["conv_dw_w"])[l]
        pp[:, l, PP_DWW:PP_DWW + 93] = dw.reshape(31, 3, 128).transpose(2, 1, 0).reshape(128, 93)
        pp[:, l, PP_DWB:PP_DWB + 3] = f(inp["conv_dw_b"])[l].reshape(3, 128).T
        pp[:, l, PP_LNG:PP_LNG + 3] = f(inp["conv_ln_g"])[l].reshape(3, 128).T
        pp[:, l, PP_LNB:PP_LNB + 3] = f(inp["conv_ln_b"])[l].reshape(3, 128).T
        pp[:, l, PP_PWB:PP_PWB + 3] = f(inp["conv_pw_b"])[l].reshape(3, 128).T
        for c in range(2):
            pp[0:64, l, PP_INVW + c] = 1.0 / wins[c, 0]
            pp[64:128, l, PP_INVW + c] = 1.0 / wins[c, 1]
        pp[:, l, PP_G2:PP_G2 + 8] = f(inp["norm2_g"])[l].reshape(8, 128).T
    sh["pp"] = pp.reshape(128, 2 * NPP)
    sh["norm1_g"] = f(inp["norm1_g"])
    sh["norm2_g"] = f(inp["norm2_g"])
    sh["final_g"] = f(inp["final_g"]).reshape(1, D)
    sh["gm_norm_g"] = f(inp["gm_norm_g"])
    sh["gm_b"] = f(inp["gm_b"]).reshape(2, 512)
    sh["w_in"] = f(inp["w_in"])
    sh["pool_w"] = f(inp["pool_w"])
    sh["gm_wsT"] = np.ascontiguousarray(f(inp["gm_ws"]).transpose(0, 1, 3, 2))
    sh["conv_pw_w"] = f(inp["conv_pw_w"])
    sh["w_out"] = f(inp["w_out"])
    sh["ffn_wg"] = f(inp["ffn_wg"])[0]
    sh["ffn_wu"] = f(inp["ffn_wu"])[0]
    sh["ffn_wd"] = f(inp["ffn_wd"])[0]
    sh["router"] = f(inp["moe_router"])[0]
    sh["moe_wg"] = f(inp["moe_wg"])[0]
    sh["moe_wu"] = f(inp["moe_wu"])[0]
    sh["moe_wd"] = f(inp["moe_wd"])[0]
    return sh


def _prep_core(x, c):
    b, q = c // 4, c % 4
    xin = np.zeros((NT * 128, D), np.float32)
    xin[128:] = x[b, q * 2048:(q + 1) * 2048]
    if q > 0:
        xin[:128] = x[b, q * 2048 - 128:q * 2048]
    flag = np.full((128, 1), 1.0 if q > 0 else 0.0, np.float32)
    pinv = np.zeros((128, 2, 16), np.float32)
    wins = [[2, 4], [8, 16]]
    for cc in range(2):
        for half in range(2):
            w = wins[cc][half]
            for j in range(16):
                cntv = min(j + 1, w) if q == 0 else w
                pinv[half * 64:(half + 1) * 64, cc, j] = 1.0 / cntv
    return {"x": xin, "flag": flag, "pool_inv": pinv.reshape(128, 32)}


_NC_CACHE = {}


def run(inputs, stop_after=None, trace=False):
    x = np.asarray(inputs["x"], dtype=np.float32)
    sh = _prep_shared(inputs)
    in_maps = []
    for c in range(8):
        m = dict(sh)
        m.update(_prep_core(x, c))
        in_maps.append(m)
    if stop_after not in _NC_CACHE:
        _NC_CACHE[stop_after] = build_program(stop_after)
    nc = _NC_CACHE[stop_after]
    res = run_bass_kernel_spmd(nc, in_maps, core_ids=list(range(8)), **({"trace": True} if trace else {}))
    out = np.zeros((2, 8192, D), np.float32)
    for c in range(8):
        b, q = c // 4, c % 4
        out[b, q * 2048:(q + 1) * 2048] = res.results[c]["y"]
    return out, res


def kernel(**inputs):
    out, _ = run(inputs)
    return out
```

```python
import numpy as np
from contextlib import ExitStack
import concourse.bass as bass
import concourse.mybir as mybir
from concourse.bass_utils import run_bass_kernel_spmd

F32 = mybir.dt.float32
BF16 = mybir.dt.bfloat16
I32 = mybir.dt.int32
AF = mybir.ActivationFunctionType
ALU = mybir.AluOpType
AX = mybir.AxisListType

PE, ACT, DVE, POOL, SP = "pe", "act", "dve", "pool", "sp"
ENGS = (PE, ACT, DVE, POOL, SP)

D = 1024
NT = 17
D_IN = 1792
D_FF = 2816
D_FFE = 3584
NE = 8
EPS = 1e-6
NPP = 120
PP_PSCALE = 0
PP_DWW = 2
PP_DWB = 95
PP_LNG = 98
PP_LNB = 101
PP_PWB = 104
PP_INVW = 107
PP_G2 = 109
TGM = 2
GF = 4
GFS = 2
CAPS = (512, 640, 768)
FORCE_CLASS = None


class Prog:
    def __init__(self, nc, stack):
        self.nc = nc
        self.stack = stack
        self.ops = {e: [] for e in ENGS}
        self.sems = {}
        self.cnt = {}
        self.seen = {e: {} for e in ENGS}
        for e in ENGS:
            self._mksem("eng_" + e)

    def _mksem(self, key):
        if key not in self.sems:
            self.sems[key] = self.stack.enter_context(self.nc.semaphore(key))
            self.cnt[key] = 0
        return self.sems[key]

    def _waits(self, eng, deps):
        out = []
        for d in deps:
            if d is None:
                continue
            key, val = d
            if eng == PE and key == "eng_pe":
                continue
            if self.seen[eng].get(key, 0) >= val:
                continue
            self.seen[eng][key] = val
            out.append((self.sems[key], val))
        return out

    def op(self, eng, fn, deps=(), inc=True):
        waits = self._waits(eng, deps)
        key = "eng_" + eng
        tok = None
        if inc:
            self.cnt[key] += 1
            tok = (key, self.cnt[key])
        self.ops[eng].append((waits, fn, (self.sems[key], 1) if inc else None))
        return tok

    def dma(self, eng, out, in_, semname, deps=()):
        self._mksem(semname)
        waits = self._waits(eng, deps)
        self.cnt[semname] += 16
        tok = (semname, self.cnt[semname])
        self.ops[eng].append(
            (waits, lambda e, o=out, i=in_: e.dma_start(out=o, in_=i), (self.sems[semname], 16))
        )
        return tok

    def wait_only(self, eng, deps):
        waits = self._waits(eng, deps)
        if waits:
            self.ops[eng].append((waits, None, None))

    def barrier(self):
        toks = [(k, v) for k, v in self.cnt.items() if v > 0]
        for e in ENGS:
            self.wait_only(e, toks)

    def cond_region(self, cond_ap, cond_deps, then_fn, else_fn):
        for e in ENGS:
            self.ops[e].append(("IF", cond_ap, self._waits(e, cond_deps)))
        snap_cnt = dict(self.cnt)
        snap_seen = {e: dict(d) for e, d in self.seen.items()}
        Buf.reset_all()
        then_fn()
        then_cnt = dict(self.cnt)
        then_end = {e: len(self.ops[e]) for e in ENGS}
        self.cnt = dict(snap_cnt)
        for kk in then_cnt:
            self.cnt.setdefault(kk, 0)
        self.seen = {e: dict(d) for e, d in snap_seen.items()}
        for e in ENGS:
            self.ops[e].append(("ELSE",))
        Buf.reset_all()
        else_fn()
        else_cnt = dict(self.cnt)
        keys = set(then_cnt) | set(else_cnt)
        final = {kk: max(then_cnt.get(kk, 0), else_cnt.get(kk, 0)) for kk in keys}

        def equalizers(branch_cnt):
            per_eng = {e: [] for e in ENGS}
            for kk in sorted(keys):
                diff = final[kk] - branch_cnt.get(kk, 0)
                if diff <= 0:
                    continue
                eng = kk[4:] if kk.startswith("eng_") else SP
                per_eng[eng].append(("EQ", self.sems[kk], branch_cnt.get(kk, 0), diff))
            return per_eng

        eq_then = equalizers(then_cnt)
        eq_else = equalizers(else_cnt)
        for e in ENGS:
            self.ops[e][then_end[e]:then_end[e]] = eq_then[e]
            self.ops[e].extend(eq_else[e])
            self.ops[e].append(("ENDIF",))
        self.cnt = final
        self.seen = snap_seen
        Buf.reset_all()

    def flush(self):
        nc = self.nc
        ops = self.ops

        def run(e, lst):
            cms = []
            for item in lst:
                tag = item[0]
                if tag == "IF":
                    for s_, v in item[2]:
                        e.wait_ge(s_, v)
                    val = e.value_load(item[1])
                    cm = e.If(val == 1)
                    cm.__enter__()
                    cms.append(cm)
                elif tag == "ELSE":
                    cms.pop().__exit__(None, None, None)
                    cm = e.Else()
                    cm.__enter__()
                    cms.append(cm)
                elif tag == "ENDIF":
                    cms.pop().__exit__(None, None, None)
                elif tag == "EQ":
                    _, sem, have, diff = item
                    if have > 0:
                        e.wait_ge(sem, have)
                    e.sem_inc(sem, diff)
                else:
                    waits, fn, inc = item
                    for s_, v in waits:
                        e.wait_ge(s_, v)
                    if fn is not None:
                        ins = fn(e)
                        if inc is not None:
                            ins.then_inc(inc[0], inc[1])

        with nc.Block() as block:
            @block.tensor
            def _(e):
                run(e, ops[PE])

            @block.scalar
            def _(e):
                run(e, ops[ACT])

            @block.vector
            def _(e):
                run(e, ops[DVE])

            @block.gpsimd
            def _(e):
                run(e, ops[POOL])

            @block.sync
            def _(e):
                run(e, ops[SP])
        self.ops = {e: [] for e in ENGS}


class Buf:
    ALL = []

    def __init__(self, name=""):
        self.name = name
        self.w = None
        self.r = {}
        Buf.ALL.append(self)

    @staticmethod
    def reset_all():
        for b in Buf.ALL:
            b.w = None
            b.r = {}

    def add_read(self, tok):
        if tok is None:
            return
        k, v = tok
        if self.r.get(k, 0) < v:
            self.r[k] = v

    def set_write(self, tok):
        self.w = tok
        self.r = {}


class K:
    def __init__(self, nc, stack):
        self.nc = nc
        self.p = Prog(nc, stack)
        self.dma_n = 0

    def deps_of(self, reads, writes):
        deps = []
        for b in reads:
            deps.append(b.w)
        for b in writes:
            deps.append(b.w)
            deps.extend(b.r.items())
        return deps

    def emit(self, eng, fn, reads=(), writes=()):
        tok = self.p.op(eng, fn, self.deps_of(reads, writes), True)
        for b in reads:
            b.add_read(tok)
        for b in writes:
            b.set_write(tok)
        return tok

    def dma(self, eng, out, in_, reads=(), writes=(), sem=None):
        assert sem is not None
        deps = []
        for b in reads:
            deps.append(b.w)
        for b in writes:
            if not (b.w is not None and b.w[0] == sem):
                deps.append(b.w)
            deps.extend(b.r.items())
        tok = self.p.dma(eng, out, in_, sem, deps)
        for b in reads:
            b.add_read(tok)
        for b in writes:
            b.set_write(tok)
        return tok

    def mm_group(self, out, pairs, reads, bank, transpose=False):
        deps = self.deps_of(reads, [bank])
        n = len(pairs)
        tok = None
        for i, (l, r) in enumerate(pairs):
            last = i == n - 1
            if transpose:
                fn = (lambda e, o=out[i], a=l, b=r: e.transpose(o, a, b))
            else:
                fn = (lambda e, o=out, a=l, b=r, s=(i == 0), t=last: e.matmul(o, lhsT=a, rhs=b, start=s, stop=t))
            tok = self.p.op(PE, fn, deps if i == 0 else (), last)
        for b in reads:
            b.add_read(tok)
        bank.set_write(tok)
        return tok

    def mm_multi(self, groups, reads, bank):
        deps = self.deps_of(reads, [bank])
        n = len(groups)
        tok = None
        for i, (o, l, r) in enumerate(groups):
            last = i == n - 1
            fn = (lambda e, o=o, a=l, b=r: e.matmul(o, lhsT=a, rhs=b, start=True, stop=True))
            tok = self.p.op(PE, fn, deps if i == 0 else (), last)
        for b in reads:
            b.add_read(tok)
        bank.set_write(tok)
        return tok

    def act(self, out, in_, func, reads, writes, bias=None, scale=None, accum=None):
        kw = {}
        if bias is not None:
            kw["bias"] = bias
        if scale is not None:
            kw["scale"] = scale
        if accum is not None:
            kw["accum_out"] = accum
        return self.emit(ACT, lambda e: e.activation(out=out, in_=in_, func=func, **kw), reads, writes)

    def tt(self, out, in0, in1, op, reads, writes, eng=DVE):
        return self.emit(eng, lambda e: e.tensor_tensor(out=out, in0=in0, in1=in1, op=op), reads, writes)

    def ts(self, out, in0, s1, s2, op0, op1, reads, writes, eng=DVE):
        return self.emit(eng, lambda e: e.tensor_scalar(out=out, in0=in0, scalar1=s1, scalar2=s2, op0=op0, op1=op1),
                         reads, writes)

    def ts1(self, out, in0, s1, op0, reads, writes, eng=DVE):
        return self.emit(eng, lambda e: e.tensor_scalar(out=out, in0=in0, scalar1=s1, scalar2=None, op0=op0),
                         reads, writes)

    def stt(self, out, in0, scalar, in1, op0, op1, reads, writes, accum=None, eng=DVE):
        kw = {}
        if accum is not None:
            kw["accum_out"] = accum
        return self.emit(eng, lambda e: e.scalar_tensor_tensor(out=out, in0=in0, scalar=scalar, in1=in1,
                                                               op0=op0, op1=op1, **kw), reads, writes)

    def copy(self, out, in_, reads, writes, eng=DVE):
        return self.emit(eng, lambda e: e.tensor_copy(out=out, in_=in_), reads, writes)

    def recip(self, out, in_, reads, writes):
        return self.emit(DVE, lambda e: e.reciprocal(out=out, in_=in_), reads, writes)

    def memset(self, ap, val, writes, eng=DVE):
        return self.emit(eng, lambda e: e.memset(ap, val), (), writes)


def build_program(stop_after=None):
    nc = bass.Bass("TRN2", target_bir_lowering=False)

    def din(name, shape):
        return nc.dram_tensor(name, list(shape), F32, kind="ExternalInput").ap()

    x_d = din("x", [NT * 128, D])
    flag_d = din("flag", [128, 1])
    pinv_d = din("pool_inv", [128, 32])
    ident_d = din("ident", [128, 128])
    ustrict_d = din("ustrict", [128, 128])
    iota_d = din("iota", [128, CAPS[-1]])
    pp_d = din("pp", [128, 2 * NPP])
    n1g_d = din("norm1_g", [2, D])
    n2g_d = din("norm2_g", [2, D])
    fg_d = din("final_g", [1, D])
    gmg_d = din("gm_norm_g", [2, 384])
    gmb_d = din("gm_b", [2, 512])
    w_in_d = din("w_in", [2, D, D_IN])
    pool_w_d = din("pool_w", [2, 4, 64, 64])
    wsT_d = din("gm_wsT", [2, 4, 128, 128])
    pw_d = din("conv_pw_w", [2, 384, 384])
    w_out_d = din("w_out", [2, D, D])
    fwg_d = din("ffn_wg", [D, D_FF])
    fwu_d = din("ffn_wu", [D, D_FF])
    fwd_d = din("ffn_wd", [D_FF, D])
    router_d = din("router", [D, NE])
    mwg_d = din("moe_wg", [NE, D, D_FFE])
    mwu_d = din("moe_wu", [NE, D, D_FFE])
    mwd_d = din("moe_wd", [NE, D_FFE, D])
    y_d = nc.dram_tensor("y", [16 * 128, D], F32, kind="ExternalOutput").ap()

    with ExitStack() as st:
        ARENA_WORDS = 53100
        arena = st.enter_context(nc.sbuf_tensor("arena", [128, ARENA_WORDS], F32))
        atop = [0]
        scopes = {}

        def _release(mark):
            atop[0] = mark

        def sb(stack, name, shape, dt):
            if stack is not st and not getattr(stack, "_arena_marked", False):
                stack._arena_marked = True
                stack.callback(_release, atop[0])
            n = 1
            for d_ in shape[1:]:
                n *= d_
            nbytes = n * (4 if dt in (F32, I32) else 2)
            words = ((nbytes + 3) // 4 + 7) // 8 * 8
            assert atop[0] + words <= ARENA_WORDS, ("SBUF arena overflow", name, atop[0], words)
            v = arena[:, atop[0]:atop[0] + words]
            atop[0] += words
            if dt != F32:
                v = v.bitcast(dt)
            v = v[:, 0:n]
            if len(shape) == 3:
                v = v.rearrange("p (a b) -> p a b", a=shape[1])
            return v

        k = K(nc, st)
        p = k.p

        x_tok = sb(st, "x_tok", [128, NT, D], F32)
        xb = [Buf("x%d" % n) for n in range(NT)]
        ident = sb(st, "ident", [128, 128], BF16)
        ones32 = sb(st, "ones32", [128, 128], F32)
        ustrict = sb(st, "ustrict", [128, 128], F32)
        ident32 = sb(st, "ident32", [128, 128], F32)
        iota = sb(st, "iota", [128, CAPS[-1]], F32)
        pp = sb(st, "pp", [128, 2 * NPP], F32)
        flag = sb(st, "flag", [128, 1], F32)
        pinv = sb(st, "pinv", [128, 2, 16], F32)
        stt_t = sb(st, "stats", [128, 8, 4], F32)
        gate = sb(st, "gate", [128, NT, NE], F32)
        cb = Buf("consts")
        stb = [Buf("st%d" % i) for i in range(8)]
        gateb = [Buf("gate%d" % n) for n in range(NT)]
        banks = [st.enter_context(nc.psum_tensor("bank%d" % i, [128, 512], F32)) for i in range(8)]
        bankb = [Buf("bank%d" % i) for i in range(8)]
        ring = [0]
        stat_i = [0]

        def next_bank():
            i = ring[0]
            ring[0] = (i + 1) % 8
            return banks[i], bankb[i]

        def next_stat():
            i = stat_i[0]
            stat_i[0] = (i + 1) % 8
            return stt_t[:, i, :], stb[i]

        def ppc(l, col, n=1, lo=0, hi=128):
            return pp[lo:hi, l * NPP + col: l * NPP + col + n]

        k.memset(ones32[:], 1.0, [cb])
        k.dma(POOL, ident[:], ident_d, writes=[cb], sem="consts")
        k.dma(POOL, pp[:], pp_d, writes=[cb], sem="consts")
        k.dma(POOL, ustrict[:], ustrict_d, writes=[cb], sem="consts")
        k.dma(POOL, ident32[:], ident_d, writes=[cb], sem="consts")
        k.dma(POOL, iota[:], iota_d, writes=[cb], sem="consts")
        k.dma(POOL, flag[:], flag_d, writes=[cb], sem="consts")
        k.dma(POOL, pinv[:].rearrange("p c j -> p (c j)"), pinv_d, writes=[cb], sem="consts")
        for n in range(NT):
            k.dma(SP, x_tok[:, n, :], x_d[n * 128:(n + 1) * 128, :], writes=[xb[n]], sem="x%d" % n)

        def rms_stats(src_ap, srcb, junk, junkb, scale):
            sap, sbf = next_stat()
            k.act(junk, src_ap, AF.Square, [srcb], [junkb, sbf], scale=scale, accum=sap[:, 0:1])
            k.act(sap[:, 1:2], sap[:, 0:1], AF.Sqrt, [sbf], [sbf], bias=EPS)
            k.recip(sap[:, 2:3], sap[:, 1:2], [sbf], [sbf])
            return sap[:, 2:3], sbf

        def norm_and_transpose(n, g_bc, gb, h_tok, h_tokb, junk, junkb, hT_dst, hTb):
            rstd, sbf = rms_stats(x_tok[:, n, :], xb[n], junk, junkb, 1.0 / 32.0)
            k.stt(h_tok, x_tok[:, n, :], rstd, g_bc, ALU.mult, ALU.mult, [xb[n], sbf, gb], [h_tokb])
            bk, bkb = next_bank()
            bkbf = bk.bitcast(BF16)
            outs = [bkbf[:, kc * 128:(kc + 1) * 128] for kc in range(8)]
            pairs = [(h_tok[:, kc * 128:(kc + 1) * 128], ident[:, :]) for kc in range(8)]
            k.mm_group(outs, pairs, [h_tokb, cb], bkb, transpose=True)
            k.act(hT_dst, bkbf[:, :].rearrange("p (k t) -> p k t", k=8), AF.Copy, [bkb], [hTb])
            return rstd, sbf

        def mixer_phase(l):
            with ExitStack() as ms:
                NMAX = TGM * 128
                LMAX = 32 + NMAX
                w_in_sb = sb(ms, "w_in_sb", [128, 8, D_IN], BF16)
                wo_a = sb(ms, "wo_a", [128, 2, D], BF16)
                wo_b = sb(ms, "wo_b", [128, 4, D], BF16)
                wo_c = sb(ms, "wo_c", [128, 3, D], BF16)
                pw_sb = sb(ms, "pw_sb", [128, 3, 384], BF16)
                wblk = sb(ms, "wblk", [128, 2, 128], BF16)
                wsT = sb(ms, "wsT", [128, 4, 128], BF16)
                g1_bc = sb(ms, "g1_bc", [128, D], F32)
                gmg_bc = sb(ms, "gmg_bc", [128, 384], F32)
                gmb_bc = sb(ms, "gmb_bc", [128, 512], F32)
                wb = Buf("mixw")
                h_tok = [sb(ms, "h_tok%d" % i, [128, D], BF16) for i in range(2)]
                h_tokb = [Buf(), Buf()]
                junk = sb(ms, "junk", [128, D], BF16)
                junkb = Buf()
                two = range(2)
                a_ext = [sb(ms, "a_ext%d" % i, [128, 2, LMAX], F32) for i in two]
                hc_ext = [sb(ms, "hc_ext%d" % i, [128, 3, LMAX], BF16) for i in two]
                yb = [sb(ms, "yb%d" % i, [128, 4, NMAX], BF16) for i in two]
                ab, hcb, ybb = ([Buf(), Buf()] for _ in range(3))

                def same2(name, shape, dt):
                    t_ = sb(ms, name, shape, dt)
                    return [t_, t_]

                def same2b():
                    b_ = Buf()
                    return [b_, b_]

                hT = same2("hT", [128, 8, NMAX], BF16)
                sig = same2("sig", [128, 3, NMAX], F32)
                acc = same2("acc", [128, 3, NMAX], F32)
                u_sb = same2("u_sb", [128, 4, NMAX], F32)
                y_p = same2("y_p", [128, 2, NMAX], BF16)
                ya = [sb(ms, "ya%d" % i, [128, 2, NMAX], BF16) for i in two]
                hs = same2("hs", [128, 3, NMAX], BF16)
                yc = same2("yc", [128, 3, NMAX], BF16)
                hTb, sigb, ub, ypb, hsb, ycb = (same2b() for _ in range(6))
                yab = [Buf(), Buf()]
                accb1 = [Buf(), Buf(), Buf()]
                accb = [accb1, accb1]
                dg = sb(ms, "dg", [128, 93, 128], BF16)
                dgb = Buf()
                sA = sb(ms, "sA", [128, 2, LMAX], F32)
                sB = sb(ms, "sB", [128, 2, LMAX], F32)
                tmp16 = sb(ms, "tmp16", [128, 16], F32)
                v_n = [sb(ms, "v_n%d" % i, [128, 384], BF16) for i in two]
                ztmp = sb(ms, "ztmp", [128, 512], F32)
                sq = sb(ms, "sq", [128, 3, NMAX], F32)
                mean = sb(ms, "mean", [128, NMAX], F32)
                var = sb(ms, "var", [128, NMAX], F32)
                rstdc = sb(ms, "rstdc", [128, NMAX], F32)
                sAb, sBb, t16b, ztb, sqb, meanb, varb, rsb = (Buf() for _ in range(8))
                vnb = [Buf(), Buf()]

                wblkb = Buf()
                wsTb = Buf()
                k.memset(wblk[:], 0.0, [wblkb])
                for c in range(2):
                    k.dma(POOL, wblk[0:64, c, 0:64], pool_w_d[l, 2 * c], writes=[wblkb], sem="wblk")
                    k.dma(POOL, wblk[64:128, c, 64:128], pool_w_d[l, 2 * c + 1], writes=[wblkb], sem="wblk")
                k.dma(POOL, wsT[:], wsT_d[l].rearrange("h j i -> j h i"), writes=[wsTb], sem="wsT")
                k.memset(wsT[64:128, :, 0:64], 0.0, [wsTb])
                k.dma(SP, g1_bc[:], n1g_d[l:l + 1, :].partition_broadcast(128), writes=[wb], sem="mixw")
                k.dma(SP, gmg_bc[:], gmg_d[l:l + 1, :].partition_broadcast(128), writes=[wb], sem="mixw")
                k.dma(SP, gmb_bc[:], gmb_d[l:l + 1, :].partition_broadcast(128), writes=[wb], sem="mixw")
                k.dma(POOL, w_in_sb[:], w_in_d[l].rearrange("(kc p) n -> p kc n", p=128), writes=[wb], sem="mixw")
                k.dma(POOL, wo_a[:], w_out_d[l, 0:256, :].rearrange("(c p) n -> p c n", p=128), writes=[wb], sem="mixw")
                k.dma(POOL, wo_b[0:96], w_out_d[l, 256:640, :].rearrange("(h p) n -> p h n", p=96), writes=[wb], sem="mixw")
                k.dma(POOL, wo_c[:], w_out_d[l, 640:1024, :].rearrange("(c p) n -> p c n", p=128), writes=[wb], sem="mixw")
                k.dma(POOL, pw_sb[:], pw_d[l].rearrange("(c p) n -> p c n", p=128), writes=[wb], sem="mixw")
                k.memset(a_ext[0][:, :, 0:32], 0.0, [ab[0]])
                k.memset(hc_ext[0][:, :, 0:32], 0.0, [hcb[0]])
                for i in range(93):
                    k.ts1(dg[:, i, :], ident[:, :], ppc(l, PP_DWW + i), ALU.mult, [cb], [dgb])

                groups = [(0, 1)] + [(t0, TGM) for t0 in range(1, NT, TGM)]

                def stage_ne(gi):
                    t0, nt = groups[gi]
                    for j in range(nt):
                        n = t0 + j
                        hb = (gi * TGM + j) % 2
                        rstd, sbf = rms_stats(x_tok[:, n, :], xb[n], junk[:], junkb, 1.0 / 32.0)
                        k.stt(h_tok[hb][:], x_tok[:, n, :], rstd, g1_bc[:], ALU.mult, ALU.mult,
                              [xb[n], sbf, wb], [h_tokb[hb]])

                def stage_nt(gi):
                    t0, nt = groups[gi]
                    q = gi % 2
                    for j in range(nt):
                        hb = (gi * TGM + j) % 2
                        bk, bkb = next_bank()
                        bkbf = bk.bitcast(BF16)
                        outs = [bkbf[:, kc * 128:(kc + 1) * 128] for kc in range(8)]
                        pairs = [(h_tok[hb][:, kc * 128:(kc + 1) * 128], ident[:, :]) for kc in range(8)]
                        k.mm_group(outs, pairs, [h_tokb[hb], cb], bkb, transpose=True)
                        k.act(hT[q][:, :, j * 128:(j + 1) * 128], bkbf[:, :].rearrange("p (k t) -> p k t", k=8),
                              AF.Copy, [bkb], [hTb[q]])

                def stage_p(gi):
                    t0, nt = groups[gi]
                    q = gi % 2
                    N = nt * 128
                    L = 32 + N
                    full = not (l == 1 and t0 == 0)
                    first_own = (t0 == 1)
                    if gi > 0:
                        pN = groups[gi - 1][1] * 128
                        k.copy(a_ext[q][:, :, 0:32], a_ext[1 - q][:, :, pN:pN + 32], [ab[1 - q]], [ab[q]], eng=POOL)
                        k.copy(hc_ext[q][:, :, 0:32], hc_ext[1 - q][:, :, pN:pN + 32], [hcb[1 - q]], [hcb[q]], eng=POOL)

                    def proj(col0, m):
                        bk, bkb = next_bank()
                        pairs = [(w_in_sb[:, kc, col0:col0 + m], hT[q][:, kc, 0:N]) for kc in range(8)]
                        k.mm_group(bk[0:m, 0:N], pairs, [wb, hTb[q]], bkb)
                        return bk, bkb

                    for c in range(2):
                        bk, bkb = proj(c * 128, 128)
                        k.act(a_ext[q][:, c, 32:L], bk[:, 0:N], AF.Copy, [bkb], [ab[q]])
                    vinfo = []
                    if full:
                        for j in range(nt):
                            vb = j % 2
                            bk, bkb = next_bank()
                            pairs = [(hT[q][:, kc, j * 128:(j + 1) * 128], w_in_sb[:, kc, 640:1024]) for kc in range(8)]
                            k.mm_group(bk[:, 0:384], pairs, [wb, hTb[q]], bkb)
                            rstd, sbf = rms_stats(bk[:, 0:384], bkb, junk[:, 0:384], junkb, float(384.0 ** -0.5))
                            k.stt(v_n[vb][:], bk[:, 0:384], rstd, gmg_bc[:], ALU.mult, ALU.mult,
                                  [bkb, sbf, wb], [vnb[vb]])
                    for c in range(3):
                        bk, bkb = proj(1408 + c * 128, 128)
                        k.act(sig[q][:, c, 0:N], bk[:, 0:N], AF.Sigmoid, [bkb], [sigb[q]])
                    for c in range(3):
                        bk, bkb = proj(1024 + c * 128, 128)
                        k.tt(hc_ext[q][:, c, 32:L], bk[:, 0:N], sig[q][:, c, 0:N], ALU.mult, [bkb, sigb[q]], [hcb[q]])
                    if full:
                        for h in range(4):
                            bk, bkb = proj(256 + h * 96, 96)
                            k.act(u_sb[q][0:96, h, 0:N], bk[0:96, 0:N], AF.Copy, [bkb], [ub[q]])
                        for j in range(nt):
                            vb = j % 2
                            zk, zkb = next_bank()
                            grp = [(zk[0:96, h * 128:(h + 1) * 128], v_n[vb][:, h * 96:(h + 1) * 96], wsT[:, h, :])
                                   for h in range(4)]
                            k.mm_multi(grp, [vnb[vb], wsTb], zkb)
                            k.tt(ztmp[0:96, :], zk[0:96, :], gmb_bc[0:96, :], ALU.add, [zkb, wb], [ztb])
                            k.tt(yb[q][0:96, :, j * 128:(j + 1) * 128],
                                 ztmp[0:96, :].rearrange("p (h i) -> p h i", h=4),
                                 u_sb[q][0:96, :, j * 128:(j + 1) * 128], ALU.mult, [ztb, ub[q]], [ybb[q]])
                        A_ = a_ext[q]

                        def pool_out(sbuf_t, sbuf_b, lo, hi, c):
                            k.stt(y_p[q][lo:hi, c, 0:N], sbuf_t[lo:hi, c, 32:L], ppc(l, PP_INVW + c, 1, lo, hi),
                                  A_[lo:hi, c, 32:L], ALU.mult, ALU.subtract, [sbuf_b, ab[q], cb], [ypb[q]])
                            if first_own:
                                k.tt(tmp16[lo:hi, :], sbuf_t[lo:hi, c, 32:48], pinv[lo:hi, c, :], ALU.mult,
                                     [sbuf_b, cb], [t16b])
                                k.tt(y_p[q][lo:hi, c, 0:16], tmp16[lo:hi, :], A_[lo:hi, c, 32:48], ALU.subtract,
                                     [t16b, ab[q]], [ypb[q]])

                        k.tt(sA[:, :, 1:L], A_[:, :, 1:L], A_[:, :, 0:L - 1], ALU.add, [ab[q]], [sAb])
                        pool_out(sA, sAb, 0, 64, 0)
                        k.tt(sB[:, :, 3:L], sA[:, :, 3:L], sA[:, :, 1:L - 2], ALU.add, [sAb], [sBb])
                        pool_out(sB, sBb, 64, 128, 0)
                        k.tt(sA[:, :, 7:L], sB[:, :, 7:L], sB[:, :, 3:L - 4], ALU.add, [sBb], [sAb])
                        pool_out(sA, sAb, 0, 64, 1)
                        k.tt(sB[:, :, 15:L], sA[:, :, 15:L], sA[:, :, 7:L - 8], ALU.add, [sAb], [sBb])
                        pool_out(sB, sBb, 64, 128, 1)

                def stage_b1a(gi):
                    t0, nt = groups[gi]
                    q = gi % 2
                    N = nt * 128
                    L = 32 + N
                    full = not (l == 1 and t0 == 0)
                    first_own = (t0 == 1)
                    if not full:
                        return
                    A_ = a_ext[q]
                    H_ = hc_ext[q]

                    for c in range(2):
                        bk, bkb = next_bank()
                        k.mm_group(bk[:, 0:N], [(wblk[:, c, :], y_p[q][:, c, 0:N])], [wblkb, ypb[q]], bkb)
                        k.act(ya[q][:, c, 0:N], bk[:, 0:N], AF.Copy, [bkb, cb], [yab[q]], scale=ppc(l, PP_PSCALE + c))

                    for c in range(3):
                        bk, bkb = next_bank()
                        pairs = [(dg[:, c * 31 + kk, :], H_[:, c, 2 + kk:2 + kk + N]) for kk in range(31)]
                        k.mm_group(bk[:, 0:N], pairs, [dgb, hcb[q]], bkb)
                        k.act(acc[q][:, c, 0:N], bk[:, 0:N], AF.Identity, [bkb, cb], [accb[q][c]],
                              bias=ppc(l, PP_DWB + c))
                    for c in range(3):
                        k.act(sq[:, c, 0:N], acc[q][:, c, 0:N], AF.Square, [accb[q][c]], [sqb])

                def stage_b1b(gi):
                    t0, nt = groups[gi]
                    q = gi % 2
                    N = nt * 128
                    full = not (l == 1 and t0 == 0)
                    if not full:
                        return
                    b1, b1b = next_bank()
                    k.mm_group(b1[:, 0:N], [(ones32[:, :], acc[q][:, c, 0:N]) for c in range(3)], accb[q] + [cb], b1b)
                    b2, b2b = next_bank()
                    k.mm_group(b2[:, 0:N], [(ones32[:, :], sq[:, c, 0:N]) for c in range(3)], [sqb, cb], b2b)
                    k.ts(mean[:, 0:N], b1[:, 0:N], 1.0 / 384.0, 0.0, ALU.mult, ALU.add, [b1b], [meanb])
                    k.tt(var[:, 0:N], mean[:, 0:N], mean[:, 0:N], ALU.mult, [meanb], [varb])
                    k.stt(var[:, 0:N], b2[:, 0:N], 1.0 / 384.0, var[:, 0:N], ALU.mult, ALU.subtract,
                          [b2b], [varb])
                    k.act(var[:, 0:N], var[:, 0:N], AF.Sqrt, [], [varb], bias=EPS)
                    k.recip(rstdc[:, 0:N], var[:, 0:N], [varb], [rsb])
                    for c in range(3):
                        k.tt(sq[:, c, 0:N], acc[q][:, c, 0:N], mean[:, 0:N], ALU.subtract, [accb[q][c], meanb], [sqb])
                        k.tt(sq[:, c, 0:N], sq[:, c, 0:N], rstdc[:, 0:N], ALU.mult, [rsb], [sqb])
                        k.act(hs[q][:, c, 0:N], sq[:, c, 0:N], AF.Silu, [sqb, cb], [hsb[q]],
                              bias=ppc(l, PP_LNB + c), scale=ppc(l, PP_LNG + c))
                def stage_b2(gi):
                    t0, nt = groups[gi]
                    q = gi % 2
                    N = nt * 128
                    full = not (l == 1 and t0 == 0)
                    if not full:
                        return
                    for co in range(3):
                        bk, bkb = next_bank()
                        pairs = [(pw_sb[:, ci, co * 128:(co + 1) * 128], hs[q][:, ci, 0:N]) for ci in range(3)]
                        k.mm_group(bk[:, 0:N], pairs, [wb, hsb[q]], bkb)
                        k.act(yc[q][:, co, 0:N], bk[:, 0:N], AF.Identity, [bkb, cb], [ycb[q]], bias=ppc(l, PP_PWB + co))

                    for j in range(nt):
                        n = t0 + j
                        ts_ = slice(j * 128, (j + 1) * 128)
                        for hf in range(2):
                            cs = slice(hf * 512, (hf + 1) * 512)
                            pairs = [(ya[q][:, c, ts_], wo_a[:, c, cs]) for c in range(2)]
                            pairs += [(yb[q][0:96, h, ts_], wo_b[0:96, h, cs]) for h in range(4)]
                            pairs += [(yc[q][:, c, ts_], wo_c[:, c, cs]) for c in range(3)]
                            bk, bkb = next_bank()
                            k.mm_group(bk[:, :], pairs, [wb, yab[q], ybb[q], ycb[q]], bkb)
                            k.tt(x_tok[:, n, cs], x_tok[:, n, cs], bk[:, :], ALU.add, [bkb], [xb[n]])

                ng = len(groups)
                stage_ne(0)
                stage_nt(0)
                if ng > 1:
                    stage_ne(1)
                stage_p(0)
                for gi in range(ng):
                    if gi + 1 < ng:
                        stage_nt(gi + 1)
                    stage_b1a(gi)
                    if gi >= 1:
                        stage_b2(gi - 1)
                    if gi + 2 < ng:
                        stage_ne(gi + 2)
                    if gi + 1 < ng:
                        stage_p(gi + 1)
                    stage_b1b(gi)
                stage_b2(ng - 1)
                p.barrier()

        def ffn_stream(scope, moe, tiles, h2T, h2b, gf, experts=None):
            GW = gf * 128
            slots = []
            for s_ in range(2):
                slots.append((sb(scope, "wg%d" % s_, [128, 8, GW], BF16),
                              sb(scope, "wu%d" % s_, [128, 8, GW], BF16),
                              sb(scope, "wd%d" % s_, [128, gf, D], BF16), Buf()))
            actt = [sb(scope, "act%d" % i, [128, gf, 512], BF16) for i in range(2)]
            actb = [Buf(), Buf()]
            sg = [sb(scope, "sg%d" % i, [128, 512], F32) for i in range(2)]
            sgb = [Buf(), Buf()]
            glist = []
            if not moe:
                nch = D_FF // 128
                for c0 in range(0, nch, gf):
                    nf = min(gf, nch - c0)
                    glist.append((fwg_d[:, c0 * 128:(c0 + nf) * 128], fwu_d[:, c0 * 128:(c0 + nf) * 128],
                                  fwd_d[c0 * 128:(c0 + nf) * 128, :], nf, None))
            else:
                nch = D_FFE // 128
                for e in (experts if experts is not None else range(NE)):
                    for c0 in range(0, nch, gf):
                        nf = min(gf, nch - c0)
                        glist.append((mwg_d[e, :, c0 * 128:(c0 + nf) * 128],
                                      mwu_d[e, :, c0 * 128:(c0 + nf) * 128],
                                      mwd_d[e, c0 * 128:(c0 + nf) * 128, :], nf, e))
            tgroups = [tiles[i:i + 4] for i in range(0, len(tiles), 4)]
            cnt = 0
            sgi = 0
            for gi, (wg_ap, wu_ap, wd_ap, nf, e) in enumerate(glist):
                wg_s, wu_s, wd_s, wsb = slots[gi % 2]
                sem = "wslot%d" % (gi % 2)
                k.dma(POOL, wg_s[:, :, 0:nf * 128], wg_ap.rearrange("(kc p) n -> p kc n", p=128),
                      writes=[wsb], sem=sem)
                k.dma(POOL, wu_s[:, :, 0:nf * 128], wu_ap.rearrange("(kc p) n -> p kc n", p=128),
                      writes=[wsb], sem=sem)
                k.dma(POOL, wd_s[:, 0:nf, :], wd_ap.rearrange("(f p) n -> p f n", p=128),
                      writes=[wsb], sem=sem)
                for tg in tgroups:
                    ab_i = cnt % 2
                    cnt += 1
                    t_lo = tg[0] * 128
                    N = len(tg) * 128
                    for f in range(nf):
                        bg, bgb = next_bank()
                        k.mm_group(bg[:, 0:N], [(wg_s[:, kc, f * 128:(f + 1) * 128], h2T[:, kc, t_lo:t_lo + N])
                                                for kc in range(8)], [wsb] + [h2b[n] for n in tg], bgb)
                        bu, bub = next_bank()
                        k.mm_group(bu[:, 0:N], [(wu_s[:, kc, f * 128:(f + 1) * 128], h2T[:, kc, t_lo:t_lo + N])
                                                for kc in range(8)], [wsb] + [h2b[n] for n in tg], bub)
                        si = sgi % 2
                        sgi += 1
                        k.act(sg[si][:, 0:N], bg[:, 0:N], AF.Silu, [bgb], [sgb[si]])
                        k.tt(actt[ab_i][:, f, 0:N], sg[si][:, 0:N], bu[:, 0:N], ALU.mult, [sgb[si], bub],
                             [actb[ab_i]])
                    for j, n in enumerate(tg):
                        for hf in range(2):
                            cs = slice(hf * 512, (hf + 1) * 512)
                            bk, bkb = next_bank()
                            k.mm_group(bk[:, :], [(actt[ab_i][:, f, j * 128:(j + 1) * 128], wd_s[:, f, cs])
                                                  for f in range(nf)], [wsb, actb[ab_i]], bkb)
                            if moe:
                                k.stt(x_tok[:, n, cs], bk[:, :], gate[:, n, e:e + 1], x_tok[:, n, cs],
                                      ALU.mult, ALU.add, [bkb, gateb[n]], [xb[n]])
                            else:
                                k.tt(x_tok[:, n, cs], x_tok[:, n, cs], bk[:, :], ALU.add, [bkb], [xb[n]])

        def ffn_phase0():
            tiles = list(range(0, NT))
            with ExitStack() as fs:
                h2T = sb(fs, "h2T", [128, 8, NT * 128], BF16)
                h2b = [Buf() for _ in range(NT)]
                g2_bc = sb(fs, "g2_bc", [128, D], F32)
                gb = Buf()
                h_tok = [sb(fs, "h_tokf%d" % i, [128, D], BF16) for i in range(2)]
                h_tokb = [Buf(), Buf()]
                junk = sb(fs, "junkf", [128, D], BF16)
                junkb = Buf()
                k.dma(SP, g2_bc[:], n2g_d[0:1, :].partition_broadcast(128), writes=[gb], sem="g2")
                for ti, n in enumerate(tiles):
                    hb = ti % 2
                    norm_and_transpose(n, g2_bc[:], gb, h_tok[hb][:], h_tokb[hb], junk[:], junkb,
                                       h2T[:, :, n * 128:(n + 1) * 128], h2b[n])
                ffn_stream(fs, False, tiles, h2T, h2b, GF)
                p.barrier()

        def moe_phase():
            tiles = list(range(1, NT))
            with ExitStack() as fs:
                h_all = sb(fs, "h_all", [128, 16, D], BF16)
                hab = [Buf() for _ in range(16)]
                sel = sb(fs, "sel", [128, 16, NE], F32)
                pos = sb(fs, "pos", [128, 16, NE], F32)
                tot = sb(fs, "tot", [128, 16, NE], F32)
                offs = sb(fs, "offs", [128, 16, NE], F32)
                misc = sb(fs, "rmisc", [128, 40], F32)
                cond_i = sb(fs, "cond_i", [128, 32], I32)
                selb, posb, totb, offb, miscb, condb = (Buf() for _ in range(6))
                with ExitStack() as f1:
                    g2_bc = sb(f1, "g2_bc", [128, D], F32)
                    gb = Buf()
                    junk = sb(f1, "junkf", [128, D], BF16)
                    junkb = Buf()
                    rk = sb(f1, "rk", [128, 8, NE], F32)
                    rkb = Buf()
                    xT32 = [sb(f1, "xT32_%d" % i, [128, 8, 128], F32) for i in range(2)]
                    xTb = [Buf(), Buf()]
                    lg = sb(f1, "lg", [128, 2, 32], F32)
                    lgb = [Buf(), Buf()]
                    k.dma(SP, g2_bc[:], n2g_d[1:2, :].partition_broadcast(128), writes=[gb], sem="g2")
                    k.dma(SP, rk[:], router_d.rearrange("(kc p) e -> p kc e", p=128), writes=[rkb], sem="rg")
                    for kc in range(8):
                        k.ts1(rk[:, kc, :], rk[:, kc, :], ppc(1, PP_G2 + kc), ALU.mult, [cb], [rkb])
                    for ti, n in enumerate(tiles):
                        T = n - 1
                        hb = ti % 2
                        rstd, sbf = rms_stats(x_tok[:, n, :], xb[n], junk[:], junkb, 1.0 / 32.0)
                        k.stt(h_all[:, T, :], x_tok[:, n, :], rstd, g2_bc[:], ALU.mult, ALU.mult,
                              [xb[n], sbf, gb], [hab[T]])
                        L_ = lg[:, hb, :]
                        lb = lgb[hb]
                        for half in range(2):
                            bk, bkb = next_bank()
                            outs = [bk[:, i * 128:(i + 1) * 128] for i in range(4)]
                            pairs = [(x_tok[:, n, (4 * half + i) * 128:(4 * half + i + 1) * 128], ident32[:, :])
                                     for i in range(4)]
                            k.mm_group(outs, pairs, [xb[n], cb], bkb, transpose=True)
                            k.act(xT32[hb][:, 4 * half:4 * half + 4, :], bk[:, :].rearrange("p (a b) -> p a b", a=4),
                                  AF.Copy, [bkb], [xTb[hb]])
                        bl, blb = next_bank()
                        k.mm_group(bl[:, 0:NE], [(xT32[hb][:, kc, :], rk[:, kc, :]) for kc in range(8)],
                                   [xTb[hb], rkb], blb)
                        k.ts1(L_[:, 0:8], bl[:, 0:NE], rstd, ALU.mult, [blb, sbf], [lb])
                        k.emit(DVE, lambda e_, o=L_[:, 24:25], i=L_[:, 0:8]: e_.reduce_max(out=o, in_=i, axis=AX.X),
                               [], [lb])
                        k.ts1(L_[:, 8:16], L_[:, 0:8], L_[:, 24:25], ALU.is_equal, [], [lb])
                        k.stt(L_[:, 16:24], L_[:, 8:16], -1e30, L_[:, 0:8], ALU.mult, ALU.add, [], [lb])
                        k.emit(DVE, lambda e_, o=L_[:, 25:26], i=L_[:, 16:24]: e_.reduce_max(out=o, in_=i, axis=AX.X),
                               [], [lb])
                        k.ts1(L_[:, 16:24], L_[:, 16:24], L_[:, 25:26], ALU.is_equal, [], [lb])
                        k.tt(sel[:, T, :], L_[:, 8:16], L_[:, 16:24], ALU.add, [lb], [selb])
                        k.tt(L_[:, 26:27], L_[:, 25:26], L_[:, 24:25], ALU.subtract, [], [lb])
                        k.act(L_[:, 26:27], L_[:, 26:27], AF.Exp, [], [lb])
                        k.ts(L_[:, 27:28], L_[:, 26:27], 1.0, 0.0, ALU.add, ALU.add, [], [lb])
                        k.recip(L_[:, 27:28], L_[:, 27:28], [], [lb])
                        k.tt(L_[:, 28:29], L_[:, 26:27], L_[:, 27:28], ALU.mult, [], [lb])
                        k.ts1(L_[:, 8:16], L_[:, 8:16], L_[:, 27:28], ALU.mult, [], [lb])
                        k.stt(gate[:, n, :], L_[:, 16:24], L_[:, 28:29], L_[:, 8:16], ALU.mult, ALU.add,
                              [lb], [gateb[n]])
                    selv = sel[:, :, :].rearrange("p t e -> p (t e)")
                    b1, b1b = next_bank()
                    k.mm_group(b1[:, 0:128], [(ustrict[:, :], selv)], [selb, cb], b1b)
                    k.copy(pos[:, :, :].rearrange("p t e -> p (t e)"), b1[:, 0:128], [b1b], [posb])
                    b2, b2b = next_bank()
                    k.mm_group(b2[:, 0:128], [(ones32[:, :], selv)], [selb, cb], b2b)
                    k.copy(tot[:, :, :].rearrange("p t e -> p (t e)"), b2[:, 0:128], [b2b], [totb])
                    k.memset(offs[:, 0, :], 0.0, [offb])
                    for T in range(1, 16):
                        k.tt(offs[:, T, :], offs[:, T - 1, :], tot[:, T - 1, :], ALU.add, [totb], [offb])
                    k.tt(pos[:, :, :], pos[:, :, :], offs[:, :, :], ALU.add, [offb], [posb])
                    k.tt(misc[:, 0:8], offs[:, 15, :], tot[:, 15, :], ALU.add, [offb, totb], [miscb])
                    for c, cap in enumerate(CAPS):
                        k.ts(misc[:, 8 + 8 * c:16 + 8 * c], misc[:, 0:8], float(cap) + 0.5, 0.0, ALU.is_lt, ALU.add,
                             [], [miscb])
                    k.copy(cond_i[:, 0:8 * len(CAPS)], misc[:, 8:8 + 8 * len(CAPS)], [miscb], [condb])
                    if FORCE_CLASS is not None:
                        for c in range(len(CAPS)):
                            k.memset(cond_i[:, 8 * c:8 * c + 8], 1 if c >= FORCE_CLASS else 0, [condb])
                    p.barrier()

                def expert_sparse(e, cap):
                    NJ = cap // 128
                    chunks = [(0, 512)] + ([(512, cap - 512)] if cap > 512 else [])
                    with ExitStack() as sp_:
                        Pb = [sb(sp_, "P%d" % i, [128, cap], BF16) for i in range(4)]
                        Pbb = [Buf() for _ in range(4)]
                        pi = [0]
                        hTe = sb(sp_, "hTe", [128, 8, cap], BF16)
                        hTeb = Buf()
                        GW = GFS * 128
                        slots = []
                        for s_ in range(2):
                            slots.append((sb(sp_, "swg%d" % s_, [128, 8, GW], BF16),
                                          sb(sp_, "swu%d" % s_, [128, 8, GW], BF16),
                                          sb(sp_, "swd%d" % s_, [128, GFS, D], BF16), Buf(), Buf()))
                        actt = [sb(sp_, "sact%d" % i, [128, GFS, cap], BF16) for i in range(2)]
                        actb = [Buf(), Buf()]
                        sg = [sb(sp_, "ssg%d" % i, [128, cap], F32) for i in range(2)]
                        sgb = [Buf(), Buf()]
                        oe32 = sb(sp_, "oe32", [128, NJ, D], F32)
                        oe_bf = sb(sp_, "oe_bf", [128, NJ, D], BF16)
                        oeb = [[Buf(), Buf()] for _ in range(NJ)]
                        oebf_b = Buf()
                        PT = [sb(sp_, "PT%d" % i, [128, NJ, 128], BF16) for i in range(2)]
                        PTb = [Buf(), Buf()]

                        def build_P(T, c0, w):
                            i = pi[0] % 4
                            pi[0] += 1
                            k.ts(Pb[i][:, 0:w], iota[:, c0:c0 + w], pos[:, T, e:e + 1], sel[:, T, e:e + 1],
                                 ALU.is_equal, ALU.mult, [cb, posb, selb], [Pbb[i]])
                            return Pb[i], Pbb[i]

                        for (c0, w) in chunks:
                            per_bank = 512 // w
                            nb = 8 // per_bank
                            bks = [next_bank() for _ in range(nb)]
                            allb = [b for _, b in bks]
                            tok = None
                            for T in range(16):
                                Pt, Ptb = build_P(T, c0, w)
                                deps = [Ptb.w, hab[T].w]
                                if T == 0:
                                    deps += k.deps_of([], allb)
                                for kc in range(8):
                                    o = bks[kc // per_bank][0][:, (kc % per_bank) * w:(kc % per_bank + 1) * w]
                                    tok = p.op(PE, (lambda e_, o=o, a=h_all[:, T, kc * 128:(kc + 1) * 128], r=Pt[:, 0:w],
                                                    s_=(T == 0), t_=(T == 15): e_.matmul(o, lhsT=a, rhs=r, start=s_, stop=t_)),
                                               deps if kc == 0 else (), kc == 7)
                                Ptb.add_read(tok)
                                hab[T].add_read(tok)
                            for b in allb:
                                b.set_write(tok)
                            for bi, (bk, bkb) in enumerate(bks):
                                k.act(hTe[:, bi * per_bank:(bi + 1) * per_bank, c0:c0 + w],
                                      bk[:, 0:per_bank * w].rearrange("p (a b) -> p a b", a=per_bank), AF.Copy,
                                      [bkb], [hTeb])
                        nch = D_FFE // 128
                        ngrp = (nch + GFS - 1) // GFS
                        ginfo = {}
                        sgi_box = [0]

                        def ffn_s1(g_):
                            c0f = g_ * GFS
                            nf = min(GFS, nch - c0f)
                            wg_s, wu_s, wd_s, wsb, wdb = slots[g_ % 2]
                            k.dma(POOL, wg_s[:, :, 0:nf * 128],
                                  mwg_d[e, :, c0f * 128:(c0f + nf) * 128].rearrange("(kc p) n -> p kc n", p=128),
                                  writes=[wsb], sem="wsA%d" % (g_ % 2))
                            k.dma(POOL, wu_s[:, :, 0:nf * 128],
                                  mwu_d[e, :, c0f * 128:(c0f + nf) * 128].rearrange("(kc p) n -> p kc n", p=128),
                                  writes=[wsb], sem="wsA%d" % (g_ % 2))
                            k.dma(POOL, wd_s[:, 0:nf, :],
                                  mwd_d[e, c0f * 128:(c0f + nf) * 128, :].rearrange("(f p) n -> p f n", p=128),
                                  writes=[wdb], sem="wsB%d" % (g_ % 2))
                            ab_i = g_ % 2
                            ginfo[g_] = (nf, wd_s, wdb, ab_i)
                            for f in range(nf):
                                fs_ = slice(f * 128, (f + 1) * 128)
                                si = sgi_box[0] % 2
                                sgi_box[0] += 1
                                for (c0, w) in chunks:
                                    if w == 512:
                                        bg, bgb = next_bank()
                                        bu, bub = next_bank()
                                        k.mm_group(bg[:, :], [(wg_s[:, kc, fs_], hTe[:, kc, c0:c0 + w]) for kc in range(8)],
                                                   [wsb, hTeb], bgb)
                                        k.mm_group(bu[:, :], [(wu_s[:, kc, fs_], hTe[:, kc, c0:c0 + w]) for kc in range(8)],
                                                   [wsb, hTeb], bub)
                                        g_ap, u_ap = bg[:, :], bu[:, :]
                                    else:
                                        bs, bsb = next_bank()
                                        deps = k.deps_of([wsb, hTeb], [bsb])
                                        tok = None
                                        for wi, w_s in enumerate((wg_s, wu_s)):
                                            for kc in range(8):
                                                tok = p.op(PE, (lambda e_, o=bs[:, wi * w:(wi + 1) * w], a=w_s[:, kc, fs_],
                                                                r=hTe[:, kc, c0:c0 + w], s_=(kc == 0), t_=(kc == 7):
                                                                e_.matmul(o, lhsT=a, rhs=r, start=s_, stop=t_)),
                                                           deps if (wi == 0 and kc == 0) else (), (wi == 1 and kc == 7))
                                        wsb.add_read(tok)
                                        hTeb.add_read(tok)
                                        bsb.set_write(tok)
                                        bgb = bub = bsb
                                        g_ap, u_ap = bs[:, 0:w], bs[:, w:2 * w]
                                    k.act(sg[si][:, c0:c0 + w], g_ap, AF.Silu, [bgb], [sgb[si]])
                                    k.tt(actt[ab_i][:, f, c0:c0 + w], sg[si][:, c0:c0 + w], u_ap, ALU.mult,
                                         [sgb[si], bub], [actb[ab_i]])

                        def ffn_s2(g_):
                            nf, wd_s, wsb, ab_i = ginfo[g_]
                            for j in range(NJ):
                                for hf in range(2):
                                    cs = slice(hf * 512, (hf + 1) * 512)
                                    bk, bkb = next_bank()
                                    k.mm_group(bk[:, :], [(actt[ab_i][:, f, j * 128:(j + 1) * 128], wd_s[:, f, cs])
                                                          for f in range(nf)], [wsb, actb[ab_i]], bkb)
                                    ob = oeb[j][hf]
                                    if g_ == 0:
                                        k.act(oe32[:, j, cs], bk[:, :], AF.Copy, [bkb], [ob])
                                    elif g_ < ngrp - 1:
                                        k.tt(oe32[:, j, cs], oe32[:, j, cs], bk[:, :], ALU.add, [bkb], [ob])
                                    else:
                                        k.tt(oe_bf[:, j, cs], oe32[:, j, cs], bk[:, :], ALU.add, [bkb, ob], [oebf_b])

                        ffn_s1(0)
                        for g_ in range(ngrp):
                            if g_ + 1 < ngrp:
                                ffn_s1(g_ + 1)
                            ffn_s2(g_)
                        Pq = {}

                        def st_x(T):
                            Pq[T] = build_P(T, 0, cap)

                        def st_y(T):
                            Pt, Ptb = Pq.pop(T)
                            bk, bkb = next_bank()
                            bkbf = bk.bitcast(BF16)
                            outs = [bkbf[:, j * 128:(j + 1) * 128] for j in range(NJ)]
                            pairs = [(Pt[:, j * 128:(j + 1) * 128], ident[:, :]) for j in range(NJ)]
                            k.mm_group(outs, pairs, [Ptb, cb], bkb, transpose=True)
                            pti = T % 2
                            k.act(PT[pti][:, :, :], bkbf[:, 0:cap].rearrange("p (j t) -> p j t", j=NJ), AF.Copy,
                                  [bkb], [PTb[pti]])

                        def st_z(T):
                            ptp = T % 2
                            n = T + 1
                            for hf in range(2):
                                cs = slice(hf * 512, (hf + 1) * 512)
                                bk2, bk2b = next_bank()
                                k.mm_group(bk2[:, :], [(PT[ptp][:, j, :], oe_bf[:, j, cs]) for j in range(NJ)],
                                           [PTb[ptp], oebf_b], bk2b)
                                k.stt(x_tok[:, n, cs], bk2[:, :], gate[:, n, e:e + 1], x_tok[:, n, cs],
                                      ALU.mult, ALU.add, [bk2b, gateb[n]], [xb[n]])

                        st_x(0)
                        st_x(1)
                        st_y(0)
                        for T in range(16):
                            if T + 2 < 16:
                                st_x(T + 2)
                            if T + 1 < 16:
                                st_y(T + 1)
                            st_z(T)
                        p.barrier()

                def expert_dense(e):
                    with ExitStack() as db:
                        h2T = sb(db, "h2T", [128, 8, NT * 128], BF16)
                        h2b = [Buf() for _ in range(NT)]
                        for n in tiles:
                            T = n - 1
                            bk, bkb = next_bank()
                            bkbf = bk.bitcast(BF16)
                            outs = [bkbf[:, kc * 128:(kc + 1) * 128] for kc in range(8)]
                            pairs = [(h_all[:, T, kc * 128:(kc + 1) * 128], ident[:, :]) for kc in range(8)]
                            k.mm_group(outs, pairs, [hab[T], cb], bkb, transpose=True)
                            k.act(h2T[:, :, n * 128:(n + 1) * 128], bkbf[:, :].rearrange("p (k t) -> p k t", k=8),
                                  AF.Copy, [bkb], [h2b[n]])
                        ffn_stream(db, True, tiles, h2T, h2b, GFS, experts=[e])
                        p.barrier()

                def cflag(c, e):
                    return cond_i[0:1, 8 * c + e:8 * c + e + 1]

                for e in range(NE):
                    p.cond_region(
                        cflag(1, e), [],
                        lambda e=e: p.cond_region(cflag(0, e), [], lambda: expert_sparse(e, CAPS[0]),
                                                  lambda: expert_sparse(e, CAPS[1])),
                        lambda e=e: p.cond_region(cflag(2, e), [], lambda: expert_sparse(e, CAPS[2]),
                                                  lambda: expert_dense(e)))
                    p.barrier()

        def final_phase(do_norm):
            with ExitStack() as os_:
                gf_bc = sb(os_, "gf_bc", [128, D], F32)
                gfb = Buf()
                outt = [sb(os_, "outt%d" % i, [128, D], F32) for i in range(2)]
                outb = [Buf(), Buf()]
                junk = sb(os_, "junko", [128, D], BF16)
                junkb = Buf()
                toks = []
                if do_norm:
                    k.dma(SP, gf_bc[:], fg_d[0:1, :].partition_broadcast(128), writes=[gfb], sem="gf")
                for n in range(1, NT):
                    if do_norm:
                        oi = n % 2
                        rstd, sbf = rms_stats(x_tok[:, n, :], xb[n], junk[:], junkb, 1.0 / 32.0)
                        k.stt(outt[oi][:], x_tok[:, n, :], rstd, gf_bc[:], ALU.mult, ALU.mult,
                              [xb[n], sbf, gfb], [outb[oi]])
                        toks.append(k.dma(SP, y_d[(n - 1) * 128:n * 128, :], outt[oi][:], reads=[outb[oi]], sem="out%d" % oi))
                    else:
                        toks.append(k.dma(SP, y_d[(n - 1) * 128:n * 128, :], x_tok[:, n, :], reads=[xb[n]], sem="out0"))
                p.wait_only(SP, toks)
                p.barrier()

        mixer_phase(0)
        if stop_after == "M0":
            final_phase(False)
            p.flush()
            return nc
        ffn_phase0()
        if stop_after == "F0":
            final_phase(False)
            p.flush()
            return nc
        k.ts1(x_tok[:, 0, :], x_tok[:, 0, :], flag[:, 0:1], ALU.mult, [cb], [xb[0]])
        mixer_phase(1)
        if stop_after == "M1":
            final_phase(False)
            p.flush()
            return nc
        moe_phase()
        final_phase(True)
        p.flush()
    return nc


def _prep_shared(inp):
    f = lambda a: np.ascontiguousarray(np.asarray(a, dtype=np.float32))
    sh = {}
    sh["ident"] = np.eye(128, dtype=np.float32)
    sh["ustrict"] = np.triu(np.ones((128, 128), np.float32), 1)
    sh["iota"] = np.ascontiguousarray(np.broadcast_to(np.arange(CAPS[-1], dtype=np.float32), (128, CAPS[-1])))
    pp = np.zeros((128, 2, NPP), np.float32)
    wins = np.array([[2.0, 4.0], [8.0, 16.0]], np.float32)
    for l in range(2):
        pp[:, l, PP_PSCALE:PP_PSCALE + 2] = f(inp["pool_scale"])[l].reshape(2, 128).T
        dw = f(inp["conv_dw_w"])[l]
        pp[:, l, PP_DWW:PP_DWW + 93] = dw.reshape(31, 3, 128).transpose(2, 1, 0).reshape(128, 93)
        pp[:, l, PP_DWB:PP_DWB + 3] = f(inp["conv_dw_b"])[l].reshape(3, 128).T
        pp[:, l, PP_LNG:PP_LNG + 3] = f(inp["conv_ln_g"])[l].reshape(3, 128).T
        pp[:, l, PP_LNB:PP_LNB + 3] = f(inp["conv_ln_b"])[l].reshape(3, 128).T
        pp[:, l, PP_PWB:PP_PWB + 3] = f(inp["conv_pw_b"])[l].reshape(3, 128).T
        for c in range(2):
            pp[0:64, l, PP_INVW + c] = 1.0 / wins[c, 0]
            pp[64:128, l, PP_INVW + c] = 1.0 / wins[c, 1]
        pp[:, l, PP_G2:PP_G2 + 8] = f(inp["norm2_g"])[l].reshape(8, 128).T
    sh["pp"] = pp.reshape(128, 2 * NPP)
    sh["norm1_g"] = f(inp["norm1_g"])
    sh["norm2_g"] = f(inp["norm2_g"])
    sh["final_g"] = f(inp["final_g"]).reshape(1, D)
    sh["gm_norm_g"] = f(inp["gm_norm_g"])
    sh["gm_b"] = f(inp["gm_b"]).reshape(2, 512)
    sh["w_in"] = f(inp["w_in"])
    sh["pool_w"] = f(inp["pool_w"])
    sh["gm_wsT"] = np.ascontiguousarray(f(inp["gm_ws"]).transpose(0, 1, 3, 2))
    sh["conv_pw_w"] = f(inp["conv_pw_w"])
    sh["w_out"] = f(inp["w_out"])
    sh["ffn_wg"] = f(inp["ffn_wg"])[0]
    sh["ffn_wu"] = f(inp["ffn_wu"])[0]
    sh["ffn_wd"] = f(inp["ffn_wd"])[0]
    sh["router"] = f(inp["moe_router"])[0]
    sh["moe_wg"] = f(inp["moe_wg"])[0]
    sh["moe_wu"] = f(inp["moe_wu"])[0]
    sh["moe_wd"] = f(inp["moe_wd"])[0]
    return sh


def _prep_core(x, c):
    b, q = c // 4, c % 4
    xin = np.zeros((NT * 128, D), np.float32)
    xin[128:] = x[b, q * 2048:(q + 1) * 2048]
    if q > 0:
        xin[:128] = x[b, q * 2048 - 128:q * 2048]
    flag = np.full((128, 1), 1.0 if q > 0 else 0.0, np.float32)
    pinv = np.zeros((128, 2, 16), np.float32)
    wins = [[2, 4], [8, 16]]
    for cc in range(2):
        for half in range(2):
            w = wins[cc][half]
            for j in range(16):
                cntv = min(j + 1, w) if q == 0 else w
                pinv[half * 64:(half + 1) * 64, cc, j] = 1.0 / cntv
    return {"x": xin, "flag": flag, "pool_inv": pinv.reshape(128, 32)}


_NC_CACHE = {}


def run(inputs, stop_after=None, trace=False):
    x = np.asarray(inputs["x"], dtype=np.float32)
    sh = _prep_shared(inputs)
    in_maps = []
    for c in range(8):
        m = dict(sh)
        m.update(_prep_core(x, c))
        in_maps.append(m)
    if stop_after not in _NC_CACHE:
        _NC_CACHE[stop_after] = build_program(stop_after)
    nc = _NC_CACHE[stop_after]
    res = run_bass_kernel_spmd(nc, in_maps, core_ids=list(range(8)), **({"trace": True} if trace else {}))
    out = np.zeros((2, 8192, D), np.float32)
    for c in range(8):
        b, q = c // 4, c % 4
        out[b, q * 2048:(q + 1) * 2048] = res.results[c]["y"]
    return out, res


def kernel(**inputs):
    out, _ = run(inputs)
    return out
```

```python
import numpy as np
from contextlib import ExitStack
import concourse.bass as bass
import concourse.mybir as mybir
from concourse.bass_utils import run_bass_kernel_spmd

F32 = mybir.dt.float32
BF16 = mybir.dt.bfloat16
I32 = mybir.dt.int32
AF = mybir.ActivationFunctionType
ALU = mybir.AluOpType
AX = mybir.AxisListType

PE, ACT, DVE, POOL, SP = "pe", "act", "dve", "pool", "sp"
ENGS = (PE, ACT, DVE, POOL, SP)

D = 1024
NT = 17
D_IN = 1792
D_FF = 2816
D_FFE = 3584
NE = 8
EPS = 1e-6
NPP = 120
PP_PSCALE = 0
PP_DWW = 2
PP_DWB = 95
PP_LNG = 98
PP_LNB = 101
PP_PWB = 104
PP_INVW = 107
PP_G2 = 109
TGM = 2
GF = 4
GFS = 2
CAPS = (512, 640, 768)
FORCE_CLASS = None


class Prog:
    def __init__(self, nc, stack):
        self.nc = nc
        self.stack = stack
        self.ops = {e: [] for e in ENGS}
        self.sems = {}
        self.cnt = {}
        self.seen = {e: {} for e in ENGS}
        for e in ENGS:
            self._mksem("eng_" + e)

    def _mksem(self, key):
        if key not in self.sems:
            self.sems[key] = self.stack.enter_context(self.nc.semaphore(key))
            self.cnt[key] = 0
        return self.sems[key]

    def _waits(self, eng, deps):
        out = []
        for d in deps:
            if d is None:
                continue
            key, val = d
            if eng == PE and key == "eng_pe":
                continue
            if self.seen[eng].get(key, 0) >= val:
                continue
            self.seen[eng][key] = val
            out.append((self.sems[key], val))
        return out

    def op(self, eng, fn, deps=(), inc=True):
        waits = self._waits(eng, deps)
        key = "eng_" + eng
        tok = None
        if inc:
            self.cnt[key] += 1
            tok = (key, self.cnt[key])
        self.ops[eng].append((waits, fn, (self.sems[key], 1) if inc else None))
        return tok

    def dma(self, eng, out, in_, semname, deps=()):
        self._mksem(semname)
        waits = self._waits(eng, deps)
        self.cnt[semname] += 16
        tok = (semname, self.cnt[semname])
        self.ops[eng].append(
            (waits, lambda e, o=out, i=in_: e.dma_start(out=o, in_=i), (self.sems[semname], 16))
        )
        return tok

    def wait_only(self, eng, deps):
        waits = self._waits(eng, deps)
        if waits:
            self.ops[eng].append((waits, None, None))

    def barrier(self):
        toks = [(k, v) for k, v in self.cnt.items() if v > 0]
        for e in ENGS:
            self.wait_only(e, toks)

    def cond_region(self, cond_ap, cond_deps, then_fn, else_fn):
        for e in ENGS:
            self.ops[e].append(("IF", cond_ap, self._waits(e, cond_deps)))
        snap_cnt = dict(self.cnt)
        snap_seen = {e: dict(d) for e, d in self.seen.items()}
        Buf.reset_all()
        then_fn()
        then_cnt = dict(self.cnt)
        then_end = {e: len(self.ops[e]) for e in ENGS}
        self.cnt = dict(snap_cnt)
        for kk in then_cnt:
            self.cnt.setdefault(kk, 0)
        self.seen = {e: dict(d) for e, d in snap_seen.items()}
        for e in ENGS:
            self.ops[e].append(("ELSE",))
        Buf.reset_all()
        else_fn()
        else_cnt = dict(self.cnt)
        keys = set(then_cnt) | set(else_cnt)
        final = {kk: max(then_cnt.get(kk, 0), else_cnt.get(kk, 0)) for kk in keys}

        def equalizers(branch_cnt):
            per_eng = {e: [] for e in ENGS}
            for kk in sorted(keys):
                diff = final[kk] - branch_cnt.get(kk, 0)
                if diff <= 0:
                    continue
                eng = kk[4:] if kk.startswith("eng_") else SP
                per_eng[eng].append(("EQ", self.sems[kk], branch_cnt.get(kk, 0), diff))
            return per_eng

        eq_then = equalizers(then_cnt)
        eq_else = equalizers(else_cnt)
        for e in ENGS:
            self.ops[e][then_end[e]:then_end[e]] = eq_then[e]
            self.ops[e].extend(eq_else[e])
            self.ops[e].append(("ENDIF",))
        self.cnt = final
        self.seen = snap_seen
        Buf.reset_all()

    def flush(self):
        nc = self.nc
        ops = self.ops

        def run(e, lst):
            cms = []
            for item in lst:
                tag = item[0]
                if tag == "IF":
                    for s_, v in item[2]:
                        e.wait_ge(s_, v)
                    val = e.value_load(item[1])
                    cm = e.If(val == 1)
                    cm.__enter__()
                    cms.append(cm)
                elif tag == "ELSE":
                    cms.pop().__exit__(None, None, None)
                    cm = e.Else()
                    cm.__enter__()
                    cms.append(cm)
                elif tag == "ENDIF":
                    cms.pop().__exit__(None, None, None)
                elif tag == "EQ":
                    _, sem, have, diff = item
                    if have > 0:
                        e.wait_ge(sem, have)
                    e.sem_inc(sem, diff)
                else:
                    waits, fn, inc = item
                    for s_, v in waits:
                        e.wait_ge(s_, v)
                    if fn is not None:
                        ins = fn(e)
                        if inc is not None:
                            ins.then_inc(inc[0], inc[1])

        with nc.Block() as block:
            @block.tensor
            def _(e):
                run(e, ops[PE])

            @block.scalar
            def _(e):
                run(e, ops[ACT])

            @block.vector
            def _(e):
                run(e, ops[DVE])

            @block.gpsimd
            def _(e):
                run(e, ops[POOL])

            @block.sync
            def _(e):
                run(e, ops[SP])
        self.ops = {e: [] for e in ENGS}


class Buf:
    ALL = []

    def __init__(self, name=""):
        self.name = name
        self.w = None
        self.r = {}
        Buf.ALL.append(self)

    @staticmethod
    def reset_all():
        for b in Buf.ALL:
            b.w = None
            b.r = {}

    def add_read(self, tok):
        if tok is None:
            return
        k, v = tok
        if self.r.get(k, 0) < v:
            self.r[k] = v

    def set_write(self, tok):
        self.w = tok
        self.r = {}


class K:
    def __init__(self, nc, stack):
        self.nc = nc
        self.p = Prog(nc, stack)
        self.dma_n = 0

    def deps_of(self, reads, writes):
        deps = []
        for b in reads:
            deps.append(b.w)
        for b in writes:
            deps.append(b.w)
            deps.extend(b.r.items())
        return deps

    def emit(self, eng, fn, reads=(), writes=()):
        tok = self.p.op(eng, fn, self.deps_of(reads, writes), True)
        for b in reads:
            b.add_read(tok)
        for b in writes:
            b.set_write(tok)
        return tok

    def dma(self, eng, out, in_, reads=(), writes=(), sem=None):
        assert sem is not None
        deps = []
        for b in reads:
            deps.append(b.w)
        for b in writes:
            if not (b.w is not None and b.w[0] == sem):
                deps.append(b.w)
            deps.extend(b.r.items())
        tok = self.p.dma(eng, out, in_, sem, deps)
        for b in reads:
            b.add_read(tok)
        for b in writes:
            b.set_write(tok)
        return tok

    def mm_group(self, out, pairs, reads, bank, transpose=False):
        deps = self.deps_of(reads, [bank])
        n = len(pairs)
        tok = None
        for i, (l, r) in enumerate(pairs):
            last = i == n - 1
            if transpose:
                fn = (lambda e, o=out[i], a=l, b=r: e.transpose(o, a, b))
            else:
                fn = (lambda e, o=out, a=l, b=r, s=(i == 0), t=last: e.matmul(o, lhsT=a, rhs=b, start=s, stop=t))
            tok = self.p.op(PE, fn, deps if i == 0 else (), last)
        for b in reads:
            b.add_read(tok)
        bank.set_write(tok)
        return tok

    def mm_multi(self, groups, reads, bank):
        deps = self.deps_of(reads, [bank])
        n = len(groups)
        tok = None
        for i, (o, l, r) in enumerate(groups):
            last = i == n - 1
            fn = (lambda e, o=o, a=l, b=r: e.matmul(o, lhsT=a, rhs=b, start=True, stop=True))
            tok = self.p.op(PE, fn, deps if i == 0 else (), last)
        for b in reads:
            b.add_read(tok)
        bank.set_write(tok)
        return tok

    def act(self, out, in_, func, reads, writes, bias=None, scale=None, accum=None):
        kw = {}
        if bias is not None:
            kw["bias"] = bias
        if scale is not None:
            kw["scale"] = scale
        if accum is not None:
            kw["accum_out"] = accum
        return self.emit(ACT, lambda e: e.activation(out=out, in_=in_, func=func, **kw), reads, writes)

    def tt(self, out, in0, in1, op, reads, writes, eng=DVE):
        return self.emit(eng, lambda e: e.tensor_tensor(out=out, in0=in0, in1=in1, op=op), reads, writes)

    def ts(self, out, in0, s1, s2, op0, op1, reads, writes, eng=DVE):
        return self.emit(eng, lambda e: e.tensor_scalar(out=out, in0=in0, scalar1=s1, scalar2=s2, op0=op0, op1=op1),
                         reads, writes)

    def ts1(self, out, in0, s1, op0, reads, writes, eng=DVE):
        return self.emit(eng, lambda e: e.tensor_scalar(out=out, in0=in0, scalar1=s1, scalar2=None, op0=op0),
                         reads, writes)

    def stt(self, out, in0, scalar, in1, op0, op1, reads, writes, accum=None, eng=DVE):
        kw = {}
        if accum is not None:
            kw["accum_out"] = accum
        return self.emit(eng, lambda e: e.scalar_tensor_tensor(out=out, in0=in0, scalar=scalar, in1=in1,
                                                               op0=op0, op1=op1, **kw), reads, writes)

    def copy(self, out, in_, reads, writes, eng=DVE):
        return self.emit(eng, lambda e: e.tensor_copy(out=out, in_=in_), reads, writes)

    def recip(self, out, in_, reads, writes):
        return self.emit(DVE, lambda e: e.reciprocal(out=out, in_=in_), reads, writes)

    def memset(self, ap, val, writes, eng=DVE):
        return self.emit(eng, lambda e: e.memset(ap, val), (), writes)


def build_program(stop_after=None):
    nc = bass.Bass("TRN2", target_bir_lowering=False)

    def din(name, shape):
        return nc.dram_tensor(name, list(shape), F32, kind="ExternalInput").ap()

    x_d = din("x", [NT * 128, D])
    flag_d = din("flag", [128, 1])
    pinv_d = din("pool_inv", [128, 32])
    ident_d = din("ident", [128, 128])
    ustrict_d = din("ustrict", [128, 128])
    iota_d = din("iota", [128, CAPS[-1]])
    pp_d = din("pp", [128, 2 * NPP])
    n1g_d = din("norm1_g", [2, D])
    n2g_d = din("norm2_g", [2, D])
    fg_d = din("final_g", [1, D])
    gmg_d = din("gm_norm_g", [2, 384])
    gmb_d = din("gm_b", [2, 512])
    w_in_d = din("w_in", [2, D, D_IN])
    pool_w_d = din("pool_w", [2, 4, 64, 64])
    wsT_d = din("gm_wsT", [2, 4, 128, 128])
    pw_d = din("conv_pw_w", [2, 384, 384])
    w_out_d = din("w_out", [2, D, D])
    fwg_d = din("ffn_wg", [D, D_FF])
    fwu_d = din("ffn_wu", [D, D_FF])
    fwd_d = din("ffn_wd", [D_FF, D])
    router_d = din("router", [D, NE])
    mwg_d = din("moe_wg", [NE, D, D_FFE])
    mwu_d = din("moe_wu", [NE, D, D_FFE])
    mwd_d = din("moe_wd", [NE, D_FFE, D])
    y_d = nc.dram_tensor("y", [16 * 128, D], F32, kind="ExternalOutput").ap()

    with ExitStack() as st:
        ARENA_WORDS = 53100
        arena = st.enter_context(nc.sbuf_tensor("arena", [128, ARENA_WORDS], F32))
        atop = [0]
        scopes = {}

        def _release(mark):
            atop[0] = mark

        def sb(stack, name, shape, dt):
            if stack is not st and not getattr(stack, "_arena_marked", False):
                stack._arena_marked = True
                stack.callback(_release, atop[0])
            n = 1
            for d_ in shape[1:]:
                n *= d_
            nbytes = n * (4 if dt in (F32, I32) else 2)
            words = ((nbytes + 3) // 4 + 7) // 8 * 8
            assert atop[0] + words <= ARENA_WORDS, ("SBUF arena overflow", name, atop[0], words)
            v = arena[:, atop[0]:atop[0] + words]
            atop[0] += words
            if dt != F32:
                v = v.bitcast(dt)
            v = v[:, 0:n]
            if len(shape) == 3:
                v = v.rearrange("p (a b) -> p a b", a=shape[1])
            return v

        k = K(nc, st)
        p = k.p

        x_tok = sb(st, "x_tok", [128, NT, D], F32)
        xb = [Buf("x%d" % n) for n in range(NT)]
        ident = sb(st, "ident", [128, 128], BF16)
        ones32 = sb(st, "ones32", [128, 128], F32)
        ustrict = sb(st, "ustrict", [128, 128], F32)
        ident32 = sb(st, "ident32", [128, 128], F32)
        iota = sb(st, "iota", [128, CAPS[-1]], F32)
        pp = sb(st, "pp", [128, 2 * NPP], F32)
        flag = sb(st, "flag", [128, 1], F32)
        pinv = sb(st, "pinv", [128, 2, 16], F32)
        stt_t = sb(st, "stats", [128, 8, 4], F32)
        gate = sb(st, "gate", [128, NT, NE], F32)
        cb = Buf("consts")
        stb = [Buf("st%d" % i) for i in range(8)]
        gateb = [Buf("gate%d" % n) for n in range(NT)]
        banks = [st.enter_context(nc.psum_tensor("bank%d" % i, [128, 512], F32)) for i in range(8)]
        bankb = [Buf("bank%d" % i) for i in range(8)]
        ring = [0]
        stat_i = [0]

        def next_bank():
            i = ring[0]
            ring[0] = (i + 1) % 8
            return banks[i], bankb[i]

        def next_stat():
            i = stat_i[0]
            stat_i[0] = (i + 1) % 8
            return stt_t[:, i, :], stb[i]

        def ppc(l, col, n=1, lo=0, hi=128):
            return pp[lo:hi, l * NPP + col: l * NPP + col + n]

        k.memset(ones32[:], 1.0, [cb])
        k.dma(POOL, ident[:], ident_d, writes=[cb], sem="consts")
        k.dma(POOL, pp[:], pp_d, writes=[cb], sem="consts")
        k.dma(POOL, ustrict[:], ustrict_d, writes=[cb], sem="consts")
        k.dma(POOL, ident32[:], ident_d, writes=[cb], sem="consts")
        k.dma(POOL, iota[:], iota_d, writes=[cb], sem="consts")
        k.dma(POOL, flag[:], flag_d, writes=[cb], sem="consts")
        k.dma(POOL, pinv[:].rearrange("p c j -> p (c j)"), pinv_d, writes=[cb], sem="consts")
        for n in range(NT):
            k.dma(SP, x_tok[:, n, :], x_d[n * 128:(n + 1) * 128, :], writes=[xb[n]], sem="x%d" % n)

        def rms_stats(src_ap, srcb, junk, junkb, scale):
            sap, sbf = next_stat()
            k.act(junk, src_ap, AF.Square, [srcb], [junkb, sbf], scale=scale, accum=sap[:, 0:1])
            k.act(sap[:, 1:2], sap[:, 0:1], AF.Sqrt, [sbf], [sbf], bias=EPS)
            k.recip(sap[:, 2:3], sap[:, 1:2], [sbf], [sbf])
            return sap[:, 2:3], sbf

        def norm_and_transpose(n, g_bc, gb, h_tok, h_tokb, junk, junkb, hT_dst, hTb):
            rstd, sbf = rms_stats(x_tok[:, n, :], xb[n], junk, junkb, 1.0 / 32.0)
            k.stt(h_tok, x_tok[:, n, :], rstd, g_bc, ALU.mult, ALU.mult, [xb[n], sbf, gb], [h_tokb])
            bk, bkb = next_bank()
            bkbf = bk.bitcast(BF16)
            outs = [bkbf[:, kc * 128:(kc + 1) * 128] for kc in range(8)]
            pairs = [(h_tok[:, kc * 128:(kc + 1) * 128], ident[:, :]) for kc in range(8)]
            k.mm_group(outs, pairs, [h_tokb, cb], bkb, transpose=True)
            k.act(hT_dst, bkbf[:, :].rearrange("p (k t) -> p k t", k=8), AF.Copy, [bkb], [hTb])
            return rstd, sbf

        def mixer_phase(l):
            with ExitStack() as ms:
                NMAX = TGM * 128
                LMAX = 32 + NMAX
                w_in_sb = sb(ms, "w_in_sb", [128, 8, D_IN], BF16)
                wo_a = sb(ms, "wo_a", [128, 2, D], BF16)
                wo_b = sb(ms, "wo_b", [128, 4, D], BF16)
                wo_c = sb(ms, "wo_c", [128, 3, D], BF16)
                pw_sb = sb(ms, "pw_sb", [128, 3, 384], BF16)
                wblk = sb(ms, "wblk", [128, 2, 128], BF16)
                wsT = sb(ms, "wsT", [128, 4, 128], BF16)
                g1_bc = sb(ms, "g1_bc", [128, D], F32)
                gmg_bc = sb(ms, "gmg_bc", [128, 384], F32)
                gmb_bc = sb(ms, "gmb_bc", [128, 512], F32)
                wb = Buf("mixw")
                h_tok = [sb(ms, "h_tok%d" % i, [128, D], BF16) for i in range(2)]
                h_tokb = [Buf(), Buf()]
                junk = sb(ms, "junk", [128, D], BF16)
                junkb = Buf()
                two = range(2)
                a_ext = [sb(ms, "a_ext%d" % i, [128, 2, LMAX], F32) for i in two]
                hc_ext = [sb(ms, "hc_ext%d" % i, [128, 3, LMAX], BF16) for i in two]
                yb = [sb(ms, "yb%d" % i, [128, 4, NMAX], BF16) for i in two]
                ab, hcb, ybb = ([Buf(), Buf()] for _ in range(3))

                def same2(name, shape, dt):
                    t_ = sb(ms, name, shape, dt)
                    return [t_, t_]

                def same2b():
                    b_ = Buf()
                    return [b_, b_]

                hT = same2("hT", [128, 8, NMAX], BF16)
                sig = same2("sig", [128, 3, NMAX], F32)
                acc = same2("acc", [128, 3, NMAX], F32)
                u_sb = same2("u_sb", [128, 4, NMAX], F32)
                y_p = same2("y_p", [128, 2, NMAX], BF16)
                ya = [sb(ms, "ya%d" % i, [128, 2, NMAX], BF16) for i in two]
                hs = same2("hs", [128, 3, NMAX], BF16)
                yc = same2("yc", [128, 3, NMAX], BF16)
                hTb, sigb, ub, ypb, hsb, ycb = (same2b() for _ in range(6))
                yab = [Buf(), Buf()]
                accb1 = [Buf(), Buf(), Buf()]
                accb = [accb1, accb1]
                dg = sb(ms, "dg", [128, 93, 128], BF16)
                dgb = Buf()
                sA = sb(ms, "sA", [128, 2, LMAX], F32)
                sB = sb(ms, "sB", [128, 2, LMAX], F32)
                tmp16 = sb(ms, "tmp16", [128, 16], F32)
                v_n = [sb(ms, "v_n%d" % i, [128, 384], BF16) for i in two]
                ztmp = sb(ms, "ztmp", [128, 512], F32)
                sq = sb(ms, "sq", [128, 3, NMAX], F32)
                mean = sb(ms, "mean", [128, NMAX], F32)
                var = sb(ms, "var", [128, NMAX], F32)
                rstdc = sb(ms, "rstdc", [128, NMAX], F32)
                sAb, sBb, t16b, ztb, sqb, meanb, varb, rsb = (Buf() for _ in range(8))
                vnb = [Buf(), Buf()]

                wblkb = Buf()
                wsTb = Buf()
                k.memset(wblk[:], 0.0, [wblkb])
                for c in range(2):
                    k.dma(POOL, wblk[0:64, c, 0:64], pool_w_d[l, 2 * c], writes=[wblkb], sem="wblk")
                    k.dma(POOL, wblk[64:128, c, 64:128], pool_w_d[l, 2 * c + 1], writes=[wblkb], sem="wblk")
                k.dma(POOL, wsT[:], wsT_d[l].rearrange("h j i -> j h i"), writes=[wsTb], sem="wsT")
                k.memset(wsT[64:128, :, 0:64], 0.0, [wsTb])
                k.dma(SP, g1_bc[:], n1g_d[l:l + 1, :].partition_broadcast(128), writes=[wb], sem="mixw")
                k.dma(SP, gmg_bc[:], gmg_d[l:l + 1, :].partition_broadcast(128), writes=[wb], sem="mixw")
                k.dma(SP, gmb_bc[:], gmb_d[l:l + 1, :].partition_broadcast(128), writes=[wb], sem="mixw")
                k.dma(POOL, w_in_sb[:], w_in_d[l].rearrange("(kc p) n -> p kc n", p=128), writes=[wb], sem="mixw")
                k.dma(POOL, wo_a[:], w_out_d[l, 0:256, :].rearrange("(c p) n -> p c n", p=128), writes=[wb], sem="mixw")
                k.dma(POOL, wo_b[0:96], w_out_d[l, 256:640, :].rearrange("(h p) n -> p h n", p=96), writes=[wb], sem="mixw")
                k.dma(POOL, wo_c[:], w_out_d[l, 640:1024, :].rearrange("(c p) n -> p c n", p=128), writes=[wb], sem="mixw")
                k.dma(POOL, pw_sb[:], pw_d[l].rearrange("(c p) n -> p c n", p=128), writes=[wb], sem="mixw")
                k.memset(a_ext[0][:, :, 0:32], 0.0, [ab[0]])
                k.memset(hc_ext[0][:, :, 0:32], 0.0, [hcb[0]])
                for i in range(93):
                    k.ts1(dg[:, i, :], ident[:, :], ppc(l, PP_DWW + i), ALU.mult, [cb], [dgb])

                groups = [(0, 1)] + [(t0, TGM) for t0 in range(1, NT, TGM)]

                def stage_ne(gi):
                    t0, nt = groups[gi]
                    for j in range(nt):
                        n = t0 + j
                        hb = (gi * TGM + j) % 2
                        rstd, sbf = rms_stats(x_tok[:, n, :], xb[n], junk[:], junkb, 1.0 / 32.0)
                        k.stt(h_tok[hb][:], x_tok[:, n, :], rstd, g1_bc[:], ALU.mult, ALU.mult,
                              [xb[n], sbf, wb], [h_tokb[hb]])

                def stage_nt(gi):
                    t0, nt = groups[gi]
                    q = gi % 2
                    for j in range(nt):
                        hb = (gi * TGM + j) % 2
                        bk, bkb = next_bank()
                        bkbf = bk.bitcast(BF16)
                        outs = [bkbf[:, kc * 128:(kc + 1) * 128] for kc in range(8)]
                        pairs = [(h_tok[hb][:, kc * 128:(kc + 1) * 128], ident[:, :]) for kc in range(8)]
                        k.mm_group(outs, pairs, [h_tokb[hb], cb], bkb, transpose=True)
                        k.act(hT[q][:, :, j * 128:(j + 1) * 128], bkbf[:, :].rearrange("p (k t) -> p k t", k=8),
                              AF.Copy, [bkb], [hTb[q]])

                def stage_p(gi):
                    t0, nt = groups[gi]
                    q = gi % 2
                    N = nt * 128
                    L = 32 + N
                    full = not (l == 1 and t0 == 0)
                    first_own = (t0 == 1)
                    if gi > 0:
                        pN = groups[gi - 1][1] * 128
                        k.copy(a_ext[q][:, :, 0:32], a_ext[1 - q][:, :, pN:pN + 32], [ab[1 - q]], [ab[q]], eng=POOL)
                        k.copy(hc_ext[q][:, :, 0:32], hc_ext[1 - q][:, :, pN:pN + 32], [hcb[1 - q]], [hcb[q]], eng=POOL)

                    def proj(col0, m):
                        bk, bkb = next_bank()
                        pairs = [(w_in_sb[:, kc, col0:col0 + m], hT[q][:, kc, 0:N]) for kc in range(8)]
                        k.mm_group(bk[0:m, 0:N], pairs, [wb, hTb[q]], bkb)
                        return bk, bkb

                    for c in range(2):
                        bk, bkb = proj(c * 128, 128)
                        k.act(a_ext[q][:, c, 32:L], bk[:, 0:N], AF.Copy, [bkb], [ab[q]])
                    vinfo = []
                    if full:
                        for j in range(nt):
                            vb = j % 2
                            bk, bkb = next_bank()
                            pairs = [(hT[q][:, kc, j * 128:(j + 1) * 128], w_in_sb[:, kc, 640:1024]) for kc in range(8)]
                            k.mm_group(bk[:, 0:384], pairs, [wb, hTb[q]], bkb)
                            rstd, sbf = rms_stats(bk[:, 0:384], bkb, junk[:, 0:384], junkb, float(384.0 ** -0.5))
                            k.stt(v_n[vb][:], bk[:, 0:384], rstd, gmg_bc[:], ALU.mult, ALU.mult,
                                  [bkb, sbf, wb], [vnb[vb]])
                    for c in range(3):
                        bk, bkb = proj(1408 + c * 128, 128)
                        k.act(sig[q][:, c, 0:N], bk[:, 0:N], AF.Sigmoid, [bkb], [sigb[q]])
                    for c in range(3):
                        bk, bkb = proj(1024 + c * 128, 128)
                        k.tt(hc_ext[q][:, c, 32:L], bk[:, 0:N], sig[q][:, c, 0:N], ALU.mult, [bkb, sigb[q]], [hcb[q]])
                    if full:
                        for h in range(4):
                            bk, bkb = proj(256 + h * 96, 96)
                            k.act(u_sb[q][0:96, h, 0:N], bk[0:96, 0:N], AF.Copy, [bkb], [ub[q]])
                        for j in range(nt):
                            vb = j % 2
                            zk, zkb = next_bank()
                            grp = [(zk[0:96, h * 128:(h + 1) * 128], v_n[vb][:, h * 96:(h + 1) * 96], wsT[:, h, :])
                                   for h in range(4)]
                            k.mm_multi(grp, [vnb[vb], wsTb], zkb)
                            k.tt(ztmp[0:96, :], zk[0:96, :], gmb_bc[0:96, :], ALU.add, [zkb, wb], [ztb])
                            k.tt(yb[q][0:96, :, j * 128:(j + 1) * 128],
                                 ztmp[0:96, :].rearrange("p (h i) -> p h i", h=4),
                                 u_sb[q][0:96, :, j * 128:(j + 1) * 128], ALU.mult, [ztb, ub[q]], [ybb[q]])
                        A_ = a_ext[q]

                        def pool_out(sbuf_t, sbuf_b, lo, hi, c):
                            k.stt(y_p[q][lo:hi, c, 0:N], sbuf_t[lo:hi, c, 32:L], ppc(l, PP_INVW + c, 1, lo, hi),
                                  A_[lo:hi, c, 32:L], ALU.mult, ALU.subtract, [sbuf_b, ab[q], cb], [ypb[q]])
                            if first_own:
                                k.tt(tmp16[lo:hi, :], sbuf_t[lo:hi, c, 32:48], pinv[lo:hi, c, :], ALU.mult,
                                     [sbuf_b, cb], [t16b])
                                k.tt(y_p[q][lo:hi, c, 0:16], tmp16[lo:hi, :], A_[lo:hi, c, 32:48], ALU.subtract,
                                     [t16b, ab[q]], [ypb[q]])

                        k.tt(sA[:, :, 1:L], A_[:, :, 1:L], A_[:, :, 0:L - 1], ALU.add, [ab[q]], [sAb])
                        pool_out(sA, sAb, 0, 64, 0)
                        k.tt(sB[:, :, 3:L], sA[:, :, 3:L], sA[:, :, 1:L - 2], ALU.add, [sAb], [sBb])
                        pool_out(sB, sBb, 64, 128, 0)
                        k.tt(sA[:, :, 7:L], sB[:, :, 7:L], sB[:, :, 3:L - 4], ALU.add, [sBb], [sAb])
                        pool_out(sA, sAb, 0, 64, 1)
                        k.tt(sB[:, :, 15:L], sA[:, :, 15:L], sA[:, :, 7:L - 8], ALU.add, [sAb], [sBb])
                        pool_out(sB, sBb, 64, 128, 1)

                def stage_b1a(gi):
                    t0, nt = groups[gi]
                    q = gi % 2
                    N = nt * 128
                    L = 32 + N
                    full = not (l == 1 and t0 == 0)
                    first_own = (t0 == 1)
                    if not full:
                        return
                    A_ = a_ext[q]
                    H_ = hc_ext[q]

                    for c in range(2):
                        bk, bkb = next_bank()
                        k.mm_group(bk[:, 0:N], [(wblk[:, c, :], y_p[q][:, c, 0:N])], [wblkb, ypb[q]], bkb)
                        k.act(ya[q][:, c, 0:N], bk[:, 0:N], AF.Copy, [bkb, cb], [yab[q]], scale=ppc(l, PP_PSCALE + c))

                    for c in range(3):
                        bk, bkb = next_bank()
                        pairs = [(dg[:, c * 31 + kk, :], H_[:, c, 2 + kk:2 + kk + N]) for kk in range(31)]
                        k.mm_group(bk[:, 0:N], pairs, [dgb, hcb[q]], bkb)
                        k.act(acc[q][:, c, 0:N], bk[:, 0:N], AF.Identity, [bkb, cb], [accb[q][c]],
                              bias=ppc(l, PP_DWB + c))
                    for c in range(3):
                        k.act(sq[:, c, 0:N], acc[q][:, c, 0:N], AF.Square, [accb[q][c]], [sqb])

                def stage_b1b(gi):
                    t0, nt = groups[gi]
                    q = gi % 2
                    N = nt * 128
                    full = not (l == 1 and t0 == 0)
                    if not full:
                        return
                    b1, b1b = next_bank()
                    k.mm_group(b1[:, 0:N], [(ones32[:, :], acc[q][:, c, 0:N]) for c in range(3)], accb[q] + [cb], b1b)
                    b2, b2b = next_bank()
                    k.mm_group(b2[:, 0:N], [(ones32[:, :], sq[:, c, 0:N]) for c in range(3)], [sqb, cb], b2b)
                    k.ts(mean[:, 0:N], b1[:, 0:N], 1.0 / 384.0, 0.0, ALU.mult, ALU.add, [b1b], [meanb])
                    k.tt(var[:, 0:N], mean[:, 0:N], mean[:, 0:N], ALU.mult, [meanb], [varb])
                    k.stt(var[:, 0:N], b2[:, 0:N], 1.0 / 384.0, var[:, 0:N], ALU.mult, ALU.subtract,
                          [b2b], [varb])
                    k.act(var[:, 0:N], var[:, 0:N], AF.Sqrt, [], [varb], bias=EPS)
                    k.recip(rstdc[:, 0:N], var[:, 0:N], [varb], [rsb])
                    for c in range(3):
                        k.tt(sq[:, c, 0:N], acc[q][:, c, 0:N], mean[:, 0:N], ALU.subtract, [accb[q][c], meanb], [sqb])
                        k.tt(sq[:, c, 0:N], sq[:, c, 0:N], rstdc[:, 0:N], ALU.mult, [rsb], [sqb])
                        k.act(hs[q][:, c, 0:N], sq[:, c, 0:N], AF.Silu, [sqb, cb], [hsb[q]],
                              bias=ppc(l, PP_LNB + c), scale=ppc(l, PP_LNG + c))
                def stage_b2(gi):
                    t0, nt = groups[gi]
                    q = gi % 2
                    N = nt * 128
                    full = not (l == 1 and t0 == 0)
                    if not full:
                        return
                    for co in range(3):
                        bk, bkb = next_bank()
                        pairs = [(pw_sb[:, ci, co * 128:(co + 1) * 128], hs[q][:, ci, 0:N]) for ci in range(3)]
                        k.mm_group(bk[:, 0:N], pairs, [wb, hsb[q]], bkb)
                        k.act(yc[q][:, co, 0:N], bk[:, 0:N], AF.Identity, [bkb, cb], [ycb[q]], bias=ppc(l, PP_PWB + co))

                    for j in range(nt):
                        n = t0 + j
                        ts_ = slice(j * 128, (j + 1) * 128)
                        for hf in range(2):
                            cs = slice(hf * 512, (hf + 1) * 512)
                            pairs = [(ya[q][:, c, ts_], wo_a[:, c, cs]) for c in range(2)]
                            pairs += [(yb[q][0:96, h, ts_], wo_b[0:96, h, cs]) for h in range(4)]
                            pairs += [(yc[q][:, c, ts_], wo_c[:, c, cs]) for c in range(3)]
                            bk, bkb = next_bank()
                            k.mm_group(bk[:, :], pairs, [wb, yab[q], ybb[q], ycb[q]], bkb)
                            k.tt(x_tok[:, n, cs], x_tok[:, n, cs], bk[:, :], ALU.add, [bkb], [xb[n]])

                ng = len(groups)
                stage_ne(0)
                stage_nt(0)
                if ng > 1:
                    stage_ne(1)
                stage_p(0)
                for gi in range(ng):
                    if gi + 1 < ng:
                        stage_nt(gi + 1)
                    stage_b1a(gi)
                    if gi >= 1:
                        stage_b2(gi - 1)
                    if gi + 2 < ng:
                        stage_ne(gi + 2)
                    if gi + 1 < ng:
                        stage_p(gi + 1)
                    stage_b1b(gi)
                stage_b2(ng - 1)
                p.barrier()

        def ffn_stream(scope, moe, tiles, h2T, h2b, gf, experts=None):
            GW = gf * 128
            slots = []
            for s_ in range(2):
                slots.append((sb(scope, "wg%d" % s_, [128, 8, GW], BF16),
                              sb(scope, "wu%d" % s_, [128, 8, GW], BF16),
                              sb(scope, "wd%d" % s_, [128, gf, D], BF16), Buf()))
            actt = [sb(scope, "act%d" % i, [128, gf, 512], BF16) for i in range(2)]
            actb = [Buf(), Buf()]
            sg = [sb(scope, "sg%d" % i, [128, 512], F32) for i in range(2)]
            sgb = [Buf(), Buf()]
            glist = []
            if not moe:
                nch = D_FF // 128
                for c0 in range(0, nch, gf):
                    nf = min(gf, nch - c0)
                    glist.append((fwg_d[:, c0 * 128:(c0 + nf) * 128], fwu_d[:, c0 * 128:(c0 + nf) * 128],
                                  fwd_d[c0 * 128:(c0 + nf) * 128, :], nf, None))
            else:
                nch = D_FFE // 128
                for e in (experts if experts is not None else range(NE)):
                    for c0 in range(0, nch, gf):
                        nf = min(gf, nch - c0)
                        glist.append((mwg_d[e, :, c0 * 128:(c0 + nf) * 128],
                                      mwu_d[e, :, c0 * 128:(c0 + nf) * 128],
                                      mwd_d[e, c0 * 128:(c0 + nf) * 128, :], nf, e))
            tgroups = [tiles[i:i + 4] for i in range(0, len(tiles), 4)]
            cnt = 0
            sgi = 0
            for gi, (wg_ap, wu_ap, wd_ap, nf, e) in enumerate(glist):
                wg_s, wu_s, wd_s, wsb = slots[gi % 2]
                sem = "wslot%d" % (gi % 2)
                k.dma(POOL, wg_s[:, :, 0:nf * 128], wg_ap.rearrange("(kc p) n -> p kc n", p=128),
                      writes=[wsb], sem=sem)
                k.dma(POOL, wu_s[:, :, 0:nf * 128], wu_ap.rearrange("(kc p) n -> p kc n", p=128),
                      writes=[wsb], sem=sem)
                k.dma(POOL, wd_s[:, 0:nf, :], wd_ap.rearrange("(f p) n -> p f n", p=128),
                      writes=[wsb], sem=sem)
                for tg in tgroups:
                    ab_i = cnt % 2
                    cnt += 1
                    t_lo = tg[0] * 128
                    N = len(tg) * 128
                    for f in range(nf):
                        bg, bgb = next_bank()
                        k.mm_group(bg[:, 0:N], [(wg_s[:, kc, f * 128:(f + 1) * 128], h2T[:, kc, t_lo:t_lo + N])
                                                for kc in range(8)], [wsb] + [h2b[n] for n in tg], bgb)
                        bu, bub = next_bank()
                        k.mm_group(bu[:, 0:N], [(wu_s[:, kc, f * 128:(f + 1) * 128], h2T[:, kc, t_lo:t_lo + N])
                                                for kc in range(8)], [wsb] + [h2b[n] for n in tg], bub)
                        si = sgi % 2
                        sgi += 1
                        k.act(sg[si][:, 0:N], bg[:, 0:N], AF.Silu, [bgb], [sgb[si]])
                        k.tt(actt[ab_i][:, f, 0:N], sg[si][:, 0:N], bu[:, 0:N], ALU.mult, [sgb[si], bub],
                             [actb[ab_i]])
                    for j, n in enumerate(tg):
                        for hf in range(2):
                            cs = slice(hf * 512, (hf + 1) * 512)
                            bk, bkb = next_bank()
                            k.mm_group(bk[:, :], [(actt[ab_i][:, f, j * 128:(j + 1) * 128], wd_s[:, f, cs])
                                                  for f in range(nf)], [wsb, actb[ab_i]], bkb)
                            if moe:
                                k.stt(x_tok[:, n, cs], bk[:, :], gate[:, n, e:e + 1], x_tok[:, n, cs],
                                      ALU.mult, ALU.add, [bkb, gateb[n]], [xb[n]])
                            else:
                                k.tt(x_tok[:, n, cs], x_tok[:, n, cs], bk[:, :], ALU.add, [bkb], [xb[n]])

        def ffn_phase0():
            tiles = list(range(0, NT))
            with ExitStack() as fs:
                h2T = sb(fs, "h2T", [128, 8, NT * 128], BF16)
                h2b = [Buf() for _ in range(NT)]
                g2_bc = sb(fs, "g2_bc", [128, D], F32)
                gb = Buf()
                h_tok = [sb(fs, "h_tokf%d" % i, [128, D], BF16) for i in range(2)]
                h_tokb = [Buf(), Buf()]
                junk = sb(fs, "junkf", [128, D], BF16)
                junkb = Buf()
                k.dma(SP, g2_bc[:], n2g_d[0:1, :].partition_broadcast(128), writes=[gb], sem="g2")
                for ti, n in enumerate(tiles):
                    hb = ti % 2
                    norm_and_transpose(n, g2_bc[:], gb, h_tok[hb][:], h_tokb[hb], junk[:], junkb,
                                       h2T[:, :, n * 128:(n + 1) * 128], h2b[n])
                ffn_stream(fs, False, tiles, h2T, h2b, GF)
                p.barrier()

        def moe_phase():
            tiles = list(range(1, NT))
            with ExitStack() as fs:
                h_all = sb(fs, "h_all", [128, 16, D], BF16)
                hab = [Buf() for _ in range(16)]
                sel = sb(fs, "sel", [128, 16, NE], F32)
                pos = sb(fs, "pos", [128, 16, NE], F32)
                tot = sb(fs, "tot", [128, 16, NE], F32)
                offs = sb(fs, "offs", [128, 16, NE], F32)
                misc = sb(fs, "rmisc", [128, 40], F32)
                cond_i = sb(fs, "cond_i", [128, 32], I32)
                selb, posb, totb, offb, miscb, condb = (Buf() for _ in range(6))
                with ExitStack() as f1:
                    g2_bc = sb(f1, "g2_bc", [128, D], F32)
                    gb = Buf()
                    junk = sb(f1, "junkf", [128, D], BF16)
                    junkb = Buf()
                    rk = sb(f1, "rk", [128, 8, NE], F32)
                    rkb = Buf()
                    xT32 = [sb(f1, "xT32_%d" % i, [128, 8, 128], F32) for i in range(2)]
                    xTb = [Buf(), Buf()]
                    lg3 = sb(f1, "lg3", [128, 16, NE], F32)
                    lg3b = Buf()
                    gw = sb(f1, "gw", [128, 8, 16], F32)
                    mk1 = sb(f1, "mk1", [128, 16, NE], F32)
                    mk2 = sb(f1, "mk2", [128, 16, NE], F32)
                    gsb = Buf()
                    k.dma(SP, g2_bc[:], n2g_d[1:2, :].partition_broadcast(128), writes=[gb], sem="g2")
                    k.dma(SP, rk[:], router_d.rearrange("(kc p) e -> p kc e", p=128), writes=[rkb], sem="rg")
                    for kc in range(8):
                        k.ts1(rk[:, kc, :], rk[:, kc, :], ppc(1, PP_G2 + kc), ALU.mult, [cb], [rkb])
                    for ti, n in enumerate(tiles):
                        T = n - 1
                        hb = ti % 2
                        rstd, sbf = rms_stats(x_tok[:, n, :], xb[n], junk[:], junkb, 1.0 / 32.0)
                        k.stt(h_all[:, T, :], x_tok[:, n, :], rstd, g2_bc[:], ALU.mult, ALU.mult,
                              [xb[n], sbf, gb], [hab[T]])
                        for half in range(2):
                            bk, bkb = next_bank()
                            outs = [bk[:, i * 128:(i + 1) * 128] for i in range(4)]
                            pairs = [(x_tok[:, n, (4 * half + i) * 128:(4 * half + i + 1) * 128], ident32[:, :])
                                     for i in range(4)]
                            k.mm_group(outs, pairs, [xb[n], cb], bkb, transpose=True)
                            k.act(xT32[hb][:, 4 * half:4 * half + 4, :], bk[:, :].rearrange("p (a b) -> p a b", a=4),
                                  AF.Copy, [bkb], [xTb[hb]])
                        bl, blb = next_bank()
                        k.mm_group(bl[:, 0:NE], [(xT32[hb][:, kc, :], rk[:, kc, :]) for kc in range(8)],
                                   [xTb[hb], rkb], blb)
                        k.ts1(lg3[:, T, :], bl[:, 0:NE], rstd, ALU.mult, [blb, sbf], [lg3b])
                    def bc(v):
                        return v.unsqueeze(2).to_broadcast([128, 16, NE])

                    k.emit(DVE, lambda e_: e_.reduce_max(out=gw[:, 0, :], in_=lg3[:, :, :], axis=AX.X), [lg3b], [gsb])
                    k.tt(mk1[:, :, :], lg3[:, :, :], bc(gw[:, 0, :]), ALU.is_equal, [lg3b], [gsb])
                    k.stt(mk2[:, :, :], mk1[:, :, :], -1e30, lg3[:, :, :], ALU.mult, ALU.add, [lg3b], [gsb])
                    k.emit(DVE, lambda e_: e_.reduce_max(out=gw[:, 1, :], in_=mk2[:, :, :], axis=AX.X), [], [gsb])
                    k.tt(mk2[:, :, :], mk2[:, :, :], bc(gw[:, 1, :]), ALU.is_equal, [], [gsb])
                    k.tt(sel[:, :, :], mk1[:, :, :], mk2[:, :, :], ALU.add, [gsb], [selb])
                    k.tt(gw[:, 2, :], gw[:, 1, :], gw[:, 0, :], ALU.subtract, [], [gsb])
                    k.act(gw[:, 2, :], gw[:, 2, :], AF.Exp, [], [gsb])
                    k.ts(gw[:, 3, :], gw[:, 2, :], 1.0, 0.0, ALU.add, ALU.add, [], [gsb])
                    k.recip(gw[:, 3, :], gw[:, 3, :], [], [gsb])
                    k.tt(gw[:, 4, :], gw[:, 2, :], gw[:, 3, :], ALU.mult, [], [gsb])
                    k.tt(mk1[:, :, :], mk1[:, :, :], bc(gw[:, 3, :]), ALU.mult, [], [gsb])
                    k.tt(mk2[:, :, :], mk2[:, :, :], bc(gw[:, 4, :]), ALU.mult, [], [gsb])
                    k.tt(gate[:, 1:NT, :], mk1[:, :, :], mk2[:, :, :], ALU.add, [gsb], [gateb[n] for n in tiles])
                    selv = sel[:, :, :].rearrange("p t e -> p (t e)")
                    b1, b1b = next_bank()
                    k.mm_group(b1[:, 0:128], [(ustrict[:, :], selv)], [selb, cb], b1b)
                    k.copy(pos[:, :, :].rearrange("p t e -> p (t e)"), b1[:, 0:128], [b1b], [posb])
                    b2, b2b = next_bank()
                    k.mm_group(b2[:, 0:128], [(ones32[:, :], selv)], [selb, cb], b2b)
                    k.copy(tot[:, :, :].rearrange("p t e -> p (t e)"), b2[:, 0:128], [b2b], [totb])
                    k.memset(offs[:, 0, :], 0.0, [offb])
                    for T in range(1, 16):
                        k.tt(offs[:, T, :], offs[:, T - 1, :], tot[:, T - 1, :], ALU.add, [totb], [offb])
                    k.tt(pos[:, :, :], pos[:, :, :], offs[:, :, :], ALU.add, [offb], [posb])
                    k.tt(misc[:, 0:8], offs[:, 15, :], tot[:, 15, :], ALU.add, [offb, totb], [miscb])
                    for c, cap in enumerate(CAPS):
                        k.ts(misc[:, 8 + 8 * c:16 + 8 * c], misc[:, 0:8], float(cap) + 0.5, 0.0, ALU.is_lt, ALU.add,
                             [], [miscb])
                    k.copy(cond_i[:, 0:8 * len(CAPS)], misc[:, 8:8 + 8 * len(CAPS)], [miscb], [condb])
                    if FORCE_CLASS is not None:
                        for c in range(len(CAPS)):
                            k.memset(cond_i[:, 8 * c:8 * c + 8], 1 if c >= FORCE_CLASS else 0, [condb])
                    p.barrier()

                def expert_sparse(e, cap):
                    NJ = cap // 128
                    chunks = [(0, 512)] + ([(512, cap - 512)] if cap > 512 else [])
                    with ExitStack() as sp_:
                        Pb = [sb(sp_, "P%d" % i, [128, cap], BF16) for i in range(4)]
                        Pbb = [Buf() for _ in range(4)]
                        pi = [0]
                        hTe = sb(sp_, "hTe", [128, 8, cap], BF16)
                        hTeb = Buf()
                        GW = GFS * 128
                        slots = []
                        for s_ in range(2):
                            slots.append((sb(sp_, "swg%d" % s_, [128, 8, GW], BF16),
                                          sb(sp_, "swu%d" % s_, [128, 8, GW], BF16),
                                          sb(sp_, "swd%d" % s_, [128, GFS, D], BF16), Buf(), Buf()))
                        actt = [sb(sp_, "sact%d" % i, [128, GFS, cap], BF16) for i in range(2)]
                        actb = [Buf(), Buf()]
                        sg = [sb(sp_, "ssg%d" % i, [128, cap], F32) for i in range(2)]
                        sgb = [Buf(), Buf()]
                        oe32 = sb(sp_, "oe32", [128, NJ, D], F32)
                        oe_bf = sb(sp_, "oe_bf", [128, NJ, D], BF16)
                        oeb = [[Buf(), Buf()] for _ in range(NJ)]
                        oebf_b = Buf()
                        PT = [sb(sp_, "PT%d" % i, [128, NJ, 128], BF16) for i in range(2)]
                        PTb = [Buf(), Buf()]

                        def build_P(T, c0, w):
                            i = pi[0] % 4
                            pi[0] += 1
                            k.ts(Pb[i][:, 0:w], iota[:, c0:c0 + w], pos[:, T, e:e + 1], sel[:, T, e:e + 1],
                                 ALU.is_equal, ALU.mult, [cb, posb, selb], [Pbb[i]])
                            return Pb[i], Pbb[i]

                        for (c0, w) in chunks:
                            per_bank = 512 // w
                            nb = 8 // per_bank
                            bks = [next_bank() for _ in range(nb)]
                            allb = [b for _, b in bks]
                            tok = None
                            for T in range(16):
                                Pt, Ptb = build_P(T, c0, w)
                                deps = [Ptb.w, hab[T].w]
                                if T == 0:
                                    deps += k.deps_of([], allb)
                                for kc in range(8):
                                    o = bks[kc // per_bank][0][:, (kc % per_bank) * w:(kc % per_bank + 1) * w]
                                    tok = p.op(PE, (lambda e_, o=o, a=h_all[:, T, kc * 128:(kc + 1) * 128], r=Pt[:, 0:w],
                                                    s_=(T == 0), t_=(T == 15): e_.matmul(o, lhsT=a, rhs=r, start=s_, stop=t_)),
                                               deps if kc == 0 else (), kc == 7)
                                Ptb.add_read(tok)
                                hab[T].add_read(tok)
                            for b in allb:
                                b.set_write(tok)
                            for bi, (bk, bkb) in enumerate(bks):
                                k.act(hTe[:, bi * per_bank:(bi + 1) * per_bank, c0:c0 + w],
                                      bk[:, 0:per_bank * w].rearrange("p (a b) -> p a b", a=per_bank), AF.Copy,
                                      [bkb], [hTeb])
                        nch = D_FFE // 128
                        ngrp = (nch + GFS - 1) // GFS
                        ginfo = {}
                        sgi_box = [0]

                        def ffn_s1(g_):
                            c0f = g_ * GFS
                            nf = min(GFS, nch - c0f)
                            wg_s, wu_s, wd_s, wsb, wdb = slots[g_ % 2]
                            k.dma(POOL, wg_s[:, :, 0:nf * 128],
                                  mwg_d[e, :, c0f * 128:(c0f + nf) * 128].rearrange("(kc p) n -> p kc n", p=128),
                                  writes=[wsb], sem="wsA%d" % (g_ % 2))
                            k.dma(POOL, wu_s[:, :, 0:nf * 128],
                                  mwu_d[e, :, c0f * 128:(c0f + nf) * 128].rearrange("(kc p) n -> p kc n", p=128),
                                  writes=[wsb], sem="wsA%d" % (g_ % 2))
                            k.dma(POOL, wd_s[:, 0:nf, :],
                                  mwd_d[e, c0f * 128:(c0f + nf) * 128, :].rearrange("(f p) n -> p f n", p=128),
                                  writes=[wdb], sem="wsB%d" % (g_ % 2))
                            ab_i = g_ % 2
                            ginfo[g_] = (nf, wd_s, wdb, ab_i)
                            for f in range(nf):
                                fs_ = slice(f * 128, (f + 1) * 128)
                                si = sgi_box[0] % 2
                                sgi_box[0] += 1
                                for (c0, w) in chunks:
                                    if w == 512:
                                        bg, bgb = next_bank()
                                        bu, bub = next_bank()
                                        k.mm_group(bg[:, :], [(wg_s[:, kc, fs_], hTe[:, kc, c0:c0 + w]) for kc in range(8)],
                                                   [wsb, hTeb], bgb)
                                        k.mm_group(bu[:, :], [(wu_s[:, kc, fs_], hTe[:, kc, c0:c0 + w]) for kc in range(8)],
                                                   [wsb, hTeb], bub)
                                        g_ap, u_ap = bg[:, :], bu[:, :]
                                    else:
                                        bs, bsb = next_bank()
                                        deps = k.deps_of([wsb, hTeb], [bsb])
                                        tok = None
                                        for wi, w_s in enumerate((wg_s, wu_s)):
                                            for kc in range(8):
                                                tok = p.op(PE, (lambda e_, o=bs[:, wi * w:(wi + 1) * w], a=w_s[:, kc, fs_],
                                                                r=hTe[:, kc, c0:c0 + w], s_=(kc == 0), t_=(kc == 7):
                                                                e_.matmul(o, lhsT=a, rhs=r, start=s_, stop=t_)),
                                                           deps if (wi == 0 and kc == 0) else (), (wi == 1 and kc == 7))
                                        wsb.add_read(tok)
                                        hTeb.add_read(tok)
                                        bsb.set_write(tok)
                                        bgb = bub = bsb
                                        g_ap, u_ap = bs[:, 0:w], bs[:, w:2 * w]
                                    k.act(sg[si][:, c0:c0 + w], g_ap, AF.Silu, [bgb], [sgb[si]])
                                    k.tt(actt[ab_i][:, f, c0:c0 + w], sg[si][:, c0:c0 + w], u_ap, ALU.mult,
                                         [sgb[si], bub], [actb[ab_i]])

                        def ffn_s2(g_):
                            nf, wd_s, wsb, ab_i = ginfo[g_]
                            for j in range(NJ):
                                for hf in range(2):
                                    cs = slice(hf * 512, (hf + 1) * 512)
                                    bk, bkb = next_bank()
                                    k.mm_group(bk[:, :], [(actt[ab_i][:, f, j * 128:(j + 1) * 128], wd_s[:, f, cs])
                                                          for f in range(nf)], [wsb, actb[ab_i]], bkb)
                                    ob = oeb[j][hf]
                                    if g_ == 0:
                                        k.act(oe32[:, j, cs], bk[:, :], AF.Copy, [bkb], [ob])
                                    elif g_ < ngrp - 1:
                                        k.tt(oe32[:, j, cs], oe32[:, j, cs], bk[:, :], ALU.add, [bkb], [ob])
                                    else:
                                        k.tt(oe_bf[:, j, cs], oe32[:, j, cs], bk[:, :], ALU.add, [bkb, ob], [oebf_b])

                        ffn_s1(0)
                        for g_ in range(ngrp):
                            if g_ + 1 < ngrp:
                                ffn_s1(g_ + 1)
                            ffn_s2(g_)
                        Pq = {}

                        def st_x(T):
                            Pq[T] = build_P(T, 0, cap)

                        def st_y(T):
                            Pt, Ptb = Pq.pop(T)
                            bk, bkb = next_bank()
                            bkbf = bk.bitcast(BF16)
                            outs = [bkbf[:, j * 128:(j + 1) * 128] for j in range(NJ)]
                            pairs = [(Pt[:, j * 128:(j + 1) * 128], ident[:, :]) for j in range(NJ)]
                            k.mm_group(outs, pairs, [Ptb, cb], bkb, transpose=True)
                            pti = T % 2
                            k.act(PT[pti][:, :, :], bkbf[:, 0:cap].rearrange("p (j t) -> p j t", j=NJ), AF.Copy,
                                  [bkb], [PTb[pti]])

                        def st_z(T):
                            ptp = T % 2
                            n = T + 1
                            for hf in range(2):
                                cs = slice(hf * 512, (hf + 1) * 512)
                                bk2, bk2b = next_bank()
                                k.mm_group(bk2[:, :], [(PT[ptp][:, j, :], oe_bf[:, j, cs]) for j in range(NJ)],
                                           [PTb[ptp], oebf_b], bk2b)
                                k.stt(x_tok[:, n, cs], bk2[:, :], gate[:, n, e:e + 1], x_tok[:, n, cs],
                                      ALU.mult, ALU.add, [bk2b, gateb[n]], [xb[n]])

                        st_x(0)
                        st_x(1)
                        st_y(0)
                        for T in range(16):
                            if T + 2 < 16:
                                st_x(T + 2)
                            if T + 1 < 16:
                                st_y(T + 1)
                            st_z(T)
                        p.barrier()

                def expert_dense(e):
                    with ExitStack() as db:
                        h2T = sb(db, "h2T", [128, 8, NT * 128], BF16)
                        h2b = [Buf() for _ in range(NT)]
                        for n in tiles:
                            T = n - 1
                            bk, bkb = next_bank()
                            bkbf = bk.bitcast(BF16)
                            outs = [bkbf[:, kc * 128:(kc + 1) * 128] for kc in range(8)]
                            pairs = [(h_all[:, T, kc * 128:(kc + 1) * 128], ident[:, :]) for kc in range(8)]
                            k.mm_group(outs, pairs, [hab[T], cb], bkb, transpose=True)
                            k.act(h2T[:, :, n * 128:(n + 1) * 128], bkbf[:, :].rearrange("p (k t) -> p k t", k=8),
                                  AF.Copy, [bkb], [h2b[n]])
                        ffn_stream(db, True, tiles, h2T, h2b, GFS, experts=[e])
                        p.barrier()

                def cflag(c, e):
                    return cond_i[0:1, 8 * c + e:8 * c + e + 1]

                for e in range(NE):
                    p.cond_region(
                        cflag(1, e), [],
                        lambda e=e: p.cond_region(cflag(0, e), [], lambda: expert_sparse(e, CAPS[0]),
                                                  lambda: expert_sparse(e, CAPS[1])),
                        lambda e=e: p.cond_region(cflag(2, e), [], lambda: expert_sparse(e, CAPS[2]),
                                                  lambda: expert_dense(e)))
                    p.barrier()

        def final_phase(do_norm):
            with ExitStack() as os_:
                gf_bc = sb(os_, "gf_bc", [128, D], F32)
                gfb = Buf()
                outt = [sb(os_, "outt%d" % i, [128, D], F32) for i in range(2)]
                outb = [Buf(), Buf()]
                junk = sb(os_, "junko", [128, D], BF16)
                junkb = Buf()
                toks = []
                if do_norm:
                    k.dma(SP, gf_bc[:], fg_d[0:1, :].partition_broadcast(128), writes=[gfb], sem="gf")
                for n in range(1, NT):
                    if do_norm:
                        oi = n % 2
                        rstd, sbf = rms_stats(x_tok[:, n, :], xb[n], junk[:], junkb, 1.0 / 32.0)
                        k.stt(outt[oi][:], x_tok[:, n, :], rstd, gf_bc[:], ALU.mult, ALU.mult,
                              [xb[n], sbf, gfb], [outb[oi]])
                        toks.append(k.dma(SP, y_d[(n - 1) * 128:n * 128, :], outt[oi][:], reads=[outb[oi]], sem="out%d" % oi))
                    else:
                        toks.append(k.dma(SP, y_d[(n - 1) * 128:n * 128, :], x_tok[:, n, :], reads=[xb[n]], sem="out0"))
                p.wait_only(SP, toks)
                p.barrier()

        mixer_phase(0)
        if stop_after == "M0":
            final_phase(False)
            p.flush()
            return nc
        ffn_phase0()
        if stop_after == "F0":
            final_phase(False)
            p.flush()
            return nc
        k.ts1(x_tok[:, 0, :], x_tok[:, 0, :], flag[:, 0:1], ALU.mult, [cb], [xb[0]])
        mixer_phase(1)
        if stop_after == "M1":
            final_phase(False)
            p.flush()
            return nc
        moe_phase()
        final_phase(True)
        p.flush()
    return nc


def _prep_shared(inp):
    f = lambda a: np.ascontiguousarray(np.asarray(a, dtype=np.float32))
    sh = {}
    sh["ident"] = np.eye(128, dtype=np.float32)
    sh["ustrict"] = np.triu(np.ones((128, 128), np.float32), 1)
    sh["iota"] = np.ascontiguousarray(np.broadcast_to(np.arange(CAPS[-1], dtype=np.float32), (128, CAPS[-1])))
    pp = np.zeros((128, 2, NPP), np.float32)
    wins = np.array([[2.0, 4.0], [8.0, 16.0]], np.float32)
    for l in range(2):
        pp[:, l, PP_PSCALE:PP_PSCALE + 2] = f(inp["pool_scale"])[l].reshape(2, 128).T
        dw = f(inp["conv_dw_w"])[l]
        pp[:, l, PP_DWW:PP_DWW + 93] = dw.reshape(31, 3, 128).transpose(2, 1, 0).reshape(128, 93)
        pp[:, l, PP_DWB:PP_DWB + 3] = f(inp["conv_dw_b"])[l].reshape(3, 128).T
        pp[:, l, PP_LNG:PP_LNG + 3] = f(inp["conv_ln_g"])[l].reshape(3, 128).T
        pp[:, l, PP_LNB:PP_LNB + 3] = f(inp["conv_ln_b"])[l].reshape(3, 128).T
        pp[:, l, PP_PWB:PP_PWB + 3] = f(inp["conv_pw_b"])[l].reshape(3, 128).T
        for c in range(2):
            pp[0:64, l, PP_INVW + c] = 1.0 / wins[c, 0]
            pp[64:128, l, PP_INVW + c] = 1.0 / wins[c, 1]
        pp[:, l, PP_G2:PP_G2 + 8] = f(inp["norm2_g"])[l].reshape(8, 128).T
    sh["pp"] = pp.reshape(128, 2 * NPP)
    sh["norm1_g"] = f(inp["norm1_g"])
    sh["norm2_g"] = f(inp["norm2_g"])
    sh["final_g"] = f(inp["final_g"]).reshape(1, D)
    sh["gm_norm_g"] = f(inp["gm_norm_g"])
    sh["gm_b"] = f(inp["gm_b"]).reshape(2, 512)
    sh["w_in"] = f(inp["w_in"])
    sh["pool_w"] = f(inp["pool_w"])
    sh["gm_wsT"] = np.ascontiguousarray(f(inp["gm_ws"]).transpose(0, 1, 3, 2))
    sh["conv_pw_w"] = f(inp["conv_pw_w"])
    sh["w_out"] = f(inp["w_out"])
    sh["ffn_wg"] = f(inp["ffn_wg"])[0]
    sh["ffn_wu"] = f(inp["ffn_wu"])[0]
    sh["ffn_wd"] = f(inp["ffn_wd"])[0]
    sh["router"] = f(inp["moe_router"])[0]
    sh["moe_wg"] = f(inp["moe_wg"])[0]
    sh["moe_wu"] = f(inp["moe_wu"])[0]
    sh["moe_wd"] = f(inp["moe_wd"])[0]
    return sh


def _prep_core(x, c):
    b, q = c // 4, c % 4
    xin = np.zeros((NT * 128, D), np.float32)
    xin[128:] = x[b, q * 2048:(q + 1) * 2048]
    if q > 0:
        xin[:128] = x[b, q * 2048 - 128:q * 2048]
    flag = np.full((128, 1), 1.0 if q > 0 else 0.0, np.float32)
    pinv = np.zeros((128, 2, 16), np.float32)
    wins = [[2, 4], [8, 16]]
    for cc in range(2):
        for half in range(2):
            w = wins[cc][half]
            for j in range(16):
                cntv = min(j + 1, w) if q == 0 else w
                pinv[half * 64:(half + 1) * 64, cc, j] = 1.0 / cntv
    return {"x": xin, "flag": flag, "pool_inv": pinv.reshape(128, 32)}


_NC_CACHE = {}


def run(inputs, stop_after=None, trace=False):
    x = np.asarray(inputs["x"], dtype=np.float32)
    sh = _prep_shared(inputs)
    in_maps = []
    for c in range(8):
        m = dict(sh)
        m.update(_prep_core(x, c))
        in_maps.append(m)
    if stop_after not in _NC_CACHE:
        _NC_CACHE[stop_after] = build_program(stop_after)
    nc = _NC_CACHE[stop_after]
    res = run_bass_kernel_spmd(nc, in_maps, core_ids=list(range(8)), **({"trace": True} if trace else {}))
    out = np.zeros((2, 8192, D), np.float32)
    for c in range(8):
        b, q = c // 4, c % 4
        out[b, q * 2048:(q + 1) * 2048] = res.results[c]["y"]
    return out, res


def kernel(**inputs):
    out, _ = run(inputs)
    return out
```
